# Optimizing a Trainium2 kernel written in Bass

```python
import math
import jax, jax.numpy as jnp
from jax import lax
import numpy as np

D_MODEL = 2048
BATCH = 4
SEQ = 4096
DEPTH = 2

CHUNK = 64
Q_BLOCK = 128
N_BRANCH = 4
HEAD_DIM = 128
HEADS = D_MODEL // (N_BRANCH * HEAD_DIM)
BRANCH_W = HEADS * HEAD_DIM
A_LEFT_CHUNKS = 8
A_BAND = (A_LEFT_CHUNKS + 1) * CHUNK
REL_CLIP = 128
DIFF_DIM = HEAD_DIM // 2
IDX_HEADS = 8
IDX_DIM = 64
TOPK_MAX = 256
ROPE_THETA = 10000.0
D_FF = 4 * D_MODEL
EPS = 1e-6

SEGMENTS = (
    ("a_q", BRANCH_W), ("a_k", BRANCH_W), ("a_v", BRANCH_W),
    ("b_q", BRANCH_W), ("b_k", BRANCH_W), ("b_v", BRANCH_W),
    ("c_q", BRANCH_W), ("c_k", BRANCH_W), ("c_v", BRANCH_W), ("c_f", HEADS),
    ("d_q", BRANCH_W), ("d_k", HEAD_DIM), ("d_v", HEAD_DIM),
    ("d_iq", IDX_HEADS * IDX_DIM), ("d_ik", IDX_DIM), ("d_iw", IDX_HEADS),
    ("gate", N_BRANCH * D_MODEL),
)
N_IN = (10 * BRANCH_W + HEADS + 2 * HEAD_DIM + IDX_HEADS * IDX_DIM + IDX_DIM
        + IDX_HEADS + N_BRANCH * D_MODEL)

kernel_name = "hybrid_gated_chunk_causal_encoder"


def _split(z):
    out = {}
    off = 0
    for name, width in SEGMENTS:
        out[name] = z[..., off:off + width]
        off += width
    return out


def rmsnorm(x, g):
    xf = x.astype(jnp.float32)
    y = xf * lax.rsqrt(jnp.mean(xf * xf, axis=-1, keepdims=True) + EPS)
    return (y * g.astype(jnp.float32)).astype(x.dtype)


def rope_tables(seq, dim):
    inv = ROPE_THETA ** (-jnp.arange(0, dim, 2, dtype=jnp.float32) / dim)
    ang = jnp.arange(seq, dtype=jnp.float32)[:, None] * inv[None, :]
    return jnp.cos(ang), jnp.sin(ang)


def apply_rope(x, cos, sin):
    half = x.shape[-1] // 2
    shape = (x.shape[1],) + (1,) * (x.ndim - 3) + (half,)
    c = cos.reshape(shape).astype(x.dtype)
    s = sin.reshape(shape).astype(x.dtype)
    x1, x2 = x[..., :half], x[..., half:]
    return jnp.concatenate([x1 * c - x2 * s, x1 * s + x2 * c], axis=-1)


def chunk_causal_mask(start, end):
    qpos = jnp.arange(start, end)
    kpos = jnp.arange(end)
    return (kpos[None, :] // CHUNK) <= (qpos[:, None] // CHUNK)


def chunk_band_attention(q, k, v, rel_bias):
    B, S, H, d = q.shape
    nc = S // CHUNK
    qc = q.reshape(B, nc, CHUNK, H, d)
    pad = ((0, 0), (A_LEFT_CHUNKS * CHUNK, 0), (0, 0), (0, 0))
    kp = jnp.pad(k, pad).reshape(B, nc + A_LEFT_CHUNKS, CHUNK, H, d)
    vp = jnp.pad(v, pad).reshape(B, nc + A_LEFT_CHUNKS, CHUNK, H, d)
    band = jnp.arange(nc)[:, None] + jnp.arange(A_LEFT_CHUNKS + 1)[None, :]
    kb = kp[:, band].reshape(B, nc, A_BAND, H, d)
    vb = vp[:, band].reshape(B, nc, A_BAND, H, d)
    s = jnp.einsum('bcqhd,bckhd->bhcqk', qc, kb).astype(jnp.float32) * (d ** -0.5)
    rel = A_LEFT_CHUNKS * CHUNK + jnp.arange(CHUNK)[:, None] - jnp.arange(A_BAND)[None, :]
    bias = rel_bias.astype(jnp.float32)[:, jnp.clip(rel, -REL_CLIP, REL_CLIP) + REL_CLIP]
    valid = jnp.repeat(band >= A_LEFT_CHUNKS, CHUNK, axis=1)
    s = jnp.where(valid[None, None, :, None, :], s + bias[None, :, None], -jnp.inf)
    p = jax.nn.softmax(s, axis=-1)
    o = jnp.einsum('bhcqk,bckhd->bcqhd', p.astype(v.dtype), vb)
    return o.reshape(B, S, H, d)


def diff_attention(q, k, v, lam, lambda_init, sub_norm_g):
    B, S, H, _, dd = q.shape
    scale = dd ** -0.5
    outs = []
    for start in range(0, S, Q_BLOCK):
        end = start + Q_BLOCK
        s = jnp.einsum('bqhnd,bkhnd->bnhqk', q[:, start:end], k[:, :end]).astype(jnp.float32) * scale
        s = jnp.where(chunk_causal_mask(start, end), s, -jnp.inf)
        p = jax.nn.softmax(s, axis=-1)
        a = p[:, 0] - lam * p[:, 1]
        outs.append(jnp.einsum('bhqk,bkhd->bqhd', a.astype(v.dtype), v[:, :end]))
    o = jnp.concatenate(outs, axis=1)
    return rmsnorm(o, sub_norm_g) * (1.0 - lambda_init)


def forgetting_attention(q, k, v, log_f):
    B, S, H, d = q.shape
    scale = d ** -0.5
    cum = jnp.transpose(lax.cumsum(log_f, axis=1), (0, 2, 1))
    outs = []
    for start in range(0, S, Q_BLOCK):
        end = start + Q_BLOCK
        s = jnp.einsum('bqhd,bkhd->bhqk', q[:, start:end], k[:, :end]).astype(jnp.float32) * scale
        s = s + (cum[:, :, start:end, None] - cum[:, :, None, :end])
        causal = jnp.arange(end)[None, :] <= jnp.arange(start, end)[:, None]
        p = jax.nn.softmax(jnp.where(causal, s, -jnp.inf), axis=-1)
        outs.append(jnp.einsum('bhqk,bkhd->bqhd', p.astype(v.dtype), v[:, :end]))
    return jnp.concatenate(outs, axis=1)


def _gather_rows(src, idx):
    return jax.vmap(lambda s_, i_: s_[i_])(src, idx)


def indexed_sparse_attention(q, k, v, iq, ik, iw, k_sel):
    B, S, H, d = q.shape
    scale = d ** -0.5
    outs = []
    for start in range(0, S, Q_BLOCK):
        end = start + Q_BLOCK
        dots = jnp.einsum('bqhd,bkd->bqhk', iq[:, start:end], ik[:, :end]).astype(jnp.float32)
        score = jnp.einsum('bqh,bqhk->bqk', iw[:, start:end].astype(jnp.float32), jax.nn.relu(dots))
        score = jnp.where(chunk_causal_mask(start, end)[None], score, -jnp.inf)
        _, sel = lax.top_k(score, min(k_sel, end))
        kg = _gather_rows(k[:, :end], sel)
        vg = _gather_rows(v[:, :end], sel)
        s = jnp.einsum('bqhd,bqkd->bhqk', q[:, start:end], kg).astype(jnp.float32) * scale
        valid = (sel // CHUNK) <= (jnp.arange(start, end) // CHUNK)[None, :, None]
        p = jax.nn.softmax(jnp.where(valid[:, None], s, -jnp.inf), axis=-1)
        outs.append(jnp.einsum('bhqk,bqkd->bqhd', p.astype(v.dtype), vg))
    return jnp.concatenate(outs, axis=1)


def setup_inputs(seed: int = 0) -> dict:
    key = jax.random.key(seed)
    ks = jax.random.split(key, 20)

    def nrm(k, shape, scale):
        return jax.random.normal(k, shape, jnp.float32) * scale

    return {
        "x": nrm(ks[0], (BATCH, SEQ, D_MODEL), 1.0),
        "norm1_g": 1.0 + nrm(ks[1], (DEPTH, D_MODEL), 0.02),
        "norm2_g": 1.0 + nrm(ks[2], (DEPTH, D_MODEL), 0.02),
        "final_g": 1.0 + nrm(ks[3], (D_MODEL,), 0.02),
        "w_in": nrm(ks[4], (DEPTH, D_MODEL, N_IN), D_MODEL ** -0.5),
        "b_gate": nrm(ks[5], (DEPTH, N_BRANCH * D_MODEL), 0.02),
        "b_forget": 2.0 + nrm(ks[6], (DEPTH, HEADS), 0.5),
        "rel_bias": nrm(ks[7], (DEPTH, HEADS, 2 * REL_CLIP + 1), 0.5),
        "lambda_q1": nrm(ks[8], (DEPTH, DIFF_DIM), 0.1),
        "lambda_k1": nrm(ks[9], (DEPTH, DIFF_DIM), 0.1),
        "lambda_q2": nrm(ks[10], (DEPTH, DIFF_DIM), 0.1),
        "lambda_k2": nrm(ks[11], (DEPTH, DIFF_DIM), 0.1),
        "diff_norm_g": 1.0 + nrm(ks[12], (DEPTH, HEAD_DIM), 0.02),
        "w_branch": nrm(ks[13], (DEPTH, N_BRANCH, BRANCH_W, D_MODEL), BRANCH_W ** -0.5),
        "w_out": nrm(ks[14], (DEPTH, D_MODEL, D_MODEL), D_MODEL ** -0.5),
        "w_ff1": nrm(ks[15], (DEPTH, D_MODEL, D_FF), D_MODEL ** -0.5),
        "w_ff2": nrm(ks[16], (DEPTH, D_FF, D_MODEL), D_FF ** -0.5),
    }


def reference(x, norm1_g, norm2_g, final_g, w_in, b_gate, b_forget, rel_bias,
              lambda_q1, lambda_k1, lambda_q2, lambda_k2, diff_norm_g,
              w_branch, w_out, w_ff1, w_ff2):
    B, S, _ = x.shape
    k_sel = min(TOPK_MAX, S // 4)
    cos128, sin128 = rope_tables(S, HEAD_DIM)
    cos64, sin64 = rope_tables(S, DIFF_DIM)
    for l in range(DEPTH):
        h = rmsnorm(x, norm1_g[l])
        p = _split(h @ w_in[l])

        oa = chunk_band_attention(p["a_q"].reshape(B, S, HEADS, HEAD_DIM),
                                  p["a_k"].reshape(B, S, HEADS, HEAD_DIM),
                                  p["a_v"].reshape(B, S, HEADS, HEAD_DIM), rel_bias[l])

        lambda_init = 0.8 - 0.6 * math.exp(-0.3 * l)
        lam = (jnp.exp(jnp.sum(lambda_q1[l].astype(jnp.float32) * lambda_k1[l].astype(jnp.float32)))
               - jnp.exp(jnp.sum(lambda_q2[l].astype(jnp.float32) * lambda_k2[l].astype(jnp.float32)))
               + lambda_init)
        qb = apply_rope(p["b_q"].reshape(B, S, HEADS, 2, DIFF_DIM), cos64, sin64)
        kb = apply_rope(p["b_k"].reshape(B, S, HEADS, 2, DIFF_DIM), cos64, sin64)
        ob = diff_attention(qb, kb, p["b_v"].reshape(B, S, HEADS, HEAD_DIM), lam, lambda_init, diff_norm_g[l])

        log_f = jax.nn.log_sigmoid(p["c_f"].astype(jnp.float32) + b_forget[l].astype(jnp.float32))
        oc = forgetting_attention(p["c_q"].reshape(B, S, HEADS, HEAD_DIM),
                                  p["c_k"].reshape(B, S, HEADS, HEAD_DIM),
                                  p["c_v"].reshape(B, S, HEADS, HEAD_DIM), log_f)

        qd = apply_rope(p["d_q"].reshape(B, S, HEADS, HEAD_DIM), cos128, sin128)
        kd = apply_rope(p["d_k"], cos128, sin128)
        iq = apply_rope(p["d_iq"].reshape(B, S, IDX_HEADS, IDX_DIM), cos64, sin64) * (IDX_DIM ** -0.5)
        ik = apply_rope(p["d_ik"], cos64, sin64)
        iw = p["d_iw"] * (IDX_HEADS ** -0.5)
        od = indexed_sparse_attention(qd, kd, p["d_v"], iq, ik, iw, k_sel)

        gates = jax.nn.sigmoid(p["gate"] + b_gate[l]).reshape(B, S, N_BRANCH, D_MODEL)
        merged = None
        for i, o in enumerate((oa, ob, oc, od)):
            term = gates[:, :, i] * (o.reshape(B, S, BRANCH_W) @ w_branch[l, i])
            merged = term if merged is None else merged + term
        x = x + merged @ w_out[l]

        h2 = rmsnorm(x, norm2_g[l])
        x = x + jnp.square(jax.nn.relu(h2 @ w_ff1[l])) @ w_ff2[l]
    return rmsnorm(x, final_g)
```

```python
import math
from contextlib import ExitStack

import numpy as np
import concourse.bass as bass
import concourse.mybir as mybir
from concourse.bass_utils import run_bass_kernel_spmd

F32 = mybir.dt.float32
BF16 = mybir.dt.bfloat16
AF = mybir.ActivationFunctionType
ALU = mybir.AluOpType
AX = mybir.AxisListType

D_MODEL = 2048
BATCH = 4
SEQ = 4096
DEPTH = 2
KC = D_MODEL // 128
NBLK = SEQ // 128
LBLK = NBLK // 2
LTOK = LBLK * 128
TT = 256
NT = LTOK // TT
D_FF = 4 * D_MODEL
EPS = 1e-6
NEG = -30000.0
N_CORES = 8


class Tok:
    __slots__ = ("w", "r", "parent", "subs", "dj")

    def __init__(self, parent=None, dj=False):
        self.w = {}
        self.r = {}
        self.parent = parent
        self.subs = []
        self.dj = dj


class Tile:
    def __init__(self, ap, name):
        self.ap = ap
        self.name = name
        self.t = Tok()
        self._subs = {}

    def s(self, key):
        if key not in self._subs:
            tk = Tok(parent=self.t)
            self.t.subs.append(tk)
            self._subs[key] = tk
        return self._subs[key]

    def __getitem__(self, idx):
        return self.ap[idx]


ENGS = ("pe", "act", "dve", "pool", "sp")


class _Rec:
    def __init__(self):
        self.call = None

    def __getattr__(self, name):
        def f(*a, **kw):
            self.call = (name, a, kw)
            return None
        return f


class Sched:
    def __init__(self, nc, es, n_dma_sems=56):
        self.nc = nc
        self.es = es
        self.ops = {e: [] for e in ENGS}
        self.cnt = {e: 0 for e in ENGS}
        self.sems = {}
        for e in ENGS + ("cc",):
            self.sems[e] = es.enter_context(nc.semaphore("sem_" + e))
        self.cc_cnt = 0
        self.dma_sems = [es.enter_context(nc.semaphore(f"dsem{i}")) for i in range(n_dma_sems)]
        self.dma_cnt = [0] * n_dma_sems
        self.dma_rr = 0
        self.tile_sem = {}
        self.seen = {e: {} for e in ENGS}
        self.final_waits = []
        self.ntiles = 0

    def sb(self, name, shape, dtype):
        self.ntiles += 1
        t = self.es.enter_context(self.nc.sbuf_tensor(f"{name}_{self.ntiles}", list(shape), dtype))
        return Tile(t, name)

    def ps(self, name, shape, dtype=F32):
        self.ntiles += 1
        t = self.es.enter_context(self.nc.psum_tensor(f"{name}_{self.ntiles}", list(shape), dtype))
        return Tile(t, name)

    @staticmethod
    def _toks(x):
        out = []
        for i in x:
            out.append(i if isinstance(i, Tok) else i.t)
        return out

    def _collect(self, reads, writes):
        ev = {}

        def add_r(d):
            for k, v in d.items():
                if ev.get(k, 0) < v:
                    ev[k] = v

        add = add_r

        for t in reads:
            add(t.w)
            if t.parent is not None:
                add(t.parent.w)
            for s_ in t.subs:
                add(s_.w)
        for t in writes:
            if not t.dj:
                add(t.w)
            add_r(t.r)
            if t.parent is not None:
                add(t.parent.w)
                add_r(t.parent.r)
            for s_ in t.subs:
                add(s_.w)
                add_r(s_.r)
        return ev

    def _update(self, reads, writes, me):
        k, v = me
        for t in reads:
            if t.r.get(k, 0) < v:
                t.r[k] = v
        for t in writes:
            if t.dj:
                if t.w.get(k, 0) < v:
                    t.w[k] = v
                continue
            t.w = {k: v}
            t.r = {}
            for s_ in t.subs:
                s_.w = {}
                s_.r = {}

    def _waits(self, eng, ev):
        seen = self.seen[eng]
        waits = []
        for k, v in ev.items():
            if eng == "pe" and k == "pe":
                continue
            if seen.get(k, 0) >= v:
                continue
            seen[k] = v
            waits.append((k, v))
        return waits

    def _semof(self, k):
        return self.sems[k] if isinstance(k, str) else self.dma_sems[k]

    def op(self, eng, fn, reads=(), writes=()):
        reads = self._toks(reads)
        writes = self._toks(writes)
        ev = self._collect(reads, writes)
        waits = self._waits(eng, ev)
        self.cnt[eng] += 1
        me = (eng, self.cnt[eng])
        rec = _Rec()
        fn(rec)
        name, a, kw = rec.call

        def fn2(e, name=name, a=a, kw=kw):
            return getattr(e, name)(*a, **kw)

        self.ops[eng].append((waits, fn2, (eng, 1)))
        self._update(reads, writes, me)

    def collective_allgather(self, in_ap, out_ap, reads=(), writes=()):
        reads = self._toks(reads)
        writes = self._toks(writes)
        ev = self._collect(reads, writes)
        waits = self._waits("pool", ev)
        self.cc_cnt += 1
        me = ("cc", self.cc_cnt)

        def fn(e):
            return e.collective_compute("AllGather", ALU.bypass, replica_groups=[list(range(N_CORES))],
                                        ins=[in_ap], outs=[out_ap])

        self.ops["pool"].append((waits, fn, ("cc", None)))
        self._update(reads, writes, me)

    def idma(self, out, in_, idx_ap, reads=(), writes=(), semtile=None):
        def fn(e):
            return e.indirect_dma_start(out=out, out_offset=None, in_=in_,
                                        in_offset=bass.IndirectOffsetOnAxis(ap=idx_ap, axis=0))
        return self.dma("pool", None, None, reads=reads, writes=writes, semtile=semtile, fn=fn)

    def dma(self, queue, out, in_, reads=(), writes=(), semtile=None, fn=None, **kw):
        reads = self._toks(reads)
        writes = self._toks(writes)
        ev = self._collect(reads, writes)
        key = semtile if semtile is not None else (writes[0] if writes else reads[0])
        if key not in self.tile_sem:
            self.tile_sem[key] = self.dma_rr % len(self.dma_sems)
            self.dma_rr += 1
        si = self.tile_sem[key]
        if self.dma_cnt[si] > 0:
            pv = self.dma_cnt[si]
            if ev.get(si, 0) < pv:
                ev[si] = pv
        waits = self._waits(queue, ev)
        self.dma_cnt[si] += 16
        me = (si, self.dma_cnt[si])

        if fn is None:
            def fn(e, out=out, in_=in_, kw=kw):
                return e.dma_start(out=out, in_=in_, **kw)

        self.ops[queue].append((waits, fn, (si, 16)))
        self._update(reads, writes, me)
        return me

    def final_all(self):
        for si, v in enumerate(self.dma_cnt):
            if v > 0:
                self.final_waits.append((si, v))

    def emit(self):
        nc = self.nc
        block = self.es.enter_context(nc.Block())

        def replay(eng_name, e, extra=None):
            for waits, fn, inc in self.ops[eng_name]:
                for k, v in waits:
                    e.wait_ge(self._semof(k), v)
                ins = fn(e)
                if inc[1] is None:
                    ins.then_inc(self._semof(inc[0]))
                else:
                    ins.then_inc(self._semof(inc[0]), inc[1])
            if extra:
                for k, v in extra:
                    e.wait_ge(self._semof(k), v)

        @block.sync
        def _(e):
            replay("sp", e, self.final_waits)

        @block.tensor
        def _(e):
            replay("pe", e)

        @block.scalar
        def _(e):
            replay("act", e)

        @block.vector
        def _(e):
            replay("dve", e)

        @block.gpsimd
        def _(e):
            replay("pool", e)


WB = 256


class Ctx:
    def __init__(self, S):
        self.S = S
        self.ones_bf = S.sb("ones_bf", [128, 128], BF16)
        S.op("pool", lambda e: e.memset(self.ones_bf[:], 1.0), writes=[self.ones_bf])
        self.wbuf = [S.sb(f"wbuf{i}", [128, KC, WB], BF16) for i in range(3)]
        self.wrr = 0
        self.psd = [S.ps(f"psd{i}", [128, 512], F32) for i in range(2)]
        self.psrr = 0
        self.evrr = 0
        self.sqb = [S.sb(f"sqb{i}", [128, TT], BF16) for i in range(2)]
        self.rstd = S.sb("rstd", [128, TT], F32)
        self.fence_t = S.sb("fence", [128, 8], F32)

    def next_w(self):
        w = self.wbuf[self.wrr % len(self.wbuf)]
        self.wrr += 1
        return w

    def next_ps(self):
        p = self.psd[self.psrr % len(self.psd)]
        self.psrr += 1
        return p

    def ev_eng(self):
        self.evrr += 1
        return "act" if self.evrr % 2 else "dve"

    def fence(self, tiles):
        self.S.op("pool", lambda e: e.memset(self.fence_t[:, 0:1], 0.0), writes=[self.fence_t] + list(tiles))


def load_w(cx, wdram, col0, ncols, nk=KC, row0=0, wtok=None):
    S = cx.S
    w = cx.next_w()
    src = wdram[row0:row0 + nk * 128, col0:col0 + ncols].rearrange("(k p) n -> p k n", p=128)
    S.dma("pool", out=w[:, 0:nk, 0:ncols], in_=src, reads=([wtok] if wtok is not None else []), writes=[w])
    return w


def evac(cx, out_ap, in_ps, reads, writes, scale=None, eng=None):
    S = cx.S
    eng = eng or cx.ev_eng()
    if eng == "act":
        if scale is None:
            S.op("act", lambda e: e.activation(out=out_ap, in_=in_ps, func=AF.Copy), reads=reads, writes=writes)
        else:
            S.op("act", lambda e: e.activation(out=out_ap, in_=in_ps, func=AF.Copy, scale=float(scale)),
                 reads=reads, writes=writes)
    else:
        if scale is None:
            S.op("dve", lambda e: e.tensor_copy(out=out_ap, in_=in_ps), reads=reads, writes=writes)
        else:
            S.op("dve", lambda e: e.tensor_scalar(out=out_ap, in0=in_ps, scalar1=float(scale), scalar2=None,
                                                  op0=ALU.mult), reads=reads, writes=writes)


def rmsnorm_fm(cx, x, g, hT, ps):
    S = cx.S
    rstd = cx.rstd
    for kc in range(KC):
        sq = cx.sqb[kc % 2]
        S.op("act", lambda e, kc=kc, sq=sq: e.activation(out=sq[:, :], in_=x[:, kc, :], func=AF.Square),
             reads=[x], writes=[sq])
        S.op("pe", lambda e, kc=kc, sq=sq: e.matmul(ps[:, 0:TT], lhsT=cx.ones_bf[:, :], rhs=sq[:, :],
                                                    start=(kc == 0), stop=(kc == KC - 1)),
             reads=[sq, cx.ones_bf], writes=[ps])
    S.op("act", lambda e: e.activation(out=rstd[:, :], in_=ps[:, 0:TT], func=AF.Sqrt, bias=EPS, scale=1.0 / D_MODEL),
         reads=[ps], writes=[rstd])
    S.op("dve", lambda e: e.reciprocal(out=rstd[:, :], in_=rstd[:, :]), reads=[rstd], writes=[rstd])
    for kc in range(KC):
        S.op("dve", lambda e, kc=kc: e.scalar_tensor_tensor(out=hT[:, kc, :], in0=x[:, kc, :], scalar=g[:, kc:kc + 1],
                                                            op0=ALU.mult, in1=rstd[:, :], op1=ALU.mult),
             reads=[x, g, rstd], writes=[hT.s(kc)])


def proj_fm(cx, w, c0, M, hT, ps, nk=KC, ncol=TT, kofs=0):
    S = cx.S
    for kc in range(nk):
        S.op("pe", lambda e, kc=kc: e.matmul(ps[0:M, 0:ncol], lhsT=w[:, kc, c0:c0 + M], rhs=hT[:, kofs + kc, :],
                                             start=(kc == 0), stop=(kc == nk - 1)),
             reads=[w, hT.s(kofs + kc)], writes=[ps])


def rope_fm(cx, ps, P, half, ctab, stab, out_ap, out_toks, tmpA, tmpB):
    S = cx.S
    S.op("dve", lambda e: e.tensor_tensor(out=tmpA[0:P, :], in0=ps[0:P, 0:TT], in1=ctab[0:P, :], op=ALU.mult),
         reads=[ps] + ctab.toks, writes=[tmpA])
    for g in range(P // (2 * half)):
        b = g * 2 * half
        S.op("dve", lambda e, b=b: e.tensor_tensor(out=tmpB[b:b + half, :], in0=ps[b + half:b + 2 * half, 0:TT],
                                                   in1=stab[b + half:b + 2 * half, :], op=ALU.mult),
             reads=[ps] + stab.toks, writes=[tmpB])
        S.op("dve", lambda e, b=b: e.tensor_tensor(out=tmpB[b + half:b + 2 * half, :], in0=ps[b:b + half, 0:TT],
                                                   in1=stab[b:b + half, :], op=ALU.mult),
             reads=[ps] + stab.toks, writes=[tmpB])
    if isinstance(out_ap, list):
        for (r0, r1, oap, otk) in out_ap:
            S.op("pool", lambda e, r0=r0, r1=r1, oap=oap: e.tensor_tensor(out=oap, in0=tmpA[r0:r1, :], in1=tmpB[r0:r1, :],
                                                                       op=ALU.add), reads=[tmpA, tmpB], writes=otk)
    else:
        S.op("pool", lambda e: e.tensor_tensor(out=out_ap, in0=tmpA[0:P, :], in1=tmpB[0:P, :], op=ALU.add),
             reads=[tmpA, tmpB], writes=out_toks)


class View:
    def __init__(self, tile, idx, toks=None):
        self.tile = tile
        self.idx = idx
        self.toks = toks if toks is not None else [tile.t]

    def __getitem__(self, sl):
        rows, cols = sl
        return self.tile.ap[(rows,) + tuple(self.idx) + (cols,)] if isinstance(self.idx, tuple) else \
            self.tile.ap[rows, self.idx, cols]


WK_COLS = 512 * 3 + 128 + 64 + 4
WV_COLS = 512 * 3 + 128


class DT:
    def __init__(self, ap, tok=None, dj=False):
        self.ap = ap
        self.t = tok if tok is not None else Tok(dj=dj)

    def __getitem__(self, i):
        return self.ap[i]


def load_x_tile(cx, c, x_src, t, ropet):
    S = cx.S
    xs, xtok = x_src(t)
    S.dma("sp", out=c["xbuf"][:, :, :], in_=xs.rearrange("(k p) n -> p k n", p=128), reads=[xtok], writes=[c["xbuf"]])
    S.dma("sp", out=c["rt"][:, :, :], in_=ropet.ap[:, :, t * TT:(t + 1) * TT].rearrange("f p n -> p f n"),
          reads=[ropet.t], writes=[c["rt"]])


def phase_kv(cx, io, x_src, c):
    S = cx.S
    x, hT, tmpA, tmpB, kst, vst, lf, rt = (c[k] for k in ("xbuf", "hT", "tmpA", "tmpB", "kst", "vst", "lf", "rt"))
    c128, s128, c64, s64 = (View(rt, i) for i in range(4))
    stn = 0
    for t in range(NT):
        tc = (t * TT, (t + 1) * TT)
        load_x_tile(cx, c, x_src, t, io["ropet"])
        rmsnorm_fm(cx, x, c["n1g"], hT, cx.next_ps())
        for (c0, dst, rk) in ((0, "kTa", None), (256, "kTa", None), (512, "kTb", 32), (768, "kTb", 32),
                              (1024, "kTc", None), (1280, "kTc", None)):
            w = load_w(cx, io["wk"].ap, c0, 256, wtok=io["wk"].t)
            st = kst[stn % 2]
            stn += 1
            for j in range(2):
                ps = cx.next_ps()
                proj_fm(cx, w, j * 128, 128, hT, ps)
                if rk is None:
                    evac(cx, st[:, j, :], ps[:, 0:TT], [ps], [st])
                else:
                    rope_fm(cx, ps, 128, rk, c64, s64, st[:, j, :], [st], tmpA, tmpB)
            r0 = c0 % 512
            S.dma("sp", out=io[dst].ap[r0:r0 + 256, tc[0]:tc[1]].rearrange("(j p) n -> p j n", p=128), in_=st[:, :, :],
                  reads=[st], writes=[io[dst]], semtile=st.t)
        w = load_w(cx, io["wk"].ap, 1536, 196, wtok=io["wk"].t)
        st = kst[stn % 2]
        stn += 1
        ps = cx.next_ps()
        proj_fm(cx, w, 0, 128, hT, ps)
        rope_fm(cx, ps, 128, 64, c128, s128, st[:, 0, :], [st], tmpA, tmpB)
        ps = cx.next_ps()
        proj_fm(cx, w, 128, 68, hT, ps)
        rope_fm(cx, ps, 64, 32, c64, s64, st[0:64, 1, :], [st], tmpA, tmpB)
        S.op("act", lambda e, ps=ps: e.activation(out=lf[64:68, :], in_=ps[64:68, 0:TT], func=AF.Exp,
                                                 bias=c["nbfg"][64:68, :], scale=-1.0),
             reads=[ps, c["nbfg"]], writes=[lf])
        S.op("act", lambda e: e.activation(out=lf[64:68, :], in_=lf[64:68, :], func=AF.Ln, bias=1.0),
             reads=[lf], writes=[lf])
        S.op("act", lambda e: e.mul(out=lf[64:68, :], in_=lf[64:68, :], mul=-1.0), reads=[lf], writes=[lf])
        S.dma("sp", out=io["kTd"].ap[:, tc[0]:tc[1]], in_=st[:, 0, :], reads=[st], writes=[io["kTd"]], semtile=st.t)
        S.dma("sp", out=io["ikT"].ap[:, tc[0]:tc[1]], in_=st[0:64, 1, :], reads=[st], writes=[io["ikT"]], semtile=st.t)
        S.dma("sp", out=io["logf"].ap[:, tc[0]:tc[1]], in_=lf[64:68, :], reads=[lf], writes=[io["logf"]], semtile=lf.t)
        for (c0, ncols, nm, d0) in ((0, 256, "va", 0), (256, 256, "va", 256), (512, 256, "vb", 0), (768, 256, "vb", 256),
                                    (1024, 256, "vc", 0), (1280, 256, "vc", 256), (1536, 128, "vd", 0)):
            w = load_w(cx, io["wv"].ap, c0, ncols, wtok=io["wv"].t)
            for sbk in range(TT // 128):
                ps = cx.next_ps()
                for kc in range(KC):
                    S.op("pe", lambda e, kc=kc, ps=ps, w=w, sbk=sbk, ncols=ncols: e.matmul(
                        ps[:, 0:ncols], lhsT=hT[:, kc, sbk * 128:(sbk + 1) * 128], rhs=w[:, kc, 0:ncols],
                        start=(kc == 0), stop=(kc == KC - 1)), reads=[w, hT.s(kc)], writes=[ps])
                st = vst[stn % 2]
                stn += 1
                evac(cx, st[:, 0:ncols], ps[:, 0:ncols], [ps], [st])
                r0 = t * TT + sbk * 128
                S.dma("sp", out=io[nm].ap[r0:r0 + 128, d0:d0 + ncols], in_=st[:, 0:ncols], reads=[st], writes=[io[nm]],
                      semtile=st.t)


def alloc_common(cx):
    S = cx.S
    c = {}
    c["xbuf"] = S.sb("xbuf", [128, KC, TT], F32)
    c["hT"] = S.sb("hT", [128, KC, TT], BF16)
    c["tmpA"] = S.sb("tmpA", [128, TT], F32)
    c["tmpB"] = S.sb("tmpB", [128, TT], F32)
    c["rt"] = S.sb("rt", [128, 4, TT], F32)
    c["n1g"] = S.sb("n1g", [128, KC], F32)
    return c


def alloc_kv(cx, c):
    S = cx.S
    c["kst"] = [S.sb(f"kst{i}", [128, 2, TT], BF16) for i in range(2)]
    c["vst"] = [S.sb(f"vst{i}", [128, 256], BF16) for i in range(2)]
    c["lf"] = S.sb("lf", [128, TT], F32)
    c["nbfg"] = S.sb("nbfg", [128, 1], F32)
    return c


def dram_in(nc, name, shape, dtype=F32):
    return DT(nc.dram_tensor(name, list(shape), dtype, kind="ExternalInput").ap())


def dram_out(nc, name, shape, dtype=F32):
    return DT(nc.dram_tensor(name, list(shape), dtype, kind="ExternalOutput").ap(), dj=True)


KV_OUT = (("kTa", [512, LTOK], BF16), ("kTb", [512, LTOK], BF16), ("kTc", [512, LTOK], BF16),
          ("kTd", [128, LTOK], BF16), ("ikT", [64, LTOK], BF16), ("logf", [4, LTOK], F32),
          ("va", [LTOK, 512], BF16), ("vb", [LTOK, 512], BF16), ("vc", [LTOK, 512], BF16),
          ("vd", [LTOK, 128], BF16))


def build_A():
    nc = bass.Bass("TRN2", target_bir_lowering=False)
    es = ExitStack()
    io = {}
    xT = dram_in(nc, "xT", [D_MODEL, LTOK])
    io["wk"] = dram_in(nc, "wk", [D_MODEL, WK_COLS])
    io["wv"] = dram_in(nc, "wv", [D_MODEL, WV_COLS])
    io["ropet"] = dram_in(nc, "ropet", [4, 128, LTOK])
    n1g = dram_in(nc, "n1g", [128, KC])
    bfg = dram_in(nc, "bfg", [4, 1])
    for nm, shp, dt in KV_OUT:
        io[nm] = dram_out(nc, nm, shp, dt)
    with es:
        S = Sched(nc, es)
        cx = Ctx(S)
        c = alloc_kv(cx, alloc_common(cx))
        S.dma("sp", out=c["n1g"][:, :], in_=n1g.ap[:, :], writes=[c["n1g"]])
        S.dma("sp", out=c["nbfg"][64:68, :], in_=bfg.ap[:, :], writes=[c["nbfg"]])
        S.op("dve", lambda e: e.tensor_scalar(out=c["nbfg"][64:68, :], in0=c["nbfg"][64:68, :], scalar1=-1.0,
                                              scalar2=None, op0=ALU.mult), reads=[c["nbfg"]], writes=[c["nbfg"]])
        phase_kv(cx, io, lambda t: (xT.ap[:, t * TT:(t + 1) * TT], xT.t), c)
        S.final_all()
        S.emit()
    return nc


SEG_W = (("a_q", 512), ("a_k", 512), ("a_v", 512), ("b_q", 512), ("b_k", 512), ("b_v", 512),
         ("c_q", 512), ("c_k", 512), ("c_v", 512), ("c_f", 4), ("d_q", 512), ("d_k", 128), ("d_v", 128),
         ("d_iq", 512), ("d_ik", 64), ("d_iw", 8), ("gate", 8192))
SEG = {}
_o = 0
for _n, _w in SEG_W:
    SEG[_n] = (_o, _o + _w)
    _o += _w


def wcols(w, names):
    return np.ascontiguousarray(np.concatenate([w[:, SEG[n][0]:SEG[n][1]] for n in names], axis=1))


def local_pos(c):
    lb = np.arange(LBLK)
    return ((2 * lb + c)[:, None] * 128 + np.arange(128)[None, :]).reshape(-1)


def rope_tabs(c):
    pos = local_pos(c).astype(np.float32)
    out = []
    for dim in (128, 64):
        inv = (np.float32(10000.0) ** (-np.arange(0, dim, 2, dtype=np.float32) / np.float32(dim))).astype(np.float32)
        ang = pos[:, None] * inv[None, :]
        cos = np.cos(ang).astype(np.float32).T
        sin = np.sin(ang).astype(np.float32).T
        rep = 128 // dim
        out.append(np.concatenate([cos, cos] * rep, axis=0))
        out.append(np.concatenate([sin, -sin] * rep, axis=0))
    return np.ascontiguousarray(np.stack(out, 0))


def to_local_T(xb, c):
    blk = xb.reshape(NBLK, 128, -1)[c::2].reshape(LTOK, -1)
    return np.ascontiguousarray(blk.T)


def pcol(v):
    return np.ascontiguousarray(v.reshape(-1, 128).T)


IWS = (8 ** -0.5) * (64 ** -0.5)
KV_ALL = (("kTa", [2, 512, LTOK], BF16), ("kTb", [2, 512, LTOK], BF16), ("kTc", [2, 512, LTOK], BF16),
          ("kTd", [2, 128, LTOK], BF16), ("ikT", [2, 64, LTOK], BF16), ("logf", [2, 4, LTOK], F32),
          ("va", [2, LTOK, 512], BF16), ("vb", [2, LTOK, 512], BF16), ("vc", [2, LTOK, 512], BF16),
          ("vd", [2, LTOK, 128], BF16))


def alloc_q(cx, c):
    S = cx.S
    c["kTd"] = S.sb("kTd_sb", [128, SEQ], BF16)
    c["ikT2"] = S.sb("ikT2", [128, SEQ], BF16)
    c["vd"] = S.sb("vd_sb", [128, NBLK, 128], BF16)
    c["score"] = S.sb("score", [128, SEQ], F32)
    c["negcum"] = S.sb("negcum", [128, NBLK * 4], F32)
    c["cum_mine"] = S.sb("cum_mine", [4, LTOK], F32)
    c["cumq"] = S.sb("cumq", [128, 4, TT], F32)
    c["biasA"] = S.sb("biasA", [128, 6, 512], BF16)
    c["mk"] = S.sb("mk", [128, 4, 128], BF16)
    c["idxmask"] = S.sb("idxmask", [128, 256], F32)
    c["ident"] = S.sb("ident", [128, 128], BF16)
    c["identf"] = S.sb("identf", [128, 128], F32)
    c["onesf"] = S.sb("onesf", [128, 256], F32)
    c["selh"] = S.sb("selh", [4, 512], F32)
    c["sel"] = S.sb("sel", [4, 2], F32)
    c["n2g"] = S.sb("n2g", [128, KC], F32)
    c["fg"] = S.sb("fg", [128, KC], F32)
    c["bgate"] = S.sb("bgate", [128, 64], F32)
    c["dng"] = S.sb("dng", [128, 1], F32)
    c["neglam"] = S.sb("neglam", [128, 1], F32)
    c["lamv"] = S.sb("lamv", [64, 4], F32)
    c["lamp"] = S.sb("lamp", [128, 2], F32)
    c["wiw"] = S.sb("wiw", [128, KC, 8], BF16)
    c["q"] = S.sb("q", [128, 4, TT], BF16)
    c["iq"] = S.sb("iq", [128, 4, TT], BF16)
    c["qz"] = [S.sb(f"qz{i}", [128, 4, TT], BF16) for i in range(2)]
    c["iw"] = S.sb("iw", [128, 16], F32)
    c["o"] = [S.sb(f"o{i}", [128, 4, TT], BF16) for i in range(4)]
    c["mg"] = S.sb("mg", [128, KC, TT], BF16)
    c["u"] = S.sb("u", [128, 32, TT], BF16)
    c["kch"] = [S.sb(f"kch{i}", [128, 4, 512], BF16) for i in range(2)]
    c["vch"] = [S.sb(f"vch{i}", [128, 4, 512], BF16) for i in range(2)]
    c["pT"] = [S.sb(f"pT{i}", [128, 512], BF16) for i in range(3)]
    c["sp"] = [S.sb(f"sp{i}", [128, 512], F32) for i in range(2)]
    c["nm"] = [S.sb(f"nm{i}", [128, 512], BF16) for i in range(2)]
    c["nmT"] = [S.sb(f"nmT{i}", [128, 4, 128], BF16) for i in range(2)]
    c["rden"] = S.sb("rden", [128, 512], F32)
    c["obr"] = S.sb("obr", [128, TT], F32)
    c["acc"] = [S.sb(f"acc{i}", [128, TT], F32) for i in range(2)]
    c["m8"] = S.sb("m8", [128, 8], F32)
    c["ost"] = [S.sb(f"ost{i}", [128, TT], F32) for i in range(2)]
    c["psS"] = [S.ps(f"psS{i}", [128, 512], F32) for i in range(2)]
    c["psO"] = S.ps("psO", [128, 512], F32)
    c["psD"] = S.ps("psD", [128, 512], F32)
    c["psm"] = S.ps("psm", [128, 512], F32)
    c["psT"] = S.ps("psT", [128, 512], BF16)
    c["rr"] = {"S": 0, "pT": 0, "sp": 0, "kv": 0, "nm": 0}
    return c


def rr(c, name, key):
    lst = c[name]
    i = c["rr"][key]
    c["rr"][key] = i + 1
    return lst[i % len(lst)]


def setup_q(cx, c, io, lambda_init):
    S = cx.S
    pq = "pool"
    S.dma(pq, out=c["biasA"][:, :, :], in_=io["biasA"].ap[:, :].rearrange("p (b f) -> p b f", b=6),
          reads=[io["biasA"]], writes=[c["biasA"]])
    S.dma(pq, out=c["mk"][:, :, :], in_=io["mk"].ap[:, :].rearrange("p (b f) -> p b f", b=4), reads=[io["mk"]],
          writes=[c["mk"]])
    S.dma(pq, out=c["ident"][:, :], in_=io["identf"].ap[:, :], reads=[io["identf"]], writes=[c["ident"]])
    S.dma(pq, out=c["wiw"][:, :, :], in_=io["wiw"].ap[:, :].rearrange("(k p) n -> p k n", p=128), reads=[io["wiw"]],
          writes=[c["wiw"]])
    for nm in ("identf", "idxmask", "selh", "sel", "n2g", "fg", "bgate", "dng"):
        S.dma("sp", out=c[nm][:, :], in_=io[nm].ap[:, :], reads=[io[nm]], writes=[c[nm]])
    S.dma("sp", out=c["lamv"][:, :], in_=io["lam4"].ap[:, :], reads=[io["lam4"]], writes=[c["lamv"]])
    S.op("pool", lambda e: e.memset(c["onesf"][:, :], 1.0), writes=[c["onesf"]])
    S.op("dve", lambda e: e.tensor_scalar(out=c["dng"][:, :], in0=c["dng"][:, :], scalar1=float(1.0 - lambda_init),
                                          scalar2=None, op0=ALU.mult), reads=[c["dng"]], writes=[c["dng"]])
    x = c["xbuf"]
    xflat = x[0:4, :, :].rearrange("p k n -> p (k n)")
    for r in range(2):
        S.dma("sp", out=c["kTd"][:, :].rearrange("p (n r j) -> p n r j", r=2, j=128)[:, :, r, :],
              in_=io["kTd_all"].ap[r].rearrange("p (n j) -> p n j", j=128), reads=[io["kTd_all"]], writes=[c["kTd"]])
        for hf in range(2):
            S.dma("sp", out=c["ikT2"][hf * 64:(hf + 1) * 64, :].rearrange("p (n r j) -> p n r j", r=2, j=128)[:, :, r, :],
                  in_=io["ikT_all"].ap[r].rearrange("p (n j) -> p n j", j=128), reads=[io["ikT_all"]],
                  writes=[c["ikT2"]])
        S.dma("sp", out=c["vd"][:, :, :].rearrange("p (n r) d -> p n r d", r=2)[:, :, r, :],
              in_=io["vd_all"].ap[r].rearrange("(n p) d -> p n d", p=128), reads=[io["vd_all"]], writes=[c["vd"]])
        S.dma("sp", out=xflat.rearrange("p (n r j) -> p n r j", r=2, j=128)[:, :, r, :],
              in_=io["logf_all"].ap[r].rearrange("p (n j) -> p n j", j=128), reads=[io["logf_all"]], writes=[x])
    score = c["score"]
    CH = 256
    for i in range(SEQ // CH):
        init = 0.0 if i == 0 else score[0:4, i * CH - 1:i * CH]
        S.op("dve", lambda e, i=i, init=init: e.tensor_tensor_scan(
            out=score[0:4, i * CH:(i + 1) * CH], data0=c["onesf"][0:4, 0:CH], data1=xflat[:, i * CH:(i + 1) * CH],
            initial=init, op0=ALU.mult, op1=ALU.add), reads=[x, c["onesf"], score], writes=[score])
    psm = c["psm"]
    for kb in range(NBLK):
        S.op("pe", lambda e, kb=kb: e.transpose(out=psm[:, kb * 4:(kb + 1) * 4], in_=score[0:4, kb * 128:(kb + 1) * 128],
                                                identity=c["identf"][0:4, 0:4]),
             reads=[score, c["identf"]], writes=[psm])
    S.op("act", lambda e: e.mul(out=c["negcum"][:, :], in_=psm[:, 0:NBLK * 4], mul=-1.0), reads=[psm],
         writes=[c["negcum"]])
    cv = score[0:4, :].rearrange("p (n r j) -> p n r j", r=2, j=128)
    cm = c["cum_mine"][0:4, :].rearrange("p (n j) -> p n j", j=128)
    S.op("dve", lambda e: e.tensor_scalar(out=cm, in0=cv[:, :, 0, :], scalar1=c["sel"][0:4, 0:1], scalar2=None,
                                          op0=ALU.mult), reads=[score, c["sel"]], writes=[c["cum_mine"]])
    S.op("dve", lambda e: e.scalar_tensor_tensor(out=cm, in0=cv[:, :, 1, :], scalar=c["sel"][0:4, 1:2], op0=ALU.mult,
                                                 in1=cm, op1=ALU.add), reads=[score, c["sel"], c["cum_mine"]],
         writes=[c["cum_mine"]])
    lv, lp = c["lamv"], c["lamp"]
    S.op("dve", lambda e: e.tensor_tensor(out=lp[0:64, 0:1], in0=lv[0:64, 0:1], in1=lv[0:64, 1:2], op=ALU.mult),
         reads=[lv], writes=[lp])
    S.op("dve", lambda e: e.tensor_tensor(out=lp[0:64, 1:2], in0=lv[0:64, 2:3], in1=lv[0:64, 3:4], op=ALU.mult),
         reads=[lv, lp], writes=[lp])
    S.op("pe", lambda e: e.matmul(psm[:, 0:2], lhsT=c["onesf"][0:64, 0:128], rhs=lp[0:64, 0:2], start=True, stop=True),
         reads=[c["onesf"], lp], writes=[psm])
    S.op("act", lambda e: e.activation(out=lp[:, 0:2], in_=psm[:, 0:2], func=AF.Exp), reads=[psm], writes=[lp])
    S.op("dve", lambda e: e.tensor_tensor(out=c["neglam"][:, :], in0=lp[:, 1:2], in1=lp[:, 0:1], op=ALU.subtract),
         reads=[lp], writes=[c["neglam"]])
    S.op("dve", lambda e: e.tensor_scalar(out=c["neglam"][:, :], in0=c["neglam"][:, :], scalar1=float(-lambda_init),
                                          scalar2=None, op0=ALU.add), reads=[c["neglam"]], writes=[c["neglam"]])


def q_proj(cx, c, io, col0, dst, mode, scale=None):
    hT, tmpA, tmpB, rt = c["hT"], c["tmpA"], c["tmpB"], c["rt"]
    for blk in range(2):
        w = load_w(cx, io["wq"].ap, col0 + blk * 256, 256, wtok=io["wq"].t)
        for j in range(2):
            ps = cx.next_ps()
            proj_fm(cx, w, j * 128, 128, hT, ps)
            if mode == "plain":
                evac(cx, dst[:, blk * 2 + j, :], ps[:, 0:TT], [ps], [dst], scale=scale)
            elif mode == "rope64z":
                qz = c["qz"]
                jj = blk * 2 + j
                rope_fm(cx, ps, 128, 32, View(rt, 2), View(rt, 3),
                        [(0, 64, qz[0][0:64, jj, :], [qz[0]]), (64, 128, qz[1][64:128, jj, :], [qz[1]])], None,
                        tmpA, tmpB)
            elif mode == "rope64":
                rope_fm(cx, ps, 128, 32, View(rt, 2), View(rt, 3), dst[:, blk * 2 + j, :], [dst], tmpA, tmpB)
            else:
                rope_fm(cx, ps, 128, 64, View(rt, 0), View(rt, 1), dst[:, blk * 2 + j, :], [dst], tmpA, tmpB)


def attn_A(cx, c, io, t):
    S = cx.S
    q, o = c["q"], c["o"][0]
    psO, psD, rden = c["psO"], c["psD"], c["rden"]
    for m in range(2):
        lb = 2 * t + m
        n0 = max(0, lb - 2)
        nn = lb - n0 + 1
        mc = slice(m * 128, (m + 1) * 128)
        kb_, vb_ = c["kch"], c["vch"]
        for r in range(2):
            S.dma("sp", out=kb_[r][:, :, 0:nn * 128],
                  in_=io["kTa_all"].ap[r].rearrange("(h p) n -> p h n", p=128)[:, :, n0 * 128:(lb + 1) * 128],
                  reads=[io["kTa_all"]], writes=[kb_[r]])
            S.dma("sp", out=vb_[r][:, 0:nn, :],
                  in_=io["va_all"].ap[r][n0 * 128:(lb + 1) * 128, :].rearrange("(n p) f -> p n f", p=128),
                  reads=[io["va_all"]], writes=[vb_[r]])
        tot = 2 * nn
        k = 0
        for r in range(2):
            for ni in range(nn):
                nrel = n0 + ni - (lb - 2)
                pss = rr(c, "psS", "S")
                pT = rr(c, "pT", "pT")
                for h in range(4):
                    S.op("pe", lambda e, h=h, r=r, ni=ni, pss=pss: e.matmul(
                        pss[:, h * 128:(h + 1) * 128], lhsT=kb_[r][:, h, ni * 128:(ni + 1) * 128], rhs=q[:, h, mc],
                        start=(h == 0), stop=False, skip_group_check=True), reads=[kb_[r], q], writes=[pss])
                S.op("pe", lambda e, r=r, nrel=nrel, pss=pss: e.matmul(
                    pss[:, :], lhsT=c["ident"][:, :], rhs=c["biasA"][:, r * 3 + nrel, :], start=False, stop=True,
                    skip_group_check=True), reads=[c["ident"], c["biasA"]], writes=[pss])
                S.op("act", lambda e, pss=pss, pT=pT: e.activation(out=pT[:, :], in_=pss[:, :], func=AF.Exp),
                     reads=[pss], writes=[pT])
                for h in range(4):
                    S.op("pe", lambda e, h=h, r=r, ni=ni, pT=pT, k=k: e.matmul(
                        psO[:, h * 128:(h + 1) * 128], lhsT=vb_[r][:, ni, h * 128:(h + 1) * 128],
                        rhs=pT[:, h * 128:(h + 1) * 128], start=(k == 0 and h == 0), stop=(k == tot - 1 and h == 3),
                        skip_group_check=True), reads=[vb_[r], pT], writes=[psO])
                S.op("pe", lambda e, pT=pT, k=k: e.matmul(psD[:, :], lhsT=cx.ones_bf[:, :], rhs=pT[:, :],
                                                           start=(k == 0), stop=(k == tot - 1)),
                     reads=[cx.ones_bf, pT], writes=[psD])
                k += 1
        S.op("dve", lambda e: e.reciprocal(out=rden[:, :], in_=psD[:, :]), reads=[psD], writes=[rden])
        S.op("dve", lambda e, mc=mc: e.tensor_tensor(out=o[:, :, mc], in0=psO[:, :].rearrange("p (h q) -> p h q", h=4),
                                                     in1=rden[:, :].rearrange("p (h q) -> p h q", h=4), op=ALU.mult),
             reads=[psO, rden], writes=[o])


def kv_chunks(t):
    nb_r = 2 * t + 2
    out = []
    for r in range(2):
        for cst in range(0, nb_r, 4):
            out.append((r, cst, min(4, nb_r - cst)))
    return out


def attn_BC(cx, c, io, t, kind):
    S = cx.S
    q = c["q"]
    psO, psD, rden = c["psO"], c["psD"], c["rden"]
    kname, vname = ("kTb_all", "vb_all") if kind == "B" else ("kTc_all", "vc_all")
    o = c["o"][1] if kind == "B" else c["o"][2]
    chunks = kv_chunks(t)
    nblocks = sum(nb for _, _, nb in chunks)
    ngroups = 4 if kind == "B" else 2
    for g in range(ngroups):
        k = 0
        for (r, cst, nb) in chunks:
            kc_ = rr(c, "kch", "kv")
            vc_ = c["vch"][(c["rr"]["kv"] - 1) % 2]
            cols = slice(cst * 128, (cst + nb) * 128)
            if kind == "B":
                S.dma("sp", out=kc_[:, 0, 0:nb * 128], in_=io[kname].ap[r][g * 128:(g + 1) * 128, cols],
                      reads=[io[kname]], writes=[kc_])
            else:
                S.dma("sp", out=kc_[:, 0:2, 0:nb * 128],
                      in_=io[kname].ap[r].rearrange("(h p) n -> p h n", p=128)[:, 2 * g:2 * g + 2, cols],
                      reads=[io[kname]], writes=[kc_])
            S.dma("sp", out=vc_[:, 0:nb, :], in_=io[vname].ap[r][cols, :].rearrange("(n p) f -> p n f", p=128),
                  reads=[io[vname]], writes=[vc_])
            for bi in range(nb):
                n = cst + bi
                npr = n - 2 * t
                q0 = 128 if npr == 1 else 0
                pss = rr(c, "psS", "S")
                pT = rr(c, "pT", "pT")
                bsl = slice(bi * 128, (bi + 1) * 128)
                first, last = (k == 0), (k == nblocks - 1)
                for s in range(2):
                    osl = slice(s * 256 + q0, (s + 1) * 256)
                    if kind == "B":
                        ps_ = slice(s * 64, (s + 1) * 64)
                        S.op("pe", lambda e, s=s, osl=osl, bsl=bsl, pss=pss, kc_=kc_, npr=npr: e.matmul(
                            pss[:, osl], lhsT=kc_[:, 0, bsl], rhs=c["qz"][s][:, g, q0:TT], start=(s == 0),
                            stop=(s == 1 and npr < 0), skip_group_check=True), reads=[kc_, c["qz"][s]], writes=[pss])
                    else:
                        S.op("pe", lambda e, s=s, osl=osl, bsl=bsl, pss=pss, kc_=kc_, npr=npr: e.matmul(
                            pss[:, osl], lhsT=kc_[:, s, bsl], rhs=q[:, 2 * g + s, q0:TT], start=(s == 0),
                            stop=(s == 1 and npr < 0), skip_group_check=True), reads=[kc_, q], writes=[pss])
                if npr >= 0:
                    mi = r if kind == "B" else 2 + r
                    for s in range(2):
                        msl = slice(s * 256 + npr * 128, s * 256 + (npr + 1) * 128)
                        S.op("pe", lambda e, s=s, msl=msl, mi=mi, pss=pss: e.matmul(
                            pss[:, msl], lhsT=c["ident"][:, :], rhs=c["mk"][:, mi, :], start=False, stop=(s == 1),
                            skip_group_check=True), reads=[c["ident"], c["mk"]], writes=[pss])
                v3 = lambda ap: ap.rearrange("p (s q) -> p s q", s=2)[:, :, q0:TT]
                if kind == "B":
                    S.op("act", lambda e, pss=pss, pT=pT, v3=v3: e.activation(out=v3(pT[:, :]), in_=v3(pss[:, :]),
                                                                             func=AF.Exp, scale=64 ** -0.5),
                         reads=[pss], writes=[pT])
                else:
                    sp = rr(c, "sp", "sp")
                    kbt = 2 * n + r
                    for s in range(2):
                        h = 2 * g + s
                        osl = slice(s * 256 + q0, (s + 1) * 256)
                        S.op("dve", lambda e, osl=osl, h=h, kbt=kbt, pss=pss, sp=sp: e.scalar_tensor_tensor(
                            out=sp[:, osl], in0=pss[:, osl], scalar=c["negcum"][:, kbt * 4 + h:kbt * 4 + h + 1],
                            op0=ALU.add, in1=c["cumq"][:, h, q0:TT], op1=ALU.add),
                             reads=[pss, c["negcum"], c["cumq"]], writes=[sp])
                    S.op("act", lambda e, sp=sp, pT=pT, v3=v3: e.activation(out=v3(pT[:, :]), in_=v3(sp[:, :]),
                                                                           func=AF.Exp), reads=[sp], writes=[pT])
                if kind == "B":
                    S.op("pe", lambda e, pT=pT, vc_=vc_, bi=bi, first=first, last=last, v3=v3: e.matmul(
                        v3(psO[:, :]), lhsT=vc_[:, bi, g * 128:(g + 1) * 128], rhs=v3(pT[:, :]), start=first, stop=last,
                        skip_group_check=True), reads=[vc_, pT], writes=[psO])
                else:
                    for s in range(2):
                        h = 2 * g + s
                        osl = slice(s * 256 + q0, (s + 1) * 256)
                        S.op("pe", lambda e, s=s, h=h, osl=osl, pT=pT, vc_=vc_, bi=bi, first=first, last=last: e.matmul(
                            psO[:, osl], lhsT=vc_[:, bi, h * 128:(h + 1) * 128], rhs=pT[:, osl],
                            start=(first and s == 0), stop=(last and s == 1), skip_group_check=True),
                             reads=[vc_, pT], writes=[psO])
                S.op("pe", lambda e, pT=pT, first=first, last=last, v3=v3: e.matmul(
                    v3(psD[:, :]), lhsT=cx.ones_bf[:, :], rhs=v3(pT[:, :]), start=first, stop=last,
                    skip_group_check=True), reads=[cx.ones_bf, pT], writes=[psD])
                k += 1
        S.op("dve", lambda e: e.reciprocal(out=rden[:, :], in_=psD[:, :]), reads=[psD], writes=[rden])
        if kind == "C":
            S.op("dve", lambda e, g=g: e.tensor_tensor(
                out=o[:, 2 * g:2 * g + 2, :], in0=psO[:, :].rearrange("p (s q) -> p s q", s=2),
                in1=rden[:, :].rearrange("p (s q) -> p s q", s=2), op=ALU.mult), reads=[psO, rden], writes=[o])
        else:
            obr, tA, tB = c["obr"], c["tmpA"], c["tmpB"]
            S.op("dve", lambda e: e.tensor_tensor(out=obr[:, :], in0=psO[:, 0:TT], in1=rden[:, 0:TT], op=ALU.mult),
                 reads=[psO, rden], writes=[obr])
            S.op("dve", lambda e: e.tensor_tensor(out=tA[:, :], in0=psO[:, TT:2 * TT], in1=rden[:, TT:2 * TT],
                                                  op=ALU.mult), reads=[psO, rden], writes=[tA])
            S.op("dve", lambda e: e.scalar_tensor_tensor(out=obr[:, :], in0=tA[:, :], scalar=c["neglam"][:, 0:1],
                                                         op0=ALU.mult, in1=obr[:, :], op1=ALU.add),
                 reads=[tA, c["neglam"], obr], writes=[obr])
            sq = cx.sqb[0]
            psm = c["psm"]
            S.op("act", lambda e: e.activation(out=sq[:, :], in_=obr[:, :], func=AF.Square), reads=[obr], writes=[sq])
            S.op("pe", lambda e: e.matmul(psm[:, 0:TT], lhsT=cx.ones_bf[:, :], rhs=sq[:, :], start=True, stop=True),
                 reads=[cx.ones_bf, sq], writes=[psm])
            S.op("act", lambda e: e.activation(out=tB[:, :], in_=psm[:, 0:TT], func=AF.Sqrt, bias=EPS, scale=1.0 / 128),
                 reads=[psm], writes=[tB])
            S.op("dve", lambda e: e.reciprocal(out=tB[:, :], in_=tB[:, :]), reads=[tB], writes=[tB])
            S.op("dve", lambda e, g=g: e.scalar_tensor_tensor(out=o[:, g, :], in0=obr[:, :], scalar=c["dng"][:, 0:1],
                                                              op0=ALU.mult, in1=tB[:, :], op1=ALU.mult),
                 reads=[obr, c["dng"], tB], writes=[o])


def attn_D(cx, c, io, t):
    S = cx.S
    q, iq, o, score = c["q"], c["iq"], c["o"][3], c["score"]
    psO, psD, psT, rden, m8 = c["psO"], c["psD"], c["psT"], c["rden"], c["m8"]
    for m in range(2):
        lb = 2 * t + m
        nk = (2 * lb + 2) * 128
        mc = slice(m * 128, (m + 1) * 128)
        for cc in range(0, nk, 512):
            ncol = min(512, nk - cc)
            for ih in range(8):
                psI = rr(c, "psS", "S")
                sp = rr(c, "sp", "sp")
                prow = slice((ih % 2) * 64, (ih % 2 + 1) * 64)
                S.op("pe", lambda e, psI=psI, prow=prow, ih=ih, cc=cc, ncol=ncol: e.matmul(
                    psI[:, 0:ncol], lhsT=iq[prow, ih // 2, mc], rhs=c["ikT2"][prow, cc:cc + ncol], start=True, stop=True),
                     reads=[iq, c["ikT2"]], writes=[psI])
                S.op("act", lambda e, psI=psI, sp=sp, ncol=ncol: e.activation(out=sp[:, 0:ncol], in_=psI[:, 0:ncol],
                                                                              func=AF.Relu), reads=[psI], writes=[sp])
                iwc = c["iw"][:, m * 8 + ih:m * 8 + ih + 1]
                if ih == 0:
                    S.op("dve", lambda e, sp=sp, cc=cc, ncol=ncol, iwc=iwc: e.tensor_scalar(
                        out=score[:, cc:cc + ncol], in0=sp[:, 0:ncol], scalar1=iwc, scalar2=None, op0=ALU.mult),
                         reads=[sp, c["iw"]], writes=[score])
                else:
                    S.op("dve", lambda e, sp=sp, cc=cc, ncol=ncol, iwc=iwc: e.scalar_tensor_tensor(
                        out=score[:, cc:cc + ncol], in0=sp[:, 0:ncol], scalar=iwc, op0=ALU.mult,
                        in1=score[:, cc:cc + ncol], op1=ALU.add), reads=[sp, c["iw"], score], writes=[score])
        S.op("dve", lambda e, nk=nk: e.tensor_tensor(out=score[:, nk - 256:nk], in0=score[:, nk - 256:nk],
                                                     in1=c["idxmask"][:, :], op=ALU.add),
             reads=[score, c["idxmask"]], writes=[score])
        if lb >= 1:
            for rd in range(32):
                S.op("dve", lambda e, nk=nk: e.max(out=m8[:, :], in_=score[:, 0:nk]), reads=[score], writes=[m8])
                S.op("dve", lambda e, nk=nk: e.match_replace(out=score[:, 0:nk], in_to_replace=m8[:, :],
                                                             in_values=score[:, 0:nk], imm_value=-3.0e38),
                     reads=[score, m8], writes=[score])
        nkb = nk // 128
        k = 0
        for cc in range(0, nkb, 4):
            nb = min(4, nkb - cc)
            nm = rr(c, "nm", "nm")
            nmT = c["nmT"][(c["rr"]["nm"] - 1) % 2]
            csl = slice(cc * 128, (cc + nb) * 128)
            if lb >= 1:
                S.op("dve", lambda e, nm=nm, csl=csl, nb=nb: e.tensor_scalar(
                    out=nm[:, 0:nb * 128], in0=score[:, csl], scalar1=-1.0e37, scalar2=NEG, op0=ALU.is_gt, op1=ALU.mult),
                     reads=[score], writes=[nm])
            else:
                S.op("dve", lambda e, nm=nm, csl=csl, nb=nb: e.tensor_scalar(
                    out=nm[:, 0:nb * 128], in0=score[:, csl], scalar1=-1.0e29, scalar2=NEG, op0=ALU.is_lt, op1=ALU.mult),
                     reads=[score], writes=[nm])
            for bi in range(nb):
                S.op("pe", lambda e, nm=nm, bi=bi: e.transpose(out=psT[:, bi * 128:(bi + 1) * 128],
                                                               in_=nm[:, bi * 128:(bi + 1) * 128],
                                                               identity=c["ident"][:, :]),
                     reads=[nm, c["ident"]], writes=[psT])
            S.op("act", lambda e, nmT=nmT, nb=nb: e.activation(
                out=nmT[:, 0:nb, :], in_=psT[:, 0:nb * 128].rearrange("p (b q) -> p b q", q=128), func=AF.Copy),
                 reads=[psT], writes=[nmT])
            for bi in range(nb):
                kb = cc + bi
                pss = rr(c, "psS", "S")
                pT = rr(c, "pT", "pT")
                first, last = (k == 0), (k == nkb - 1)
                S.op("pe", lambda e, kb=kb, pss=pss: e.matmul(
                    pss[:, :].rearrange("p (h q) -> p h q", h=4), lhsT=c["kTd"][:, kb * 128:(kb + 1) * 128],
                    rhs=q[:, :, mc], start=True, stop=False, skip_group_check=True), reads=[c["kTd"], q], writes=[pss])
                S.op("pe", lambda e, bi=bi, nmT=nmT, pss=pss: e.matmul(
                    pss[:, :].rearrange("p (h q) -> p h q", h=4), lhsT=c["ident"][:, :],
                    rhs=nmT[:, bi:bi + 1, :].broadcast_to([128, 4, 128]), start=False, stop=True, skip_group_check=True),
                     reads=[c["ident"], nmT], writes=[pss])
                S.op("act", lambda e, pss=pss, pT=pT: e.activation(out=pT[:, :], in_=pss[:, :], func=AF.Exp,
                                                                  scale=128 ** -0.5), reads=[pss], writes=[pT])
                S.op("pe", lambda e, kb=kb, pT=pT, first=first, last=last: e.matmul(
                    psO[:, :], lhsT=c["vd"][:, kb, :], rhs=pT[:, :], start=first, stop=last), reads=[c["vd"], pT],
                     writes=[psO])
                S.op("pe", lambda e, pT=pT, first=first, last=last: e.matmul(
                    psD[:, :], lhsT=cx.ones_bf[:, :], rhs=pT[:, :], start=first, stop=last), reads=[cx.ones_bf, pT],
                     writes=[psD])
                k += 1
        S.op("dve", lambda e: e.reciprocal(out=rden[:, :], in_=psD[:, :]), reads=[psD], writes=[rden])
        S.op("dve", lambda e, mc=mc: e.tensor_tensor(out=o[:, :, mc], in0=psO[:, :].rearrange("p (h q) -> p h q", h=4),
                                                     in1=rden[:, :].rearrange("p (h q) -> p h q", h=4), op=ALU.mult),
             reads=[psO, rden], writes=[o])


def dense_tail(cx, c, io, t, x_dst, last_layer):
    S = cx.S
    x, hT, mg, u, acc = c["xbuf"], c["hT"], c["mg"], c["u"], c["acc"]
    sp = c["sp"]
    for j2 in range(8):
        for i in range(4):
            wg = load_w(cx, io["wg"].ap, i * 2048 + j2 * 256, 256, wtok=io["wg"].t)
            wb = load_w(cx, io["wbr"].ap, j2 * 256, 256, nk=4, row0=i * 512, wtok=io["wbr"].t)
            for j in range(2):
                psg = cx.next_ps()
                proj_fm(cx, wg, j * 128, 128, hT, psg)
                psb = cx.next_ps()
                proj_fm(cx, wb, j * 128, 128, c["o"][i], psb, nk=4)
                sg = sp[j]
                bcol = i * 16 + j2 * 2 + j
                S.op("act", lambda e, psg=psg, sg=sg, bcol=bcol: e.activation(
                    out=sg[:, 0:TT], in_=psg[:, 0:TT], func=AF.Sigmoid, bias=c["bgate"][:, bcol:bcol + 1]),
                     reads=[psg, c["bgate"]], writes=[sg])
                if i == 0:
                    S.op("dve", lambda e, psb=psb, sg=sg, j=j: e.tensor_tensor(out=acc[j][:, :], in0=psb[:, 0:TT],
                                                                              in1=sg[:, 0:TT], op=ALU.mult),
                         reads=[psb, sg], writes=[acc[j]])
                else:
                    S.op("dve", lambda e, psb=psb, sg=sg: e.tensor_tensor(out=sg[:, 0:TT], in0=psb[:, 0:TT],
                                                                         in1=sg[:, 0:TT], op=ALU.mult),
                         reads=[psb, sg], writes=[sg])
                    if i < 3:
                        S.op("pool", lambda e, sg=sg, j=j: e.tensor_tensor(out=acc[j][:, :], in0=acc[j][:, :],
                                                                          in1=sg[:, 0:TT], op=ALU.add),
                             reads=[acc[j], sg], writes=[acc[j]])
                    else:
                        jj = j2 * 2 + j
                        S.op("pool", lambda e, sg=sg, j=j, jj=jj: e.tensor_tensor(out=mg[:, jj, :], in0=acc[j][:, :],
                                                                                 in1=sg[:, 0:TT], op=ALU.add),
                             reads=[acc[j], sg], writes=[mg.s(jj)])
    for j2 in range(8):
        w = load_w(cx, io["wo"].ap, j2 * 256, 256, wtok=io["wo"].t)
        for j in range(2):
            jj = j2 * 2 + j
            ps = cx.next_ps()
            proj_fm(cx, w, j * 128, 128, mg, ps)
            S.op("dve", lambda e, ps=ps, jj=jj: e.tensor_tensor(out=x[:, jj, :], in0=ps[:, 0:TT], in1=x[:, jj, :],
                                                                op=ALU.add), reads=[ps, x], writes=[x])
    rmsnorm_fm(cx, x, c["n2g"], hT, c["psm"])
    for half in range(2):
        for j2 in range(16):
            w = load_w(cx, io["wf1"].ap, half * 4096 + j2 * 256, 256, wtok=io["wf1"].t)
            for j in range(2):
                jj = j2 * 2 + j
                ps = cx.next_ps()
                proj_fm(cx, w, j * 128, 128, hT, ps)
                r_ = sp[j]
                S.op("act", lambda e, ps=ps, r_=r_: e.activation(out=r_[:, 0:TT], in_=ps[:, 0:TT], func=AF.Relu),
                     reads=[ps], writes=[r_])
                S.op("pool", lambda e, r_=r_, jj=jj: e.tensor_tensor(out=u[:, jj, :], in0=r_[:, 0:TT], in1=r_[:, 0:TT],
                                                                    op=ALU.mult), reads=[r_], writes=[u.s(jj)])
        for j2 in range(8):
            ws = [load_w(cx, io["wf2"].ap, j2 * 256, 256, nk=16, row0=half * 4096 + kk * 2048, wtok=io["wf2"].t)
                  for kk in range(2)]
            for j in range(2):
                jj = j2 * 2 + j
                ps = cx.next_ps()
                for kk in range(2):
                    for kc in range(16):
                        kidx = kk * 16 + kc
                        S.op("pe", lambda e, kk=kk, kc=kc, kidx=kidx, ps=ps, j=j: e.matmul(
                            ps[:, 0:TT], lhsT=ws[kk][:, kc, j * 128:(j + 1) * 128], rhs=u[:, kidx, :],
                            start=(kidx == 0), stop=(kidx == 31)), reads=[ws[kk], u.s(kidx)], writes=[ps])
                S.op("dve", lambda e, ps=ps, jj=jj: e.tensor_tensor(out=x[:, jj, :], in0=ps[:, 0:TT], in1=x[:, jj, :],
                                                                    op=ALU.add), reads=[ps, x], writes=[x])
    tc0 = t * TT
    if not last_layer:
        S.dma("sp", out=x_dst.ap[:, tc0:tc0 + TT].rearrange("(k p) n -> p k n", p=128), in_=x[:, :, :], reads=[x],
              writes=[x_dst])
    else:
        ps = c["psm"]
        rstd = cx.rstd
        for kc in range(KC):
            sq = cx.sqb[kc % 2]
            S.op("act", lambda e, kc=kc, sq=sq: e.activation(out=sq[:, :], in_=x[:, kc, :], func=AF.Square),
                 reads=[x], writes=[sq])
            S.op("pe", lambda e, kc=kc, sq=sq: e.matmul(ps[:, 0:TT], lhsT=cx.ones_bf[:, :], rhs=sq[:, :],
                                                        start=(kc == 0), stop=(kc == KC - 1)),
                 reads=[sq, cx.ones_bf], writes=[ps])
        S.op("act", lambda e: e.activation(out=rstd[:, :], in_=ps[:, 0:TT], func=AF.Sqrt, bias=EPS,
                                           scale=1.0 / D_MODEL), reads=[ps], writes=[rstd])
        S.op("dve", lambda e: e.reciprocal(out=rstd[:, :], in_=rstd[:, :]), reads=[rstd], writes=[rstd])
        for kc in range(KC):
            S.op("dve", lambda e, kc=kc: e.scalar_tensor_tensor(out=x[:, kc, :], in0=x[:, kc, :],
                                                                scalar=c["fg"][:, kc:kc + 1], op0=ALU.mult,
                                                                in1=rstd[:, :], op1=ALU.mult),
                 reads=[x, c["fg"], rstd], writes=[x])
        S.dma("sp", out=x_dst.ap[:, tc0:tc0 + TT].rearrange("(k p) n -> p k n", p=128), in_=x[:, :, :], reads=[x],
              writes=[x_dst])


def phase_q(cx, c, io, x_src, x_dst, lambda_init, last_layer, dbg=None):
    S = cx.S
    x, hT, psm = c["xbuf"], c["hT"], c["psm"]
    for i in range(2):
        S.op("pool", lambda e, i=i: e.memset(c["qz"][i][:, :, :], 0.0), writes=[c["qz"][i]])
    for t in range(NT):
        load_x_tile(cx, c, x_src, t, io["ropet"])
        rmsnorm_fm(cx, x, c["n1g"], hT, psm)
        for m in range(2):
            for kc in range(KC):
                S.op("pe", lambda e, m=m, kc=kc: e.matmul(psm[:, m * 8:(m + 1) * 8], lhsT=hT[:, kc, m * 128:(m + 1) * 128],
                                                          rhs=c["wiw"][:, kc, :], start=(kc == 0), stop=(kc == KC - 1)),
                     reads=[hT.s(kc), c["wiw"]], writes=[psm])
        S.op("act", lambda e: e.mul(out=c["iw"][:, :], in_=psm[:, 0:16], mul=IWS), reads=[psm], writes=[c["iw"]])
        q_proj(cx, c, io, 0, c["q"], "plain", scale=128 ** -0.5)
        attn_A(cx, c, io, t)
        q_proj(cx, c, io, 512, None, "rope64z")
        attn_BC(cx, c, io, t, "B")
        q_proj(cx, c, io, 1024, c["q"], "plain", scale=128 ** -0.5)
        for h in range(4):
            S.op("pe", lambda e, h=h, t=t: e.matmul(psm[:, 0:TT], lhsT=c["selh"][0:4, h * 128:(h + 1) * 128],
                                                    rhs=c["cum_mine"][0:4, t * TT:(t + 1) * TT], start=True, stop=True),
                 reads=[c["selh"], c["cum_mine"]], writes=[psm])
            S.op("act", lambda e, h=h: e.activation(out=c["cumq"][:, h, :], in_=psm[:, 0:TT], func=AF.Copy),
                 reads=[psm], writes=[c["cumq"]])
        attn_BC(cx, c, io, t, "C")
        q_proj(cx, c, io, 1536, c["q"], "rope128")
        q_proj(cx, c, io, 2048, c["iq"], "rope64")
        attn_D(cx, c, io, t)
        if dbg is not None:
            for i in range(4):
                S.dma("sp", out=dbg[i].ap[:, t * TT:(t + 1) * TT].rearrange("(h p) n -> p h n", p=128),
                      in_=c["o"][i][:, :, :], reads=[c["o"][i]], writes=[dbg[i]])
        dense_tail(cx, c, io, t, x_dst, last_layer)


Q_IN = (("wq", [D_MODEL, 2560]), ("wiw", [D_MODEL, 8]), ("wg", [D_MODEL, 8192]), ("wbr", [2048, D_MODEL]),
        ("wo", [D_MODEL, D_MODEL]), ("wf1", [D_MODEL, D_FF]), ("wf2", [D_FF, D_MODEL]),
        ("ropet", [4, 128, LTOK]), ("biasA", [128, 6 * 512]), ("mk", [128, 4 * 128]), ("identf", [128, 128]),
        ("idxmask", [128, 256]), ("selh", [4, 512]), ("sel", [4, 2]), ("n1g", [128, KC]), ("n2g", [128, KC]),
        ("fg", [128, KC]), ("bgate", [128, 64]), ("dng", [128, 1]), ("lam4", [64, 4]))


def build_B(layer, dbg=False):
    lambda_init = 0.8 - 0.6 * math.exp(-0.3 * layer)
    nc = bass.Bass("TRN2", target_bir_lowering=False)
    es = ExitStack()
    io = {}
    xT = dram_in(nc, "xT", [D_MODEL, LTOK])
    for nm, shp in Q_IN:
        io[nm] = dram_in(nc, nm, shp)
    for nm, shp, dt in KV_ALL:
        io[nm + "_all"] = dram_in(nc, nm + "_all", shp, dt)
    xo = dram_out(nc, "xo", [D_MODEL, LTOK])
    dbgs = [dram_out(nc, f"dbg_o{i}", [512, LTOK], BF16) for i in range(4)] if dbg else None
    with es:
        S = Sched(nc, es)
        cx = Ctx(S)
        c = alloc_q(cx, alloc_common(cx))
        S.dma("sp", out=c["n1g"][:, :], in_=io["n1g"].ap[:, :], reads=[io["n1g"]], writes=[c["n1g"]])
        setup_q(cx, c, io, lambda_init)
        phase_q(cx, c, io, lambda t: (xT.ap[:, t * TT:(t + 1) * TT], xT.t), xo, lambda_init, layer == DEPTH - 1, dbgs)
        S.final_all()
        S.emit()
    return nc


def table_biasA(rel_bias_l, c):
    out = np.full((128, 6, 4, 128), NEG, np.float32)
    j = np.arange(128)[:, None]
    qi = np.arange(128)[None, :]
    for r in range(2):
        for nrel in range(3):
            dblk = (2 * (nrel - 2) + r) - c
            dist = -dblk * 128 + qi - j
            kch = 2 * dblk + j // 64
            qch = qi // 64
            valid = (kch <= qch) & (kch >= qch - 8)
            idx = np.clip(dist, -128, 128) + 128
            for h in range(4):
                vals = rel_bias_l[h][idx]
                out[:, r * 3 + nrel, h, :] = np.where(valid, vals, np.float32(NEG))
    return np.ascontiguousarray(out.reshape(128, 6 * 512))


def table_mk(c):
    out = np.zeros((128, 4, 128), np.float32)
    j = np.arange(128)[:, None]
    qi = np.arange(128)[None, :]
    diagB = np.where(j // 64 <= qi // 64, 0.0, NEG).astype(np.float32)
    diagC = np.where(j <= qi, 0.0, NEG).astype(np.float32)
    for r in range(2):
        if r < c:
            mB = mC = np.zeros((128, 128), np.float32)
        elif r == c:
            mB, mC = diagB, diagC
        else:
            mB = mC = np.full((128, 128), NEG, np.float32)
        out[:, r, :] = mB
        out[:, 2 + r, :] = mC
    return np.ascontiguousarray(out.reshape(128, 512))


def table_idxmask(c):
    out = np.zeros((128, 2, 128), np.float32)
    qi = np.arange(128)[:, None]
    j = np.arange(128)[None, :]
    for r in range(2):
        if r < c:
            pass
        elif r == c:
            out[:, r, :] = np.where(j // 64 <= qi // 64, 0.0, -1.0e30)
        else:
            out[:, r, :] = -1.0e30
    return np.ascontiguousarray(out.reshape(128, 256))


def host_q_inputs(inp, l, c):
    w_in = inp["w_in"][l]
    m = {}
    m["wq"] = wcols(w_in, ["a_q", "b_q", "c_q", "d_q", "d_iq"])
    m["wiw"] = wcols(w_in, ["d_iw"])
    m["wg"] = wcols(w_in, ["gate"])
    m["wbr"] = np.ascontiguousarray(inp["w_branch"][l].reshape(4 * 512, D_MODEL))
    m["wo"] = np.ascontiguousarray(inp["w_out"][l])
    m["wf1"] = np.ascontiguousarray(inp["w_ff1"][l])
    m["wf2"] = np.ascontiguousarray(inp["w_ff2"][l])
    m["ropet"] = rope_tabs(c)
    m["biasA"] = table_biasA(inp["rel_bias"][l], c)
    m["mk"] = table_mk(c)
    m["identf"] = np.eye(128, dtype=np.float32)
    m["idxmask"] = table_idxmask(c)
    selh = np.zeros((4, 4, 128), np.float32)
    for h in range(4):
        selh[h, h, :] = 1.0
    m["selh"] = selh.reshape(4, 512)
    sel = np.zeros((4, 2), np.float32)
    sel[:, c] = 1.0
    m["sel"] = sel
    m["n1g"] = pcol(inp["norm1_g"][l])
    m["n2g"] = pcol(inp["norm2_g"][l])
    m["fg"] = pcol(inp["final_g"])
    m["bgate"] = pcol(inp["b_gate"][l])
    m["dng"] = np.ascontiguousarray(inp["diff_norm_g"][l].reshape(128, 1))
    m["lam4"] = np.ascontiguousarray(np.stack([inp["lambda_q1"][l], inp["lambda_k1"][l], inp["lambda_q2"][l],
                                               inp["lambda_k2"][l]], axis=1))
    return m


def host_kv_inputs(inp, l, c):
    w_in = inp["w_in"][l]
    return {"wk": wcols(w_in, ["a_k", "b_k", "c_k", "d_k", "d_ik", "c_f"]),
            "wv": wcols(w_in, ["a_v", "b_v", "c_v", "d_v"]),
            "ropet": rope_tabs(c), "n1g": pcol(inp["norm1_g"][l]),
            "bfg": np.ascontiguousarray(inp["b_forget"][l].reshape(4, 1))}


I32 = mybir.dt.int32
PK = {"kTa": 0, "kTb": 512, "kTc": 1024, "kTd": 1536, "ikT": 1664, "logf": 1728, "va": 1792, "vb": 2304,
      "vc": 2816, "vd": 3328}
RP = 3456
NSEL = 2 * RP // 128


def pack_views(ap2d, base):
    v = {}
    for nm, rows in (("kTa", 512), ("kTb", 512), ("kTc", 512), ("kTd", 128), ("ikT", 64)):
        v[nm] = ap2d[base + PK[nm]:base + PK[nm] + rows, :]
    v["logf"] = ap2d[base + PK["logf"]:base + PK["logf"] + 8, :].bitcast(F32).rearrange("(h a) n -> h (a n)", a=2)
    for nm in ("va", "vb", "vc"):
        v[nm] = ap2d[base + PK[nm]:base + PK[nm] + 512, :].rearrange("r (a f) -> (r a) f", f=512)
    v["vd"] = ap2d[base + PK["vd"]:base + PK["vd"] + 128, :].rearrange("r (a f) -> (r a) f", f=128)
    return v


A_IN = (("wk", [D_MODEL, WK_COLS]), ("wv", [D_MODEL, WV_COLS]), ("bfg", [4, 1]))


def build_fused():
    nc = bass.Bass("TRN2", target_bir_lowering=False)
    es = ExitStack()
    xT = dram_in(nc, "xT", [D_MODEL, LTOK])
    pidx_d = dram_in(nc, "pidx", [128, NSEL], I32)
    ios = []
    for l in range(DEPTH):
        io = {}
        for nm, shp in A_IN + Q_IN:
            if nm in ("ropet", "identf", "mk", "idxmask", "selh", "sel", "fg"):
                continue
            io[nm] = dram_in(nc, f"{nm}{l}", shp)
        ios.append(io)
    shared = {nm: dram_in(nc, nm, dict(Q_IN)[nm]) for nm in ("ropet", "identf", "mk", "idxmask", "selh", "sel", "fg")}
    xo = dram_out(nc, "xo", [D_MODEL, LTOK])
    kvpack_h = nc.dram_tensor("kvpack", [RP, LTOK], BF16)
    kvg_h = nc.dram_tensor("kvg", [N_CORES * RP, LTOK], BF16)
    kvall_h = nc.dram_tensor("kvall", [2 * RP, LTOK], BF16)
    xs1_h = nc.dram_tensor("xs1", [D_MODEL, LTOK], F32)
    t_pack, t_g, t_all = Tok(dj=True), Tok(dj=True), Tok(dj=True)
    xs1 = DT(xs1_h.ap(), dj=True)
    pv = pack_views(kvpack_h.ap(), 0)
    av = [pack_views(kvall_h.ap(), r * RP) for r in range(2)]
    with es:
        S = Sched(nc, es)
        cx = Ctx(S)
        c = alloc_q(cx, alloc_kv(cx, alloc_common(cx)))
        c["pidx"] = S.sb("pidx", [128, NSEL], I32)
        S.dma("sp", out=c["pidx"][:, :], in_=pidx_d.ap[:, :], writes=[c["pidx"]])
        for l in range(DEPTH):
            lambda_init = 0.8 - 0.6 * math.exp(-0.3 * l)
            io = dict(ios[l])
            io.update(shared)
            for nm in pv:
                io[nm] = DT(pv[nm], tok=t_pack)
                io[nm + "_all"] = DT([av[0][nm], av[1][nm]], tok=t_all)
            if l == 0:
                x_src = lambda t: (xT.ap[:, t * TT:(t + 1) * TT], xT.t)
                x_dst = xs1
            else:
                x_src = lambda t: (xs1.ap[:, t * TT:(t + 1) * TT], xs1.t)
                x_dst = xo
            S.dma("sp", out=c["n1g"][:, :], in_=io["n1g"].ap[:, :], reads=[io["n1g"]], writes=[c["n1g"]])
            S.dma("sp", out=c["nbfg"][64:68, :], in_=io["bfg"].ap[:, :], reads=[io["bfg"]], writes=[c["nbfg"]])
            S.op("dve", lambda e: e.tensor_scalar(out=c["nbfg"][64:68, :], in0=c["nbfg"][64:68, :], scalar1=-1.0,
                                                  scalar2=None, op0=ALU.mult), reads=[c["nbfg"]], writes=[c["nbfg"]])
            phase_kv(cx, io, x_src, c)
            S.collective_allgather(kvpack_h.ap().opt(), kvg_h.ap().opt(), reads=[t_pack], writes=[t_g])
            u = c["u"]
            for i in range(NSEL):
                k = i % 4
                toks = [u.s(8 * k + j) for j in range(8)]
                stv = u[:, 8 * k:8 * k + 8, :].rearrange("p k n -> p (k n)")
                S.idma(out=stv, in_=kvg_h.ap()[:, :], idx_ap=c["pidx"][:, i:i + 1], reads=[t_g, c["pidx"]],
                       writes=toks, semtile=toks[0])
                S.dma("sp", out=kvall_h.ap()[i * 128:(i + 1) * 128, :], in_=stv, reads=toks, writes=[t_all],
                      semtile=toks[0])
            setup_q(cx, c, io, lambda_init)
            phase_q(cx, c, io, x_src, x_dst, lambda_init, l == DEPTH - 1, None)
        S.final_all()
        print("ops per engine:", {k: len(v) for k, v in S.ops.items()})
        S.emit()
    return nc


def pair_index(core):
    b = core // 2
    idx = np.zeros((128, NSEL), np.int32)
    nb = RP // 128
    for i in range(NSEL):
        idx[:, i] = (2 * b + i // nb) * RP + (i % nb) * 128 + np.arange(128)
    return idx


def kernel_fused(**inputs):
    inp = {k: np.asarray(v) for k, v in inputs.items()}
    x = inp["x"]
    cores = list(range(N_CORES))
    nc = build_fused()
    per_layer = []
    for l in range(DEPTH):
        d = {}
        for c in range(2):
            m = host_q_inputs(inp, l, c)
            m.update({k: v for k, v in host_kv_inputs(inp, l, c).items() if k in ("wk", "wv", "bfg")})
            d[c] = m
        per_layer.append(d)
    maps = []
    for core in cores:
        b, c = core // 2, core % 2
        m = {"xT": to_local_T(x[b], c), "pidx": pair_index(core)}
        for l in range(DEPTH):
            src = per_layer[l][c]
            for nm, _ in A_IN + Q_IN:
                if nm in ("ropet", "identf", "mk", "idxmask", "selh", "sel", "fg"):
                    m[nm] = per_layer[0][c][nm]
                else:
                    m[f"{nm}{l}"] = per_layer[l][0][nm] if nm not in ("biasA",) else src[nm]
        maps.append(m)
    res = run_bass_kernel_spmd(nc, maps, core_ids=cores)
    xo = [np.asarray(res.results[core]["xo"]) for core in cores]
    out = np.stack([_from_local(xo[2 * b], xo[2 * b + 1]) for b in range(BATCH)], 0)
    return out.astype(np.float32)


def _from_local(loc0, loc1):
    out = np.empty((SEQ, D_MODEL), np.float32)
    o3 = out.reshape(NBLK, 128, D_MODEL)
    o3[0::2] = np.asarray(loc0).T.reshape(LBLK, 128, D_MODEL)
    o3[1::2] = np.asarray(loc1).T.reshape(LBLK, 128, D_MODEL)
    return out


def kernel_unfused(**inputs):
    inp = {k: np.asarray(v) for k, v in inputs.items()}
    x = inp["x"]
    cores = list(range(N_CORES))
    xT = [to_local_T(x[core // 2], core % 2) for core in cores]
    for l in range(DEPTH):
        ncA = build_A()
        mapsA = []
        for core in cores:
            m = host_kv_inputs(inp, l, core % 2)
            m["xT"] = xT[core]
            mapsA.append(m)
        resA = run_bass_kernel_spmd(ncA, mapsA, core_ids=cores)
        del mapsA
        ncB = build_B(l)
        mapsB = []
        for core in cores:
            b, c = core // 2, core % 2
            m = host_q_inputs(inp, l, c)
            m["xT"] = xT[core]
            for nm, shp, dt in KV_ALL:
                m[nm + "_all"] = np.ascontiguousarray(
                    np.stack([resA.results[2 * b][nm], resA.results[2 * b + 1][nm]], 0))
            mapsB.append(m)
        resB = run_bass_kernel_spmd(ncB, mapsB, core_ids=cores)
        del mapsB
        xT = [np.asarray(resB.results[core]["xo"]) for core in cores]
    out = np.stack([_from_local(xT[2 * b], xT[2 * b + 1]) for b in range(BATCH)], 0)
    return out.astype(np.float32)


def kernel(**inputs):
    return kernel_fused(**inputs)
```

```python
import math
from contextlib import ExitStack

import numpy as np
import concourse.bass as bass
import concourse.mybir as mybir
from concourse.bass_utils import run_bass_kernel_spmd

F32 = mybir.dt.float32
BF16 = mybir.dt.bfloat16
AF = mybir.ActivationFunctionType
ALU = mybir.AluOpType
AX = mybir.AxisListType

D_MODEL = 2048
BATCH = 4
SEQ = 4096
DEPTH = 2
KC = D_MODEL // 128
NBLK = SEQ // 128
LBLK = NBLK // 2
LTOK = LBLK * 128
TT = 256
NT = LTOK // TT
D_FF = 4 * D_MODEL
EPS = 1e-6
NEG = -30000.0
N_CORES = 8


class Tok:
    __slots__ = ("w", "r", "parent", "subs", "dj")

    def __init__(self, parent=None, dj=False):
        self.w = {}
        self.r = {}
        self.parent = parent
        self.subs = []
        self.dj = dj


class Tile:
    def __init__(self, ap, name):
        self.ap = ap
        self.name = name
        self.t = Tok()
        self._subs = {}

    def s(self, key):
        if key not in self._subs:
            tk = Tok(parent=self.t)
            self.t.subs.append(tk)
            self._subs[key] = tk
        return self._subs[key]

    def __getitem__(self, idx):
        return self.ap[idx]


ENGS = ("pe", "act", "dve", "pool", "sp")


class _Rec:
    def __init__(self):
        self.call = None

    def __getattr__(self, name):
        def f(*a, **kw):
            self.call = (name, a, kw)
            return None
        return f


class Sched:
    def __init__(self, nc, es, n_dma_sems=56):
        self.nc = nc
        self.es = es
        self.ops = {e: [] for e in ENGS}
        self.cnt = {e: 0 for e in ENGS}
        self.sems = {}
        for e in ENGS + ("cc",):
            self.sems[e] = es.enter_context(nc.semaphore("sem_" + e))
        self.cc_cnt = 0
        self.dma_sems = [es.enter_context(nc.semaphore(f"dsem{i}")) for i in range(n_dma_sems)]
        self.dma_cnt = [0] * n_dma_sems
        self.dma_rr = 0
        self.tile_sem = {}
        self.seen = {e: {} for e in ENGS}
        self.final_waits = []
        self.ntiles = 0

    def sb(self, name, shape, dtype):
        self.ntiles += 1
        t = self.es.enter_context(self.nc.sbuf_tensor(f"{name}_{self.ntiles}", list(shape), dtype))
        return Tile(t, name)

    def ps(self, name, shape, dtype=F32):
        self.ntiles += 1
        t = self.es.enter_context(self.nc.psum_tensor(f"{name}_{self.ntiles}", list(shape), dtype))
        return Tile(t, name)

    @staticmethod
    def _toks(x):
        out = []
        for i in x:
            out.append(i if isinstance(i, Tok) else i.t)
        return out

    def _collect(self, reads, writes):
        ev = {}

        def add_r(d):
            for k, v in d.items():
                if ev.get(k, 0) < v:
                    ev[k] = v

        add = add_r

        for t in reads:
            add(t.w)
            if t.parent is not None:
                add(t.parent.w)
            for s_ in t.subs:
                add(s_.w)
        for t in writes:
            if not t.dj:
                add(t.w)
            add_r(t.r)
            if t.parent is not None:
                add(t.parent.w)
                add_r(t.parent.r)
            for s_ in t.subs:
                add(s_.w)
                add_r(s_.r)
        return ev

    def _update(self, reads, writes, me):
        k, v = me
        for t in reads:
            if t.r.get(k, 0) < v:
                t.r[k] = v
        for t in writes:
            if t.dj:
                if t.w.get(k, 0) < v:
                    t.w[k] = v
                continue
            t.w = {k: v}
            t.r = {}
            for s_ in t.subs:
                s_.w = {}
                s_.r = {}

    def _waits(self, eng, ev):
        seen = self.seen[eng]
        waits = []
        for k, v in ev.items():
            if eng == "pe" and k == "pe":
                continue
            if seen.get(k, 0) >= v:
                continue
            seen[k] = v
            waits.append((k, v))
        return waits

    def _semof(self, k):
        return self.sems[k] if isinstance(k, str) else self.dma_sems[k]

    def op(self, eng, fn, reads=(), writes=()):
        reads = self._toks(reads)
        writes = self._toks(writes)
        ev = self._collect(reads, writes)
        waits = self._waits(eng, ev)
        self.cnt[eng] += 1
        me = (eng, self.cnt[eng])
        rec = _Rec()
        fn(rec)
        name, a, kw = rec.call

        def fn2(e, name=name, a=a, kw=kw):
            return getattr(e, name)(*a, **kw)

        self.ops[eng].append((waits, fn2, (eng, 1)))
        self._update(reads, writes, me)

    def collective_allgather(self, in_ap, out_ap, reads=(), writes=()):
        reads = self._toks(reads)
        writes = self._toks(writes)
        ev = self._collect(reads, writes)
        waits = self._waits("pool", ev)
        self.cc_cnt += 1
        me = ("cc", self.cc_cnt)

        def fn(e):
            return e.collective_compute("AllGather", ALU.bypass, replica_groups=[list(range(N_CORES))],
                                        ins=[in_ap], outs=[out_ap])

        self.ops["pool"].append((waits, fn, ("cc", None)))
        self._update(reads, writes, me)

    def idma(self, out, in_, idx_ap, reads=(), writes=(), semtile=None):
        def fn(e):
            return e.indirect_dma_start(out=out, out_offset=None, in_=in_,
                                        in_offset=bass.IndirectOffsetOnAxis(ap=idx_ap, axis=0))
        return self.dma("pool", None, None, reads=reads, writes=writes, semtile=semtile, fn=fn)

    def dma(self, queue, out, in_, reads=(), writes=(), semtile=None, fn=None, **kw):
        reads = self._toks(reads)
        writes = self._toks(writes)
        ev = self._collect(reads, writes)
        key = semtile if semtile is not None else (writes[0] if writes else reads[0])
        if key not in self.tile_sem:
            self.tile_sem[key] = self.dma_rr % len(self.dma_sems)
            self.dma_rr += 1
        si = self.tile_sem[key]
        if self.dma_cnt[si] > 0:
            pv = self.dma_cnt[si]
            if ev.get(si, 0) < pv:
                ev[si] = pv
        waits = self._waits(queue, ev)
        self.dma_cnt[si] += 16
        me = (si, self.dma_cnt[si])

        if fn is None:
            def fn(e, out=out, in_=in_, kw=kw):
                return e.dma_start(out=out, in_=in_, **kw)

        self.ops[queue].append((waits, fn, (si, 16)))
        self._update(reads, writes, me)
        return me

    def final_all(self):
        for si, v in enumerate(self.dma_cnt):
            if v > 0:
                self.final_waits.append((si, v))

    def emit(self):
        nc = self.nc
        block = self.es.enter_context(nc.Block())

        def replay(eng_name, e, extra=None):
            for waits, fn, inc in self.ops[eng_name]:
                for k, v in waits:
                    e.wait_ge(self._semof(k), v)
                ins = fn(e)
                if inc[1] is None:
                    ins.then_inc(self._semof(inc[0]))
                else:
                    ins.then_inc(self._semof(inc[0]), inc[1])
            if extra:
                for k, v in extra:
                    e.wait_ge(self._semof(k), v)

        @block.sync
        def _(e):
            replay("sp", e, self.final_waits)

        @block.tensor
        def _(e):
            replay("pe", e)

        @block.scalar
        def _(e):
            replay("act", e)

        @block.vector
        def _(e):
            replay("dve", e)

        @block.gpsimd
        def _(e):
            replay("pool", e)


WB = 256
USE_WSCR = False


class Ctx:
    def __init__(self, S):
        self.S = S
        self.ones_bf = S.sb("ones_bf", [128, 128], BF16)
        S.op("dve", lambda e: e.memset(self.ones_bf[:], 1.0), writes=[self.ones_bf])
        self.wscr = None
        self.wslots = {}
        self.wtoks = {}
        self.first_tile = True
        self.layer = 0
        self.wbuf = [S.sb(f"wbuf{i}", [128, KC, WB], BF16) for i in range(3)]
        self.wrr = 0
        self.psd = [S.ps(f"psd{i}", [128, 512], F32) for i in range(2)]
        self.psrr = 0
        self.evrr = 0
        self.sqb = [S.sb(f"sqb{i}", [128, TT], BF16) for i in range(2)]
        self.rstd = S.sb("rstd", [128, TT], F32)
        self.fence_t = S.sb("fence", [128, 8], F32)

    def next_w(self):
        w = self.wbuf[self.wrr % len(self.wbuf)]
        self.wrr += 1
        return w

    def next_ps(self):
        p = self.psd[self.psrr % len(self.psd)]
        self.psrr += 1
        return p

    def ev_eng(self):
        self.evrr += 1
        return "act" if self.evrr % 2 else "dve"

    def fence(self, tiles):
        self.S.op("pool", lambda e: e.memset(self.fence_t[:, 0:1], 0.0), writes=[self.fence_t] + list(tiles))


def load_w(cx, wdram, col0, ncols, nk=KC, row0=0, wtok=None, key=None):
    S = cx.S
    w = cx.next_w()
    src = wdram[row0:row0 + nk * 128, col0:col0 + ncols].rearrange("(k p) n -> p k n", p=128)
    if cx.wscr is None or key is None:
        S.dma("pool", out=w[:, 0:nk, 0:ncols], in_=src, reads=([wtok] if wtok is not None else []), writes=[w])
        return w
    k = (cx.layer, key, col0, row0)
    if k not in cx.wslots:
        cx.wslots[k] = len([1 for kk in cx.wslots if kk[0] == cx.layer])
        cx.wtoks[k] = Tok()
    slot = cx.wslots[k]
    sv = cx.wscr[cx.layer][slot][:, 0:nk * ncols].rearrange("p (k n) -> p k n", n=ncols)
    if cx.first_tile:
        S.dma("pool", out=w[:, 0:nk, 0:ncols], in_=src, reads=([wtok] if wtok is not None else []), writes=[w])
        S.dma("sp", out=sv, in_=w[:, 0:nk, 0:ncols], reads=[w], writes=[cx.wtoks[k]], semtile=w.t)
    else:
        S.dma("sp", out=w[:, 0:nk, 0:ncols], in_=sv, reads=[cx.wtoks[k]], writes=[w])
    return w


def evac(cx, out_ap, in_ps, reads, writes, scale=None, eng=None):
    S = cx.S
    eng = eng or cx.ev_eng()
    if eng == "act":
        if scale is None:
            S.op("act", lambda e: e.activation(out=out_ap, in_=in_ps, func=AF.Copy), reads=reads, writes=writes)
        else:
            S.op("act", lambda e: e.activation(out=out_ap, in_=in_ps, func=AF.Copy, scale=float(scale)),
                 reads=reads, writes=writes)
    else:
        if scale is None:
            S.op("dve", lambda e: e.tensor_copy(out=out_ap, in_=in_ps), reads=reads, writes=writes)
        else:
            S.op("dve", lambda e: e.tensor_scalar(out=out_ap, in0=in_ps, scalar1=float(scale), scalar2=None,
                                                  op0=ALU.mult), reads=reads, writes=writes)


def rmsnorm_fm(cx, x, g, hT, ps):
    S = cx.S
    rstd = cx.rstd
    for kc in range(KC):
        sq = cx.sqb[kc % 2]
        S.op("act", lambda e, kc=kc, sq=sq: e.activation(out=sq[:, :], in_=x[:, kc, :], func=AF.Square),
             reads=[x], writes=[sq])
        S.op("pe", lambda e, kc=kc, sq=sq: e.matmul(ps[:, 0:TT], lhsT=cx.ones_bf[:, :], rhs=sq[:, :],
                                                    start=(kc == 0), stop=(kc == KC - 1)),
             reads=[sq, cx.ones_bf], writes=[ps])
    S.op("act", lambda e: e.activation(out=rstd[:, :], in_=ps[:, 0:TT], func=AF.Sqrt, bias=EPS, scale=1.0 / D_MODEL),
         reads=[ps], writes=[rstd])
    S.op("dve", lambda e: e.reciprocal(out=rstd[:, :], in_=rstd[:, :]), reads=[rstd], writes=[rstd])
    for kc in range(KC):
        S.op("dve", lambda e, kc=kc: e.scalar_tensor_tensor(out=hT[:, kc, :], in0=x[:, kc, :], scalar=g[:, kc:kc + 1],
                                                            op0=ALU.mult, in1=rstd[:, :], op1=ALU.mult),
             reads=[x, g, rstd], writes=[hT.s(kc)])


def proj_fm(cx, w, c0, M, hT, ps, nk=KC, ncol=TT, kofs=0):
    S = cx.S
    for kc in range(nk):
        S.op("pe", lambda e, kc=kc: e.matmul(ps[0:M, 0:ncol], lhsT=w[:, kc, c0:c0 + M], rhs=hT[:, kofs + kc, :],
                                             start=(kc == 0), stop=(kc == nk - 1)),
             reads=[w, hT.s(kofs + kc)], writes=[ps])


def rope_fm(cx, ps, P, half, ctab, stab, out_ap, out_toks, tmpA, tmpB):
    S = cx.S
    S.op("dve", lambda e: e.tensor_tensor(out=tmpA[0:P, :], in0=ps[0:P, 0:TT], in1=ctab[0:P, :], op=ALU.mult),
         reads=[ps] + ctab.toks, writes=[tmpA])
    for g in range(P // (2 * half)):
        b = g * 2 * half
        S.op("dve", lambda e, b=b: e.tensor_tensor(out=tmpB[b:b + half, :], in0=ps[b + half:b + 2 * half, 0:TT],
                                                   in1=stab[b + half:b + 2 * half, :], op=ALU.mult),
             reads=[ps] + stab.toks, writes=[tmpB])
        S.op("dve", lambda e, b=b: e.tensor_tensor(out=tmpB[b + half:b + 2 * half, :], in0=ps[b:b + half, 0:TT],
                                                   in1=stab[b:b + half, :], op=ALU.mult),
             reads=[ps] + stab.toks, writes=[tmpB])
    if isinstance(out_ap, list):
        for (r0, r1, oap, otk) in out_ap:
            S.op("dve", lambda e, r0=r0, r1=r1, oap=oap: e.tensor_tensor(out=oap, in0=tmpA[r0:r1, :], in1=tmpB[r0:r1, :],
                                                                      op=ALU.add), reads=[tmpA, tmpB], writes=otk)
    else:
        S.op("dve", lambda e: e.tensor_tensor(out=out_ap, in0=tmpA[0:P, :], in1=tmpB[0:P, :], op=ALU.add),
             reads=[tmpA, tmpB], writes=out_toks)


class View:
    def __init__(self, tile, idx, toks=None):
        self.tile = tile
        self.idx = idx
        self.toks = toks if toks is not None else [tile.t]

    def __getitem__(self, sl):
        rows, cols = sl
        return self.tile.ap[(rows,) + tuple(self.idx) + (cols,)] if isinstance(self.idx, tuple) else \
            self.tile.ap[rows, self.idx, cols]


WK_COLS = 512 * 3 + 128 + 64 + 4
WV_COLS = 512 * 3 + 128


class DT:
    def __init__(self, ap, tok=None, dj=False):
        self.ap = ap
        self.t = tok if tok is not None else Tok(dj=dj)

    def __getitem__(self, i):
        return self.ap[i]


def load_x_tile(cx, c, x_src, t, ropet):
    S = cx.S
    xs, xtok = x_src(t)
    S.dma("sp", out=c["xbuf"][:, :, :], in_=xs.rearrange("(k p) n -> p k n", p=128), reads=[xtok], writes=[c["xbuf"]])
    S.dma("sp", out=c["rt"][:, :, :], in_=ropet.ap[:, :, t * TT:(t + 1) * TT].rearrange("f p n -> p f n"),
          reads=[ropet.t], writes=[c["rt"]])


def phase_kv(cx, io, x_src, c):
    S = cx.S
    x, hT, tmpA, tmpB, kst, vst, lf, rt = (c[k] for k in ("xbuf", "hT", "tmpA", "tmpB", "kst", "vst", "lf", "rt"))
    c128, s128, c64, s64 = (View(rt, i) for i in range(4))
    stn = 0
    for t in range(NT):
        cx.first_tile = (t == 0)
        tc = (t * TT, (t + 1) * TT)
        load_x_tile(cx, c, x_src, t, io["ropet"])
        rmsnorm_fm(cx, x, c["n1g"], hT, cx.next_ps())
        for (c0, dst, rk) in ((0, "kTa", None), (256, "kTa", None), (512, "kTb", 32), (768, "kTb", 32),
                              (1024, "kTc", None), (1280, "kTc", None)):
            w = load_w(cx, io["wk"].ap, c0, 256, wtok=io["wk"].t, key="wk")
            st = kst[stn % 2]
            stn += 1
            for j in range(2):
                ps = cx.next_ps()
                proj_fm(cx, w, j * 128, 128, hT, ps)
                if rk is None:
                    evac(cx, st[:, j, :], ps[:, 0:TT], [ps], [st])
                else:
                    rope_fm(cx, ps, 128, rk, c64, s64, st[:, j, :], [st], tmpA, tmpB)
            r0 = c0 % 512
            S.dma("sp", out=io[dst].ap[r0:r0 + 256, tc[0]:tc[1]].rearrange("(j p) n -> p j n", p=128), in_=st[:, :, :],
                  reads=[st], writes=[io[dst]], semtile=st.t)
        w = load_w(cx, io["wk"].ap, 1536, 196, wtok=io["wk"].t, key="wk")
        st = kst[stn % 2]
        stn += 1
        ps = cx.next_ps()
        proj_fm(cx, w, 0, 128, hT, ps)
        rope_fm(cx, ps, 128, 64, c128, s128, st[:, 0, :], [st], tmpA, tmpB)
        ps = cx.next_ps()
        proj_fm(cx, w, 128, 68, hT, ps)
        rope_fm(cx, ps, 64, 32, c64, s64, st[0:64, 1, :], [st], tmpA, tmpB)
        S.op("act", lambda e, ps=ps: e.activation(out=lf[64:68, :], in_=ps[64:68, 0:TT], func=AF.Exp,
                                                 bias=c["nbfg"][64:68, :], scale=-1.0),
             reads=[ps, c["nbfg"]], writes=[lf])
        S.op("act", lambda e: e.activation(out=lf[64:68, :], in_=lf[64:68, :], func=AF.Ln, bias=1.0),
             reads=[lf], writes=[lf])
        S.op("act", lambda e: e.mul(out=lf[64:68, :], in_=lf[64:68, :], mul=-1.0), reads=[lf], writes=[lf])
        S.dma("sp", out=io["kTd"].ap[:, tc[0]:tc[1]], in_=st[:, 0, :], reads=[st], writes=[io["kTd"]], semtile=st.t)
        S.dma("sp", out=io["ikT"].ap[:, tc[0]:tc[1]], in_=st[0:64, 1, :], reads=[st], writes=[io["ikT"]], semtile=st.t)
        S.dma("sp", out=io["logf"].ap[:, tc[0]:tc[1]], in_=lf[64:68, :], reads=[lf], writes=[io["logf"]], semtile=lf.t)
        for (c0, ncols, nm, d0) in ((0, 256, "va", 0), (256, 256, "va", 256), (512, 256, "vb", 0), (768, 256, "vb", 256),
                                    (1024, 256, "vc", 0), (1280, 256, "vc", 256), (1536, 128, "vd", 0)):
            w = load_w(cx, io["wv"].ap, c0, ncols, wtok=io["wv"].t, key="wv")
            for sbk in range(TT // 128):
                ps = cx.next_ps()
                for kc in range(KC):
                    S.op("pe", lambda e, kc=kc, ps=ps, w=w, sbk=sbk, ncols=ncols: e.matmul(
                        ps[:, 0:ncols], lhsT=hT[:, kc, sbk * 128:(sbk + 1) * 128], rhs=w[:, kc, 0:ncols],
                        start=(kc == 0), stop=(kc == KC - 1)), reads=[w, hT.s(kc)], writes=[ps])
                st = vst[stn % 2]
                stn += 1
                evac(cx, st[:, 0:ncols], ps[:, 0:ncols], [ps], [st])
                r0 = t * TT + sbk * 128
                S.dma("sp", out=io[nm].ap[r0:r0 + 128, d0:d0 + ncols], in_=st[:, 0:ncols], reads=[st], writes=[io[nm]],
                      semtile=st.t)


def alloc_common(cx):
    S = cx.S
    c = {}
    c["xbuf"] = S.sb("xbuf", [128, KC, TT], F32)
    c["hT"] = S.sb("hT", [128, KC, TT], BF16)
    c["tmpA"] = S.sb("tmpA", [128, TT], F32)
    c["tmpB"] = S.sb("tmpB", [128, TT], F32)
    c["rt"] = S.sb("rt", [128, 4, TT], F32)
    c["n1g"] = S.sb("n1g", [128, KC], F32)
    return c


def alloc_kv(cx, c):
    S = cx.S
    c["kst"] = [S.sb(f"kst{i}", [128, 2, TT], BF16) for i in range(2)]
    c["vst"] = [S.sb(f"vst{i}", [128, 256], BF16) for i in range(2)]
    c["lf"] = S.sb("lf", [128, TT], F32)
    c["nbfg"] = S.sb("nbfg", [128, 1], F32)
    return c


def dram_in(nc, name, shape, dtype=F32):
    return DT(nc.dram_tensor(name, list(shape), dtype, kind="ExternalInput").ap())


def dram_out(nc, name, shape, dtype=F32):
    return DT(nc.dram_tensor(name, list(shape), dtype, kind="ExternalOutput").ap(), dj=True)


KV_OUT = (("kTa", [512, LTOK], BF16), ("kTb", [512, LTOK], BF16), ("kTc", [512, LTOK], BF16),
          ("kTd", [128, LTOK], BF16), ("ikT", [64, LTOK], BF16), ("logf", [4, LTOK], F32),
          ("va", [LTOK, 512], BF16), ("vb", [LTOK, 512], BF16), ("vc", [LTOK, 512], BF16),
          ("vd", [LTOK, 128], BF16))


def build_A():
    nc = bass.Bass("TRN2", target_bir_lowering=False)
    es = ExitStack()
    io = {}
    xT = dram_in(nc, "xT", [D_MODEL, LTOK])
    io["wk"] = dram_in(nc, "wk", [D_MODEL, WK_COLS])
    io["wv"] = dram_in(nc, "wv", [D_MODEL, WV_COLS])
    io["ropet"] = dram_in(nc, "ropet", [4, 128, LTOK])
    n1g = dram_in(nc, "n1g", [128, KC])
    bfg = dram_in(nc, "bfg", [4, 1])
    for nm, shp, dt in KV_OUT:
        io[nm] = dram_out(nc, nm, shp, dt)
    with es:
        S = Sched(nc, es)
        cx = Ctx(S)
        c = alloc_kv(cx, alloc_common(cx))
        S.dma("sp", out=c["n1g"][:, :], in_=n1g.ap[:, :], writes=[c["n1g"]])
        S.dma("sp", out=c["nbfg"][64:68, :], in_=bfg.ap[:, :], writes=[c["nbfg"]])
        S.op("dve", lambda e: e.tensor_scalar(out=c["nbfg"][64:68, :], in0=c["nbfg"][64:68, :], scalar1=-1.0,
                                              scalar2=None, op0=ALU.mult), reads=[c["nbfg"]], writes=[c["nbfg"]])
        phase_kv(cx, io, lambda t: (xT.ap[:, t * TT:(t + 1) * TT], xT.t), c)
        S.final_all()
        S.emit()
    return nc


SEG_W = (("a_q", 512), ("a_k", 512), ("a_v", 512), ("b_q", 512), ("b_k", 512), ("b_v", 512),
         ("c_q", 512), ("c_k", 512), ("c_v", 512), ("c_f", 4), ("d_q", 512), ("d_k", 128), ("d_v", 128),
         ("d_iq", 512), ("d_ik", 64), ("d_iw", 8), ("gate", 8192))
SEG = {}
_o = 0
for _n, _w in SEG_W:
    SEG[_n] = (_o, _o + _w)
    _o += _w


def wcols(w, names):
    return np.ascontiguousarray(np.concatenate([w[:, SEG[n][0]:SEG[n][1]] for n in names], axis=1))


def local_pos(c):
    lb = np.arange(LBLK)
    return ((2 * lb + c)[:, None] * 128 + np.arange(128)[None, :]).reshape(-1)


def rope_tabs(c):
    pos = local_pos(c).astype(np.float32)
    out = []
    for dim in (128, 64):
        inv = (np.float32(10000.0) ** (-np.arange(0, dim, 2, dtype=np.float32) / np.float32(dim))).astype(np.float32)
        ang = pos[:, None] * inv[None, :]
        cos = np.cos(ang).astype(np.float32).T
        sin = np.sin(ang).astype(np.float32).T
        rep = 128 // dim
        out.append(np.concatenate([cos, cos] * rep, axis=0))
        out.append(np.concatenate([sin, -sin] * rep, axis=0))
    return np.ascontiguousarray(np.stack(out, 0))


def to_local_T(xb, c):
    blk = xb.reshape(NBLK, 128, -1)[c::2].reshape(LTOK, -1)
    return np.ascontiguousarray(blk.T)


def pcol(v):
    return np.ascontiguousarray(v.reshape(-1, 128).T)


IWS = (8 ** -0.5) * (64 ** -0.5)
BIS_W = 256.0
BIS_IT = 24
KV_ALL = (("kTa", [2, 512, LTOK], BF16), ("kTb", [2, 512, LTOK], BF16), ("kTc", [2, 512, LTOK], BF16),
          ("kTd", [2, 128, LTOK], BF16), ("ikT", [2, 64, LTOK], BF16), ("logf", [2, 4, LTOK], F32),
          ("va", [2, LTOK, 512], BF16), ("vb", [2, LTOK, 512], BF16), ("vc", [2, LTOK, 512], BF16),
          ("vd", [2, LTOK, 128], BF16))


def alloc_q(cx, c):
    S = cx.S
    c["kTd"] = S.sb("kTd_sb", [128, SEQ], BF16)
    c["ikT2"] = S.sb("ikT2", [128, SEQ], BF16)
    c["vd"] = S.sb("vd_sb", [128, NBLK, 128], BF16)
    c["score"] = S.sb("score", [128, SEQ], F32)
    c["negcum"] = S.sb("negcum", [128, NBLK * 4], F32)
    c["cum_mine"] = S.sb("cum_mine", [4, LTOK], F32)
    c["cumq"] = S.sb("cumq", [128, 4, TT], F32)
    c["biasA"] = S.sb("biasA", [128, 6, 512], BF16)
    c["mk"] = S.sb("mk", [128, 4, 128], BF16)
    c["idxmask"] = S.sb("idxmask", [128, 256], F32)
    c["ident"] = S.sb("ident", [128, 128], BF16)
    c["identf"] = S.sb("identf", [128, 128], F32)
    c["onesf"] = S.sb("onesf", [128, 256], F32)
    c["selh"] = S.sb("selh", [4, 512], F32)
    c["sel"] = S.sb("sel", [4, 2], F32)
    c["n2g"] = S.sb("n2g", [128, KC], F32)
    c["fg"] = S.sb("fg", [128, KC], F32)
    c["bgate"] = S.sb("bgate", [128, 64], F32)
    c["dng"] = S.sb("dng", [128, 1], F32)
    c["neglam"] = S.sb("neglam", [128, 1], F32)
    c["lamv"] = S.sb("lamv", [64, 4], F32)
    c["lamp"] = S.sb("lamp", [128, 2], F32)
    c["wiw"] = S.sb("wiw", [128, KC, 8], BF16)
    c["q"] = S.sb("q", [128, 4, TT], BF16)
    c["iq"] = S.sb("iq", [128, 4, TT], BF16)
    c["qz"] = [S.sb(f"qz{i}", [128, 4, TT], BF16) for i in range(2)]
    c["iw"] = S.sb("iw", [128, 16], F32)
    c["o"] = [S.sb(f"o{i}", [128, 4, TT], BF16) for i in range(4)]
    c["mg"] = S.sb("mg", [128, KC, TT], BF16)
    c["u"] = S.sb("u", [128, 32, TT], BF16)
    c["kch"] = [S.sb(f"kch{i}", [128, 4, 512], BF16) for i in range(2)]
    c["vch"] = [S.sb(f"vch{i}", [128, 4, 512], BF16) for i in range(2)]
    c["pT"] = [S.sb(f"pT{i}", [128, 512], BF16) for i in range(3)]
    c["sp"] = [S.sb(f"sp{i}", [128, 512], F32) for i in range(2)]
    c["nm"] = [S.sb(f"nm{i}", [128, 512], BF16) for i in range(2)]
    c["nmT"] = [S.sb(f"nmT{i}", [128, 4, 128], BF16) for i in range(2)]
    c["rden"] = S.sb("rden", [128, 512], F32)
    c["obr"] = S.sb("obr", [128, TT], F32)
    c["acc"] = [S.sb(f"acc{i}", [128, TT], F32) for i in range(2)]
    c["m8"] = S.sb("m8", [128, 8], F32)
    c["bis"] = S.sb("bis", [128, 4], F32)
    c["ost"] = [S.sb(f"ost{i}", [128, TT], F32) for i in range(2)]
    c["psS"] = [S.ps(f"psS{i}", [128, 512], F32) for i in range(2)]
    c["psO"] = S.ps("psO", [128, 512], F32)
    c["psD"] = S.ps("psD", [128, 512], F32)
    c["psm"] = S.ps("psm", [128, 512], F32)
    c["psT"] = S.ps("psT", [128, 512], BF16)
    c["rr"] = {"S": 0, "pT": 0, "sp": 0, "kv": 0, "nm": 0}
    return c


def rr(c, name, key):
    lst = c[name]
    i = c["rr"][key]
    c["rr"][key] = i + 1
    return lst[i % len(lst)]


def setup_q(cx, c, io, lambda_init):
    S = cx.S
    pq = "pool"
    S.dma(pq, out=c["biasA"][:, :, :], in_=io["biasA"].ap[:, :].rearrange("p (b f) -> p b f", b=6),
          reads=[io["biasA"]], writes=[c["biasA"]])
    S.dma(pq, out=c["mk"][:, :, :], in_=io["mk"].ap[:, :].rearrange("p (b f) -> p b f", b=4), reads=[io["mk"]],
          writes=[c["mk"]])
    S.dma(pq, out=c["ident"][:, :], in_=io["identf"].ap[:, :], reads=[io["identf"]], writes=[c["ident"]])
    S.dma(pq, out=c["wiw"][:, :, :], in_=io["wiw"].ap[:, :].rearrange("(k p) n -> p k n", p=128), reads=[io["wiw"]],
          writes=[c["wiw"]])
    for nm in ("identf", "idxmask", "selh", "sel", "n2g", "fg", "bgate", "dng"):
        S.dma("sp", out=c[nm][:, :], in_=io[nm].ap[:, :], reads=[io[nm]], writes=[c[nm]])
    S.dma("sp", out=c["lamv"][:, :], in_=io["lam4"].ap[:, :], reads=[io["lam4"]], writes=[c["lamv"]])
    S.op("dve", lambda e: e.memset(c["onesf"][:, :], 1.0), writes=[c["onesf"]])
    S.op("dve", lambda e: e.tensor_scalar(out=c["dng"][:, :], in0=c["dng"][:, :], scalar1=float(1.0 - lambda_init),
                                          scalar2=None, op0=ALU.mult), reads=[c["dng"]], writes=[c["dng"]])
    x = c["xbuf"]
    xflat = x[0:4, :, :].rearrange("p k n -> p (k n)")
    for r in range(2):
        S.dma("sp", out=c["kTd"][:, :].rearrange("p (n r j) -> p n r j", r=2, j=128)[:, :, r, :],
              in_=io["kTd_all"].ap[r].rearrange("p (n j) -> p n j", j=128), reads=[io["kTd_all"]], writes=[c["kTd"]])
        for hf in range(2):
            S.dma("sp", out=c["ikT2"][hf * 64:(hf + 1) * 64, :].rearrange("p (n r j) -> p n r j", r=2, j=128)[:, :, r, :],
                  in_=io["ikT_all"].ap[r].rearrange("p (n j) -> p n j", j=128), reads=[io["ikT_all"]],
                  writes=[c["ikT2"]])
        S.dma("sp", out=c["vd"][:, :, :].rearrange("p (n r) d -> p n r d", r=2)[:, :, r, :],
              in_=io["vd_all"].ap[r].rearrange("(n p) d -> p n d", p=128), reads=[io["vd_all"]], writes=[c["vd"]])
        S.dma("sp", out=xflat.rearrange("p (n r j) -> p n r j", r=2, j=128)[:, :, r, :],
              in_=io["logf_all"].ap[r].rearrange("p (n j) -> p n j", j=128), reads=[io["logf_all"]], writes=[x])
    score = c["score"]
    CH = 256
    for i in range(SEQ // CH):
        init = 0.0 if i == 0 else score[0:4, i * CH - 1:i * CH]
        S.op("dve", lambda e, i=i, init=init: e.tensor_tensor_scan(
            out=score[0:4, i * CH:(i + 1) * CH], data0=c["onesf"][0:4, 0:CH], data1=xflat[:, i * CH:(i + 1) * CH],
            initial=init, op0=ALU.mult, op1=ALU.add), reads=[x, c["onesf"], score], writes=[score])
    psm = c["psm"]
    for kb in range(NBLK):
        S.op("pe", lambda e, kb=kb: e.transpose(out=psm[:, kb * 4:(kb + 1) * 4], in_=score[0:4, kb * 128:(kb + 1) * 128],
                                                identity=c["identf"][0:4, 0:4]),
             reads=[score, c["identf"]], writes=[psm])
    S.op("act", lambda e: e.mul(out=c["negcum"][:, :], in_=psm[:, 0:NBLK * 4], mul=-1.0), reads=[psm],
         writes=[c["negcum"]])
    cv = score[0:4, :].rearrange("p (n r j) -> p n r j", r=2, j=128)
    cm = c["cum_mine"][0:4, :].rearrange("p (n j) -> p n j", j=128)
    S.op("dve", lambda e: e.tensor_scalar(out=cm, in0=cv[:, :, 0, :], scalar1=c["sel"][0:4, 0:1], scalar2=None,
                                          op0=ALU.mult), reads=[score, c["sel"]], writes=[c["cum_mine"]])
    S.op("dve", lambda e: e.scalar_tensor_tensor(out=cm, in0=cv[:, :, 1, :], scalar=c["sel"][0:4, 1:2], op0=ALU.mult,
                                                 in1=cm, op1=ALU.add), reads=[score, c["sel"], c["cum_mine"]],
         writes=[c["cum_mine"]])
    lv, lp = c["lamv"], c["lamp"]
    S.op("dve", lambda e: e.tensor_tensor(out=lp[0:64, 0:1], in0=lv[0:64, 0:1], in1=lv[0:64, 1:2], op=ALU.mult),
         reads=[lv], writes=[lp])
    S.op("dve", lambda e: e.tensor_tensor(out=lp[0:64, 1:2], in0=lv[0:64, 2:3], in1=lv[0:64, 3:4], op=ALU.mult),
         reads=[lv, lp], writes=[lp])
    S.op("pe", lambda e: e.matmul(psm[:, 0:2], lhsT=c["onesf"][0:64, 0:128], rhs=lp[0:64, 0:2], start=True, stop=True),
         reads=[c["onesf"], lp], writes=[psm])
    S.op("act", lambda e: e.activation(out=lp[:, 0:2], in_=psm[:, 0:2], func=AF.Exp), reads=[psm], writes=[lp])
    S.op("dve", lambda e: e.tensor_tensor(out=c["neglam"][:, :], in0=lp[:, 1:2], in1=lp[:, 0:1], op=ALU.subtract),
         reads=[lp], writes=[c["neglam"]])
    S.op("dve", lambda e: e.tensor_scalar(out=c["neglam"][:, :], in0=c["neglam"][:, :], scalar1=float(-lambda_init),
                                          scalar2=None, op0=ALU.add), reads=[c["neglam"]], writes=[c["neglam"]])


def q_proj(cx, c, io, col0, dst, mode, scale=None):
    hT, tmpA, tmpB, rt = c["hT"], c["tmpA"], c["tmpB"], c["rt"]
    for blk in range(2):
        w = load_w(cx, io["wq"].ap, col0 + blk * 256, 256, wtok=io["wq"].t, key="wq")
        for j in range(2):
            ps = cx.next_ps()
            proj_fm(cx, w, j * 128, 128, hT, ps)
            if mode == "plain":
                evac(cx, dst[:, blk * 2 + j, :], ps[:, 0:TT], [ps], [dst], scale=scale)
            elif mode == "rope64z":
                qz = c["qz"]
                jj = blk * 2 + j
                rope_fm(cx, ps, 128, 32, View(rt, 2), View(rt, 3),
                        [(0, 64, qz[0][0:64, jj, :], [qz[0]]), (64, 128, qz[1][64:128, jj, :], [qz[1]])], None,
                        tmpA, tmpB)
            elif mode == "rope64":
                rope_fm(cx, ps, 128, 32, View(rt, 2), View(rt, 3), dst[:, blk * 2 + j, :], [dst], tmpA, tmpB)
            else:
                rope_fm(cx, ps, 128, 64, View(rt, 0), View(rt, 1), dst[:, blk * 2 + j, :], [dst], tmpA, tmpB)


def attn_A(cx, c, io, t):
    S = cx.S
    q, o = c["q"], c["o"][0]
    psO, psD, rden = c["psO"], c["psD"], c["rden"]
    for m in range(2):
        lb = 2 * t + m
        n0 = max(0, lb - 2)
        nn = lb - n0 + 1
        mc = slice(m * 128, (m + 1) * 128)
        kb_, vb_ = c["kch"], c["vch"]
        for r in range(2):
            S.dma("sp", out=kb_[r][:, :, 0:nn * 128],
                  in_=io["kTa_all"].ap[r].rearrange("(h p) n -> p h n", p=128)[:, :, n0 * 128:(lb + 1) * 128],
                  reads=[io["kTa_all"]], writes=[kb_[r]])
            S.dma("sp", out=vb_[r][:, 0:nn, :],
                  in_=io["va_all"].ap[r][n0 * 128:(lb + 1) * 128, :].rearrange("(n p) f -> p n f", p=128),
                  reads=[io["va_all"]], writes=[vb_[r]])
        tot = 2 * nn
        k = 0
        for r in range(2):
            for ni in range(nn):
                nrel = n0 + ni - (lb - 2)
                pss = rr(c, "psS", "S")
                pT = rr(c, "pT", "pT")
                for h in range(4):
                    S.op("pe", lambda e, h=h, r=r, ni=ni, pss=pss: e.matmul(
                        pss[:, h * 128:(h + 1) * 128], lhsT=kb_[r][:, h, ni * 128:(ni + 1) * 128], rhs=q[:, h, mc],
                        start=(h == 0), stop=False, skip_group_check=True), reads=[kb_[r], q], writes=[pss])
                S.op("pe", lambda e, r=r, nrel=nrel, pss=pss: e.matmul(
                    pss[:, :], lhsT=c["ident"][:, :], rhs=c["biasA"][:, r * 3 + nrel, :], start=False, stop=True,
                    skip_group_check=True), reads=[c["ident"], c["biasA"]], writes=[pss])
                S.op("act", lambda e, pss=pss, pT=pT: e.activation(out=pT[:, :], in_=pss[:, :], func=AF.Exp),
                     reads=[pss], writes=[pT])
                for h in range(4):
                    S.op("pe", lambda e, h=h, r=r, ni=ni, pT=pT, k=k: e.matmul(
                        psO[:, h * 128:(h + 1) * 128], lhsT=vb_[r][:, ni, h * 128:(h + 1) * 128],
                        rhs=pT[:, h * 128:(h + 1) * 128], start=(k == 0 and h == 0), stop=(k == tot - 1 and h == 3),
                        skip_group_check=True), reads=[vb_[r], pT], writes=[psO])
                S.op("pe", lambda e, pT=pT, k=k: e.matmul(psD[:, :], lhsT=cx.ones_bf[:, :], rhs=pT[:, :],
                                                           start=(k == 0), stop=(k == tot - 1)),
                     reads=[cx.ones_bf, pT], writes=[psD])
                k += 1
        S.op("dve", lambda e: e.reciprocal(out=rden[:, :], in_=psD[:, :]), reads=[psD], writes=[rden])
        S.op("dve", lambda e, mc=mc: e.tensor_tensor(out=o[:, :, mc], in0=psO[:, :].rearrange("p (h q) -> p h q", h=4),
                                                     in1=rden[:, :].rearrange("p (h q) -> p h q", h=4), op=ALU.mult),
             reads=[psO, rden], writes=[o])


def kv_chunks(t):
    nb_r = 2 * t + 2
    out = []
    for r in range(2):
        for cst in range(0, nb_r, 4):
            out.append((r, cst, min(4, nb_r - cst)))
    return out


def attn_BC(cx, c, io, t, kind):
    S = cx.S
    q = c["q"]
    psO, psD, rden = c["psO"], c["psD"], c["rden"]
    kname, vname = ("kTb_all", "vb_all") if kind == "B" else ("kTc_all", "vc_all")
    o = c["o"][1] if kind == "B" else c["o"][2]
    chunks = kv_chunks(t)
    nblocks = sum(nb for _, _, nb in chunks)
    ngroups = 4 if kind == "B" else 2
    for g in range(ngroups):
        k = 0
        for (r, cst, nb) in chunks:
            kc_ = rr(c, "kch", "kv")
            vc_ = c["vch"][(c["rr"]["kv"] - 1) % 2]
            cols = slice(cst * 128, (cst + nb) * 128)
            if kind == "B":
                S.dma("sp", out=kc_[:, 0, 0:nb * 128], in_=io[kname].ap[r][g * 128:(g + 1) * 128, cols],
                      reads=[io[kname]], writes=[kc_])
            else:
                S.dma("sp", out=kc_[:, 0:2, 0:nb * 128],
                      in_=io[kname].ap[r].rearrange("(h p) n -> p h n", p=128)[:, 2 * g:2 * g + 2, cols],
                      reads=[io[kname]], writes=[kc_])
            S.dma("sp", out=vc_[:, 0:nb, :], in_=io[vname].ap[r][cols, :].rearrange("(n p) f -> p n f", p=128),
                  reads=[io[vname]], writes=[vc_])
            for bi in range(nb):
                n = cst + bi
                npr = n - 2 * t
                q0 = 128 if npr == 1 else 0
                pss = rr(c, "psS", "S")
                pT = rr(c, "pT", "pT")
                bsl = slice(bi * 128, (bi + 1) * 128)
                first, last = (k == 0), (k == nblocks - 1)
                for s in range(2):
                    osl = slice(s * 256 + q0, (s + 1) * 256)
                    if kind == "B":
                        ps_ = slice(s * 64, (s + 1) * 64)
                        S.op("pe", lambda e, s=s, osl=osl, bsl=bsl, pss=pss, kc_=kc_, npr=npr: e.matmul(
                            pss[:, osl], lhsT=kc_[:, 0, bsl], rhs=c["qz"][s][:, g, q0:TT], start=(s == 0),
                            stop=(s == 1 and npr < 0), skip_group_check=True), reads=[kc_, c["qz"][s]], writes=[pss])
                    else:
                        S.op("pe", lambda e, s=s, osl=osl, bsl=bsl, pss=pss, kc_=kc_, npr=npr: e.matmul(
                            pss[:, osl], lhsT=kc_[:, s, bsl], rhs=q[:, 2 * g + s, q0:TT], start=(s == 0),
                            stop=(s == 1 and npr < 0), skip_group_check=True), reads=[kc_, q], writes=[pss])
                if npr >= 0:
                    mi = r if kind == "B" else 2 + r
                    for s in range(2):
                        msl = slice(s * 256 + npr * 128, s * 256 + (npr + 1) * 128)
                        S.op("pe", lambda e, s=s, msl=msl, mi=mi, pss=pss: e.matmul(
                            pss[:, msl], lhsT=c["ident"][:, :], rhs=c["mk"][:, mi, :], start=False, stop=(s == 1),
                            skip_group_check=True), reads=[c["ident"], c["mk"]], writes=[pss])
                v3 = lambda ap: ap.rearrange("p (s q) -> p s q", s=2)[:, :, q0:TT]
                if kind == "B":
                    S.op("act", lambda e, pss=pss, pT=pT, v3=v3: e.activation(out=v3(pT[:, :]), in_=v3(pss[:, :]),
                                                                             func=AF.Exp, scale=64 ** -0.5),
                         reads=[pss], writes=[pT])
                else:
                    sp = rr(c, "sp", "sp")
                    kbt = 2 * n + r
                    for s in range(2):
                        h = 2 * g + s
                        osl = slice(s * 256 + q0, (s + 1) * 256)
                        S.op("dve", lambda e, osl=osl, h=h, kbt=kbt, pss=pss, sp=sp: e.scalar_tensor_tensor(
                            out=sp[:, osl], in0=pss[:, osl], scalar=c["negcum"][:, kbt * 4 + h:kbt * 4 + h + 1],
                            op0=ALU.add, in1=c["cumq"][:, h, q0:TT], op1=ALU.add),
                             reads=[pss, c["negcum"], c["cumq"]], writes=[sp])
                    S.op("act", lambda e, sp=sp, pT=pT, v3=v3: e.activation(out=v3(pT[:, :]), in_=v3(sp[:, :]),
                                                                           func=AF.Exp), reads=[sp], writes=[pT])
                if kind == "B":
                    S.op("pe", lambda e, pT=pT, vc_=vc_, bi=bi, first=first, last=last, v3=v3: e.matmul(
                        v3(psO[:, :]), lhsT=vc_[:, bi, g * 128:(g + 1) * 128], rhs=v3(pT[:, :]), start=first, stop=last,
                        skip_group_check=True), reads=[vc_, pT], writes=[psO])
                else:
                    for s in range(2):
                        h = 2 * g + s
                        osl = slice(s * 256 + q0, (s + 1) * 256)
                        S.op("pe", lambda e, s=s, h=h, osl=osl, pT=pT, vc_=vc_, bi=bi, first=first, last=last: e.matmul(
                            psO[:, osl], lhsT=vc_[:, bi, h * 128:(h + 1) * 128], rhs=pT[:, osl],
                            start=(first and s == 0), stop=(last and s == 1), skip_group_check=True),
                             reads=[vc_, pT], writes=[psO])
                S.op("pe", lambda e, pT=pT, first=first, last=last, v3=v3: e.matmul(
                    v3(psD[:, :]), lhsT=cx.ones_bf[:, :], rhs=v3(pT[:, :]), start=first, stop=last,
                    skip_group_check=True), reads=[cx.ones_bf, pT], writes=[psD])
                k += 1
        S.op("dve", lambda e: e.reciprocal(out=rden[:, :], in_=psD[:, :]), reads=[psD], writes=[rden])
        if kind == "C":
            S.op("dve", lambda e, g=g: e.tensor_tensor(
                out=o[:, 2 * g:2 * g + 2, :], in0=psO[:, :].rearrange("p (s q) -> p s q", s=2),
                in1=rden[:, :].rearrange("p (s q) -> p s q", s=2), op=ALU.mult), reads=[psO, rden], writes=[o])
        else:
            obr, tA, tB = c["obr"], c["tmpA"], c["tmpB"]
            S.op("dve", lambda e: e.tensor_tensor(out=obr[:, :], in0=psO[:, 0:TT], in1=rden[:, 0:TT], op=ALU.mult),
                 reads=[psO, rden], writes=[obr])
            S.op("dve", lambda e: e.tensor_tensor(out=tA[:, :], in0=psO[:, TT:2 * TT], in1=rden[:, TT:2 * TT],
                                                  op=ALU.mult), reads=[psO, rden], writes=[tA])
            S.op("dve", lambda e: e.scalar_tensor_tensor(out=obr[:, :], in0=tA[:, :], scalar=c["neglam"][:, 0:1],
                                                         op0=ALU.mult, in1=obr[:, :], op1=ALU.add),
                 reads=[tA, c["neglam"], obr], writes=[obr])
            sq = cx.sqb[0]
            psm = c["psm"]
            S.op("act", lambda e: e.activation(out=sq[:, :], in_=obr[:, :], func=AF.Square), reads=[obr], writes=[sq])
            S.op("pe", lambda e: e.matmul(psm[:, 0:TT], lhsT=cx.ones_bf[:, :], rhs=sq[:, :], start=True, stop=True),
                 reads=[cx.ones_bf, sq], writes=[psm])
            S.op("act", lambda e: e.activation(out=tB[:, :], in_=psm[:, 0:TT], func=AF.Sqrt, bias=EPS, scale=1.0 / 128),
                 reads=[psm], writes=[tB])
            S.op("dve", lambda e: e.reciprocal(out=tB[:, :], in_=tB[:, :]), reads=[tB], writes=[tB])
            S.op("dve", lambda e, g=g: e.scalar_tensor_tensor(out=o[:, g, :], in0=obr[:, :], scalar=c["dng"][:, 0:1],
                                                              op0=ALU.mult, in1=tB[:, :], op1=ALU.mult),
                 reads=[obr, c["dng"], tB], writes=[o])


def attn_D(cx, c, io, t):
    S = cx.S
    q, iq, o, score = c["q"], c["iq"], c["o"][3], c["score"]
    psO, psD, psT, rden, m8 = c["psO"], c["psD"], c["psT"], c["rden"], c["m8"]
    for m in range(2):
        lb = 2 * t + m
        nk = (2 * lb + 2) * 128
        mc = slice(m * 128, (m + 1) * 128)
        for cc in range(0, nk, 512):
            ncol = min(512, nk - cc)
            for ih in range(8):
                psI = rr(c, "psS", "S")
                sp = rr(c, "sp", "sp")
                prow = slice((ih % 2) * 64, (ih % 2 + 1) * 64)
                S.op("pe", lambda e, psI=psI, prow=prow, ih=ih, cc=cc, ncol=ncol: e.matmul(
                    psI[:, 0:ncol], lhsT=iq[prow, ih // 2, mc], rhs=c["ikT2"][prow, cc:cc + ncol], start=True, stop=True),
                     reads=[iq, c["ikT2"]], writes=[psI])
                S.op("act", lambda e, psI=psI, sp=sp, ncol=ncol: e.activation(out=sp[:, 0:ncol], in_=psI[:, 0:ncol],
                                                                              func=AF.Relu), reads=[psI], writes=[sp])
                iwc = c["iw"][:, m * 8 + ih:m * 8 + ih + 1]
                if ih == 0:
                    S.op("dve", lambda e, sp=sp, cc=cc, ncol=ncol, iwc=iwc: e.tensor_scalar(
                        out=score[:, cc:cc + ncol], in0=sp[:, 0:ncol], scalar1=iwc, scalar2=None, op0=ALU.mult),
                         reads=[sp, c["iw"]], writes=[score])
                else:
                    S.op("dve", lambda e, sp=sp, cc=cc, ncol=ncol, iwc=iwc: e.scalar_tensor_tensor(
                        out=score[:, cc:cc + ncol], in0=sp[:, 0:ncol], scalar=iwc, op0=ALU.mult,
                        in1=score[:, cc:cc + ncol], op1=ALU.add), reads=[sp, c["iw"], score], writes=[score])
        S.op("dve", lambda e, nk=nk: e.tensor_tensor(out=score[:, nk - 256:nk], in0=score[:, nk - 256:nk],
                                                     in1=c["idxmask"][:, :], op=ALU.add),
             reads=[score, c["idxmask"]], writes=[score])
        if lb >= 1:
            u = c["u"]
            junk = u[:, :, :].rearrange("p k n -> p (k n)")
            lo, cand, cnt, gg = (c["bis"][:, i:i + 1] for i in range(4))
            S.op("dve", lambda e, nk=nk: e.max(out=m8[:, :], in_=score[:, 0:nk]), reads=[score], writes=[m8])
            S.op("dve", lambda e: e.tensor_scalar(out=lo, in0=m8[:, 0:1], scalar1=-BIS_W, scalar2=None, op0=ALU.add),
                 reads=[m8], writes=[c["bis"]])
            for it in range(BIS_IT):
                ck = BIS_W / float(2 ** (it + 1))
                S.op("dve", lambda e, ck=ck: e.tensor_scalar(out=cand, in0=lo, scalar1=ck, scalar2=None, op0=ALU.add),
                     reads=[c["bis"]], writes=[c["bis"]])
                S.op("dve", lambda e, nk=nk: e.tensor_scalar(out=junk[:, 0:nk], in0=score[:, 0:nk], scalar1=cand,
                                                             scalar2=None, op0=ALU.is_ge, op1=ALU.add, accum_out=cnt),
                     reads=[score, c["bis"]], writes=[u, c["bis"]])
                S.op("dve", lambda e, ck=ck: e.tensor_scalar(out=gg, in0=cnt, scalar1=256.0, scalar2=ck, op0=ALU.is_ge,
                                                             op1=ALU.mult), reads=[c["bis"]], writes=[c["bis"]])
                S.op("dve", lambda e: e.tensor_tensor(out=lo, in0=lo, in1=gg, op=ALU.add), reads=[c["bis"]],
                     writes=[c["bis"]])
        nkb = nk // 128
        k = 0
        for cc in range(0, nkb, 4):
            nb = min(4, nkb - cc)
            nm = rr(c, "nm", "nm")
            nmT = c["nmT"][(c["rr"]["nm"] - 1) % 2]
            csl = slice(cc * 128, (cc + nb) * 128)
            if lb >= 1:
                S.op("dve", lambda e, nm=nm, csl=csl, nb=nb: e.tensor_scalar(
                    out=nm[:, 0:nb * 128], in0=score[:, csl], scalar1=c["bis"][:, 0:1], scalar2=NEG, op0=ALU.is_lt,
                    op1=ALU.mult), reads=[score, c["bis"]], writes=[nm])
            else:
                S.op("dve", lambda e, nm=nm, csl=csl, nb=nb: e.tensor_scalar(
                    out=nm[:, 0:nb * 128], in0=score[:, csl], scalar1=-1.0e29, scalar2=NEG, op0=ALU.is_lt, op1=ALU.mult),
                     reads=[score], writes=[nm])
            for bi in range(nb):
                S.op("pe", lambda e, nm=nm, bi=bi: e.transpose(out=psT[:, bi * 128:(bi + 1) * 128],
                                                               in_=nm[:, bi * 128:(bi + 1) * 128],
                                                               identity=c["ident"][:, :]),
                     reads=[nm, c["ident"]], writes=[psT])
            S.op("act", lambda e, nmT=nmT, nb=nb: e.activation(
                out=nmT[:, 0:nb, :], in_=psT[:, 0:nb * 128].rearrange("p (b q) -> p b q", q=128), func=AF.Copy),
                 reads=[psT], writes=[nmT])
            for bi in range(nb):
                kb = cc + bi
                pss = rr(c, "psS", "S")
                pT = rr(c, "pT", "pT")
                first, last = (k == 0), (k == nkb - 1)
                S.op("pe", lambda e, kb=kb, pss=pss: e.matmul(
                    pss[:, :].rearrange("p (h q) -> p h q", h=4), lhsT=c["kTd"][:, kb * 128:(kb + 1) * 128],
                    rhs=q[:, :, mc], start=True, stop=False, skip_group_check=True), reads=[c["kTd"], q], writes=[pss])
                S.op("pe", lambda e, bi=bi, nmT=nmT, pss=pss: e.matmul(
                    pss[:, :].rearrange("p (h q) -> p h q", h=4), lhsT=c["ident"][:, :],
                    rhs=nmT[:, bi:bi + 1, :].broadcast_to([128, 4, 128]), start=False, stop=True, skip_group_check=True),
                     reads=[c["ident"], nmT], writes=[pss])
                S.op("act", lambda e, pss=pss, pT=pT: e.activation(out=pT[:, :], in_=pss[:, :], func=AF.Exp,
                                                                  scale=128 ** -0.5), reads=[pss], writes=[pT])
                S.op("pe", lambda e, kb=kb, pT=pT, first=first, last=last: e.matmul(
                    psO[:, :], lhsT=c["vd"][:, kb, :], rhs=pT[:, :], start=first, stop=last), reads=[c["vd"], pT],
                     writes=[psO])
                S.op("pe", lambda e, pT=pT, first=first, last=last: e.matmul(
                    psD[:, :], lhsT=cx.ones_bf[:, :], rhs=pT[:, :], start=first, stop=last), reads=[cx.ones_bf, pT],
                     writes=[psD])
                k += 1
        S.op("dve", lambda e: e.reciprocal(out=rden[:, :], in_=psD[:, :]), reads=[psD], writes=[rden])
        S.op("dve", lambda e, mc=mc: e.tensor_tensor(out=o[:, :, mc], in0=psO[:, :].rearrange("p (h q) -> p h q", h=4),
                                                     in1=rden[:, :].rearrange("p (h q) -> p h q", h=4), op=ALU.mult),
             reads=[psO, rden], writes=[o])


def dense_tail(cx, c, io, t, x_dst, last_layer):
    S = cx.S
    x, hT, mg, u, acc = c["xbuf"], c["hT"], c["mg"], c["u"], c["acc"]
    sp = c["sp"]
    for j2 in range(8):
        for i in range(4):
            wg = load_w(cx, io["wg"].ap, i * 2048 + j2 * 256, 256, wtok=io["wg"].t, key="wg")
            wb = load_w(cx, io["wbr"].ap, j2 * 256, 256, nk=4, row0=i * 512, wtok=io["wbr"].t, key="wbr")
            for j in range(2):
                psg = cx.next_ps()
                proj_fm(cx, wg, j * 128, 128, hT, psg)
                psb = cx.next_ps()
                proj_fm(cx, wb, j * 128, 128, c["o"][i], psb, nk=4)
                sg = sp[j]
                bcol = i * 16 + j2 * 2 + j
                S.op("act", lambda e, psg=psg, sg=sg, bcol=bcol: e.activation(
                    out=sg[:, 0:TT], in_=psg[:, 0:TT], func=AF.Sigmoid, bias=c["bgate"][:, bcol:bcol + 1]),
                     reads=[psg, c["bgate"]], writes=[sg])
                if i == 0:
                    S.op("dve", lambda e, psb=psb, sg=sg, j=j: e.tensor_tensor(out=acc[j][:, :], in0=psb[:, 0:TT],
                                                                              in1=sg[:, 0:TT], op=ALU.mult),
                         reads=[psb, sg], writes=[acc[j]])
                else:
                    S.op("dve", lambda e, psb=psb, sg=sg: e.tensor_tensor(out=sg[:, 0:TT], in0=psb[:, 0:TT],
                                                                         in1=sg[:, 0:TT], op=ALU.mult),
                         reads=[psb, sg], writes=[sg])
                    if i < 3:
                        S.op("dve", lambda e, sg=sg, j=j: e.tensor_tensor(out=acc[j][:, :], in0=acc[j][:, :],
                                                                         in1=sg[:, 0:TT], op=ALU.add),
                             reads=[acc[j], sg], writes=[acc[j]])
                    else:
                        jj = j2 * 2 + j
                        S.op("dve", lambda e, sg=sg, j=j, jj=jj: e.tensor_tensor(out=mg[:, jj, :], in0=acc[j][:, :],
                                                                                in1=sg[:, 0:TT], op=ALU.add),
                             reads=[acc[j], sg], writes=[mg.s(jj)])
    for j2 in range(8):
        w = load_w(cx, io["wo"].ap, j2 * 256, 256, wtok=io["wo"].t, key="wo")
        for j in range(2):
            jj = j2 * 2 + j
            ps = cx.next_ps()
            proj_fm(cx, w, j * 128, 128, mg, ps)
            S.op("dve", lambda e, ps=ps, jj=jj: e.tensor_tensor(out=x[:, jj, :], in0=ps[:, 0:TT], in1=x[:, jj, :],
                                                                op=ALU.add), reads=[ps, x], writes=[x])
    rmsnorm_fm(cx, x, c["n2g"], hT, c["psm"])
    for half in range(2):
        for j2 in range(16):
            w = load_w(cx, io["wf1"].ap, half * 4096 + j2 * 256, 256, wtok=io["wf1"].t, key="wf1")
            for j in range(2):
                jj = j2 * 2 + j
                ps = cx.next_ps()
                proj_fm(cx, w, j * 128, 128, hT, ps)
                r_ = sp[j]
                S.op("act", lambda e, ps=ps, r_=r_: e.activation(out=r_[:, 0:TT], in_=ps[:, 0:TT], func=AF.Relu),
                     reads=[ps], writes=[r_])
                S.op("dve", lambda e, r_=r_, jj=jj, ps=ps: e.tensor_tensor(out=u[:, jj, :], in0=ps[:, 0:TT],
                                                                          in1=r_[:, 0:TT], op=ALU.mult),
                     reads=[r_, ps], writes=[u.s(jj)])
        for j2 in range(8):
            ws = [load_w(cx, io["wf2"].ap, j2 * 256, 256, nk=16, row0=half * 4096 + kk * 2048, wtok=io["wf2"].t, key="wf2")
                  for kk in range(2)]
            for j in range(2):
                jj = j2 * 2 + j
                ps = cx.next_ps()
                for kk in range(2):
                    for kc in range(16):
                        kidx = kk * 16 + kc
                        S.op("pe", lambda e, kk=kk, kc=kc, kidx=kidx, ps=ps, j=j: e.matmul(
                            ps[:, 0:TT], lhsT=ws[kk][:, kc, j * 128:(j + 1) * 128], rhs=u[:, kidx, :],
                            start=(kidx == 0), stop=(kidx == 31)), reads=[ws[kk], u.s(kidx)], writes=[ps])
                S.op("dve", lambda e, ps=ps, jj=jj: e.tensor_tensor(out=x[:, jj, :], in0=ps[:, 0:TT], in1=x[:, jj, :],
                                                                    op=ALU.add), reads=[ps, x], writes=[x])
    tc0 = t * TT
    if not last_layer:
        S.dma("sp", out=x_dst.ap[:, tc0:tc0 + TT].rearrange("(k p) n -> p k n", p=128), in_=x[:, :, :], reads=[x],
              writes=[x_dst])
    else:
        ps = c["psm"]
        rstd = cx.rstd
        for kc in range(KC):
            sq = cx.sqb[kc % 2]
            S.op("act", lambda e, kc=kc, sq=sq: e.activation(out=sq[:, :], in_=x[:, kc, :], func=AF.Square),
                 reads=[x], writes=[sq])
            S.op("pe", lambda e, kc=kc, sq=sq: e.matmul(ps[:, 0:TT], lhsT=cx.ones_bf[:, :], rhs=sq[:, :],
                                                        start=(kc == 0), stop=(kc == KC - 1)),
                 reads=[sq, cx.ones_bf], writes=[ps])
        S.op("act", lambda e: e.activation(out=rstd[:, :], in_=ps[:, 0:TT], func=AF.Sqrt, bias=EPS,
                                           scale=1.0 / D_MODEL), reads=[ps], writes=[rstd])
        S.op("dve", lambda e: e.reciprocal(out=rstd[:, :], in_=rstd[:, :]), reads=[rstd], writes=[rstd])
        for kc in range(KC):
            S.op("dve", lambda e, kc=kc: e.scalar_tensor_tensor(out=x[:, kc, :], in0=x[:, kc, :],
                                                                scalar=c["fg"][:, kc:kc + 1], op0=ALU.mult,
                                                                in1=rstd[:, :], op1=ALU.mult),
                 reads=[x, c["fg"], rstd], writes=[x])
        S.dma("sp", out=x_dst.ap[:, tc0:tc0 + TT].rearrange("(k p) n -> p k n", p=128), in_=x[:, :, :], reads=[x],
              writes=[x_dst])


def phase_q(cx, c, io, x_src, x_dst, lambda_init, last_layer, dbg=None):
    S = cx.S
    x, hT, psm = c["xbuf"], c["hT"], c["psm"]
    for i in range(2):
        S.op("dve", lambda e, i=i: e.memset(c["qz"][i][:, :, :], 0.0), writes=[c["qz"][i]])
    for t in range(NT):
        cx.first_tile = (t == 0)
        load_x_tile(cx, c, x_src, t, io["ropet"])
        rmsnorm_fm(cx, x, c["n1g"], hT, psm)
        for m in range(2):
            for kc in range(KC):
                S.op("pe", lambda e, m=m, kc=kc: e.matmul(psm[:, m * 8:(m + 1) * 8], lhsT=hT[:, kc, m * 128:(m + 1) * 128],
                                                          rhs=c["wiw"][:, kc, :], start=(kc == 0), stop=(kc == KC - 1)),
                     reads=[hT.s(kc), c["wiw"]], writes=[psm])
        S.op("act", lambda e: e.mul(out=c["iw"][:, :], in_=psm[:, 0:16], mul=IWS), reads=[psm], writes=[c["iw"]])
        q_proj(cx, c, io, 0, c["q"], "plain", scale=128 ** -0.5)
        attn_A(cx, c, io, t)
        q_proj(cx, c, io, 512, None, "rope64z")
        attn_BC(cx, c, io, t, "B")
        q_proj(cx, c, io, 1024, c["q"], "plain", scale=128 ** -0.5)
        for h in range(4):
            S.op("pe", lambda e, h=h, t=t: e.matmul(psm[:, 0:TT], lhsT=c["selh"][0:4, h * 128:(h + 1) * 128],
                                                    rhs=c["cum_mine"][0:4, t * TT:(t + 1) * TT], start=True, stop=True),
                 reads=[c["selh"], c["cum_mine"]], writes=[psm])
            S.op("act", lambda e, h=h: e.activation(out=c["cumq"][:, h, :], in_=psm[:, 0:TT], func=AF.Copy),
                 reads=[psm], writes=[c["cumq"]])
        attn_BC(cx, c, io, t, "C")
        q_proj(cx, c, io, 1536, c["q"], "rope128")
        q_proj(cx, c, io, 2048, c["iq"], "rope64")
        attn_D(cx, c, io, t)
        if dbg is not None:
            for i in range(4):
                S.dma("sp", out=dbg[i].ap[:, t * TT:(t + 1) * TT].rearrange("(h p) n -> p h n", p=128),
                      in_=c["o"][i][:, :, :], reads=[c["o"][i]], writes=[dbg[i]])
        dense_tail(cx, c, io, t, x_dst, last_layer)


Q_IN = (("wq", [D_MODEL, 2560]), ("wiw", [D_MODEL, 8]), ("wg", [D_MODEL, 8192]), ("wbr", [2048, D_MODEL]),
        ("wo", [D_MODEL, D_MODEL]), ("wf1", [D_MODEL, D_FF]), ("wf2", [D_FF, D_MODEL]),
        ("ropet", [4, 128, LTOK]), ("biasA", [128, 6 * 512]), ("mk", [128, 4 * 128]), ("identf", [128, 128]),
        ("idxmask", [128, 256]), ("selh", [4, 512]), ("sel", [4, 2]), ("n1g", [128, KC]), ("n2g", [128, KC]),
        ("fg", [128, KC]), ("bgate", [128, 64]), ("dng", [128, 1]), ("lam4", [64, 4]))


def build_B(layer, dbg=False):
    lambda_init = 0.8 - 0.6 * math.exp(-0.3 * layer)
    nc = bass.Bass("TRN2", target_bir_lowering=False)
    es = ExitStack()
    io = {}
    xT = dram_in(nc, "xT", [D_MODEL, LTOK])
    for nm, shp in Q_IN:
        io[nm] = dram_in(nc, nm, shp)
    for nm, shp, dt in KV_ALL:
        io[nm + "_all"] = dram_in(nc, nm + "_all", shp, dt)
    xo = dram_out(nc, "xo", [D_MODEL, LTOK])
    dbgs = [dram_out(nc, f"dbg_o{i}", [512, LTOK], BF16) for i in range(4)] if dbg else None
    with es:
        S = Sched(nc, es)
        cx = Ctx(S)
        c = alloc_q(cx, alloc_common(cx))
        S.dma("sp", out=c["n1g"][:, :], in_=io["n1g"].ap[:, :], reads=[io["n1g"]], writes=[c["n1g"]])
        setup_q(cx, c, io, lambda_init)
        phase_q(cx, c, io, lambda t: (xT.ap[:, t * TT:(t + 1) * TT], xT.t), xo, lambda_init, layer == DEPTH - 1, dbgs)
        S.final_all()
        S.emit()
    return nc


def table_biasA(rel_bias_l, c):
    out = np.full((128, 6, 4, 128), NEG, np.float32)
    j = np.arange(128)[:, None]
    qi = np.arange(128)[None, :]
    for r in range(2):
        for nrel in range(3):
            dblk = (2 * (nrel - 2) + r) - c
            dist = -dblk * 128 + qi - j
            kch = 2 * dblk + j // 64
            qch = qi // 64
            valid = (kch <= qch) & (kch >= qch - 8)
            idx = np.clip(dist, -128, 128) + 128
            for h in range(4):
                vals = rel_bias_l[h][idx]
                out[:, r * 3 + nrel, h, :] = np.where(valid, vals, np.float32(NEG))
    return np.ascontiguousarray(out.reshape(128, 6 * 512))


def table_mk(c):
    out = np.zeros((128, 4, 128), np.float32)
    j = np.arange(128)[:, None]
    qi = np.arange(128)[None, :]
    diagB = np.where(j // 64 <= qi // 64, 0.0, NEG).astype(np.float32)
    diagC = np.where(j <= qi, 0.0, NEG).astype(np.float32)
    for r in range(2):
        if r < c:
            mB = mC = np.zeros((128, 128), np.float32)
        elif r == c:
            mB, mC = diagB, diagC
        else:
            mB = mC = np.full((128, 128), NEG, np.float32)
        out[:, r, :] = mB
        out[:, 2 + r, :] = mC
    return np.ascontiguousarray(out.reshape(128, 512))


def table_idxmask(c):
    out = np.zeros((128, 2, 128), np.float32)
    qi = np.arange(128)[:, None]
    j = np.arange(128)[None, :]
    for r in range(2):
        if r < c:
            pass
        elif r == c:
            out[:, r, :] = np.where(j // 64 <= qi // 64, 0.0, -1.0e30)
        else:
            out[:, r, :] = -1.0e30
    return np.ascontiguousarray(out.reshape(128, 256))


def host_q_inputs(inp, l, c):
    w_in = inp["w_in"][l]
    m = {}
    m["wq"] = wcols(w_in, ["a_q", "b_q", "c_q", "d_q", "d_iq"])
    m["wiw"] = wcols(w_in, ["d_iw"])
    m["wg"] = wcols(w_in, ["gate"])
    m["wbr"] = np.ascontiguousarray(inp["w_branch"][l].reshape(4 * 512, D_MODEL))
    m["wo"] = np.ascontiguousarray(inp["w_out"][l])
    m["wf1"] = np.ascontiguousarray(inp["w_ff1"][l])
    m["wf2"] = np.ascontiguousarray(inp["w_ff2"][l])
    m["ropet"] = rope_tabs(c)
    m["biasA"] = table_biasA(inp["rel_bias"][l], c)
    m["mk"] = table_mk(c)
    m["identf"] = np.eye(128, dtype=np.float32)
    m["idxmask"] = table_idxmask(c)
    selh = np.zeros((4, 4, 128), np.float32)
    for h in range(4):
        selh[h, h, :] = 1.0
    m["selh"] = selh.reshape(4, 512)
    sel = np.zeros((4, 2), np.float32)
    sel[:, c] = 1.0
    m["sel"] = sel
    m["n1g"] = pcol(inp["norm1_g"][l])
    m["n2g"] = pcol(inp["norm2_g"][l])
    m["fg"] = pcol(inp["final_g"])
    m["bgate"] = pcol(inp["b_gate"][l])
    m["dng"] = np.ascontiguousarray(inp["diff_norm_g"][l].reshape(128, 1))
    m["lam4"] = np.ascontiguousarray(np.stack([inp["lambda_q1"][l], inp["lambda_k1"][l], inp["lambda_q2"][l],
                                               inp["lambda_k2"][l]], axis=1))
    return m


def host_kv_inputs(inp, l, c):
    w_in = inp["w_in"][l]
    return {"wk": wcols(w_in, ["a_k", "b_k", "c_k", "d_k", "d_ik", "c_f"]),
            "wv": wcols(w_in, ["a_v", "b_v", "c_v", "d_v"]),
            "ropet": rope_tabs(c), "n1g": pcol(inp["norm1_g"][l]),
            "bfg": np.ascontiguousarray(inp["b_forget"][l].reshape(4, 1))}


I32 = mybir.dt.int32
PK = {"kTa": 0, "kTb": 512, "kTc": 1024, "kTd": 1536, "ikT": 1664, "logf": 1728, "va": 1792, "vb": 2304,
      "vc": 2816, "vd": 3328}
RP = 3456
NSEL = 2 * RP // 128


def pack_views(ap2d, base):
    v = {}
    for nm, rows in (("kTa", 512), ("kTb", 512), ("kTc", 512), ("kTd", 128), ("ikT", 64)):
        v[nm] = ap2d[base + PK[nm]:base + PK[nm] + rows, :]
    v["logf"] = ap2d[base + PK["logf"]:base + PK["logf"] + 8, :].bitcast(F32).rearrange("(h a) n -> h (a n)", a=2)
    for nm in ("va", "vb", "vc"):
        v[nm] = ap2d[base + PK[nm]:base + PK[nm] + 512, :].rearrange("r (a f) -> (r a) f", f=512)
    v["vd"] = ap2d[base + PK["vd"]:base + PK["vd"] + 128, :].rearrange("r (a f) -> (r a) f", f=128)
    return v


A_IN = (("wk", [D_MODEL, WK_COLS]), ("wv", [D_MODEL, WV_COLS]), ("bfg", [4, 1]))


def build_fused():
    nc = bass.Bass("TRN2", target_bir_lowering=False)
    es = ExitStack()
    xT = dram_in(nc, "xT", [D_MODEL, LTOK])
    pidx_d = dram_in(nc, "pidx", [128, NSEL], I32)
    ios = []
    for l in range(DEPTH):
        io = {}
        for nm, shp in A_IN + Q_IN:
            if nm in ("ropet", "identf", "mk", "idxmask", "selh", "sel", "fg"):
                continue
            io[nm] = dram_in(nc, f"{nm}{l}", shp)
        ios.append(io)
    shared = {nm: dram_in(nc, nm, dict(Q_IN)[nm]) for nm in ("ropet", "identf", "mk", "idxmask", "selh", "sel", "fg")}
    xo = dram_out(nc, "xo", [D_MODEL, LTOK])
    kvpack_h = nc.dram_tensor("kvpack", [RP, LTOK], BF16)
    kvg_h = nc.dram_tensor("kvg", [N_CORES * RP, LTOK], BF16)
    kvall_h = nc.dram_tensor("kvall", [2 * RP, LTOK], BF16)
    xs1_h = nc.dram_tensor("xs1", [D_MODEL, LTOK], F32)
    NSLOT = 160
    wscr_h = [nc.dram_tensor(f"wscr{l}", [NSLOT, 128, KC * WB], BF16) for l in range(DEPTH)] if USE_WSCR else []
    t_pack, t_g, t_all = Tok(dj=True), Tok(dj=True), Tok(dj=True)
    xs1 = DT(xs1_h.ap(), dj=True)
    pv = pack_views(kvpack_h.ap(), 0)
    av = [pack_views(kvall_h.ap(), r * RP) for r in range(2)]
    with es:
        S = Sched(nc, es)
        cx = Ctx(S)
        c = alloc_q(cx, alloc_kv(cx, alloc_common(cx)))
        c["pidx"] = S.sb("pidx", [128, NSEL], I32)
        cx.wscr = [h.ap() for h in wscr_h] if USE_WSCR else None
        S.dma("sp", out=c["pidx"][:, :], in_=pidx_d.ap[:, :], writes=[c["pidx"]])
        for l in range(DEPTH):
            lambda_init = 0.8 - 0.6 * math.exp(-0.3 * l)
            cx.layer = l
            io = dict(ios[l])
            io.update(shared)
            for nm in pv:
                io[nm] = DT(pv[nm], tok=t_pack)
                io[nm + "_all"] = DT([av[0][nm], av[1][nm]], tok=t_all)
            if l == 0:
                x_src = lambda t: (xT.ap[:, t * TT:(t + 1) * TT], xT.t)
                x_dst = xs1
            else:
                x_src = lambda t: (xs1.ap[:, t * TT:(t + 1) * TT], xs1.t)
                x_dst = xo
            S.dma("sp", out=c["n1g"][:, :], in_=io["n1g"].ap[:, :], reads=[io["n1g"]], writes=[c["n1g"]])
            S.dma("sp", out=c["nbfg"][64:68, :], in_=io["bfg"].ap[:, :], reads=[io["bfg"]], writes=[c["nbfg"]])
            S.op("dve", lambda e: e.tensor_scalar(out=c["nbfg"][64:68, :], in0=c["nbfg"][64:68, :], scalar1=-1.0,
                                                  scalar2=None, op0=ALU.mult), reads=[c["nbfg"]], writes=[c["nbfg"]])
            phase_kv(cx, io, x_src, c)
            S.collective_allgather(kvpack_h.ap().opt(), kvg_h.ap().opt(), reads=[t_pack], writes=[t_g])
            u = c["u"]
            for i in range(NSEL):
                k = i % 4
                toks = [u.s(8 * k + j) for j in range(8)]
                stv = u[:, 8 * k:8 * k + 8, :].rearrange("p k n -> p (k n)")
                S.idma(out=stv, in_=kvg_h.ap()[:, :], idx_ap=c["pidx"][:, i:i + 1], reads=[t_g, c["pidx"]],
                       writes=toks, semtile=toks[0])
                S.dma("sp", out=kvall_h.ap()[i * 128:(i + 1) * 128, :], in_=stv, reads=toks, writes=[t_all],
                      semtile=toks[0])
            setup_q(cx, c, io, lambda_init)
            phase_q(cx, c, io, x_src, x_dst, lambda_init, l == DEPTH - 1, None)
        S.final_all()
        print("ops per engine:", {k: len(v) for k, v in S.ops.items()})
        S.emit()
    return nc


def pair_index(core):
    b = core // 2
    idx = np.zeros((128, NSEL), np.int32)
    nb = RP // 128
    for i in range(NSEL):
        idx[:, i] = (2 * b + i // nb) * RP + (i % nb) * 128 + np.arange(128)
    return idx


def kernel_fused(**inputs):
    inp = {k: np.asarray(v) for k, v in inputs.items()}
    x = inp["x"]
    cores = list(range(N_CORES))
    nc = build_fused()
    per_layer = []
    for l in range(DEPTH):
        d = {}
        for c in range(2):
            m = host_q_inputs(inp, l, c)
            m.update({k: v for k, v in host_kv_inputs(inp, l, c).items() if k in ("wk", "wv", "bfg")})
            d[c] = m
        per_layer.append(d)
    maps = []
    for core in cores:
        b, c = core // 2, core % 2
        m = {"xT": to_local_T(x[b], c), "pidx": pair_index(core)}
        for l in range(DEPTH):
            src = per_layer[l][c]
            for nm, _ in A_IN + Q_IN:
                if nm in ("ropet", "identf", "mk", "idxmask", "selh", "sel", "fg"):
                    m[nm] = per_layer[0][c][nm]
                else:
                    m[f"{nm}{l}"] = per_layer[l][0][nm] if nm not in ("biasA",) else src[nm]
        maps.append(m)
    res = run_bass_kernel_spmd(nc, maps, core_ids=cores)
    xo = [np.asarray(res.results[core]["xo"]) for core in cores]
    out = np.stack([_from_local(xo[2 * b], xo[2 * b + 1]) for b in range(BATCH)], 0)
    return out.astype(np.float32)


def _from_local(loc0, loc1):
    out = np.empty((SEQ, D_MODEL), np.float32)
    o3 = out.reshape(NBLK, 128, D_MODEL)
    o3[0::2] = np.asarray(loc0).T.reshape(LBLK, 128, D_MODEL)
    o3[1::2] = np.asarray(loc1).T.reshape(LBLK, 128, D_MODEL)
    return out


def kernel_unfused(**inputs):
    inp = {k: np.asarray(v) for k, v in inputs.items()}
    x = inp["x"]
    cores = list(range(N_CORES))
    xT = [to_local_T(x[core // 2], core % 2) for core in cores]
    for l in range(DEPTH):
        ncA = build_A()
        mapsA = []
        for core in cores:
            m = host_kv_inputs(inp, l, core % 2)
            m["xT"] = xT[core]
            mapsA.append(m)
        resA = run_bass_kernel_spmd(ncA, mapsA, core_ids=cores)
        del mapsA
        ncB = build_B(l)
        mapsB = []
        for core in cores:
            b, c = core // 2, core % 2
            m = host_q_inputs(inp, l, c)
            m["xT"] = xT[core]
            for nm, shp, dt in KV_ALL:
                m[nm + "_all"] = np.ascontiguousarray(
                    np.stack([resA.results[2 * b][nm], resA.results[2 * b + 1][nm]], 0))
            mapsB.append(m)
        resB = run_bass_kernel_spmd(ncB, mapsB, core_ids=cores)
        del mapsB
        xT = [np.asarray(resB.results[core]["xo"]) for core in cores]
    out = np.stack([_from_local(xT[2 * b], xT[2 * b + 1]) for b in range(BATCH)], 0)
    return out.astype(np.float32)


def kernel(**inputs):
    return kernel_fused(**inputs)


def _sim_sched(S):
    sem = {}
    pos = {e: 0 for e in ENGS}
    total = sum(len(v) for v in S.ops.values())
    done = 0
    while done < total:
        progressed = False
        for e in ENGS:
            ops = S.ops[e]
            while pos[e] < len(ops):
                waits, fn, inc = ops[pos[e]]
                if all(sem.get(k, 0) >= v for k, v in waits):
                    sem[inc[0]] = sem.get(inc[0], 0) + (1 if inc[1] is None else inc[1])
                    pos[e] += 1
                    done += 1
                    progressed = True
                else:
                    break
        if not progressed:
            for e in ENGS:
                if pos[e] < len(S.ops[e]):
                    waits, fn, inc = S.ops[e][pos[e]]
                    bad = [(k, v, sem.get(k, 0)) for k, v in waits if sem.get(k, 0) < v]
                    print("STUCK", e, pos[e], "/", len(S.ops[e]), "unsatisfied (key, need, have):", bad)
            return False
    return True
```

```python
import math
from contextlib import ExitStack

import numpy as np
import concourse.bass as bass
import concourse.mybir as mybir
from concourse.bass_utils import run_bass_kernel_spmd

F32 = mybir.dt.float32
BF16 = mybir.dt.bfloat16
AF = mybir.ActivationFunctionType
ALU = mybir.AluOpType
AX = mybir.AxisListType

D_MODEL = 2048
BATCH = 4
SEQ = 4096
DEPTH = 2
KC = D_MODEL // 128
NBLK = SEQ // 128
LBLK = NBLK // 2
LTOK = LBLK * 128
TT = 256
NT = LTOK // TT
D_FF = 4 * D_MODEL
EPS = 1e-6
NEG = -30000.0
N_CORES = 8


class Tok:
    __slots__ = ("w", "r", "parent", "subs", "dj")

    def __init__(self, parent=None, dj=False):
        self.w = {}
        self.r = {}
        self.parent = parent
        self.subs = []
        self.dj = dj


class Tile:
    def __init__(self, ap, name):
        self.ap = ap
        self.name = name
        self.t = Tok()
        self._subs = {}

    def s(self, key):
        if key not in self._subs:
            tk = Tok(parent=self.t)
            self.t.subs.append(tk)
            self._subs[key] = tk
        return self._subs[key]

    def __getitem__(self, idx):
        return self.ap[idx]


ENGS = ("pe", "act", "dve", "pool", "sp")


class _Rec:
    def __init__(self):
        self.call = None

    def __getattr__(self, name):
        def f(*a, **kw):
            self.call = (name, a, kw)
            return None
        return f


class Sched:
    def __init__(self, nc, es, n_dma_sems=56):
        self.nc = nc
        self.es = es
        self.ops = {e: [] for e in ENGS}
        self.cnt = {e: 0 for e in ENGS}
        self.sems = {}
        for e in ENGS + ("cc",):
            self.sems[e] = es.enter_context(nc.semaphore("sem_" + e))
        self.cc_cnt = 0
        self.dma_sems = [es.enter_context(nc.semaphore(f"dsem{i}")) for i in range(n_dma_sems)]
        self.dma_cnt = [0] * n_dma_sems
        self.dma_rr = 0
        self.tile_sem = {}
        self.seen = {e: {} for e in ENGS}
        self.final_waits = []
        self.ntiles = 0

    def sb(self, name, shape, dtype):
        self.ntiles += 1
        t = self.es.enter_context(self.nc.sbuf_tensor(f"{name}_{self.ntiles}", list(shape), dtype))
        return Tile(t, name)

    def ps(self, name, shape, dtype=F32):
        self.ntiles += 1
        t = self.es.enter_context(self.nc.psum_tensor(f"{name}_{self.ntiles}", list(shape), dtype))
        return Tile(t, name)

    @staticmethod
    def _toks(x):
        out = []
        for i in x:
            out.append(i if isinstance(i, Tok) else i.t)
        return out

    def _collect(self, reads, writes):
        ev = {}

        def add_r(d):
            for k, v in d.items():
                if ev.get(k, 0) < v:
                    ev[k] = v

        add = add_r

        for t in reads:
            add(t.w)
            if t.parent is not None:
                add(t.parent.w)
            for s_ in t.subs:
                add(s_.w)
        for t in writes:
            if not t.dj:
                add(t.w)
            add_r(t.r)
            if t.parent is not None:
                add(t.parent.w)
                add_r(t.parent.r)
            for s_ in t.subs:
                add(s_.w)
                add_r(s_.r)
        return ev

    def _update(self, reads, writes, me):
        k, v = me
        for t in reads:
            if t.r.get(k, 0) < v:
                t.r[k] = v
        for t in writes:
            if t.dj:
                if t.w.get(k, 0) < v:
                    t.w[k] = v
                continue
            t.w = {k: v}
            t.r = {}
            for s_ in t.subs:
                s_.w = {}
                s_.r = {}

    def _waits(self, eng, ev):
        seen = self.seen[eng]
        waits = []
        for k, v in ev.items():
            if eng == "pe" and k == "pe":
                continue
            if seen.get(k, 0) >= v:
                continue
            seen[k] = v
            waits.append((k, v))
        return waits

    def _semof(self, k):
        return self.sems[k] if isinstance(k, str) else self.dma_sems[k]

    def op(self, eng, fn, reads=(), writes=()):
        reads = self._toks(reads)
        writes = self._toks(writes)
        ev = self._collect(reads, writes)
        waits = self._waits(eng, ev)
        self.cnt[eng] += 1
        me = (eng, self.cnt[eng])
        rec = _Rec()
        fn(rec)
        name, a, kw = rec.call

        def fn2(e, name=name, a=a, kw=kw):
            return getattr(e, name)(*a, **kw)

        self.ops[eng].append((waits, fn2, (eng, 1)))
        self._update(reads, writes, me)

    def collective_allgather(self, in_ap, out_ap, reads=(), writes=()):
        reads = self._toks(reads)
        writes = self._toks(writes)
        ev = self._collect(reads, writes)
        waits = self._waits("pool", ev)
        self.cc_cnt += 1
        me = ("cc", self.cc_cnt)

        def fn(e):
            return e.collective_compute("AllGather", ALU.bypass, replica_groups=[list(range(N_CORES))],
                                        ins=[in_ap], outs=[out_ap])

        self.ops["pool"].append((waits, fn, ("cc", None)))
        self._update(reads, writes, me)

    def idma(self, out, in_, idx_ap, reads=(), writes=(), semtile=None):
        def fn(e):
            return e.indirect_dma_start(out=out, out_offset=None, in_=in_,
                                        in_offset=bass.IndirectOffsetOnAxis(ap=idx_ap, axis=0))
        return self.dma("pool", None, None, reads=reads, writes=writes, semtile=semtile, fn=fn)

    def dma(self, queue, out, in_, reads=(), writes=(), semtile=None, fn=None, **kw):
        reads = self._toks(reads)
        writes = self._toks(writes)
        ev = self._collect(reads, writes)
        key = semtile if semtile is not None else (writes[0] if writes else reads[0])
        if key not in self.tile_sem:
            self.tile_sem[key] = self.dma_rr % len(self.dma_sems)
            self.dma_rr += 1
        si = self.tile_sem[key]
        if self.dma_cnt[si] > 0:
            pv = self.dma_cnt[si]
            if ev.get(si, 0) < pv:
                ev[si] = pv
        waits = self._waits(queue, ev)
        self.dma_cnt[si] += 16
        me = (si, self.dma_cnt[si])

        if fn is None:
            def fn(e, out=out, in_=in_, kw=kw):
                return e.dma_start(out=out, in_=in_, **kw)

        self.ops[queue].append((waits, fn, (si, 16)))
        self._update(reads, writes, me)
        return me

    def final_all(self):
        for si, v in enumerate(self.dma_cnt):
            if v > 0:
                self.final_waits.append((si, v))

    def emit(self):
        nc = self.nc
        block = self.es.enter_context(nc.Block())

        def replay(eng_name, e, extra=None):
            for waits, fn, inc in self.ops[eng_name]:
                for k, v in waits:
                    e.wait_ge(self._semof(k), v)
                ins = fn(e)
                if inc[1] is None:
                    ins.then_inc(self._semof(inc[0]))
                else:
                    ins.then_inc(self._semof(inc[0]), inc[1])
            if extra:
                for k, v in extra:
                    e.wait_ge(self._semof(k), v)

        @block.sync
        def _(e):
            replay("sp", e, self.final_waits)

        @block.tensor
        def _(e):
            replay("pe", e)

        @block.scalar
        def _(e):
            replay("act", e)

        @block.vector
        def _(e):
            replay("dve", e)

        @block.gpsimd
        def _(e):
            replay("pool", e)


WB = 512
USE_WSCR = False


class Ctx:
    def __init__(self, S):
        self.S = S
        self.ones_bf = S.sb("ones_bf", [128, 128], BF16)
        S.op("dve", lambda e: e.memset(self.ones_bf[:], 1.0), writes=[self.ones_bf])
        self.wscr = None
        self.wslots = {}
        self.wtoks = {}
        self.first_tile = True
        self.layer = 0
        self.wbuf = [S.sb(f"wbuf{i}", [128, KC, WB], BF16) for i in range(2)]
        self.wrr = 0
        self.wsm = [S.sb(f"wsm{i}", [128, 4, WB], BF16) for i in range(2)]
        self.wsrr = 0
        self.psd = [S.ps(f"psd{i}", [128, 512], F32) for i in range(2)]
        self.psrr = 0
        self.evrr = 0
        self.sqb = [S.sb(f"sqb{i}", [128, TT], BF16) for i in range(2)]
        self.rstd = S.sb("rstd", [128, TT], F32)
        self.fence_t = S.sb("fence", [128, 8], F32)

    def next_w(self, small=False):
        if small:
            w = self.wsm[self.wsrr % len(self.wsm)]
            self.wsrr += 1
            return w
        w = self.wbuf[self.wrr % len(self.wbuf)]
        self.wrr += 1
        return w

    def next_ps(self):
        p = self.psd[self.psrr % len(self.psd)]
        self.psrr += 1
        return p

    def ev_eng(self):
        self.evrr += 1
        return "act" if self.evrr % 2 else "dve"

    def fence(self, tiles):
        self.S.op("pool", lambda e: e.memset(self.fence_t[:, 0:1], 0.0), writes=[self.fence_t] + list(tiles))


def load_w(cx, wdram, col0, ncols, nk=KC, row0=0, wtok=None, key=None):
    S = cx.S
    w = cx.next_w(small=(nk <= 4))
    src = wdram[row0:row0 + nk * 128, col0:col0 + ncols].rearrange("(k p) n -> p k n", p=128)
    if cx.wscr is None or key is None:
        S.dma("pool", out=w[:, 0:nk, 0:ncols], in_=src, reads=([wtok] if wtok is not None else []), writes=[w])
        return w
    k = (cx.layer, key, col0, row0)
    if k not in cx.wslots:
        cx.wslots[k] = len([1 for kk in cx.wslots if kk[0] == cx.layer])
        cx.wtoks[k] = Tok()
    slot = cx.wslots[k]
    sv = cx.wscr[cx.layer][slot][:, 0:nk * ncols].rearrange("p (k n) -> p k n", n=ncols)
    if cx.first_tile:
        S.dma("pool", out=w[:, 0:nk, 0:ncols], in_=src, reads=([wtok] if wtok is not None else []), writes=[w])
        S.dma("sp", out=sv, in_=w[:, 0:nk, 0:ncols], reads=[w], writes=[cx.wtoks[k]], semtile=w.t)
    else:
        S.dma("sp", out=w[:, 0:nk, 0:ncols], in_=sv, reads=[cx.wtoks[k]], writes=[w])
    return w


def evac(cx, out_ap, in_ps, reads, writes, scale=None, eng=None):
    S = cx.S
    eng = eng or cx.ev_eng()
    if eng == "act":
        if scale is None:
            S.op("act", lambda e: e.activation(out=out_ap, in_=in_ps, func=AF.Copy), reads=reads, writes=writes)
        else:
            S.op("act", lambda e: e.activation(out=out_ap, in_=in_ps, func=AF.Copy, scale=float(scale)),
                 reads=reads, writes=writes)
    else:
        if scale is None:
            S.op("dve", lambda e: e.tensor_copy(out=out_ap, in_=in_ps), reads=reads, writes=writes)
        else:
            S.op("dve", lambda e: e.tensor_scalar(out=out_ap, in0=in_ps, scalar1=float(scale), scalar2=None,
                                                  op0=ALU.mult), reads=reads, writes=writes)


def rmsnorm_fm(cx, x, g, hT, ps):
    S = cx.S
    rstd = cx.rstd
    for kc in range(KC):
        sq = cx.sqb[kc % 2]
        S.op("act", lambda e, kc=kc, sq=sq: e.activation(out=sq[:, :], in_=x[:, kc, :], func=AF.Square),
             reads=[x], writes=[sq])
        S.op("pe", lambda e, kc=kc, sq=sq: e.matmul(ps[:, 0:TT], lhsT=cx.ones_bf[:, :], rhs=sq[:, :],
                                                    start=(kc == 0), stop=(kc == KC - 1)),
             reads=[sq, cx.ones_bf], writes=[ps])
    S.op("act", lambda e: e.activation(out=rstd[:, :], in_=ps[:, 0:TT], func=AF.Sqrt, bias=EPS, scale=1.0 / D_MODEL),
         reads=[ps], writes=[rstd])
    S.op("dve", lambda e: e.reciprocal(out=rstd[:, :], in_=rstd[:, :]), reads=[rstd], writes=[rstd])
    for kc in range(KC):
        S.op("dve", lambda e, kc=kc: e.scalar_tensor_tensor(out=hT[:, kc, :], in0=x[:, kc, :], scalar=g[:, kc:kc + 1],
                                                            op0=ALU.mult, in1=rstd[:, :], op1=ALU.mult),
             reads=[x, g, rstd], writes=[hT.s(kc)])


def proj_fm(cx, w, c0, M, hT, ps, nk=KC, ncol=TT, kofs=0):
    S = cx.S
    for kc in range(nk):
        S.op("pe", lambda e, kc=kc: e.matmul(ps[0:M, 0:ncol], lhsT=w[:, kc, c0:c0 + M], rhs=hT[:, kofs + kc, :],
                                             start=(kc == 0), stop=(kc == nk - 1)),
             reads=[w, hT.s(kofs + kc)], writes=[ps])


def rope_fm(cx, ps, P, half, ctab, stab, out_ap, out_toks, tmpA, tmpB):
    S = cx.S
    S.op("dve", lambda e: e.tensor_tensor(out=tmpA[0:P, :], in0=ps[0:P, 0:TT], in1=ctab[0:P, :], op=ALU.mult),
         reads=[ps] + ctab.toks, writes=[tmpA])
    for g in range(P // (2 * half)):
        b = g * 2 * half
        S.op("dve", lambda e, b=b: e.tensor_tensor(out=tmpB[b:b + half, :], in0=ps[b + half:b + 2 * half, 0:TT],
                                                   in1=stab[b + half:b + 2 * half, :], op=ALU.mult),
             reads=[ps] + stab.toks, writes=[tmpB])
        S.op("dve", lambda e, b=b: e.tensor_tensor(out=tmpB[b + half:b + 2 * half, :], in0=ps[b:b + half, 0:TT],
                                                   in1=stab[b:b + half, :], op=ALU.mult),
             reads=[ps] + stab.toks, writes=[tmpB])
    if isinstance(out_ap, list):
        for (r0, r1, oap, otk) in out_ap:
            S.op("dve", lambda e, r0=r0, r1=r1, oap=oap: e.tensor_tensor(out=oap, in0=tmpA[r0:r1, :], in1=tmpB[r0:r1, :],
                                                                      op=ALU.add), reads=[tmpA, tmpB], writes=otk)
    else:
        S.op("dve", lambda e: e.tensor_tensor(out=out_ap, in0=tmpA[0:P, :], in1=tmpB[0:P, :], op=ALU.add),
             reads=[tmpA, tmpB], writes=out_toks)


class View:
    def __init__(self, tile, idx, toks=None):
        self.tile = tile
        self.idx = idx
        self.toks = toks if toks is not None else [tile.t]

    def __getitem__(self, sl):
        rows, cols = sl
        return self.tile.ap[(rows,) + tuple(self.idx) + (cols,)] if isinstance(self.idx, tuple) else \
            self.tile.ap[rows, self.idx, cols]


WK_COLS = 512 * 3 + 128 + 64 + 4
WV_COLS = 512 * 3 + 128


class DT:
    def __init__(self, ap, tok=None, dj=False):
        self.ap = ap
        self.t = tok if tok is not None else Tok(dj=dj)

    def __getitem__(self, i):
        return self.ap[i]


def load_x_tile(cx, c, x_src, t, ropet):
    S = cx.S
    xs, xtok = x_src(t)
    S.dma("sp", out=c["xbuf"][:, :, :], in_=xs.rearrange("(k p) n -> p k n", p=128), reads=[xtok], writes=[c["xbuf"]])
    S.dma("sp", out=c["rt"][:, :, :], in_=ropet.ap[:, :, t * TT:(t + 1) * TT].rearrange("f p n -> p f n"),
          reads=[ropet.t], writes=[c["rt"]])


def phase_kv(cx, io, x_src, c):
    S = cx.S
    x, hT, tmpA, tmpB, kst, vst, lf, rt = (c[k] for k in ("xbuf", "hT", "tmpA", "tmpB", "kst", "vst", "lf", "rt"))
    c128, s128, c64, s64 = (View(rt, i) for i in range(4))
    stn = 0
    for t in range(NT):
        cx.first_tile = (t == 0)
        tc = (t * TT, (t + 1) * TT)
        load_x_tile(cx, c, x_src, t, io["ropet"])
        rmsnorm_fm(cx, x, c["n1g"], hT, cx.next_ps())
        for (c0, dst, rk) in ((0, "kTa", None), (512, "kTb", 32), (1024, "kTc", None)):
            w = load_w(cx, io["wk"].ap, c0, 512, wtok=io["wk"].t, key="wk")
            for jp in range(2):
                st = kst[stn % 2]
                stn += 1
                for j in range(2):
                    ps = cx.next_ps()
                    proj_fm(cx, w, (jp * 2 + j) * 128, 128, hT, ps)
                    if rk is None:
                        evac(cx, st[:, j, :], ps[:, 0:TT], [ps], [st])
                    else:
                        rope_fm(cx, ps, 128, rk, c64, s64, st[:, j, :], [st], tmpA, tmpB)
                r0 = jp * 256
                S.dma("sp", out=io[dst].ap[r0:r0 + 256, tc[0]:tc[1]].rearrange("(j p) n -> p j n", p=128),
                      in_=st[:, :, :], reads=[st], writes=[io[dst]], semtile=st.t)
        w = load_w(cx, io["wk"].ap, 1536, 196, wtok=io["wk"].t, key="wk")
        st = kst[stn % 2]
        stn += 1
        ps = cx.next_ps()
        proj_fm(cx, w, 0, 128, hT, ps)
        rope_fm(cx, ps, 128, 64, c128, s128, st[:, 0, :], [st], tmpA, tmpB)
        ps = cx.next_ps()
        proj_fm(cx, w, 128, 68, hT, ps)
        rope_fm(cx, ps, 64, 32, c64, s64, st[0:64, 1, :], [st], tmpA, tmpB)
        S.op("act", lambda e, ps=ps: e.activation(out=lf[64:68, :], in_=ps[64:68, 0:TT], func=AF.Exp,
                                                 bias=c["nbfg"][64:68, :], scale=-1.0),
             reads=[ps, c["nbfg"]], writes=[lf])
        S.op("act", lambda e: e.activation(out=lf[64:68, :], in_=lf[64:68, :], func=AF.Ln, bias=1.0),
             reads=[lf], writes=[lf])
        S.op("act", lambda e: e.mul(out=lf[64:68, :], in_=lf[64:68, :], mul=-1.0), reads=[lf], writes=[lf])
        S.dma("sp", out=io["kTd"].ap[:, tc[0]:tc[1]], in_=st[:, 0, :], reads=[st], writes=[io["kTd"]], semtile=st.t)
        S.dma("sp", out=io["ikT"].ap[:, tc[0]:tc[1]], in_=st[0:64, 1, :], reads=[st], writes=[io["ikT"]], semtile=st.t)
        S.dma("sp", out=io["logf"].ap[:, tc[0]:tc[1]], in_=lf[64:68, :], reads=[lf], writes=[io["logf"]], semtile=lf.t)
        for (c0, ncols, nm) in ((0, 512, "va"), (512, 512, "vb"), (1024, 512, "vc"), (1536, 128, "vd")):
            w = load_w(cx, io["wv"].ap, c0, ncols, wtok=io["wv"].t, key="wv")
            for sbk in range(TT // 128):
                ps = cx.next_ps()
                for kc in range(KC):
                    S.op("pe", lambda e, kc=kc, ps=ps, w=w, sbk=sbk, ncols=ncols: e.matmul(
                        ps[:, 0:ncols], lhsT=hT[:, kc, sbk * 128:(sbk + 1) * 128], rhs=w[:, kc, 0:ncols],
                        start=(kc == 0), stop=(kc == KC - 1)), reads=[w, hT.s(kc)], writes=[ps])
                st = vst[stn % 2]
                stn += 1
                evac(cx, st[:, 0:ncols], ps[:, 0:ncols], [ps], [st])
                r0 = t * TT + sbk * 128
                S.dma("sp", out=io[nm].ap[r0:r0 + 128, 0:ncols], in_=st[:, 0:ncols], reads=[st], writes=[io[nm]],
                      semtile=st.t)


def alloc_common(cx):
    S = cx.S
    c = {}
    c["xbuf"] = S.sb("xbuf", [128, KC, TT], F32)
    c["hT"] = S.sb("hT", [128, KC, TT], BF16)
    c["tmpA"] = S.sb("tmpA", [128, TT], F32)
    c["tmpB"] = S.sb("tmpB", [128, TT], F32)
    c["rt"] = S.sb("rt", [128, 4, TT], F32)
    c["n1g"] = S.sb("n1g", [128, KC], F32)
    return c


def alloc_kv(cx, c):
    S = cx.S
    c["kst"] = [S.sb(f"kst{i}", [128, 2, TT], BF16) for i in range(2)]
    c["vst"] = [S.sb(f"vst{i}", [128, 512], BF16) for i in range(2)]
    c["lf"] = S.sb("lf", [128, TT], F32)
    c["nbfg"] = S.sb("nbfg", [128, 1], F32)
    return c


def dram_in(nc, name, shape, dtype=F32):
    return DT(nc.dram_tensor(name, list(shape), dtype, kind="ExternalInput").ap())


def dram_out(nc, name, shape, dtype=F32):
    return DT(nc.dram_tensor(name, list(shape), dtype, kind="ExternalOutput").ap(), dj=True)


KV_OUT = (("kTa", [512, LTOK], BF16), ("kTb", [512, LTOK], BF16), ("kTc", [512, LTOK], BF16),
          ("kTd", [128, LTOK], BF16), ("ikT", [64, LTOK], BF16), ("logf", [4, LTOK], F32),
          ("va", [LTOK, 512], BF16), ("vb", [LTOK, 512], BF16), ("vc", [LTOK, 512], BF16),
          ("vd", [LTOK, 128], BF16))


def build_A():
    nc = bass.Bass("TRN2", target_bir_lowering=False)
    es = ExitStack()
    io = {}
    xT = dram_in(nc, "xT", [D_MODEL, LTOK])
    io["wk"] = dram_in(nc, "wk", [D_MODEL, WK_COLS])
    io["wv"] = dram_in(nc, "wv", [D_MODEL, WV_COLS])
    io["ropet"] = dram_in(nc, "ropet", [4, 128, LTOK])
    n1g = dram_in(nc, "n1g", [128, KC])
    bfg = dram_in(nc, "bfg", [4, 1])
    for nm, shp, dt in KV_OUT:
        io[nm] = dram_out(nc, nm, shp, dt)
    with es:
        S = Sched(nc, es)
        cx = Ctx(S)
        c = alloc_kv(cx, alloc_common(cx))
        S.dma("sp", out=c["n1g"][:, :], in_=n1g.ap[:, :], writes=[c["n1g"]])
        S.dma("sp", out=c["nbfg"][64:68, :], in_=bfg.ap[:, :], writes=[c["nbfg"]])
        S.op("dve", lambda e: e.tensor_scalar(out=c["nbfg"][64:68, :], in0=c["nbfg"][64:68, :], scalar1=-1.0,
                                              scalar2=None, op0=ALU.mult), reads=[c["nbfg"]], writes=[c["nbfg"]])
        phase_kv(cx, io, lambda t: (xT.ap[:, t * TT:(t + 1) * TT], xT.t), c)
        S.final_all()
        S.emit()
    return nc


SEG_W = (("a_q", 512), ("a_k", 512), ("a_v", 512), ("b_q", 512), ("b_k", 512), ("b_v", 512),
         ("c_q", 512), ("c_k", 512), ("c_v", 512), ("c_f", 4), ("d_q", 512), ("d_k", 128), ("d_v", 128),
         ("d_iq", 512), ("d_ik", 64), ("d_iw", 8), ("gate", 8192))
SEG = {}
_o = 0
for _n, _w in SEG_W:
    SEG[_n] = (_o, _o + _w)
    _o += _w


def wcols(w, names):
    return np.ascontiguousarray(np.concatenate([w[:, SEG[n][0]:SEG[n][1]] for n in names], axis=1))


def local_pos(c):
    lb = np.arange(LBLK)
    return ((2 * lb + c)[:, None] * 128 + np.arange(128)[None, :]).reshape(-1)


def rope_tabs(c):
    pos = local_pos(c).astype(np.float32)
    out = []
    for dim in (128, 64):
        inv = (np.float32(10000.0) ** (-np.arange(0, dim, 2, dtype=np.float32) / np.float32(dim))).astype(np.float32)
        ang = pos[:, None] * inv[None, :]
        cos = np.cos(ang).astype(np.float32).T
        sin = np.sin(ang).astype(np.float32).T
        rep = 128 // dim
        out.append(np.concatenate([cos, cos] * rep, axis=0))
        out.append(np.concatenate([sin, -sin] * rep, axis=0))
    return np.ascontiguousarray(np.stack(out, 0))


def to_local_T(xb, c):
    blk = xb.reshape(NBLK, 128, -1)[c::2].reshape(LTOK, -1)
    return np.ascontiguousarray(blk.T)


def pcol(v):
    return np.ascontiguousarray(v.reshape(-1, 128).T)


IWS = (8 ** -0.5) * (64 ** -0.5)
BIS_W = 256.0
BIS_IT = 24
KV_ALL = (("kTa", [2, 512, LTOK], BF16), ("kTb", [2, 512, LTOK], BF16), ("kTc", [2, 512, LTOK], BF16),
          ("kTd", [2, 128, LTOK], BF16), ("ikT", [2, 64, LTOK], BF16), ("logf", [2, 4, LTOK], F32),
          ("va", [2, LTOK, 512], BF16), ("vb", [2, LTOK, 512], BF16), ("vc", [2, LTOK, 512], BF16),
          ("vd", [2, LTOK, 128], BF16))


class UView:
    def __init__(self, tile):
        self.tile = tile
        self.t = tile.t
        self.v = tile.ap[:, :].bitcast(BF16).rearrange("p (k n) -> p k n", n=TT)

    def s(self, k):
        return self.tile.s(("u", k))

    def __getitem__(self, idx):
        return self.v[idx]


def alloc_q(cx, c):
    S = cx.S
    c["kTd"] = S.sb("kTd_sb", [128, SEQ], BF16)
    c["ikT2"] = S.sb("ikT2", [128, SEQ], BF16)
    c["vd"] = S.sb("vd_sb", [128, NBLK, 128], BF16)
    c["score"] = S.sb("score", [128, SEQ], F32)
    c["negcum"] = S.sb("negcum", [128, NBLK * 4], F32)
    c["cum_mine"] = S.sb("cum_mine", [4, LTOK], F32)
    c["cumq"] = S.sb("cumq", [128, 4, TT], F32)
    c["biasA"] = S.sb("biasA", [128, 6, 512], BF16)
    c["mk"] = S.sb("mk", [128, 4, 128], BF16)
    c["idxmask"] = S.sb("idxmask", [128, 256], F32)
    c["ident"] = S.sb("ident", [128, 128], BF16)
    c["identf"] = S.sb("identf", [128, 128], F32)
    c["onesf"] = S.sb("onesf", [128, 256], F32)
    c["selh"] = S.sb("selh", [4, 512], F32)
    c["sel"] = S.sb("sel", [4, 2], F32)
    c["n2g"] = S.sb("n2g", [128, KC], F32)
    c["fg"] = S.sb("fg", [128, KC], F32)
    c["bgate"] = S.sb("bgate", [128, 64], F32)
    c["dng"] = S.sb("dng", [128, 1], F32)
    c["neglam"] = S.sb("neglam", [128, 1], F32)
    c["lamv"] = S.sb("lamv", [64, 4], F32)
    c["lamp"] = S.sb("lamp", [128, 2], F32)
    c["wiw"] = S.sb("wiw", [128, KC, 8], BF16)
    c["q"] = S.sb("q", [128, 4, TT], BF16)
    c["iq"] = S.sb("iq", [128, 4, TT], BF16)
    c["qz"] = [S.sb(f"qz{i}", [128, 4, TT], BF16) for i in range(2)]
    c["iw"] = S.sb("iw", [128, 16], F32)
    c["o"] = [S.sb(f"o{i}", [128, 4, TT], BF16) for i in range(4)]
    c["mg"] = S.sb("mg", [128, KC, TT], BF16)
    c["u"] = UView(c["score"])
    c["kch"] = [S.sb(f"kch{i}", [128, 4, 512], BF16) for i in range(2)]
    c["vch"] = [S.sb(f"vch{i}", [128, 4, 512], BF16) for i in range(2)]
    c["pT"] = [S.sb(f"pT{i}", [128, 512], BF16) for i in range(3)]
    c["sp"] = [S.sb(f"sp{i}", [128, 512], F32) for i in range(2)]
    c["nm"] = [S.sb(f"nm{i}", [128, 512], BF16) for i in range(2)]
    c["nmT"] = [S.sb(f"nmT{i}", [128, 4, 128], BF16) for i in range(2)]
    c["rden"] = S.sb("rden", [128, 512], F32)
    c["obr"] = S.sb("obr", [128, TT], F32)
    c["acc"] = [S.sb(f"acc{i}", [128, TT], F32) for i in range(4)]
    c["m8"] = S.sb("m8", [128, 8], F32)
    c["bis"] = S.sb("bis", [128, 4], F32)
    c["ost"] = [S.sb(f"ost{i}", [128, TT], F32) for i in range(2)]
    c["psS"] = [S.ps(f"psS{i}", [128, 512], F32) for i in range(2)]
    c["psO"] = S.ps("psO", [128, 512], F32)
    c["psD"] = S.ps("psD", [128, 512], F32)
    c["psm"] = S.ps("psm", [128, 512], F32)
    c["psT"] = S.ps("psT", [128, 512], BF16)
    c["rr"] = {"S": 0, "pT": 0, "sp": 0, "kv": 0, "nm": 0}
    return c


def rr(c, name, key):
    lst = c[name]
    i = c["rr"][key]
    c["rr"][key] = i + 1
    return lst[i % len(lst)]


def setup_q(cx, c, io, lambda_init):
    S = cx.S
    pq = "pool"
    S.dma(pq, out=c["biasA"][:, :, :], in_=io["biasA"].ap[:, :].rearrange("p (b f) -> p b f", b=6),
          reads=[io["biasA"]], writes=[c["biasA"]])
    S.dma(pq, out=c["mk"][:, :, :], in_=io["mk"].ap[:, :].rearrange("p (b f) -> p b f", b=4), reads=[io["mk"]],
          writes=[c["mk"]])
    S.dma(pq, out=c["ident"][:, :], in_=io["identf"].ap[:, :], reads=[io["identf"]], writes=[c["ident"]])
    S.dma(pq, out=c["wiw"][:, :, :], in_=io["wiw"].ap[:, :].rearrange("(k p) n -> p k n", p=128), reads=[io["wiw"]],
          writes=[c["wiw"]])
    for nm in ("identf", "idxmask", "selh", "sel", "n2g", "fg", "bgate", "dng"):
        S.dma("sp", out=c[nm][:, :], in_=io[nm].ap[:, :], reads=[io[nm]], writes=[c[nm]])
    S.dma("sp", out=c["lamv"][:, :], in_=io["lam4"].ap[:, :], reads=[io["lam4"]], writes=[c["lamv"]])
    S.op("dve", lambda e: e.memset(c["onesf"][:, :], 1.0), writes=[c["onesf"]])
    S.op("dve", lambda e: e.tensor_scalar(out=c["dng"][:, :], in0=c["dng"][:, :], scalar1=float(1.0 - lambda_init),
                                          scalar2=None, op0=ALU.mult), reads=[c["dng"]], writes=[c["dng"]])
    x = c["xbuf"]
    xflat = x[0:4, :, :].rearrange("p k n -> p (k n)")
    for r in range(2):
        S.dma("sp", out=c["kTd"][:, :].rearrange("p (n r j) -> p n r j", r=2, j=128)[:, :, r, :],
              in_=io["kTd_all"].ap[r].rearrange("p (n j) -> p n j", j=128), reads=[io["kTd_all"]], writes=[c["kTd"]])
        for hf in range(2):
            S.dma("sp", out=c["ikT2"][hf * 64:(hf + 1) * 64, :].rearrange("p (n r j) -> p n r j", r=2, j=128)[:, :, r, :],
                  in_=io["ikT_all"].ap[r].rearrange("p (n j) -> p n j", j=128), reads=[io["ikT_all"]],
                  writes=[c["ikT2"]])
        S.dma("sp", out=c["vd"][:, :, :].rearrange("p (n r) d -> p n r d", r=2)[:, :, r, :],
              in_=io["vd_all"].ap[r].rearrange("(n p) d -> p n d", p=128), reads=[io["vd_all"]], writes=[c["vd"]])
        S.dma("sp", out=xflat.rearrange("p (n r j) -> p n r j", r=2, j=128)[:, :, r, :],
              in_=io["logf_all"].ap[r].rearrange("p (n j) -> p n j", j=128), reads=[io["logf_all"]], writes=[x])
    score = c["score"]
    CH = 256
    for i in range(SEQ // CH):
        init = 0.0 if i == 0 else score[0:4, i * CH - 1:i * CH]
        S.op("dve", lambda e, i=i, init=init: e.tensor_tensor_scan(
            out=score[0:4, i * CH:(i + 1) * CH], data0=c["onesf"][0:4, 0:CH], data1=xflat[:, i * CH:(i + 1) * CH],
            initial=init, op0=ALU.mult, op1=ALU.add), reads=[x, c["onesf"], score], writes=[score])
    psm = c["psm"]
    for kb in range(NBLK):
        S.op("pe", lambda e, kb=kb: e.transpose(out=psm[:, kb * 4:(kb + 1) * 4], in_=score[0:4, kb * 128:(kb + 1) * 128],
                                                identity=c["identf"][0:4, 0:4]),
             reads=[score, c["identf"]], writes=[psm])
    S.op("act", lambda e: e.mul(out=c["negcum"][:, :], in_=psm[:, 0:NBLK * 4], mul=-1.0), reads=[psm],
         writes=[c["negcum"]])
    cv = score[0:4, :].rearrange("p (n r j) -> p n r j", r=2, j=128)
    cm = c["cum_mine"][0:4, :].rearrange("p (n j) -> p n j", j=128)
    S.op("dve", lambda e: e.tensor_scalar(out=cm, in0=cv[:, :, 0, :], scalar1=c["sel"][0:4, 0:1], scalar2=None,
                                          op0=ALU.mult), reads=[score, c["sel"]], writes=[c["cum_mine"]])
    S.op("dve", lambda e: e.scalar_tensor_tensor(out=cm, in0=cv[:, :, 1, :], scalar=c["sel"][0:4, 1:2], op0=ALU.mult,
                                                 in1=cm, op1=ALU.add), reads=[score, c["sel"], c["cum_mine"]],
         writes=[c["cum_mine"]])
    lv, lp = c["lamv"], c["lamp"]
    S.op("dve", lambda e: e.tensor_tensor(out=lp[0:64, 0:1], in0=lv[0:64, 0:1], in1=lv[0:64, 1:2], op=ALU.mult),
         reads=[lv], writes=[lp])
    S.op("dve", lambda e: e.tensor_tensor(out=lp[0:64, 1:2], in0=lv[0:64, 2:3], in1=lv[0:64, 3:4], op=ALU.mult),
         reads=[lv, lp], writes=[lp])
    S.op("pe", lambda e: e.matmul(psm[:, 0:2], lhsT=c["onesf"][0:64, 0:128], rhs=lp[0:64, 0:2], start=True, stop=True),
         reads=[c["onesf"], lp], writes=[psm])
    S.op("act", lambda e: e.activation(out=lp[:, 0:2], in_=psm[:, 0:2], func=AF.Exp), reads=[psm], writes=[lp])
    S.op("dve", lambda e: e.tensor_tensor(out=c["neglam"][:, :], in0=lp[:, 1:2], in1=lp[:, 0:1], op=ALU.subtract),
         reads=[lp], writes=[c["neglam"]])
    S.op("dve", lambda e: e.tensor_scalar(out=c["neglam"][:, :], in0=c["neglam"][:, :], scalar1=float(-lambda_init),
                                          scalar2=None, op0=ALU.add), reads=[c["neglam"]], writes=[c["neglam"]])


def q_proj(cx, c, io, col0, dst, mode, scale=None):
    hT, tmpA, tmpB, rt = c["hT"], c["tmpA"], c["tmpB"], c["rt"]
    w = load_w(cx, io["wq"].ap, col0, 512, wtok=io["wq"].t, key="wq")
    for jj in range(4):
        ps = cx.next_ps()
        proj_fm(cx, w, jj * 128, 128, hT, ps)
        if mode == "plain":
            evac(cx, dst[:, jj, :], ps[:, 0:TT], [ps], [dst], scale=scale)
        elif mode == "rope64z":
            qz = c["qz"]
            rope_fm(cx, ps, 128, 32, View(rt, 2), View(rt, 3),
                    [(0, 64, qz[0][0:64, jj, :], [qz[0]]), (64, 128, qz[1][64:128, jj, :], [qz[1]])], None,
                    tmpA, tmpB)
        elif mode == "rope64":
            rope_fm(cx, ps, 128, 32, View(rt, 2), View(rt, 3), dst[:, jj, :], [dst], tmpA, tmpB)
        else:
            rope_fm(cx, ps, 128, 64, View(rt, 0), View(rt, 1), dst[:, jj, :], [dst], tmpA, tmpB)


def attn_A(cx, c, io, t):
    S = cx.S
    q, o = c["q"], c["o"][0]
    psO, psD, rden = c["psO"], c["psD"], c["rden"]
    for m in range(2):
        lb = 2 * t + m
        n0 = max(0, lb - 2)
        nn = lb - n0 + 1
        mc = slice(m * 128, (m + 1) * 128)
        kb_, vb_ = c["kch"], c["vch"]
        for r in range(2):
            S.dma("sp", out=kb_[r][:, :, 0:nn * 128],
                  in_=io["kTa_all"].ap[r].rearrange("(h p) n -> p h n", p=128)[:, :, n0 * 128:(lb + 1) * 128],
                  reads=[io["kTa_all"]], writes=[kb_[r]])
            S.dma("sp", out=vb_[r][:, 0:nn, :],
                  in_=io["va_all"].ap[r][n0 * 128:(lb + 1) * 128, :].rearrange("(n p) f -> p n f", p=128),
                  reads=[io["va_all"]], writes=[vb_[r]])
        tot = 2 * nn
        k = 0
        for r in range(2):
            for ni in range(nn):
                nrel = n0 + ni - (lb - 2)
                pss = rr(c, "psS", "S")
                pT = rr(c, "pT", "pT")
                for h in range(4):
                    S.op("pe", lambda e, h=h, r=r, ni=ni, pss=pss: e.matmul(
                        pss[:, h * 128:(h + 1) * 128], lhsT=kb_[r][:, h, ni * 128:(ni + 1) * 128], rhs=q[:, h, mc],
                        start=(h == 0), stop=False, skip_group_check=True), reads=[kb_[r], q], writes=[pss])
                S.op("pe", lambda e, r=r, nrel=nrel, pss=pss: e.matmul(
                    pss[:, :], lhsT=c["ident"][:, :], rhs=c["biasA"][:, r * 3 + nrel, :], start=False, stop=True,
                    skip_group_check=True), reads=[c["ident"], c["biasA"]], writes=[pss])
                S.op("act", lambda e, pss=pss, pT=pT: e.activation(out=pT[:, :], in_=pss[:, :], func=AF.Exp),
                     reads=[pss], writes=[pT])
                for h in range(4):
                    S.op("pe", lambda e, h=h, r=r, ni=ni, pT=pT, k=k: e.matmul(
                        psO[:, h * 128:(h + 1) * 128], lhsT=vb_[r][:, ni, h * 128:(h + 1) * 128],
                        rhs=pT[:, h * 128:(h + 1) * 128], start=(k == 0 and h == 0), stop=(k == tot - 1 and h == 3),
                        skip_group_check=True), reads=[vb_[r], pT], writes=[psO])
                S.op("pe", lambda e, pT=pT, k=k: e.matmul(psD[:, :], lhsT=cx.ones_bf[:, :], rhs=pT[:, :],
                                                           start=(k == 0), stop=(k == tot - 1)),
                     reads=[cx.ones_bf, pT], writes=[psD])
                k += 1
        S.op("dve", lambda e: e.reciprocal(out=rden[:, :], in_=psD[:, :]), reads=[psD], writes=[rden])
        S.op("dve", lambda e, mc=mc: e.tensor_tensor(out=o[:, :, mc], in0=psO[:, :].rearrange("p (h q) -> p h q", h=4),
                                                     in1=rden[:, :].rearrange("p (h q) -> p h q", h=4), op=ALU.mult),
             reads=[psO, rden], writes=[o])


def kv_chunks(t):
    nb_r = 2 * t + 2
    out = []
    for r in range(2):
        for cst in range(0, nb_r, 4):
            out.append((r, cst, min(4, nb_r - cst)))
    return out


def attn_BC(cx, c, io, t, kind):
    S = cx.S
    q = c["q"]
    psO, psD, rden = c["psO"], c["psD"], c["rden"]
    kname, vname = ("kTb_all", "vb_all") if kind == "B" else ("kTc_all", "vc_all")
    o = c["o"][1] if kind == "B" else c["o"][2]
    chunks = kv_chunks(t)
    nblocks = sum(nb for _, _, nb in chunks)
    ngroups = 4 if kind == "B" else 2
    for g in range(ngroups):
        k = 0
        for (r, cst, nb) in chunks:
            kc_ = rr(c, "kch", "kv")
            vc_ = c["vch"][(c["rr"]["kv"] - 1) % 2]
            cols = slice(cst * 128, (cst + nb) * 128)
            if kind == "B":
                S.dma("sp", out=kc_[:, 0, 0:nb * 128], in_=io[kname].ap[r][g * 128:(g + 1) * 128, cols],
                      reads=[io[kname]], writes=[kc_])
            else:
                S.dma("sp", out=kc_[:, 0:2, 0:nb * 128],
                      in_=io[kname].ap[r].rearrange("(h p) n -> p h n", p=128)[:, 2 * g:2 * g + 2, cols],
                      reads=[io[kname]], writes=[kc_])
            S.dma("sp", out=vc_[:, 0:nb, :], in_=io[vname].ap[r][cols, :].rearrange("(n p) f -> p n f", p=128),
                  reads=[io[vname]], writes=[vc_])
            for bi in range(nb):
                n = cst + bi
                npr = n - 2 * t
                q0 = 128 if npr == 1 else 0
                pss = rr(c, "psS", "S")
                pT = rr(c, "pT", "pT")
                bsl = slice(bi * 128, (bi + 1) * 128)
                first, last = (k == 0), (k == nblocks - 1)
                for s in range(2):
                    osl = slice(s * 256 + q0, (s + 1) * 256)
                    if kind == "B":
                        ps_ = slice(s * 64, (s + 1) * 64)
                        S.op("pe", lambda e, s=s, osl=osl, bsl=bsl, pss=pss, kc_=kc_, npr=npr: e.matmul(
                            pss[:, osl], lhsT=kc_[:, 0, bsl], rhs=c["qz"][s][:, g, q0:TT], start=(s == 0),
                            stop=(s == 1 and npr < 0), skip_group_check=True), reads=[kc_, c["qz"][s]], writes=[pss])
                    else:
                        S.op("pe", lambda e, s=s, osl=osl, bsl=bsl, pss=pss, kc_=kc_, npr=npr: e.matmul(
                            pss[:, osl], lhsT=kc_[:, s, bsl], rhs=q[:, 2 * g + s, q0:TT], start=(s == 0),
                            stop=(s == 1 and npr < 0), skip_group_check=True), reads=[kc_, q], writes=[pss])
                if npr >= 0:
                    mi = r if kind == "B" else 2 + r
                    for s in range(2):
                        msl = slice(s * 256 + npr * 128, s * 256 + (npr + 1) * 128)
                        S.op("pe", lambda e, s=s, msl=msl, mi=mi, pss=pss: e.matmul(
                            pss[:, msl], lhsT=c["ident"][:, :], rhs=c["mk"][:, mi, :], start=False, stop=(s == 1),
                            skip_group_check=True), reads=[c["ident"], c["mk"]], writes=[pss])
                v3 = lambda ap: ap.rearrange("p (s q) -> p s q", s=2)[:, :, q0:TT]
                if kind == "B":
                    S.op("act", lambda e, pss=pss, pT=pT, v3=v3: e.activation(out=v3(pT[:, :]), in_=v3(pss[:, :]),
                                                                             func=AF.Exp, scale=64 ** -0.5),
                         reads=[pss], writes=[pT])
                else:
                    sp = rr(c, "sp", "sp")
                    kbt = 2 * n + r
                    for s in range(2):
                        h = 2 * g + s
                        osl = slice(s * 256 + q0, (s + 1) * 256)
                        S.op("dve", lambda e, osl=osl, h=h, kbt=kbt, pss=pss, sp=sp: e.scalar_tensor_tensor(
                            out=sp[:, osl], in0=pss[:, osl], scalar=c["negcum"][:, kbt * 4 + h:kbt * 4 + h + 1],
                            op0=ALU.add, in1=c["cumq"][:, h, q0:TT], op1=ALU.add),
                             reads=[pss, c["negcum"], c["cumq"]], writes=[sp])
                    S.op("act", lambda e, sp=sp, pT=pT, v3=v3: e.activation(out=v3(pT[:, :]), in_=v3(sp[:, :]),
                                                                           func=AF.Exp), reads=[sp], writes=[pT])
                if kind == "B":
                    S.op("pe", lambda e, pT=pT, vc_=vc_, bi=bi, first=first, last=last, v3=v3: e.matmul(
                        v3(psO[:, :]), lhsT=vc_[:, bi, g * 128:(g + 1) * 128], rhs=v3(pT[:, :]), start=first, stop=last,
                        skip_group_check=True), reads=[vc_, pT], writes=[psO])
                else:
                    for s in range(2):
                        h = 2 * g + s
                        osl = slice(s * 256 + q0, (s + 1) * 256)
                        S.op("pe", lambda e, s=s, h=h, osl=osl, pT=pT, vc_=vc_, bi=bi, first=first, last=last: e.matmul(
                            psO[:, osl], lhsT=vc_[:, bi, h * 128:(h + 1) * 128], rhs=pT[:, osl],
                            start=(first and s == 0), stop=(last and s == 1), skip_group_check=True),
                             reads=[vc_, pT], writes=[psO])
                S.op("pe", lambda e, pT=pT, first=first, last=last, v3=v3: e.matmul(
                    v3(psD[:, :]), lhsT=cx.ones_bf[:, :], rhs=v3(pT[:, :]), start=first, stop=last,
                    skip_group_check=True), reads=[cx.ones_bf, pT], writes=[psD])
                k += 1
        S.op("dve", lambda e: e.reciprocal(out=rden[:, :], in_=psD[:, :]), reads=[psD], writes=[rden])
        if kind == "C":
            S.op("dve", lambda e, g=g: e.tensor_tensor(
                out=o[:, 2 * g:2 * g + 2, :], in0=psO[:, :].rearrange("p (s q) -> p s q", s=2),
                in1=rden[:, :].rearrange("p (s q) -> p s q", s=2), op=ALU.mult), reads=[psO, rden], writes=[o])
        else:
            obr, tA, tB = c["obr"], c["tmpA"], c["tmpB"]
            S.op("dve", lambda e: e.tensor_tensor(out=obr[:, :], in0=psO[:, 0:TT], in1=rden[:, 0:TT], op=ALU.mult),
                 reads=[psO, rden], writes=[obr])
            S.op("dve", lambda e: e.tensor_tensor(out=tA[:, :], in0=psO[:, TT:2 * TT], in1=rden[:, TT:2 * TT],
                                                  op=ALU.mult), reads=[psO, rden], writes=[tA])
            S.op("dve", lambda e: e.scalar_tensor_tensor(out=obr[:, :], in0=tA[:, :], scalar=c["neglam"][:, 0:1],
                                                         op0=ALU.mult, in1=obr[:, :], op1=ALU.add),
                 reads=[tA, c["neglam"], obr], writes=[obr])
            sq = cx.sqb[0]
            psm = c["psm"]
            S.op("act", lambda e: e.activation(out=sq[:, :], in_=obr[:, :], func=AF.Square), reads=[obr], writes=[sq])
            S.op("pe", lambda e: e.matmul(psm[:, 0:TT], lhsT=cx.ones_bf[:, :], rhs=sq[:, :], start=True, stop=True),
                 reads=[cx.ones_bf, sq], writes=[psm])
            S.op("act", lambda e: e.activation(out=tB[:, :], in_=psm[:, 0:TT], func=AF.Sqrt, bias=EPS, scale=1.0 / 128),
                 reads=[psm], writes=[tB])
            S.op("dve", lambda e: e.reciprocal(out=tB[:, :], in_=tB[:, :]), reads=[tB], writes=[tB])
            S.op("dve", lambda e, g=g: e.scalar_tensor_tensor(out=o[:, g, :], in0=obr[:, :], scalar=c["dng"][:, 0:1],
                                                              op0=ALU.mult, in1=tB[:, :], op1=ALU.mult),
                 reads=[obr, c["dng"], tB], writes=[o])


def attn_D(cx, c, io, t):
    S = cx.S
    q, iq, o, score = c["q"], c["iq"], c["o"][3], c["score"]
    psO, psD, psT, rden, m8 = c["psO"], c["psD"], c["psT"], c["rden"], c["m8"]
    for m in range(2):
        lb = 2 * t + m
        nk = (2 * lb + 2) * 128
        mc = slice(m * 128, (m + 1) * 128)
        for cc in range(0, nk, 512):
            ncol = min(512, nk - cc)
            for ih in range(8):
                psI = rr(c, "psS", "S")
                sp = rr(c, "sp", "sp")
                prow = slice((ih % 2) * 64, (ih % 2 + 1) * 64)
                S.op("pe", lambda e, psI=psI, prow=prow, ih=ih, cc=cc, ncol=ncol: e.matmul(
                    psI[:, 0:ncol], lhsT=iq[prow, ih // 2, mc], rhs=c["ikT2"][prow, cc:cc + ncol], start=True, stop=True),
                     reads=[iq, c["ikT2"]], writes=[psI])
                S.op("act", lambda e, psI=psI, sp=sp, ncol=ncol: e.activation(out=sp[:, 0:ncol], in_=psI[:, 0:ncol],
                                                                              func=AF.Relu), reads=[psI], writes=[sp])
                iwc = c["iw"][:, m * 8 + ih:m * 8 + ih + 1]
                if ih == 0:
                    S.op("dve", lambda e, sp=sp, cc=cc, ncol=ncol, iwc=iwc: e.tensor_scalar(
                        out=score[:, cc:cc + ncol], in0=sp[:, 0:ncol], scalar1=iwc, scalar2=None, op0=ALU.mult),
                         reads=[sp, c["iw"]], writes=[score])
                else:
                    S.op("dve", lambda e, sp=sp, cc=cc, ncol=ncol, iwc=iwc: e.scalar_tensor_tensor(
                        out=score[:, cc:cc + ncol], in0=sp[:, 0:ncol], scalar=iwc, op0=ALU.mult,
                        in1=score[:, cc:cc + ncol], op1=ALU.add), reads=[sp, c["iw"], score], writes=[score])
        S.op("dve", lambda e, nk=nk: e.tensor_tensor(out=score[:, nk - 256:nk], in0=score[:, nk - 256:nk],
                                                     in1=c["idxmask"][:, :], op=ALU.add),
             reads=[score, c["idxmask"]], writes=[score])
        if lb >= 1:
            mgj = c["mg"]
            junk = mgj[:, :, :].rearrange("p k n -> p (k n)")
            lo, cand, cnt, gg = (c["bis"][:, i:i + 1] for i in range(4))
            S.op("dve", lambda e, nk=nk: e.max(out=m8[:, :], in_=score[:, 0:nk]), reads=[score], writes=[m8])
            S.op("dve", lambda e: e.tensor_scalar(out=lo, in0=m8[:, 0:1], scalar1=-BIS_W, scalar2=None, op0=ALU.add),
                 reads=[m8], writes=[c["bis"]])
            for it in range(BIS_IT):
                ck = BIS_W / float(2 ** (it + 1))
                S.op("dve", lambda e, ck=ck: e.tensor_scalar(out=cand, in0=lo, scalar1=ck, scalar2=None, op0=ALU.add),
                     reads=[c["bis"]], writes=[c["bis"]])
                S.op("dve", lambda e, nk=nk: e.tensor_scalar(out=junk[:, 0:nk], in0=score[:, 0:nk], scalar1=cand,
                                                             scalar2=None, op0=ALU.is_ge, op1=ALU.add, accum_out=cnt),
                     reads=[score, c["bis"]], writes=[mgj, c["bis"]])
                S.op("dve", lambda e, ck=ck: e.tensor_scalar(out=gg, in0=cnt, scalar1=256.0, scalar2=ck, op0=ALU.is_ge,
                                                             op1=ALU.mult), reads=[c["bis"]], writes=[c["bis"]])
                S.op("dve", lambda e: e.tensor_tensor(out=lo, in0=lo, in1=gg, op=ALU.add), reads=[c["bis"]],
                     writes=[c["bis"]])
        nkb = nk // 128
        k = 0
        for cc in range(0, nkb, 4):
            nb = min(4, nkb - cc)
            nm = rr(c, "nm", "nm")
            nmT = c["nmT"][(c["rr"]["nm"] - 1) % 2]
            csl = slice(cc * 128, (cc + nb) * 128)
            if lb >= 1:
                S.op("dve", lambda e, nm=nm, csl=csl, nb=nb: e.tensor_scalar(
                    out=nm[:, 0:nb * 128], in0=score[:, csl], scalar1=c["bis"][:, 0:1], scalar2=NEG, op0=ALU.is_lt,
                    op1=ALU.mult), reads=[score, c["bis"]], writes=[nm])
            else:
                S.op("dve", lambda e, nm=nm, csl=csl, nb=nb: e.tensor_scalar(
                    out=nm[:, 0:nb * 128], in0=score[:, csl], scalar1=-1.0e29, scalar2=NEG, op0=ALU.is_lt, op1=ALU.mult),
                     reads=[score], writes=[nm])
            for bi in range(nb):
                S.op("pe", lambda e, nm=nm, bi=bi: e.transpose(out=psT[:, bi * 128:(bi + 1) * 128],
                                                               in_=nm[:, bi * 128:(bi + 1) * 128],
                                                               identity=c["ident"][:, :]),
                     reads=[nm, c["ident"]], writes=[psT])
            S.op("act", lambda e, nmT=nmT, nb=nb: e.activation(
                out=nmT[:, 0:nb, :], in_=psT[:, 0:nb * 128].rearrange("p (b q) -> p b q", q=128), func=AF.Copy),
                 reads=[psT], writes=[nmT])
            for bi in range(nb):
                kb = cc + bi
                pss = rr(c, "psS", "S")
                pT = rr(c, "pT", "pT")
                first, last = (k == 0), (k == nkb - 1)
                S.op("pe", lambda e, kb=kb, pss=pss: e.matmul(
                    pss[:, :].rearrange("p (h q) -> p h q", h=4), lhsT=c["kTd"][:, kb * 128:(kb + 1) * 128],
                    rhs=q[:, :, mc], start=True, stop=False, skip_group_check=True), reads=[c["kTd"], q], writes=[pss])
                S.op("pe", lambda e, bi=bi, nmT=nmT, pss=pss: e.matmul(
                    pss[:, :].rearrange("p (h q) -> p h q", h=4), lhsT=c["ident"][:, :],
                    rhs=nmT[:, bi:bi + 1, :].broadcast_to([128, 4, 128]), start=False, stop=True, skip_group_check=True),
                     reads=[c["ident"], nmT], writes=[pss])
                S.op("act", lambda e, pss=pss, pT=pT: e.activation(out=pT[:, :], in_=pss[:, :], func=AF.Exp,
                                                                  scale=128 ** -0.5), reads=[pss], writes=[pT])
                S.op("pe", lambda e, kb=kb, pT=pT, first=first, last=last: e.matmul(
                    psO[:, :], lhsT=c["vd"][:, kb, :], rhs=pT[:, :], start=first, stop=last), reads=[c["vd"], pT],
                     writes=[psO])
                S.op("pe", lambda e, pT=pT, first=first, last=last: e.matmul(
                    psD[:, :], lhsT=cx.ones_bf[:, :], rhs=pT[:, :], start=first, stop=last), reads=[cx.ones_bf, pT],
                     writes=[psD])
                k += 1
        S.op("dve", lambda e: e.reciprocal(out=rden[:, :], in_=psD[:, :]), reads=[psD], writes=[rden])
        S.op("dve", lambda e, mc=mc: e.tensor_tensor(out=o[:, :, mc], in0=psO[:, :].rearrange("p (h q) -> p h q", h=4),
                                                     in1=rden[:, :].rearrange("p (h q) -> p h q", h=4), op=ALU.mult),
             reads=[psO, rden], writes=[o])


def dense_tail(cx, c, io, t, x_dst, last_layer):
    S = cx.S
    x, hT, mg, u, acc = c["xbuf"], c["hT"], c["mg"], c["u"], c["acc"]
    sp = c["sp"]
    for j2 in range(4):
        for i in range(4):
            wg = load_w(cx, io["wg"].ap, i * 2048 + j2 * 512, 512, wtok=io["wg"].t, key="wg")
            wb = load_w(cx, io["wbr"].ap, j2 * 512, 512, nk=4, row0=i * 512, wtok=io["wbr"].t, key="wbr")
            for j in range(4):
                psg = cx.next_ps()
                proj_fm(cx, wg, j * 128, 128, hT, psg)
                psb = cx.next_ps()
                proj_fm(cx, wb, j * 128, 128, c["o"][i], psb, nk=4)
                sg = sp[j // 2][:, (j % 2) * TT:(j % 2 + 1) * TT]
                sgt = sp[j // 2]
                bcol = i * 16 + j2 * 4 + j
                S.op("act", lambda e, psg=psg, sg=sg, bcol=bcol: e.activation(
                    out=sg, in_=psg[:, 0:TT], func=AF.Sigmoid, bias=c["bgate"][:, bcol:bcol + 1]),
                     reads=[psg, c["bgate"]], writes=[sgt])
                if i == 0:
                    S.op("dve", lambda e, psb=psb, sg=sg, j=j: e.tensor_tensor(out=acc[j][:, :], in0=psb[:, 0:TT],
                                                                              in1=sg, op=ALU.mult),
                         reads=[psb, sgt], writes=[acc[j]])
                else:
                    S.op("dve", lambda e, psb=psb, sg=sg: e.tensor_tensor(out=sg, in0=psb[:, 0:TT], in1=sg, op=ALU.mult),
                         reads=[psb, sgt], writes=[sgt])
                    if i < 3:
                        S.op("dve", lambda e, sg=sg, j=j: e.tensor_tensor(out=acc[j][:, :], in0=acc[j][:, :], in1=sg,
                                                                         op=ALU.add),
                             reads=[acc[j], sgt], writes=[acc[j]])
                    else:
                        jj = j2 * 4 + j
                        S.op("dve", lambda e, sg=sg, j=j, jj=jj: e.tensor_tensor(out=mg[:, jj, :], in0=acc[j][:, :],
                                                                                in1=sg, op=ALU.add),
                             reads=[acc[j], sgt], writes=[mg.s(jj)])
    for j2 in range(4):
        w = load_w(cx, io["wo"].ap, j2 * 512, 512, wtok=io["wo"].t, key="wo")
        for j in range(4):
            jj = j2 * 4 + j
            ps = cx.next_ps()
            proj_fm(cx, w, j * 128, 128, mg, ps)
            S.op("dve", lambda e, ps=ps, jj=jj: e.tensor_tensor(out=x[:, jj, :], in0=ps[:, 0:TT], in1=x[:, jj, :],
                                                                op=ALU.add), reads=[ps, x], writes=[x])
    rmsnorm_fm(cx, x, c["n2g"], hT, c["psm"])
    for half in range(2):
        for j2 in range(8):
            w = load_w(cx, io["wf1"].ap, half * 4096 + j2 * 512, 512, wtok=io["wf1"].t, key="wf1")
            for j in range(4):
                jj = j2 * 4 + j
                ps = cx.next_ps()
                proj_fm(cx, w, j * 128, 128, hT, ps)
                r_ = sp[j // 2][:, (j % 2) * TT:(j % 2 + 1) * TT]
                rt_ = sp[j // 2]
                S.op("act", lambda e, ps=ps, r_=r_: e.activation(out=r_, in_=ps[:, 0:TT], func=AF.Relu),
                     reads=[ps], writes=[rt_])
                S.op("dve", lambda e, r_=r_, jj=jj, ps=ps: e.tensor_tensor(out=u[:, jj, :], in0=ps[:, 0:TT], in1=r_,
                                                                          op=ALU.mult),
                     reads=[rt_, ps], writes=[u.s(jj)])
        for j2 in range(4):
            for kk in range(2):
                w = load_w(cx, io["wf2"].ap, j2 * 512, 512, nk=16, row0=half * 4096 + kk * 2048, wtok=io["wf2"].t,
                           key="wf2")
                for j in range(4):
                    jj = j2 * 4 + j
                    ps = cx.next_ps()
                    for kc in range(16):
                        kidx = kk * 16 + kc
                        S.op("pe", lambda e, kc=kc, kidx=kidx, ps=ps, j=j, w=w: e.matmul(
                            ps[:, 0:TT], lhsT=w[:, kc, j * 128:(j + 1) * 128], rhs=u[:, kidx, :],
                            start=(kc == 0), stop=(kc == 15)), reads=[w, u.s(kidx)], writes=[ps])
                    S.op("dve", lambda e, ps=ps, jj=jj: e.tensor_tensor(out=x[:, jj, :], in0=ps[:, 0:TT], in1=x[:, jj, :],
                                                                        op=ALU.add), reads=[ps, x], writes=[x])
    tc0 = t * TT
    if not last_layer:
        S.dma("sp", out=x_dst.ap[:, tc0:tc0 + TT].rearrange("(k p) n -> p k n", p=128), in_=x[:, :, :], reads=[x],
              writes=[x_dst])
    else:
        ps = c["psm"]
        rstd = cx.rstd
        for kc in range(KC):
            sq = cx.sqb[kc % 2]
            S.op("act", lambda e, kc=kc, sq=sq: e.activation(out=sq[:, :], in_=x[:, kc, :], func=AF.Square),
                 reads=[x], writes=[sq])
            S.op("pe", lambda e, kc=kc, sq=sq: e.matmul(ps[:, 0:TT], lhsT=cx.ones_bf[:, :], rhs=sq[:, :],
                                                        start=(kc == 0), stop=(kc == KC - 1)),
                 reads=[sq, cx.ones_bf], writes=[ps])
        S.op("act", lambda e: e.activation(out=rstd[:, :], in_=ps[:, 0:TT], func=AF.Sqrt, bias=EPS,
                                           scale=1.0 / D_MODEL), reads=[ps], writes=[rstd])
        S.op("dve", lambda e: e.reciprocal(out=rstd[:, :], in_=rstd[:, :]), reads=[rstd], writes=[rstd])
        for kc in range(KC):
            S.op("dve", lambda e, kc=kc: e.scalar_tensor_tensor(out=x[:, kc, :], in0=x[:, kc, :],
                                                                scalar=c["fg"][:, kc:kc + 1], op0=ALU.mult,
                                                                in1=rstd[:, :], op1=ALU.mult),
                 reads=[x, c["fg"], rstd], writes=[x])
        S.dma("sp", out=x_dst.ap[:, tc0:tc0 + TT].rearrange("(k p) n -> p k n", p=128), in_=x[:, :, :], reads=[x],
              writes=[x_dst])


def phase_q(cx, c, io, x_src, x_dst, lambda_init, last_layer, dbg=None):
    S = cx.S
    x, hT, psm = c["xbuf"], c["hT"], c["psm"]
    for i in range(2):
        S.op("dve", lambda e, i=i: e.memset(c["qz"][i][:, :, :], 0.0), writes=[c["qz"][i]])
    for t in range(NT):
        cx.first_tile = (t == 0)
        load_x_tile(cx, c, x_src, t, io["ropet"])
        rmsnorm_fm(cx, x, c["n1g"], hT, psm)
        for m in range(2):
            for kc in range(KC):
                S.op("pe", lambda e, m=m, kc=kc: e.matmul(psm[:, m * 8:(m + 1) * 8], lhsT=hT[:, kc, m * 128:(m + 1) * 128],
                                                          rhs=c["wiw"][:, kc, :], start=(kc == 0), stop=(kc == KC - 1)),
                     reads=[hT.s(kc), c["wiw"]], writes=[psm])
        S.op("act", lambda e: e.mul(out=c["iw"][:, :], in_=psm[:, 0:16], mul=IWS), reads=[psm], writes=[c["iw"]])
        q_proj(cx, c, io, 0, c["q"], "plain", scale=128 ** -0.5)
        attn_A(cx, c, io, t)
        q_proj(cx, c, io, 512, None, "rope64z")
        attn_BC(cx, c, io, t, "B")
        q_proj(cx, c, io, 1024, c["q"], "plain", scale=128 ** -0.5)
        for h in range(4):
            S.op("pe", lambda e, h=h, t=t: e.matmul(psm[:, 0:TT], lhsT=c["selh"][0:4, h * 128:(h + 1) * 128],
                                                    rhs=c["cum_mine"][0:4, t * TT:(t + 1) * TT], start=True, stop=True),
                 reads=[c["selh"], c["cum_mine"]], writes=[psm])
            S.op("act", lambda e, h=h: e.activation(out=c["cumq"][:, h, :], in_=psm[:, 0:TT], func=AF.Copy),
                 reads=[psm], writes=[c["cumq"]])
        attn_BC(cx, c, io, t, "C")
        q_proj(cx, c, io, 1536, c["q"], "rope128")
        q_proj(cx, c, io, 2048, c["iq"], "rope64")
        attn_D(cx, c, io, t)
        if dbg is not None:
            for i in range(4):
                S.dma("sp", out=dbg[i].ap[:, t * TT:(t + 1) * TT].rearrange("(h p) n -> p h n", p=128),
                      in_=c["o"][i][:, :, :], reads=[c["o"][i]], writes=[dbg[i]])
        dense_tail(cx, c, io, t, x_dst, last_layer)


Q_IN = (("wq", [D_MODEL, 2560]), ("wiw", [D_MODEL, 8]), ("wg", [D_MODEL, 8192]), ("wbr", [2048, D_MODEL]),
        ("wo", [D_MODEL, D_MODEL]), ("wf1", [D_MODEL, D_FF]), ("wf2", [D_FF, D_MODEL]),
        ("ropet", [4, 128, LTOK]), ("biasA", [128, 6 * 512]), ("mk", [128, 4 * 128]), ("identf", [128, 128]),
        ("idxmask", [128, 256]), ("selh", [4, 512]), ("sel", [4, 2]), ("n1g", [128, KC]), ("n2g", [128, KC]),
        ("fg", [128, KC]), ("bgate", [128, 64]), ("dng", [128, 1]), ("lam4", [64, 4]))


def build_B(layer, dbg=False):
    lambda_init = 0.8 - 0.6 * math.exp(-0.3 * layer)
    nc = bass.Bass("TRN2", target_bir_lowering=False)
    es = ExitStack()
    io = {}
    xT = dram_in(nc, "xT", [D_MODEL, LTOK])
    for nm, shp in Q_IN:
        io[nm] = dram_in(nc, nm, shp)
    for nm, shp, dt in KV_ALL:
        io[nm + "_all"] = dram_in(nc, nm + "_all", shp, dt)
    xo = dram_out(nc, "xo", [D_MODEL, LTOK])
    dbgs = [dram_out(nc, f"dbg_o{i}", [512, LTOK], BF16) for i in range(4)] if dbg else None
    with es:
        S = Sched(nc, es)
        cx = Ctx(S)
        c = alloc_q(cx, alloc_common(cx))
        S.dma("sp", out=c["n1g"][:, :], in_=io["n1g"].ap[:, :], reads=[io["n1g"]], writes=[c["n1g"]])
        setup_q(cx, c, io, lambda_init)
        phase_q(cx, c, io, lambda t: (xT.ap[:, t * TT:(t + 1) * TT], xT.t), xo, lambda_init, layer == DEPTH - 1, dbgs)
        S.final_all()
        S.emit()
    return nc


def table_biasA(rel_bias_l, c):
    out = np.full((128, 6, 4, 128), NEG, np.float32)
    j = np.arange(128)[:, None]
    qi = np.arange(128)[None, :]
    for r in range(2):
        for nrel in range(3):
            dblk = (2 * (nrel - 2) + r) - c
            dist = -dblk * 128 + qi - j
            kch = 2 * dblk + j // 64
            qch = qi // 64
            valid = (kch <= qch) & (kch >= qch - 8)
            idx = np.clip(dist, -128, 128) + 128
            for h in range(4):
                vals = rel_bias_l[h][idx]
                out[:, r * 3 + nrel, h, :] = np.where(valid, vals, np.float32(NEG))
    return np.ascontiguousarray(out.reshape(128, 6 * 512))


def table_mk(c):
    out = np.zeros((128, 4, 128), np.float32)
    j = np.arange(128)[:, None]
    qi = np.arange(128)[None, :]
    diagB = np.where(j // 64 <= qi // 64, 0.0, NEG).astype(np.float32)
    diagC = np.where(j <= qi, 0.0, NEG).astype(np.float32)
    for r in range(2):
        if r < c:
            mB = mC = np.zeros((128, 128), np.float32)
        elif r == c:
            mB, mC = diagB, diagC
        else:
            mB = mC = np.full((128, 128), NEG, np.float32)
        out[:, r, :] = mB
        out[:, 2 + r, :] = mC
    return np.ascontiguousarray(out.reshape(128, 512))


def table_idxmask(c):
    out = np.zeros((128, 2, 128), np.float32)
    qi = np.arange(128)[:, None]
    j = np.arange(128)[None, :]
    for r in range(2):
        if r < c:
            pass
        elif r == c:
            out[:, r, :] = np.where(j // 64 <= qi // 64, 0.0, -1.0e30)
        else:
            out[:, r, :] = -1.0e30
    return np.ascontiguousarray(out.reshape(128, 256))


def host_q_inputs(inp, l, c):
    w_in = inp["w_in"][l]
    m = {}
    m["wq"] = wcols(w_in, ["a_q", "b_q", "c_q", "d_q", "d_iq"])
    m["wiw"] = wcols(w_in, ["d_iw"])
    m["wg"] = wcols(w_in, ["gate"])
    m["wbr"] = np.ascontiguousarray(inp["w_branch"][l].reshape(4 * 512, D_MODEL))
    m["wo"] = np.ascontiguousarray(inp["w_out"][l])
    m["wf1"] = np.ascontiguousarray(inp["w_ff1"][l])
    m["wf2"] = np.ascontiguousarray(inp["w_ff2"][l])
    m["ropet"] = rope_tabs(c)
    m["biasA"] = table_biasA(inp["rel_bias"][l], c)
    m["mk"] = table_mk(c)
    m["identf"] = np.eye(128, dtype=np.float32)
    m["idxmask"] = table_idxmask(c)
    selh = np.zeros((4, 4, 128), np.float32)
    for h in range(4):
        selh[h, h, :] = 1.0
    m["selh"] = selh.reshape(4, 512)
    sel = np.zeros((4, 2), np.float32)
    sel[:, c] = 1.0
    m["sel"] = sel
    m["n1g"] = pcol(inp["norm1_g"][l])
    m["n2g"] = pcol(inp["norm2_g"][l])
    m["fg"] = pcol(inp["final_g"])
    m["bgate"] = pcol(inp["b_gate"][l])
    m["dng"] = np.ascontiguousarray(inp["diff_norm_g"][l].reshape(128, 1))
    m["lam4"] = np.ascontiguousarray(np.stack([inp["lambda_q1"][l], inp["lambda_k1"][l], inp["lambda_q2"][l],
                                               inp["lambda_k2"][l]], axis=1))
    return m


def host_kv_inputs(inp, l, c):
    w_in = inp["w_in"][l]
    return {"wk": wcols(w_in, ["a_k", "b_k", "c_k", "d_k", "d_ik", "c_f"]),
            "wv": wcols(w_in, ["a_v", "b_v", "c_v", "d_v"]),
            "ropet": rope_tabs(c), "n1g": pcol(inp["norm1_g"][l]),
            "bfg": np.ascontiguousarray(inp["b_forget"][l].reshape(4, 1))}


I32 = mybir.dt.int32
PK = {"kTa": 0, "kTb": 512, "kTc": 1024, "kTd": 1536, "ikT": 1664, "logf": 1728, "va": 1792, "vb": 2304,
      "vc": 2816, "vd": 3328}
RP = 3456
NSEL = 2 * RP // 128


def pack_views(ap2d, base):
    v = {}
    for nm, rows in (("kTa", 512), ("kTb", 512), ("kTc", 512), ("kTd", 128), ("ikT", 64)):
        v[nm] = ap2d[base + PK[nm]:base + PK[nm] + rows, :]
    v["logf"] = ap2d[base + PK["logf"]:base + PK["logf"] + 8, :].bitcast(F32).rearrange("(h a) n -> h (a n)", a=2)
    for nm in ("va", "vb", "vc"):
        v[nm] = ap2d[base + PK[nm]:base + PK[nm] + 512, :].rearrange("r (a f) -> (r a) f", f=512)
    v["vd"] = ap2d[base + PK["vd"]:base + PK["vd"] + 128, :].rearrange("r (a f) -> (r a) f", f=128)
    return v


A_IN = (("wk", [D_MODEL, WK_COLS]), ("wv", [D_MODEL, WV_COLS]), ("bfg", [4, 1]))


def build_fused():
    nc = bass.Bass("TRN2", target_bir_lowering=False)
    es = ExitStack()
    xT = dram_in(nc, "xT", [D_MODEL, LTOK])
    pidx_d = dram_in(nc, "pidx", [128, NSEL], I32)
    ios = []
    for l in range(DEPTH):
        io = {}
        for nm, shp in A_IN + Q_IN:
            if nm in ("ropet", "identf", "mk", "idxmask", "selh", "sel", "fg"):
                continue
            io[nm] = dram_in(nc, f"{nm}{l}", shp)
        ios.append(io)
    shared = {nm: dram_in(nc, nm, dict(Q_IN)[nm]) for nm in ("ropet", "identf", "mk", "idxmask", "selh", "sel", "fg")}
    xo = dram_out(nc, "xo", [D_MODEL, LTOK])
    kvpack_h = nc.dram_tensor("kvpack", [RP, LTOK], BF16)
    kvg_h = nc.dram_tensor("kvg", [N_CORES * RP, LTOK], BF16)
    kvall_h = nc.dram_tensor("kvall", [2 * RP, LTOK], BF16)
    xs1_h = nc.dram_tensor("xs1", [D_MODEL, LTOK], F32)
    NSLOT = 160
    wscr_h = [nc.dram_tensor(f"wscr{l}", [NSLOT, 128, KC * WB], BF16) for l in range(DEPTH)] if USE_WSCR else []
    t_pack, t_g, t_all = Tok(dj=True), Tok(dj=True), Tok(dj=True)
    xs1 = DT(xs1_h.ap(), dj=True)
    pv = pack_views(kvpack_h.ap(), 0)
    av = [pack_views(kvall_h.ap(), r * RP) for r in range(2)]
    with es:
        S = Sched(nc, es)
        cx = Ctx(S)
        c = alloc_q(cx, alloc_kv(cx, alloc_common(cx)))
        c["pidx"] = S.sb("pidx", [128, NSEL], I32)
        cx.wscr = [h.ap() for h in wscr_h] if USE_WSCR else None
        S.dma("sp", out=c["pidx"][:, :], in_=pidx_d.ap[:, :], writes=[c["pidx"]])
        for l in range(DEPTH):
            lambda_init = 0.8 - 0.6 * math.exp(-0.3 * l)
            cx.layer = l
            io = dict(ios[l])
            io.update(shared)
            for nm in pv:
                io[nm] = DT(pv[nm], tok=t_pack)
                io[nm + "_all"] = DT([av[0][nm], av[1][nm]], tok=t_all)
            if l == 0:
                x_src = lambda t: (xT.ap[:, t * TT:(t + 1) * TT], xT.t)
                x_dst = xs1
            else:
                x_src = lambda t: (xs1.ap[:, t * TT:(t + 1) * TT], xs1.t)
                x_dst = xo
            S.dma("sp", out=c["n1g"][:, :], in_=io["n1g"].ap[:, :], reads=[io["n1g"]], writes=[c["n1g"]])
            S.dma("sp", out=c["nbfg"][64:68, :], in_=io["bfg"].ap[:, :], reads=[io["bfg"]], writes=[c["nbfg"]])
            S.op("dve", lambda e: e.tensor_scalar(out=c["nbfg"][64:68, :], in0=c["nbfg"][64:68, :], scalar1=-1.0,
                                                  scalar2=None, op0=ALU.mult), reads=[c["nbfg"]], writes=[c["nbfg"]])
            phase_kv(cx, io, x_src, c)
            S.collective_allgather(kvpack_h.ap().opt(), kvg_h.ap().opt(), reads=[t_pack], writes=[t_g])
            u = c["u"]
            for i in range(NSEL):
                k = i % 4
                toks = [u.s(8 * k + j) for j in range(8)]
                stv = u[:, 8 * k:8 * k + 8, :].rearrange("p k n -> p (k n)")
                S.idma(out=stv, in_=kvg_h.ap()[:, :], idx_ap=c["pidx"][:, i:i + 1], reads=[t_g, c["pidx"]],
                       writes=toks, semtile=toks[0])
                S.dma("sp", out=kvall_h.ap()[i * 128:(i + 1) * 128, :], in_=stv, reads=toks, writes=[t_all],
                      semtile=toks[0])
            setup_q(cx, c, io, lambda_init)
            phase_q(cx, c, io, x_src, x_dst, lambda_init, l == DEPTH - 1, None)
        S.final_all()
        print("ops per engine:", {k: len(v) for k, v in S.ops.items()})
        S.emit()
    return nc


def pair_index(core):
    b = core // 2
    idx = np.zeros((128, NSEL), np.int32)
    nb = RP // 128
    for i in range(NSEL):
        idx[:, i] = (2 * b + i // nb) * RP + (i % nb) * 128 + np.arange(128)
    return idx


def kernel_fused(**inputs):
    inp = {k: np.asarray(v) for k, v in inputs.items()}
    x = inp["x"]
    cores = list(range(N_CORES))
    nc = build_fused()
    per_layer = []
    for l in range(DEPTH):
        d = {}
        for c in range(2):
            m = host_q_inputs(inp, l, c)
            m.update({k: v for k, v in host_kv_inputs(inp, l, c).items() if k in ("wk", "wv", "bfg")})
            d[c] = m
        per_layer.append(d)
    maps = []
    for core in cores:
        b, c = core // 2, core % 2
        m = {"xT": to_local_T(x[b], c), "pidx": pair_index(core)}
        for l in range(DEPTH):
            src = per_layer[l][c]
            for nm, _ in A_IN + Q_IN:
                if nm in ("ropet", "identf", "mk", "idxmask", "selh", "sel", "fg"):
                    m[nm] = per_layer[0][c][nm]
                else:
                    m[f"{nm}{l}"] = per_layer[l][0][nm] if nm not in ("biasA",) else src[nm]
        maps.append(m)
    res = run_bass_kernel_spmd(nc, maps, core_ids=cores)
    xo = [np.asarray(res.results[core]["xo"]) for core in cores]
    out = np.stack([_from_local(xo[2 * b], xo[2 * b + 1]) for b in range(BATCH)], 0)
    return out.astype(np.float32)


def _from_local(loc0, loc1):
    out = np.empty((SEQ, D_MODEL), np.float32)
    o3 = out.reshape(NBLK, 128, D_MODEL)
    o3[0::2] = np.asarray(loc0).T.reshape(LBLK, 128, D_MODEL)
    o3[1::2] = np.asarray(loc1).T.reshape(LBLK, 128, D_MODEL)
    return out


def kernel_unfused(**inputs):
    inp = {k: np.asarray(v) for k, v in inputs.items()}
    x = inp["x"]
    cores = list(range(N_CORES))
    xT = [to_local_T(x[core // 2], core % 2) for core in cores]
    for l in range(DEPTH):
        ncA = build_A()
        mapsA = []
        for core in cores:
            m = host_kv_inputs(inp, l, core % 2)
            m["xT"] = xT[core]
            mapsA.append(m)
        resA = run_bass_kernel_spmd(ncA, mapsA, core_ids=cores)
        del mapsA
        ncB = build_B(l)
        mapsB = []
        for core in cores:
            b, c = core // 2, core % 2
            m = host_q_inputs(inp, l, c)
            m["xT"] = xT[core]
            for nm, shp, dt in KV_ALL:
                m[nm + "_all"] = np.ascontiguousarray(
                    np.stack([resA.results[2 * b][nm], resA.results[2 * b + 1][nm]], 0))
            mapsB.append(m)
        resB = run_bass_kernel_spmd(ncB, mapsB, core_ids=cores)
        del mapsB
        xT = [np.asarray(resB.results[core]["xo"]) for core in cores]
    out = np.stack([_from_local(xT[2 * b], xT[2 * b + 1]) for b in range(BATCH)], 0)
    return out.astype(np.float32)


def kernel(**inputs):
    return kernel_fused(**inputs)


def _sim_sched(S):
    sem = {}
    pos = {e: 0 for e in ENGS}
    total = sum(len(v) for v in S.ops.values())
    done = 0
    while done < total:
        progressed = False
        for e in ENGS:
            ops = S.ops[e]
            while pos[e] < len(ops):
                waits, fn, inc = ops[pos[e]]
                if all(sem.get(k, 0) >= v for k, v in waits):
                    sem[inc[0]] = sem.get(inc[0], 0) + (1 if inc[1] is None else inc[1])
                    pos[e] += 1
                    done += 1
                    progressed = True
                else:
                    break
        if not progressed:
            for e in ENGS:
                if pos[e] < len(S.ops[e]):
                    waits, fn, inc = S.ops[e][pos[e]]
                    bad = [(k, v, sem.get(k, 0)) for k, v in waits if sem.get(k, 0) < v]
                    print("STUCK", e, pos[e], "/", len(S.ops[e]), "unsatisfied (key, need, have):", bad)
            return False
    return True
```

```python
import math
from contextlib import ExitStack

import numpy as np
import concourse.bass as bass
import concourse.mybir as mybir
from concourse.bass_utils import run_bass_kernel_spmd

F32 = mybir.dt.float32
BF16 = mybir.dt.bfloat16
AF = mybir.ActivationFunctionType
ALU = mybir.AluOpType
AX = mybir.AxisListType

D_MODEL = 2048
BATCH = 4
SEQ = 4096
DEPTH = 2
KC = D_MODEL // 128
NBLK = SEQ // 128
LBLK = NBLK // 2
LTOK = LBLK * 128
TT = 256
NT = LTOK // TT
D_FF = 4 * D_MODEL
EPS = 1e-6
NEG = -30000.0
N_CORES = 8


class Tok:
    __slots__ = ("w", "r", "parent", "subs", "dj")

    def __init__(self, parent=None, dj=False):
        self.w = {}
        self.r = {}
        self.parent = parent
        self.subs = []
        self.dj = dj


class Tile:
    def __init__(self, ap, name):
        self.ap = ap
        self.name = name
        self.t = Tok()
        self._subs = {}

    def s(self, key):
        if key not in self._subs:
            tk = Tok(parent=self.t)
            self.t.subs.append(tk)
            self._subs[key] = tk
        return self._subs[key]

    def __getitem__(self, idx):
        return self.ap[idx]


ENGS = ("pe", "act", "dve", "pool", "sp")


class _Rec:
    def __init__(self):
        self.call = None

    def __getattr__(self, name):
        def f(*a, **kw):
            self.call = (name, a, kw)
            return None
        return f


class Sched:
    def __init__(self, nc, es, n_dma_sems=56):
        self.nc = nc
        self.es = es
        self.ops = {e: [] for e in ENGS}
        self.cnt = {e: 0 for e in ENGS}
        self.sems = {}
        for e in ENGS + ("cc",):
            self.sems[e] = es.enter_context(nc.semaphore("sem_" + e))
        self.cc_cnt = 0
        self.dma_sems = [es.enter_context(nc.semaphore(f"dsem{i}")) for i in range(n_dma_sems)]
        self.dma_cnt = [0] * n_dma_sems
        self.dma_rr = 0
        self.tile_sem = {}
        self.seen = {e: {} for e in ENGS}
        self.final_waits = []
        self.ntiles = 0

    def sb(self, name, shape, dtype):
        self.ntiles += 1
        t = self.es.enter_context(self.nc.sbuf_tensor(f"{name}_{self.ntiles}", list(shape), dtype))
        return Tile(t, name)

    def ps(self, name, shape, dtype=F32):
        self.ntiles += 1
        t = self.es.enter_context(self.nc.psum_tensor(f"{name}_{self.ntiles}", list(shape), dtype))
        return Tile(t, name)

    @staticmethod
    def _toks(x):
        out = []
        for i in x:
            out.append(i if isinstance(i, Tok) else i.t)
        return out

    def _collect(self, reads, writes):
        ev = {}

        def add_r(d):
            for k, v in d.items():
                if ev.get(k, 0) < v:
                    ev[k] = v

        add = add_r

        for t in reads:
            add(t.w)
            if t.parent is not None:
                add(t.parent.w)
            for s_ in t.subs:
                add(s_.w)
        for t in writes:
            if not t.dj:
                add(t.w)
            add_r(t.r)
            if t.parent is not None:
                add(t.parent.w)
                add_r(t.parent.r)
            for s_ in t.subs:
                add(s_.w)
                add_r(s_.r)
        return ev

    def _update(self, reads, writes, me):
        k, v = me
        for t in reads:
            if t.r.get(k, 0) < v:
                t.r[k] = v
        for t in writes:
            if t.dj:
                if t.w.get(k, 0) < v:
                    t.w[k] = v
                continue
            t.w = {k: v}
            t.r = {}
            for s_ in t.subs:
                s_.w = {}
                s_.r = {}

    def _waits(self, eng, ev):
        seen = self.seen[eng]
        waits = []
        for k, v in ev.items():
            if eng == "pe" and k == "pe":
                continue
            if seen.get(k, 0) >= v:
                continue
            seen[k] = v
            waits.append((k, v))
        return waits

    def _semof(self, k):
        return self.sems[k] if isinstance(k, str) else self.dma_sems[k]

    def op(self, eng, fn, reads=(), writes=()):
        reads = self._toks(reads)
        writes = self._toks(writes)
        ev = self._collect(reads, writes)
        waits = self._waits(eng, ev)
        self.cnt[eng] += 1
        me = (eng, self.cnt[eng])
        rec = _Rec()
        fn(rec)
        name, a, kw = rec.call

        def fn2(e, name=name, a=a, kw=kw):
            return getattr(e, name)(*a, **kw)

        self.ops[eng].append((waits, fn2, (eng, 1)))
        self._update(reads, writes, me)

    def collective_allgather(self, in_ap, out_ap, reads=(), writes=()):
        reads = self._toks(reads)
        writes = self._toks(writes)
        ev = self._collect(reads, writes)
        waits = self._waits("pool", ev)
        self.cc_cnt += 1
        me = ("cc", self.cc_cnt)

        def fn(e):
            return e.collective_compute("AllGather", ALU.bypass, replica_groups=[list(range(N_CORES))],
                                        ins=[in_ap], outs=[out_ap])

        self.ops["pool"].append((waits, fn, ("cc", None)))
        self._update(reads, writes, me)

    def idma(self, out, in_, idx_ap, reads=(), writes=(), semtile=None):
        def fn(e):
            return e.indirect_dma_start(out=out, out_offset=None, in_=in_,
                                        in_offset=bass.IndirectOffsetOnAxis(ap=idx_ap, axis=0))
        return self.dma("pool", None, None, reads=reads, writes=writes, semtile=semtile, fn=fn)

    def dma(self, queue, out, in_, reads=(), writes=(), semtile=None, fn=None, **kw):
        reads = self._toks(reads)
        writes = self._toks(writes)
        ev = self._collect(reads, writes)
        key = semtile if semtile is not None else (writes[0] if writes else reads[0])
        if key not in self.tile_sem:
            self.tile_sem[key] = self.dma_rr % len(self.dma_sems)
            self.dma_rr += 1
        si = self.tile_sem[key]
        if self.dma_cnt[si] > 0:
            pv = self.dma_cnt[si]
            if ev.get(si, 0) < pv:
                ev[si] = pv
        waits = self._waits(queue, ev)
        self.dma_cnt[si] += 16
        me = (si, self.dma_cnt[si])

        if fn is None:
            def fn(e, out=out, in_=in_, kw=kw):
                return e.dma_start(out=out, in_=in_, **kw)

        self.ops[queue].append((waits, fn, (si, 16)))
        self._update(reads, writes, me)
        return me

    def final_all(self):
        for si, v in enumerate(self.dma_cnt):
            if v > 0:
                self.final_waits.append((si, v))

    def emit(self):
        nc = self.nc
        block = self.es.enter_context(nc.Block())

        def replay(eng_name, e, extra=None):
            for waits, fn, inc in self.ops[eng_name]:
                for k, v in waits:
                    e.wait_ge(self._semof(k), v)
                ins = fn(e)
                if inc[1] is None:
                    ins.then_inc(self._semof(inc[0]))
                else:
                    ins.then_inc(self._semof(inc[0]), inc[1])
            if extra:
                for k, v in extra:
                    e.wait_ge(self._semof(k), v)

        @block.sync
        def _(e):
            replay("sp", e, self.final_waits)

        @block.tensor
        def _(e):
            replay("pe", e)

        @block.scalar
        def _(e):
            replay("act", e)

        @block.vector
        def _(e):
            replay("dve", e)

        @block.gpsimd
        def _(e):
            replay("pool", e)


WB = 512
USE_WSCR = False


class Ctx:
    def __init__(self, S):
        self.S = S
        self.ones_bf = S.sb("ones_bf", [128, 128], BF16)
        S.op("dve", lambda e: e.memset(self.ones_bf[:], 1.0), writes=[self.ones_bf])
        self.wscr = None
        self.wslots = {}
        self.wtoks = {}
        self.first_tile = True
        self.layer = 0
        self.wbuf = [S.sb(f"wbuf{i}", [128, KC, WB], BF16) for i in range(2)]
        self.wrr = 0
        self.wsm = [S.sb(f"wsm{i}", [128, 4, WB], BF16) for i in range(2)]
        self.wsrr = 0
        self.psd = [S.ps(f"psd{i}", [128, 512], F32) for i in range(2)]
        self.psrr = 0
        self.evrr = 0
        self.sqb = [S.sb(f"sqb{i}", [128, TT], BF16) for i in range(2)]
        self.rstd = S.sb("rstd", [128, TT], F32)
        self.fence_t = S.sb("fence", [128, 8], F32)

    def next_w(self, small=False):
        if small:
            w = self.wsm[self.wsrr % len(self.wsm)]
            self.wsrr += 1
            return w
        w = self.wbuf[self.wrr % len(self.wbuf)]
        self.wrr += 1
        return w

    def next_ps(self):
        p = self.psd[self.psrr % len(self.psd)]
        self.psrr += 1
        return p

    def ev_eng(self):
        self.evrr += 1
        return "act" if self.evrr % 2 else "dve"

    def fence(self, tiles):
        self.S.op("pool", lambda e: e.memset(self.fence_t[:, 0:1], 0.0), writes=[self.fence_t] + list(tiles))


def load_w(cx, wdram, col0, ncols, nk=KC, row0=0, wtok=None, key=None):
    S = cx.S
    w = cx.next_w(small=(nk <= 4))
    src = wdram[row0:row0 + nk * 128, col0:col0 + ncols].rearrange("(k p) n -> p k n", p=128)
    if cx.wscr is None or key is None:
        S.dma("pool", out=w[:, 0:nk, 0:ncols], in_=src, reads=([wtok] if wtok is not None else []), writes=[w])
        return w
    k = (cx.layer, key, col0, row0)
    if k not in cx.wslots:
        cx.wslots[k] = len([1 for kk in cx.wslots if kk[0] == cx.layer])
        cx.wtoks[k] = Tok()
    slot = cx.wslots[k]
    sv = cx.wscr[cx.layer][slot][:, 0:nk * ncols].rearrange("p (k n) -> p k n", n=ncols)
    if cx.first_tile:
        S.dma("pool", out=w[:, 0:nk, 0:ncols], in_=src, reads=([wtok] if wtok is not None else []), writes=[w])
        S.dma("sp", out=sv, in_=w[:, 0:nk, 0:ncols], reads=[w], writes=[cx.wtoks[k]], semtile=w.t)
    else:
        S.dma("sp", out=w[:, 0:nk, 0:ncols], in_=sv, reads=[cx.wtoks[k]], writes=[w])
    return w


def evac(cx, out_ap, in_ps, reads, writes, scale=None, eng=None):
    S = cx.S
    eng = eng or cx.ev_eng()
    if eng == "act":
        if scale is None:
            S.op("act", lambda e: e.activation(out=out_ap, in_=in_ps, func=AF.Copy), reads=reads, writes=writes)
        else:
            S.op("act", lambda e: e.activation(out=out_ap, in_=in_ps, func=AF.Copy, scale=float(scale)),
                 reads=reads, writes=writes)
    else:
        if scale is None:
            S.op("dve", lambda e: e.tensor_copy(out=out_ap, in_=in_ps), reads=reads, writes=writes)
        else:
            S.op("dve", lambda e: e.tensor_scalar(out=out_ap, in0=in_ps, scalar1=float(scale), scalar2=None,
                                                  op0=ALU.mult), reads=reads, writes=writes)


def rmsnorm_fm(cx, x, g, hT, ps):
    S = cx.S
    rstd = cx.rstd
    for kc in range(KC):
        sq = cx.sqb[kc % 2]
        S.op("act", lambda e, kc=kc, sq=sq: e.activation(out=sq[:, :], in_=x[:, kc, :], func=AF.Square),
             reads=[x], writes=[sq])
        S.op("pe", lambda e, kc=kc, sq=sq: e.matmul(ps[:, 0:TT], lhsT=cx.ones_bf[:, :], rhs=sq[:, :],
                                                    start=(kc == 0), stop=(kc == KC - 1)),
             reads=[sq, cx.ones_bf], writes=[ps])
    S.op("act", lambda e: e.activation(out=rstd[:, :], in_=ps[:, 0:TT], func=AF.Sqrt, bias=EPS, scale=1.0 / D_MODEL),
         reads=[ps], writes=[rstd])
    S.op("dve", lambda e: e.reciprocal(out=rstd[:, :], in_=rstd[:, :]), reads=[rstd], writes=[rstd])
    for kc in range(KC):
        S.op("dve", lambda e, kc=kc: e.scalar_tensor_tensor(out=hT[:, kc, :], in0=x[:, kc, :], scalar=g[:, kc:kc + 1],
                                                            op0=ALU.mult, in1=rstd[:, :], op1=ALU.mult),
             reads=[x, g, rstd], writes=[hT.s(kc)])


def proj_fm(cx, w, c0, M, hT, ps, nk=KC, ncol=TT, kofs=0):
    S = cx.S
    for kc in range(nk):
        S.op("pe", lambda e, kc=kc: e.matmul(ps[0:M, 0:ncol], lhsT=w[:, kc, c0:c0 + M], rhs=hT[:, kofs + kc, :],
                                             start=(kc == 0), stop=(kc == nk - 1)),
             reads=[w, hT.s(kofs + kc)], writes=[ps])


def rope_fm(cx, ps, P, half, ctab, stab, out_ap, out_toks, tmpA, tmpB):
    S = cx.S
    S.op("dve", lambda e: e.tensor_tensor(out=tmpA[0:P, :], in0=ps[0:P, 0:TT], in1=ctab[0:P, :], op=ALU.mult),
         reads=[ps] + ctab.toks, writes=[tmpA])
    for g in range(P // (2 * half)):
        b = g * 2 * half
        S.op("dve", lambda e, b=b: e.tensor_tensor(out=tmpB[b:b + half, :], in0=ps[b + half:b + 2 * half, 0:TT],
                                                   in1=stab[b + half:b + 2 * half, :], op=ALU.mult),
             reads=[ps] + stab.toks, writes=[tmpB])
        S.op("dve", lambda e, b=b: e.tensor_tensor(out=tmpB[b + half:b + 2 * half, :], in0=ps[b:b + half, 0:TT],
                                                   in1=stab[b:b + half, :], op=ALU.mult),
             reads=[ps] + stab.toks, writes=[tmpB])
    if isinstance(out_ap, list):
        for (r0, r1, oap, otk) in out_ap:
            S.op("dve", lambda e, r0=r0, r1=r1, oap=oap: e.tensor_tensor(out=oap, in0=tmpA[r0:r1, :], in1=tmpB[r0:r1, :],
                                                                      op=ALU.add), reads=[tmpA, tmpB], writes=otk)
    else:
        S.op("dve", lambda e: e.tensor_tensor(out=out_ap, in0=tmpA[0:P, :], in1=tmpB[0:P, :], op=ALU.add),
             reads=[tmpA, tmpB], writes=out_toks)


class View:
    def __init__(self, tile, idx, toks=None):
        self.tile = tile
        self.idx = idx
        self.toks = toks if toks is not None else [tile.t]

    def __getitem__(self, sl):
        rows, cols = sl
        return self.tile.ap[(rows,) + tuple(self.idx) + (cols,)] if isinstance(self.idx, tuple) else \
            self.tile.ap[rows, self.idx, cols]


WK_COLS = 512 * 3 + 128 + 64 + 4
WV_COLS = 512 * 3 + 128


class DT:
    def __init__(self, ap, tok=None, dj=False):
        self.ap = ap
        self.t = tok if tok is not None else Tok(dj=dj)

    def __getitem__(self, i):
        return self.ap[i]


def load_x_tile(cx, c, x_src, t, ropet):
    S = cx.S
    xs, xtok = x_src(t)
    S.dma("sp", out=c["xbuf"][:, :, :], in_=xs.rearrange("(k p) n -> p k n", p=128), reads=[xtok], writes=[c["xbuf"]])
    S.dma("sp", out=c["rt"][:, :, :], in_=ropet.ap[:, :, t * TT:(t + 1) * TT].rearrange("f p n -> p f n"),
          reads=[ropet.t], writes=[c["rt"]])


def phase_kv(cx, io, x_src, c):
    S = cx.S
    x, hT, tmpA, tmpB, kst, vst, lf, rt = (c[k] for k in ("xbuf", "hT", "tmpA", "tmpB", "kst", "vst", "lf", "rt"))
    c128, s128, c64, s64 = (View(rt, i) for i in range(4))
    stn = 0
    for t in range(NT):
        cx.first_tile = (t == 0)
        tc = (t * TT, (t + 1) * TT)
        load_x_tile(cx, c, x_src, t, io["ropet"])
        rmsnorm_fm(cx, x, c["n1g"], hT, cx.next_ps())
        for (c0, dst, rk) in ((0, "kTa", None), (512, "kTb", 32), (1024, "kTc", None)):
            w = load_w(cx, io["wk"].ap, c0, 512, wtok=io["wk"].t, key="wk")
            for jp in range(2):
                st = kst[stn % 2]
                stn += 1
                for j in range(2):
                    ps = cx.next_ps()
                    proj_fm(cx, w, (jp * 2 + j) * 128, 128, hT, ps)
                    if rk is None:
                        evac(cx, st[:, j, :], ps[:, 0:TT], [ps], [st])
                    else:
                        rope_fm(cx, ps, 128, rk, c64, s64, st[:, j, :], [st], tmpA, tmpB)
                r0 = jp * 256
                S.dma("sp", out=io[dst].ap[r0:r0 + 256, tc[0]:tc[1]].rearrange("(j p) n -> p j n", p=128),
                      in_=st[:, :, :], reads=[st], writes=[io[dst]], semtile=st.t)
        w = load_w(cx, io["wk"].ap, 1536, 196, wtok=io["wk"].t, key="wk")
        st = kst[stn % 2]
        stn += 1
        ps = cx.next_ps()
        proj_fm(cx, w, 0, 128, hT, ps)
        rope_fm(cx, ps, 128, 64, c128, s128, st[:, 0, :], [st], tmpA, tmpB)
        ps = cx.next_ps()
        proj_fm(cx, w, 128, 68, hT, ps)
        rope_fm(cx, ps, 64, 32, c64, s64, st[0:64, 1, :], [st], tmpA, tmpB)
        S.op("act", lambda e, ps=ps: e.activation(out=lf[64:68, :], in_=ps[64:68, 0:TT], func=AF.Exp,
                                                 bias=c["nbfg"][64:68, :], scale=-1.0),
             reads=[ps, c["nbfg"]], writes=[lf])
        S.op("act", lambda e: e.activation(out=lf[64:68, :], in_=lf[64:68, :], func=AF.Ln, bias=1.0),
             reads=[lf], writes=[lf])
        S.op("act", lambda e: e.mul(out=lf[64:68, :], in_=lf[64:68, :], mul=-1.0), reads=[lf], writes=[lf])
        S.dma("sp", out=io["kTd"].ap[:, tc[0]:tc[1]], in_=st[:, 0, :], reads=[st], writes=[io["kTd"]], semtile=st.t)
        S.dma("sp", out=io["ikT"].ap[:, tc[0]:tc[1]], in_=st[0:64, 1, :], reads=[st], writes=[io["ikT"]], semtile=st.t)
        S.dma("sp", out=io["logf"].ap[:, tc[0]:tc[1]], in_=lf[64:68, :], reads=[lf], writes=[io["logf"]], semtile=lf.t)
        for (c0, ncols, nm) in ((0, 512, "va"), (512, 512, "vb"), (1024, 512, "vc"), (1536, 128, "vd")):
            w = load_w(cx, io["wv"].ap, c0, ncols, wtok=io["wv"].t, key="wv")
            for sbk in range(TT // 128):
                ps = cx.next_ps()
                for kc in range(KC):
                    S.op("pe", lambda e, kc=kc, ps=ps, w=w, sbk=sbk, ncols=ncols: e.matmul(
                        ps[:, 0:ncols], lhsT=hT[:, kc, sbk * 128:(sbk + 1) * 128], rhs=w[:, kc, 0:ncols],
                        start=(kc == 0), stop=(kc == KC - 1)), reads=[w, hT.s(kc)], writes=[ps])
                st = vst[stn % 2]
                stn += 1
                evac(cx, st[:, 0:ncols], ps[:, 0:ncols], [ps], [st])
                r0 = t * TT + sbk * 128
                S.dma("sp", out=io[nm].ap[r0:r0 + 128, 0:ncols], in_=st[:, 0:ncols], reads=[st], writes=[io[nm]],
                      semtile=st.t)


def alloc_common(cx):
    S = cx.S
    c = {}
    c["xbuf"] = S.sb("xbuf", [128, KC, TT], F32)
    c["hT"] = S.sb("hT", [128, KC, TT], BF16)
    c["tmpA"] = S.sb("tmpA", [128, TT], F32)
    c["tmpB"] = S.sb("tmpB", [128, TT], F32)
    c["rt"] = S.sb("rt", [128, 4, TT], F32)
    c["n1g"] = S.sb("n1g", [128, KC], F32)
    return c


def alloc_kv(cx, c):
    S = cx.S
    c["kst"] = [S.sb(f"kst{i}", [128, 2, TT], BF16) for i in range(2)]
    c["vst"] = [S.sb(f"vst{i}", [128, 512], BF16) for i in range(2)]
    c["lf"] = S.sb("lf", [128, TT], F32)
    c["nbfg"] = S.sb("nbfg", [128, 1], F32)
    return c


def dram_in(nc, name, shape, dtype=F32):
    return DT(nc.dram_tensor(name, list(shape), dtype, kind="ExternalInput").ap())


def dram_out(nc, name, shape, dtype=F32):
    return DT(nc.dram_tensor(name, list(shape), dtype, kind="ExternalOutput").ap(), dj=True)


KV_OUT = (("kTa", [512, LTOK], BF16), ("kTb", [512, LTOK], BF16), ("kTc", [512, LTOK], BF16),
          ("kTd", [128, LTOK], BF16), ("ikT", [64, LTOK], BF16), ("logf", [4, LTOK], F32),
          ("va", [LTOK, 512], BF16), ("vb", [LTOK, 512], BF16), ("vc", [LTOK, 512], BF16),
          ("vd", [LTOK, 128], BF16))


def build_A():
    nc = bass.Bass("TRN2", target_bir_lowering=False)
    es = ExitStack()
    io = {}
    xT = dram_in(nc, "xT", [D_MODEL, LTOK])
    io["wk"] = dram_in(nc, "wk", [D_MODEL, WK_COLS])
    io["wv"] = dram_in(nc, "wv", [D_MODEL, WV_COLS])
    io["ropet"] = dram_in(nc, "ropet", [4, 128, LTOK])
    n1g = dram_in(nc, "n1g", [128, KC])
    bfg = dram_in(nc, "bfg", [4, 1])
    for nm, shp, dt in KV_OUT:
        io[nm] = dram_out(nc, nm, shp, dt)
    with es:
        S = Sched(nc, es)
        cx = Ctx(S)
        c = alloc_kv(cx, alloc_common(cx))
        S.dma("sp", out=c["n1g"][:, :], in_=n1g.ap[:, :], writes=[c["n1g"]])
        S.dma("sp", out=c["nbfg"][64:68, :], in_=bfg.ap[:, :], writes=[c["nbfg"]])
        S.op("dve", lambda e: e.tensor_scalar(out=c["nbfg"][64:68, :], in0=c["nbfg"][64:68, :], scalar1=-1.0,
                                              scalar2=None, op0=ALU.mult), reads=[c["nbfg"]], writes=[c["nbfg"]])
        phase_kv(cx, io, lambda t: (xT.ap[:, t * TT:(t + 1) * TT], xT.t), c)
        S.final_all()
        S.emit()
    return nc


SEG_W = (("a_q", 512), ("a_k", 512), ("a_v", 512), ("b_q", 512), ("b_k", 512), ("b_v", 512),
         ("c_q", 512), ("c_k", 512), ("c_v", 512), ("c_f", 4), ("d_q", 512), ("d_k", 128), ("d_v", 128),
         ("d_iq", 512), ("d_ik", 64), ("d_iw", 8), ("gate", 8192))
SEG = {}
_o = 0
for _n, _w in SEG_W:
    SEG[_n] = (_o, _o + _w)
    _o += _w


def wcols(w, names):
    return np.ascontiguousarray(np.concatenate([w[:, SEG[n][0]:SEG[n][1]] for n in names], axis=1))


def local_pos(c):
    lb = np.arange(LBLK)
    return ((2 * lb + c)[:, None] * 128 + np.arange(128)[None, :]).reshape(-1)


def rope_tabs(c):
    pos = local_pos(c).astype(np.float32)
    out = []
    for dim in (128, 64):
        inv = (np.float32(10000.0) ** (-np.arange(0, dim, 2, dtype=np.float32) / np.float32(dim))).astype(np.float32)
        ang = pos[:, None] * inv[None, :]
        cos = np.cos(ang).astype(np.float32).T
        sin = np.sin(ang).astype(np.float32).T
        rep = 128 // dim
        out.append(np.concatenate([cos, cos] * rep, axis=0))
        out.append(np.concatenate([sin, -sin] * rep, axis=0))
    return np.ascontiguousarray(np.stack(out, 0))


def to_local_T(xb, c):
    blk = xb.reshape(NBLK, 128, -1)[c::2].reshape(LTOK, -1)
    return np.ascontiguousarray(blk.T)


def pcol(v):
    return np.ascontiguousarray(v.reshape(-1, 128).T)


IWS = (8 ** -0.5) * (64 ** -0.5)
BIS_W = 256.0
BIS_IT = 24
KV_ALL = (("kTa", [2, 512, LTOK], BF16), ("kTb", [2, 512, LTOK], BF16), ("kTc", [2, 512, LTOK], BF16),
          ("kTd", [2, 128, LTOK], BF16), ("ikT", [2, 64, LTOK], BF16), ("logf", [2, 4, LTOK], F32),
          ("va", [2, LTOK, 512], BF16), ("vb", [2, LTOK, 512], BF16), ("vc", [2, LTOK, 512], BF16),
          ("vd", [2, LTOK, 128], BF16))


class UView:
    def __init__(self, tile):
        self.tile = tile
        self.t = tile.t
        self.v = tile.ap[:, :].bitcast(BF16).rearrange("p (k n) -> p k n", n=TT)

    def s(self, k):
        return self.tile.s(("u", k))

    def __getitem__(self, idx):
        return self.v[idx]


def alloc_q(cx, c):
    S = cx.S
    c["kTd"] = S.sb("kTd_sb", [128, SEQ], BF16)
    c["ikT2"] = S.sb("ikT2", [128, SEQ], BF16)
    c["vd"] = S.sb("vd_sb", [128, NBLK, 128], BF16)
    c["score"] = S.sb("score", [128, SEQ], F32)
    c["negcum"] = S.sb("negcum", [128, NBLK * 4], F32)
    c["cum_mine"] = S.sb("cum_mine", [4, LTOK], F32)
    c["cumq"] = S.sb("cumq", [128, 4, TT], F32)
    c["biasA"] = S.sb("biasA", [128, 6, 512], BF16)
    c["mk"] = S.sb("mk", [128, 4, 128], BF16)
    c["idxmask"] = S.sb("idxmask", [128, 256], F32)
    c["ident"] = S.sb("ident", [128, 128], BF16)
    c["identf"] = S.sb("identf", [128, 128], F32)
    c["onesf"] = S.sb("onesf", [128, 256], F32)
    c["selh"] = S.sb("selh", [4, 512], F32)
    c["sel"] = S.sb("sel", [4, 2], F32)
    c["n2g"] = S.sb("n2g", [128, KC], F32)
    c["fg"] = S.sb("fg", [128, KC], F32)
    c["bgate"] = S.sb("bgate", [128, 64], F32)
    c["dng"] = S.sb("dng", [128, 1], F32)
    c["neglam"] = S.sb("neglam", [128, 1], F32)
    c["lamv"] = S.sb("lamv", [64, 4], F32)
    c["lamp"] = S.sb("lamp", [128, 2], F32)
    c["wiw"] = S.sb("wiw", [128, KC, 8], BF16)
    c["q"] = S.sb("q", [128, 4, TT], BF16)
    c["iq"] = S.sb("iq", [128, 4, TT], BF16)
    c["qz"] = [S.sb(f"qz{i}", [128, 4, TT], BF16) for i in range(2)]
    c["iw"] = S.sb("iw", [128, 16], F32)
    c["o"] = [S.sb(f"o{i}", [128, 4, TT], BF16) for i in range(4)]
    c["mg"] = S.sb("mg", [128, KC, TT], BF16)
    c["u"] = UView(c["score"])
    c["kch"] = [S.sb(f"kch{i}", [128, 4, 512], BF16) for i in range(2)]
    c["vch"] = [S.sb(f"vch{i}", [128, 4, 512], BF16) for i in range(2)]
    c["pT"] = [S.sb(f"pT{i}", [128, 512], BF16) for i in range(3)]
    c["sp"] = [S.sb(f"sp{i}", [128, 512], F32) for i in range(2)]
    c["nm"] = [S.sb(f"nm{i}", [128, 512], BF16) for i in range(2)]
    c["nmT"] = [S.sb(f"nmT{i}", [128, 4, 128], BF16) for i in range(2)]
    c["rden"] = S.sb("rden", [128, 512], F32)
    c["obr"] = S.sb("obr", [128, TT], F32)
    c["acc"] = [S.sb(f"acc{i}", [128, TT], F32) for i in range(4)]
    c["m8"] = S.sb("m8", [128, 8], F32)
    c["bis"] = S.sb("bis", [128, 4], F32)
    c["ost"] = [S.sb(f"ost{i}", [128, TT], F32) for i in range(2)]
    c["psS"] = [S.ps(f"psS{i}", [128, 512], F32) for i in range(2)]
    c["psO"] = S.ps("psO", [128, 512], F32)
    c["psD"] = S.ps("psD", [128, 512], F32)
    c["psm"] = S.ps("psm", [128, 512], F32)
    c["psT"] = S.ps("psT", [128, 512], BF16)
    c["rr"] = {"S": 0, "pT": 0, "sp": 0, "kv": 0, "nm": 0}
    return c


def rr(c, name, key):
    lst = c[name]
    i = c["rr"][key]
    c["rr"][key] = i + 1
    return lst[i % len(lst)]


def setup_q(cx, c, io, lambda_init):
    S = cx.S
    pq = "pool"
    S.dma(pq, out=c["biasA"][:, :, :], in_=io["biasA"].ap[:, :].rearrange("p (b f) -> p b f", b=6),
          reads=[io["biasA"]], writes=[c["biasA"]])
    S.dma(pq, out=c["mk"][:, :, :], in_=io["mk"].ap[:, :].rearrange("p (b f) -> p b f", b=4), reads=[io["mk"]],
          writes=[c["mk"]])
    S.dma(pq, out=c["ident"][:, :], in_=io["identf"].ap[:, :], reads=[io["identf"]], writes=[c["ident"]])
    S.dma(pq, out=c["wiw"][:, :, :], in_=io["wiw"].ap[:, :].rearrange("(k p) n -> p k n", p=128), reads=[io["wiw"]],
          writes=[c["wiw"]])
    for nm in ("identf", "idxmask", "selh", "sel", "n2g", "fg", "bgate", "dng"):
        S.dma("sp", out=c[nm][:, :], in_=io[nm].ap[:, :], reads=[io[nm]], writes=[c[nm]])
    S.dma("sp", out=c["lamv"][:, :], in_=io["lam4"].ap[:, :], reads=[io["lam4"]], writes=[c["lamv"]])
    S.op("dve", lambda e: e.memset(c["onesf"][:, :], 1.0), writes=[c["onesf"]])
    S.op("dve", lambda e: e.tensor_scalar(out=c["dng"][:, :], in0=c["dng"][:, :], scalar1=float(1.0 - lambda_init),
                                          scalar2=None, op0=ALU.mult), reads=[c["dng"]], writes=[c["dng"]])
    x = c["xbuf"]
    xflat = x[0:4, :, :].rearrange("p k n -> p (k n)")
    for r in range(2):
        S.dma("sp", out=c["kTd"][:, :].rearrange("p (n r j) -> p n r j", r=2, j=128)[:, :, r, :],
              in_=io["kTd_all"].ap[r].rearrange("p (n j) -> p n j", j=128), reads=[io["kTd_all"]], writes=[c["kTd"]])
        for hf in range(2):
            S.dma("sp", out=c["ikT2"][hf * 64:(hf + 1) * 64, :].rearrange("p (n r j) -> p n r j", r=2, j=128)[:, :, r, :],
                  in_=io["ikT_all"].ap[r].rearrange("p (n j) -> p n j", j=128), reads=[io["ikT_all"]],
                  writes=[c["ikT2"]])
        S.dma("sp", out=c["vd"][:, :, :].rearrange("p (n r) d -> p n r d", r=2)[:, :, r, :],
              in_=io["vd_all"].ap[r].rearrange("(n p) d -> p n d", p=128), reads=[io["vd_all"]], writes=[c["vd"]])
        S.dma("sp", out=xflat.rearrange("p (n r j) -> p n r j", r=2, j=128)[:, :, r, :],
              in_=io["logf_all"].ap[r].rearrange("p (n j) -> p n j", j=128), reads=[io["logf_all"]], writes=[x])
    score = c["score"]
    CH = 256
    for i in range(SEQ // CH):
        init = 0.0 if i == 0 else score[0:4, i * CH - 1:i * CH]
        S.op("dve", lambda e, i=i, init=init: e.tensor_tensor_scan(
            out=score[0:4, i * CH:(i + 1) * CH], data0=c["onesf"][0:4, 0:CH], data1=xflat[:, i * CH:(i + 1) * CH],
            initial=init, op0=ALU.mult, op1=ALU.add), reads=[x, c["onesf"], score], writes=[score])
    psm = c["psm"]
    for kb in range(NBLK):
        S.op("pe", lambda e, kb=kb: e.transpose(out=psm[:, kb * 4:(kb + 1) * 4], in_=score[0:4, kb * 128:(kb + 1) * 128],
                                                identity=c["identf"][0:4, 0:4]),
             reads=[score, c["identf"]], writes=[psm])
    S.op("act", lambda e: e.mul(out=c["negcum"][:, :], in_=psm[:, 0:NBLK * 4], mul=-1.0), reads=[psm],
         writes=[c["negcum"]])
    cv = score[0:4, :].rearrange("p (n r j) -> p n r j", r=2, j=128)
    cm = c["cum_mine"][0:4, :].rearrange("p (n j) -> p n j", j=128)
    S.op("dve", lambda e: e.tensor_scalar(out=cm, in0=cv[:, :, 0, :], scalar1=c["sel"][0:4, 0:1], scalar2=None,
                                          op0=ALU.mult), reads=[score, c["sel"]], writes=[c["cum_mine"]])
    S.op("dve", lambda e: e.scalar_tensor_tensor(out=cm, in0=cv[:, :, 1, :], scalar=c["sel"][0:4, 1:2], op0=ALU.mult,
                                                 in1=cm, op1=ALU.add), reads=[score, c["sel"], c["cum_mine"]],
         writes=[c["cum_mine"]])
    lv, lp = c["lamv"], c["lamp"]
    S.op("dve", lambda e: e.tensor_tensor(out=lp[0:64, 0:1], in0=lv[0:64, 0:1], in1=lv[0:64, 1:2], op=ALU.mult),
         reads=[lv], writes=[lp])
    S.op("dve", lambda e: e.tensor_tensor(out=lp[0:64, 1:2], in0=lv[0:64, 2:3], in1=lv[0:64, 3:4], op=ALU.mult),
         reads=[lv, lp], writes=[lp])
    S.op("pe", lambda e: e.matmul(psm[:, 0:2], lhsT=c["onesf"][0:64, 0:128], rhs=lp[0:64, 0:2], start=True, stop=True),
         reads=[c["onesf"], lp], writes=[psm])
    S.op("act", lambda e: e.activation(out=lp[:, 0:2], in_=psm[:, 0:2], func=AF.Exp), reads=[psm], writes=[lp])
    S.op("dve", lambda e: e.tensor_tensor(out=c["neglam"][:, :], in0=lp[:, 1:2], in1=lp[:, 0:1], op=ALU.subtract),
         reads=[lp], writes=[c["neglam"]])
    S.op("dve", lambda e: e.tensor_scalar(out=c["neglam"][:, :], in0=c["neglam"][:, :], scalar1=float(-lambda_init),
                                          scalar2=None, op0=ALU.add), reads=[c["neglam"]], writes=[c["neglam"]])


def q_proj(cx, c, io, col0, dst, mode, scale=None):
    hT, tmpA, tmpB, rt = c["hT"], c["tmpA"], c["tmpB"], c["rt"]
    w = load_w(cx, io["wq"].ap, col0, 512, wtok=io["wq"].t, key="wq")
    for jj in range(4):
        ps = cx.next_ps()
        proj_fm(cx, w, jj * 128, 128, hT, ps)
        if mode == "plain":
            evac(cx, dst[:, jj, :], ps[:, 0:TT], [ps], [dst], scale=scale)
        elif mode == "rope64z":
            qz = c["qz"]
            rope_fm(cx, ps, 128, 32, View(rt, 2), View(rt, 3),
                    [(0, 64, qz[0][0:64, jj, :], [qz[0]]), (64, 128, qz[1][64:128, jj, :], [qz[1]])], None,
                    tmpA, tmpB)
        elif mode == "rope64":
            rope_fm(cx, ps, 128, 32, View(rt, 2), View(rt, 3), dst[:, jj, :], [dst], tmpA, tmpB)
        else:
            rope_fm(cx, ps, 128, 64, View(rt, 0), View(rt, 1), dst[:, jj, :], [dst], tmpA, tmpB)


def attn_A(cx, c, io, t):
    S = cx.S
    q, o = c["q"], c["o"][0]
    psO, psD, rden = c["psO"], c["psD"], c["rden"]
    for m in range(2):
        lb = 2 * t + m
        n0 = max(0, lb - 2)
        nn = lb - n0 + 1
        mc = slice(m * 128, (m + 1) * 128)
        kb_, vb_ = c["kch"], c["vch"]
        for r in range(2):
            S.dma("sp", out=kb_[r][:, :, 0:nn * 128],
                  in_=io["kTa_all"].ap[r].rearrange("(h p) n -> p h n", p=128)[:, :, n0 * 128:(lb + 1) * 128],
                  reads=[io["kTa_all"]], writes=[kb_[r]])
            S.dma("sp", out=vb_[r][:, 0:nn, :],
                  in_=io["va_all"].ap[r][n0 * 128:(lb + 1) * 128, :].rearrange("(n p) f -> p n f", p=128),
                  reads=[io["va_all"]], writes=[vb_[r]])
        tot = 2 * nn
        k = 0
        for r in range(2):
            for ni in range(nn):
                nrel = n0 + ni - (lb - 2)
                pss = rr(c, "psS", "S")
                pT = rr(c, "pT", "pT")
                for h in range(4):
                    S.op("pe", lambda e, h=h, r=r, ni=ni, pss=pss: e.matmul(
                        pss[:, h * 128:(h + 1) * 128], lhsT=kb_[r][:, h, ni * 128:(ni + 1) * 128], rhs=q[:, h, mc],
                        start=(h == 0), stop=False, skip_group_check=True), reads=[kb_[r], q], writes=[pss])
                S.op("pe", lambda e, r=r, nrel=nrel, pss=pss: e.matmul(
                    pss[:, :], lhsT=c["ident"][:, :], rhs=c["biasA"][:, r * 3 + nrel, :], start=False, stop=True,
                    skip_group_check=True), reads=[c["ident"], c["biasA"]], writes=[pss])
                S.op("act", lambda e, pss=pss, pT=pT: e.activation(out=pT[:, :], in_=pss[:, :], func=AF.Exp),
                     reads=[pss], writes=[pT])
                for h in range(4):
                    S.op("pe", lambda e, h=h, r=r, ni=ni, pT=pT, k=k: e.matmul(
                        psO[:, h * 128:(h + 1) * 128], lhsT=vb_[r][:, ni, h * 128:(h + 1) * 128],
                        rhs=pT[:, h * 128:(h + 1) * 128], start=(k == 0 and h == 0), stop=(k == tot - 1 and h == 3),
                        skip_group_check=True), reads=[vb_[r], pT], writes=[psO])
                S.op("pe", lambda e, pT=pT, k=k: e.matmul(psD[:, :], lhsT=cx.ones_bf[:, :], rhs=pT[:, :],
                                                           start=(k == 0), stop=(k == tot - 1)),
                     reads=[cx.ones_bf, pT], writes=[psD])
                k += 1
        S.op("dve", lambda e: e.reciprocal(out=rden[:, :], in_=psD[:, :]), reads=[psD], writes=[rden])
        S.op("dve", lambda e, mc=mc: e.tensor_tensor(out=o[:, :, mc], in0=psO[:, :].rearrange("p (h q) -> p h q", h=4),
                                                     in1=rden[:, :].rearrange("p (h q) -> p h q", h=4), op=ALU.mult),
             reads=[psO, rden], writes=[o])


def kv_chunks(t):
    nb_r = 2 * t + 2
    out = []
    for r in range(2):
        for cst in range(0, nb_r, 4):
            out.append((r, cst, min(4, nb_r - cst)))
    return out


def attn_BC(cx, c, io, t, kind):
    S = cx.S
    q = c["q"]
    psO, psD, rden = c["psO"], c["psD"], c["rden"]
    kname, vname = ("kTb_all", "vb_all") if kind == "B" else ("kTc_all", "vc_all")
    o = c["o"][1] if kind == "B" else c["o"][2]
    chunks = kv_chunks(t)
    nblocks = sum(nb for _, _, nb in chunks)
    ngroups = 4 if kind == "B" else 2
    for g in range(ngroups):
        k = 0
        for (r, cst, nb) in chunks:
            kc_ = rr(c, "kch", "kv")
            vc_ = c["vch"][(c["rr"]["kv"] - 1) % 2]
            cols = slice(cst * 128, (cst + nb) * 128)
            if kind == "B":
                S.dma("sp", out=kc_[:, 0, 0:nb * 128], in_=io[kname].ap[r][g * 128:(g + 1) * 128, cols],
                      reads=[io[kname]], writes=[kc_])
            else:
                S.dma("sp", out=kc_[:, 0:2, 0:nb * 128],
                      in_=io[kname].ap[r].rearrange("(h p) n -> p h n", p=128)[:, 2 * g:2 * g + 2, cols],
                      reads=[io[kname]], writes=[kc_])
            S.dma("sp", out=vc_[:, 0:nb, :], in_=io[vname].ap[r][cols, :].rearrange("(n p) f -> p n f", p=128),
                  reads=[io[vname]], writes=[vc_])
            for bi in range(nb):
                n = cst + bi
                npr = n - 2 * t
                q0 = 128 if npr == 1 else 0
                pss = rr(c, "psS", "S")
                pT = rr(c, "pT", "pT")
                bsl = slice(bi * 128, (bi + 1) * 128)
                first, last = (k == 0), (k == nblocks - 1)
                for s in range(2):
                    osl = slice(s * 256 + q0, (s + 1) * 256)
                    if kind == "B":
                        ps_ = slice(s * 64, (s + 1) * 64)
                        S.op("pe", lambda e, s=s, osl=osl, bsl=bsl, pss=pss, kc_=kc_, npr=npr: e.matmul(
                            pss[:, osl], lhsT=kc_[:, 0, bsl], rhs=c["qz"][s][:, g, q0:TT], start=(s == 0),
                            stop=(s == 1 and npr < 0), skip_group_check=True), reads=[kc_, c["qz"][s]], writes=[pss])
                    else:
                        S.op("pe", lambda e, s=s, osl=osl, bsl=bsl, pss=pss, kc_=kc_, npr=npr: e.matmul(
                            pss[:, osl], lhsT=kc_[:, s, bsl], rhs=q[:, 2 * g + s, q0:TT], start=(s == 0),
                            stop=(s == 1 and npr < 0), skip_group_check=True), reads=[kc_, q], writes=[pss])
                if npr >= 0:
                    mi = r if kind == "B" else 2 + r
                    for s in range(2):
                        msl = slice(s * 256 + npr * 128, s * 256 + (npr + 1) * 128)
                        S.op("pe", lambda e, s=s, msl=msl, mi=mi, pss=pss: e.matmul(
                            pss[:, msl], lhsT=c["ident"][:, :], rhs=c["mk"][:, mi, :], start=False, stop=(s == 1),
                            skip_group_check=True), reads=[c["ident"], c["mk"]], writes=[pss])
                v3 = lambda ap: ap.rearrange("p (s q) -> p s q", s=2)[:, :, q0:TT]
                if kind == "B":
                    S.op("act", lambda e, pss=pss, pT=pT, v3=v3: e.activation(out=v3(pT[:, :]), in_=v3(pss[:, :]),
                                                                             func=AF.Exp, scale=64 ** -0.5),
                         reads=[pss], writes=[pT])
                else:
                    sp = rr(c, "sp", "sp")
                    kbt = 2 * n + r
                    for s in range(2):
                        h = 2 * g + s
                        osl = slice(s * 256 + q0, (s + 1) * 256)
                        S.op("dve", lambda e, osl=osl, h=h, kbt=kbt, pss=pss, sp=sp: e.scalar_tensor_tensor(
                            out=sp[:, osl], in0=pss[:, osl], scalar=c["negcum"][:, kbt * 4 + h:kbt * 4 + h + 1],
                            op0=ALU.add, in1=c["cumq"][:, h, q0:TT], op1=ALU.add),
                             reads=[pss, c["negcum"], c["cumq"]], writes=[sp])
                    S.op("act", lambda e, sp=sp, pT=pT, v3=v3: e.activation(out=v3(pT[:, :]), in_=v3(sp[:, :]),
                                                                           func=AF.Exp), reads=[sp], writes=[pT])
                if kind == "B":
                    S.op("pe", lambda e, pT=pT, vc_=vc_, bi=bi, first=first, last=last, v3=v3: e.matmul(
                        v3(psO[:, :]), lhsT=vc_[:, bi, g * 128:(g + 1) * 128], rhs=v3(pT[:, :]), start=first, stop=last,
                        skip_group_check=True), reads=[vc_, pT], writes=[psO])
                else:
                    for s in range(2):
                        h = 2 * g + s
                        osl = slice(s * 256 + q0, (s + 1) * 256)
                        S.op("pe", lambda e, s=s, h=h, osl=osl, pT=pT, vc_=vc_, bi=bi, first=first, last=last: e.matmul(
                            psO[:, osl], lhsT=vc_[:, bi, h * 128:(h + 1) * 128], rhs=pT[:, osl],
                            start=(first and s == 0), stop=(last and s == 1), skip_group_check=True),
                             reads=[vc_, pT], writes=[psO])
                S.op("pe", lambda e, pT=pT, first=first, last=last, v3=v3: e.matmul(
                    v3(psD[:, :]), lhsT=cx.ones_bf[:, :], rhs=v3(pT[:, :]), start=first, stop=last,
                    skip_group_check=True), reads=[cx.ones_bf, pT], writes=[psD])
                k += 1
        S.op("dve", lambda e: e.reciprocal(out=rden[:, :], in_=psD[:, :]), reads=[psD], writes=[rden])
        if kind == "C":
            S.op("dve", lambda e, g=g: e.tensor_tensor(
                out=o[:, 2 * g:2 * g + 2, :], in0=psO[:, :].rearrange("p (s q) -> p s q", s=2),
                in1=rden[:, :].rearrange("p (s q) -> p s q", s=2), op=ALU.mult), reads=[psO, rden], writes=[o])
        else:
            obr, tA, tB = c["obr"], c["tmpA"], c["tmpB"]
            S.op("dve", lambda e: e.tensor_tensor(out=obr[:, :], in0=psO[:, 0:TT], in1=rden[:, 0:TT], op=ALU.mult),
                 reads=[psO, rden], writes=[obr])
            S.op("dve", lambda e: e.tensor_tensor(out=tA[:, :], in0=psO[:, TT:2 * TT], in1=rden[:, TT:2 * TT],
                                                  op=ALU.mult), reads=[psO, rden], writes=[tA])
            S.op("dve", lambda e: e.scalar_tensor_tensor(out=obr[:, :], in0=tA[:, :], scalar=c["neglam"][:, 0:1],
                                                         op0=ALU.mult, in1=obr[:, :], op1=ALU.add),
                 reads=[tA, c["neglam"], obr], writes=[obr])
            sq = cx.sqb[0]
            psm = c["psm"]
            S.op("act", lambda e: e.activation(out=sq[:, :], in_=obr[:, :], func=AF.Square), reads=[obr], writes=[sq])
            S.op("pe", lambda e: e.matmul(psm[:, 0:TT], lhsT=cx.ones_bf[:, :], rhs=sq[:, :], start=True, stop=True),
                 reads=[cx.ones_bf, sq], writes=[psm])
            S.op("act", lambda e: e.activation(out=tB[:, :], in_=psm[:, 0:TT], func=AF.Sqrt, bias=EPS, scale=1.0 / 128),
                 reads=[psm], writes=[tB])
            S.op("dve", lambda e: e.reciprocal(out=tB[:, :], in_=tB[:, :]), reads=[tB], writes=[tB])
            S.op("dve", lambda e, g=g: e.scalar_tensor_tensor(out=o[:, g, :], in0=obr[:, :], scalar=c["dng"][:, 0:1],
                                                              op0=ALU.mult, in1=tB[:, :], op1=ALU.mult),
                 reads=[obr, c["dng"], tB], writes=[o])


def attn_D(cx, c, io, t):
    S = cx.S
    q, iq, o, score = c["q"], c["iq"], c["o"][3], c["score"]
    psO, psD, psT, rden, m8 = c["psO"], c["psD"], c["psT"], c["rden"], c["m8"]
    for m in range(2):
        lb = 2 * t + m
        nk = (2 * lb + 2) * 128
        mc = slice(m * 128, (m + 1) * 128)
        for cc in range(0, nk, 512):
            ncol = min(512, nk - cc)
            for ih in range(8):
                psI = rr(c, "psS", "S")
                sp = rr(c, "sp", "sp")
                prow = slice((ih % 2) * 64, (ih % 2 + 1) * 64)
                S.op("pe", lambda e, psI=psI, prow=prow, ih=ih, cc=cc, ncol=ncol: e.matmul(
                    psI[:, 0:ncol], lhsT=iq[prow, ih // 2, mc], rhs=c["ikT2"][prow, cc:cc + ncol], start=True, stop=True),
                     reads=[iq, c["ikT2"]], writes=[psI])
                S.op("act", lambda e, psI=psI, sp=sp, ncol=ncol: e.activation(out=sp[:, 0:ncol], in_=psI[:, 0:ncol],
                                                                              func=AF.Relu), reads=[psI], writes=[sp])
                iwc = c["iw"][:, m * 8 + ih:m * 8 + ih + 1]
                if ih == 0:
                    S.op("dve", lambda e, sp=sp, cc=cc, ncol=ncol, iwc=iwc: e.tensor_scalar(
                        out=score[:, cc:cc + ncol], in0=sp[:, 0:ncol], scalar1=iwc, scalar2=None, op0=ALU.mult),
                         reads=[sp, c["iw"]], writes=[score])
                else:
                    S.op("dve", lambda e, sp=sp, cc=cc, ncol=ncol, iwc=iwc: e.scalar_tensor_tensor(
                        out=score[:, cc:cc + ncol], in0=sp[:, 0:ncol], scalar=iwc, op0=ALU.mult,
                        in1=score[:, cc:cc + ncol], op1=ALU.add), reads=[sp, c["iw"], score], writes=[score])
        S.op("dve", lambda e, nk=nk: e.tensor_tensor(out=score[:, nk - 256:nk], in0=score[:, nk - 256:nk],
                                                     in1=c["idxmask"][:, :], op=ALU.add),
             reads=[score, c["idxmask"]], writes=[score])
        if lb >= 1:
            mgj = c["mg"]
            junk = mgj[:, :, :].rearrange("p k n -> p (k n)")
            lo, cand, cnt, gg = (c["bis"][:, i:i + 1] for i in range(4))
            S.op("dve", lambda e, nk=nk: e.max(out=m8[:, :], in_=score[:, 0:nk]), reads=[score], writes=[m8])
            S.op("dve", lambda e: e.tensor_scalar(out=lo, in0=m8[:, 0:1], scalar1=-BIS_W, scalar2=None, op0=ALU.add),
                 reads=[m8], writes=[c["bis"]])
            for it in range(BIS_IT):
                ck = BIS_W / float(2 ** (it + 1))
                S.op("dve", lambda e, ck=ck: e.tensor_scalar(out=cand, in0=lo, scalar1=ck, scalar2=None, op0=ALU.add),
                     reads=[c["bis"]], writes=[c["bis"]])
                S.op("dve", lambda e, nk=nk: e.tensor_scalar(out=junk[:, 0:nk], in0=score[:, 0:nk], scalar1=cand,
                                                             scalar2=None, op0=ALU.is_ge, op1=ALU.add, accum_out=cnt),
                     reads=[score, c["bis"]], writes=[mgj, c["bis"]])
                S.op("dve", lambda e, ck=ck: e.tensor_scalar(out=gg, in0=cnt, scalar1=256.0, scalar2=ck, op0=ALU.is_ge,
                                                             op1=ALU.mult), reads=[c["bis"]], writes=[c["bis"]])
                S.op("dve", lambda e: e.tensor_tensor(out=lo, in0=lo, in1=gg, op=ALU.add), reads=[c["bis"]],
                     writes=[c["bis"]])
        nkb = nk // 128
        k = 0
        for cc in range(0, nkb, 4):
            nb = min(4, nkb - cc)
            nm = rr(c, "nm", "nm")
            nmT = c["nmT"][(c["rr"]["nm"] - 1) % 2]
            csl = slice(cc * 128, (cc + nb) * 128)
            if lb >= 1:
                S.op("dve", lambda e, nm=nm, csl=csl, nb=nb: e.tensor_scalar(
                    out=nm[:, 0:nb * 128], in0=score[:, csl], scalar1=c["bis"][:, 0:1], scalar2=NEG, op0=ALU.is_lt,
                    op1=ALU.mult), reads=[score, c["bis"]], writes=[nm])
            else:
                S.op("dve", lambda e, nm=nm, csl=csl, nb=nb: e.tensor_scalar(
                    out=nm[:, 0:nb * 128], in0=score[:, csl], scalar1=-1.0e29, scalar2=NEG, op0=ALU.is_lt, op1=ALU.mult),
                     reads=[score], writes=[nm])
            for bi in range(nb):
                S.op("pe", lambda e, nm=nm, bi=bi: e.transpose(out=psT[:, bi * 128:(bi + 1) * 128],
                                                               in_=nm[:, bi * 128:(bi + 1) * 128],
                                                               identity=c["ident"][:, :]),
                     reads=[nm, c["ident"]], writes=[psT])
            S.op("act", lambda e, nmT=nmT, nb=nb: e.activation(
                out=nmT[:, 0:nb, :], in_=psT[:, 0:nb * 128].rearrange("p (b q) -> p b q", q=128), func=AF.Copy),
                 reads=[psT], writes=[nmT])
            for bi in range(nb):
                kb = cc + bi
                pss = rr(c, "psS", "S")
                pT = rr(c, "pT", "pT")
                first, last = (k == 0), (k == nkb - 1)
                S.op("pe", lambda e, kb=kb, pss=pss: e.matmul(
                    pss[:, :].rearrange("p (h q) -> p h q", h=4), lhsT=c["kTd"][:, kb * 128:(kb + 1) * 128],
                    rhs=q[:, :, mc], start=True, stop=False, skip_group_check=True), reads=[c["kTd"], q], writes=[pss])
                S.op("pe", lambda e, bi=bi, nmT=nmT, pss=pss: e.matmul(
                    pss[:, :].rearrange("p (h q) -> p h q", h=4), lhsT=c["ident"][:, :],
                    rhs=nmT[:, bi:bi + 1, :].broadcast_to([128, 4, 128]), start=False, stop=True, skip_group_check=True),
                     reads=[c["ident"], nmT], writes=[pss])
                S.op("act", lambda e, pss=pss, pT=pT: e.activation(out=pT[:, :], in_=pss[:, :], func=AF.Exp,
                                                                  scale=128 ** -0.5), reads=[pss], writes=[pT])
                S.op("pe", lambda e, kb=kb, pT=pT, first=first, last=last: e.matmul(
                    psO[:, :], lhsT=c["vd"][:, kb, :], rhs=pT[:, :], start=first, stop=last), reads=[c["vd"], pT],
                     writes=[psO])
                S.op("pe", lambda e, pT=pT, first=first, last=last: e.matmul(
                    psD[:, :], lhsT=cx.ones_bf[:, :], rhs=pT[:, :], start=first, stop=last), reads=[cx.ones_bf, pT],
                     writes=[psD])
                k += 1
        S.op("dve", lambda e: e.reciprocal(out=rden[:, :], in_=psD[:, :]), reads=[psD], writes=[rden])
        S.op("dve", lambda e, mc=mc: e.tensor_tensor(out=o[:, :, mc], in0=psO[:, :].rearrange("p (h q) -> p h q", h=4),
                                                     in1=rden[:, :].rearrange("p (h q) -> p h q", h=4), op=ALU.mult),
             reads=[psO, rden], writes=[o])


def dense_tail(cx, c, io, t, x_dst, last_layer):
    S = cx.S
    x, hT, mg, u, acc = c["xbuf"], c["hT"], c["mg"], c["u"], c["acc"]
    sp = c["sp"]
    for j2 in range(4):
        for i in range(4):
            wg = load_w(cx, io["wg"].ap, i * 2048 + j2 * 512, 512, wtok=io["wg"].t, key="wg")
            wb = load_w(cx, io["wbr"].ap, j2 * 512, 512, nk=4, row0=i * 512, wtok=io["wbr"].t, key="wbr")
            for j in range(4):
                psg = cx.next_ps()
                proj_fm(cx, wg, j * 128, 128, hT, psg)
                psb = cx.next_ps()
                proj_fm(cx, wb, j * 128, 128, c["o"][i], psb, nk=4)
                sg = sp[j // 2][:, (j % 2) * TT:(j % 2 + 1) * TT]
                sgt = sp[j // 2]
                bcol = i * 16 + j2 * 4 + j
                S.op("act", lambda e, psg=psg, sg=sg, bcol=bcol: e.activation(
                    out=sg, in_=psg[:, 0:TT], func=AF.Sigmoid, bias=c["bgate"][:, bcol:bcol + 1]),
                     reads=[psg, c["bgate"]], writes=[sgt])
                if i == 0:
                    S.op("dve", lambda e, psb=psb, sg=sg, j=j: e.tensor_tensor(out=acc[j][:, :], in0=psb[:, 0:TT],
                                                                              in1=sg, op=ALU.mult),
                         reads=[psb, sgt], writes=[acc[j]])
                else:
                    S.op("dve", lambda e, psb=psb, sg=sg: e.tensor_tensor(out=sg, in0=psb[:, 0:TT], in1=sg, op=ALU.mult),
                         reads=[psb, sgt], writes=[sgt])
                    if i < 3:
                        S.op("dve", lambda e, sg=sg, j=j: e.tensor_tensor(out=acc[j][:, :], in0=acc[j][:, :], in1=sg,
                                                                         op=ALU.add),
                             reads=[acc[j], sgt], writes=[acc[j]])
                    else:
                        jj = j2 * 4 + j
                        S.op("dve", lambda e, sg=sg, j=j, jj=jj: e.tensor_tensor(out=mg[:, jj, :], in0=acc[j][:, :],
                                                                                in1=sg, op=ALU.add),
                             reads=[acc[j], sgt], writes=[mg.s(jj)])
    for j2 in range(4):
        w = load_w(cx, io["wo"].ap, j2 * 512, 512, wtok=io["wo"].t, key="wo")
        for j in range(4):
            jj = j2 * 4 + j
            ps = cx.next_ps()
            proj_fm(cx, w, j * 128, 128, mg, ps)
            S.op("dve", lambda e, ps=ps, jj=jj: e.tensor_tensor(out=x[:, jj, :], in0=ps[:, 0:TT], in1=x[:, jj, :],
                                                                op=ALU.add), reads=[ps, x], writes=[x])
    rmsnorm_fm(cx, x, c["n2g"], hT, c["psm"])
    for half in range(2):
        for j2 in range(8):
            w = load_w(cx, io["wf1"].ap, half * 4096 + j2 * 512, 512, wtok=io["wf1"].t, key="wf1")
            for j in range(4):
                jj = j2 * 4 + j
                ps = cx.next_ps()
                proj_fm(cx, w, j * 128, 128, hT, ps)
                r_ = sp[j // 2][:, (j % 2) * TT:(j % 2 + 1) * TT]
                rt_ = sp[j // 2]
                S.op("act", lambda e, ps=ps, r_=r_: e.activation(out=r_, in_=ps[:, 0:TT], func=AF.Relu),
                     reads=[ps], writes=[rt_])
                S.op("dve", lambda e, r_=r_, jj=jj, ps=ps: e.tensor_tensor(out=u[:, jj, :], in0=ps[:, 0:TT], in1=r_,
                                                                          op=ALU.mult),
                     reads=[rt_, ps], writes=[u.s(jj)])
        for j2 in range(4):
            for kk in range(2):
                w = load_w(cx, io["wf2"].ap, j2 * 512, 512, nk=16, row0=half * 4096 + kk * 2048, wtok=io["wf2"].t,
                           key="wf2")
                for j in range(4):
                    jj = j2 * 4 + j
                    ps = cx.next_ps()
                    for kc in range(16):
                        kidx = kk * 16 + kc
                        S.op("pe", lambda e, kc=kc, kidx=kidx, ps=ps, j=j, w=w: e.matmul(
                            ps[:, 0:TT], lhsT=w[:, kc, j * 128:(j + 1) * 128], rhs=u[:, kidx, :],
                            start=(kc == 0), stop=(kc == 15)), reads=[w, u.s(kidx)], writes=[ps])
                    S.op("dve", lambda e, ps=ps, jj=jj: e.tensor_tensor(out=x[:, jj, :], in0=ps[:, 0:TT], in1=x[:, jj, :],
                                                                        op=ALU.add), reads=[ps, x], writes=[x])
    tc0 = t * TT
    if not last_layer:
        S.dma("sp", out=x_dst.ap[:, tc0:tc0 + TT].rearrange("(k p) n -> p k n", p=128), in_=x[:, :, :], reads=[x],
              writes=[x_dst])
    else:
        ps = c["psm"]
        rstd = cx.rstd
        for kc in range(KC):
            sq = cx.sqb[kc % 2]
            S.op("act", lambda e, kc=kc, sq=sq: e.activation(out=sq[:, :], in_=x[:, kc, :], func=AF.Square),
                 reads=[x], writes=[sq])
            S.op("pe", lambda e, kc=kc, sq=sq: e.matmul(ps[:, 0:TT], lhsT=cx.ones_bf[:, :], rhs=sq[:, :],
                                                        start=(kc == 0), stop=(kc == KC - 1)),
                 reads=[sq, cx.ones_bf], writes=[ps])
        S.op("act", lambda e: e.activation(out=rstd[:, :], in_=ps[:, 0:TT], func=AF.Sqrt, bias=EPS,
                                           scale=1.0 / D_MODEL), reads=[ps], writes=[rstd])
        S.op("dve", lambda e: e.reciprocal(out=rstd[:, :], in_=rstd[:, :]), reads=[rstd], writes=[rstd])
        for kc in range(KC):
            S.op("dve", lambda e, kc=kc: e.scalar_tensor_tensor(out=x[:, kc, :], in0=x[:, kc, :],
                                                                scalar=c["fg"][:, kc:kc + 1], op0=ALU.mult,
                                                                in1=rstd[:, :], op1=ALU.mult),
                 reads=[x, c["fg"], rstd], writes=[x])
        S.dma("sp", out=x_dst.ap[:, tc0:tc0 + TT].rearrange("(k p) n -> p k n", p=128), in_=x[:, :, :], reads=[x],
              writes=[x_dst])


def phase_q(cx, c, io, x_src, x_dst, lambda_init, last_layer, dbg=None):
    S = cx.S
    x, hT, psm = c["xbuf"], c["hT"], c["psm"]
    for i in range(2):
        S.op("dve", lambda e, i=i: e.memset(c["qz"][i][:, :, :], 0.0), writes=[c["qz"][i]])
    for t in range(NT):
        cx.first_tile = (t == 0)
        load_x_tile(cx, c, x_src, t, io["ropet"])
        rmsnorm_fm(cx, x, c["n1g"], hT, psm)
        for m in range(2):
            for kc in range(KC):
                S.op("pe", lambda e, m=m, kc=kc: e.matmul(psm[:, m * 8:(m + 1) * 8], lhsT=hT[:, kc, m * 128:(m + 1) * 128],
                                                          rhs=c["wiw"][:, kc, :], start=(kc == 0), stop=(kc == KC - 1)),
                     reads=[hT.s(kc), c["wiw"]], writes=[psm])
        S.op("act", lambda e: e.mul(out=c["iw"][:, :], in_=psm[:, 0:16], mul=IWS), reads=[psm], writes=[c["iw"]])
        q_proj(cx, c, io, 0, c["q"], "plain", scale=128 ** -0.5)
        attn_A(cx, c, io, t)
        q_proj(cx, c, io, 512, None, "rope64z")
        attn_BC(cx, c, io, t, "B")
        q_proj(cx, c, io, 1024, c["q"], "plain", scale=128 ** -0.5)
        for h in range(4):
            S.op("pe", lambda e, h=h, t=t: e.matmul(psm[:, 0:TT], lhsT=c["selh"][0:4, h * 128:(h + 1) * 128],
                                                    rhs=c["cum_mine"][0:4, t * TT:(t + 1) * TT], start=True, stop=True),
                 reads=[c["selh"], c["cum_mine"]], writes=[psm])
            S.op("act", lambda e, h=h: e.activation(out=c["cumq"][:, h, :], in_=psm[:, 0:TT], func=AF.Copy),
                 reads=[psm], writes=[c["cumq"]])
        attn_BC(cx, c, io, t, "C")
        q_proj(cx, c, io, 1536, c["q"], "rope128")
        q_proj(cx, c, io, 2048, c["iq"], "rope64")
        attn_D(cx, c, io, t)
        if dbg is not None:
            for i in range(4):
                S.dma("sp", out=dbg[i].ap[:, t * TT:(t + 1) * TT].rearrange("(h p) n -> p h n", p=128),
                      in_=c["o"][i][:, :, :], reads=[c["o"][i]], writes=[dbg[i]])
        dense_tail(cx, c, io, t, x_dst, last_layer)


Q_IN = (("wq", [D_MODEL, 2560]), ("wiw", [D_MODEL, 8]), ("wg", [D_MODEL, 8192]), ("wbr", [2048, D_MODEL]),
        ("wo", [D_MODEL, D_MODEL]), ("wf1", [D_MODEL, D_FF]), ("wf2", [D_FF, D_MODEL]),
        ("ropet", [4, 128, LTOK]), ("biasA", [128, 6 * 512]), ("mk", [128, 4 * 128]), ("identf", [128, 128]),
        ("idxmask", [128, 256]), ("selh", [4, 512]), ("sel", [4, 2]), ("n1g", [128, KC]), ("n2g", [128, KC]),
        ("fg", [128, KC]), ("bgate", [128, 64]), ("dng", [128, 1]), ("lam4", [64, 4]))


def build_B(layer, dbg=False):
    lambda_init = 0.8 - 0.6 * math.exp(-0.3 * layer)
    nc = bass.Bass("TRN2", target_bir_lowering=False)
    es = ExitStack()
    io = {}
    xT = dram_in(nc, "xT", [D_MODEL, LTOK])
    for nm, shp in Q_IN:
        io[nm] = dram_in(nc, nm, shp)
    for nm, shp, dt in KV_ALL:
        io[nm + "_all"] = dram_in(nc, nm + "_all", shp, dt)
    xo = dram_out(nc, "xo", [D_MODEL, LTOK])
    dbgs = [dram_out(nc, f"dbg_o{i}", [512, LTOK], BF16) for i in range(4)] if dbg else None
    with es:
        S = Sched(nc, es)
        cx = Ctx(S)
        c = alloc_q(cx, alloc_common(cx))
        S.dma("sp", out=c["n1g"][:, :], in_=io["n1g"].ap[:, :], reads=[io["n1g"]], writes=[c["n1g"]])
        setup_q(cx, c, io, lambda_init)
        phase_q(cx, c, io, lambda t: (xT.ap[:, t * TT:(t + 1) * TT], xT.t), xo, lambda_init, layer == DEPTH - 1, dbgs)
        S.final_all()
        S.emit()
    return nc


def table_biasA(rel_bias_l, c):
    out = np.full((128, 6, 4, 128), NEG, np.float32)
    j = np.arange(128)[:, None]
    qi = np.arange(128)[None, :]
    for r in range(2):
        for nrel in range(3):
            dblk = (2 * (nrel - 2) + r) - c
            dist = -dblk * 128 + qi - j
            kch = 2 * dblk + j // 64
            qch = qi // 64
            valid = (kch <= qch) & (kch >= qch - 8)
            idx = np.clip(dist, -128, 128) + 128
            for h in range(4):
                vals = rel_bias_l[h][idx]
                out[:, r * 3 + nrel, h, :] = np.where(valid, vals, np.float32(NEG))
    return np.ascontiguousarray(out.reshape(128, 6 * 512))


def table_mk(c):
    out = np.zeros((128, 4, 128), np.float32)
    j = np.arange(128)[:, None]
    qi = np.arange(128)[None, :]
    diagB = np.where(j // 64 <= qi // 64, 0.0, NEG).astype(np.float32)
    diagC = np.where(j <= qi, 0.0, NEG).astype(np.float32)
    for r in range(2):
        if r < c:
            mB = mC = np.zeros((128, 128), np.float32)
        elif r == c:
            mB, mC = diagB, diagC
        else:
            mB = mC = np.full((128, 128), NEG, np.float32)
        out[:, r, :] = mB
        out[:, 2 + r, :] = mC
    return np.ascontiguousarray(out.reshape(128, 512))


def table_idxmask(c):
    out = np.zeros((128, 2, 128), np.float32)
    qi = np.arange(128)[:, None]
    j = np.arange(128)[None, :]
    for r in range(2):
        if r < c:
            pass
        elif r == c:
            out[:, r, :] = np.where(j // 64 <= qi // 64, 0.0, -1.0e30)
        else:
            out[:, r, :] = -1.0e30
    return np.ascontiguousarray(out.reshape(128, 256))


def host_q_inputs(inp, l, c):
    w_in = inp["w_in"][l]
    m = {}
    m["wq"] = wcols(w_in, ["a_q", "b_q", "c_q", "d_q", "d_iq"])
    m["wiw"] = wcols(w_in, ["d_iw"])
    m["wg"] = wcols(w_in, ["gate"])
    m["wbr"] = np.ascontiguousarray(inp["w_branch"][l].reshape(4 * 512, D_MODEL))
    m["wo"] = np.ascontiguousarray(inp["w_out"][l])
    m["wf1"] = np.ascontiguousarray(inp["w_ff1"][l])
    m["wf2"] = np.ascontiguousarray(inp["w_ff2"][l])
    m["ropet"] = rope_tabs(c)
    m["biasA"] = table_biasA(inp["rel_bias"][l], c)
    m["mk"] = table_mk(c)
    m["identf"] = np.eye(128, dtype=np.float32)
    m["idxmask"] = table_idxmask(c)
    selh = np.zeros((4, 4, 128), np.float32)
    for h in range(4):
        selh[h, h, :] = 1.0
    m["selh"] = selh.reshape(4, 512)
    sel = np.zeros((4, 2), np.float32)
    sel[:, c] = 1.0
    m["sel"] = sel
    m["n1g"] = pcol(inp["norm1_g"][l])
    m["n2g"] = pcol(inp["norm2_g"][l])
    m["fg"] = pcol(inp["final_g"])
    m["bgate"] = pcol(inp["b_gate"][l])
    m["dng"] = np.ascontiguousarray(inp["diff_norm_g"][l].reshape(128, 1))
    m["lam4"] = np.ascontiguousarray(np.stack([inp["lambda_q1"][l], inp["lambda_k1"][l], inp["lambda_q2"][l],
                                               inp["lambda_k2"][l]], axis=1))
    return m


def host_kv_inputs(inp, l, c):
    w_in = inp["w_in"][l]
    return {"wk": wcols(w_in, ["a_k", "b_k", "c_k", "d_k", "d_ik", "c_f"]),
            "wv": wcols(w_in, ["a_v", "b_v", "c_v", "d_v"]),
            "ropet": rope_tabs(c), "n1g": pcol(inp["norm1_g"][l]),
            "bfg": np.ascontiguousarray(inp["b_forget"][l].reshape(4, 1))}


I32 = mybir.dt.int32
PK = {"kTa": 0, "kTb": 512, "kTc": 1024, "kTd": 1536, "ikT": 1664, "logf": 1728, "va": 1792, "vb": 2304,
      "vc": 2816, "vd": 3328}
RP = 3456
NSEL = 2 * RP // 128


def pack_views(ap2d, base):
    v = {}
    for nm, rows in (("kTa", 512), ("kTb", 512), ("kTc", 512), ("kTd", 128), ("ikT", 64)):
        v[nm] = ap2d[base + PK[nm]:base + PK[nm] + rows, :]
    v["logf"] = ap2d[base + PK["logf"]:base + PK["logf"] + 8, :].bitcast(F32).rearrange("(h a) n -> h (a n)", a=2)
    for nm in ("va", "vb", "vc"):
        v[nm] = ap2d[base + PK[nm]:base + PK[nm] + 512, :].rearrange("r (a f) -> (r a) f", f=512)
    v["vd"] = ap2d[base + PK["vd"]:base + PK["vd"] + 128, :].rearrange("r (a f) -> (r a) f", f=128)
    return v


A_IN = (("wk", [D_MODEL, WK_COLS]), ("wv", [D_MODEL, WV_COLS]), ("bfg", [4, 1]))


def build_fused():
    nc = bass.Bass("TRN2", target_bir_lowering=False)
    es = ExitStack()
    xT = dram_in(nc, "xT", [D_MODEL, LTOK])
    pidx_d = dram_in(nc, "pidx", [128, NSEL], I32)
    ios = []
    for l in range(DEPTH):
        io = {}
        for nm, shp in A_IN + Q_IN:
            if nm in ("ropet", "identf", "mk", "idxmask", "selh", "sel", "fg"):
                continue
            io[nm] = dram_in(nc, f"{nm}{l}", shp)
        ios.append(io)
    shared = {nm: dram_in(nc, nm, dict(Q_IN)[nm]) for nm in ("ropet", "identf", "mk", "idxmask", "selh", "sel", "fg")}
    xo = dram_out(nc, "xo", [D_MODEL, LTOK])
    kvpack_h = nc.dram_tensor("kvpack", [RP, LTOK], BF16)
    kvg_h = nc.dram_tensor("kvg", [N_CORES * RP, LTOK], BF16)
    kvall_h = nc.dram_tensor("kvall", [2 * RP, LTOK], BF16)
    xs1_h = nc.dram_tensor("xs1", [D_MODEL, LTOK], F32)
    NSLOT = 160
    wscr_h = [nc.dram_tensor(f"wscr{l}", [NSLOT, 128, KC * WB], BF16) for l in range(DEPTH)] if USE_WSCR else []
    t_pack, t_g, t_all = Tok(dj=True), Tok(dj=True), Tok(dj=True)
    xs1 = DT(xs1_h.ap(), dj=True)
    pv = pack_views(kvpack_h.ap(), 0)
    av = [pack_views(kvall_h.ap(), r * RP) for r in range(2)]
    with es:
        S = Sched(nc, es)
        cx = Ctx(S)
        c = alloc_q(cx, alloc_kv(cx, alloc_common(cx)))
        c["pidx"] = S.sb("pidx", [128, NSEL], I32)
        cx.psd = cx.psd + [c["psS"][0], c["psS"][1], c["psO"], c["psD"]]
        cx.wscr = [h.ap() for h in wscr_h] if USE_WSCR else None
        S.dma("sp", out=c["pidx"][:, :], in_=pidx_d.ap[:, :], writes=[c["pidx"]])
        for l in range(DEPTH):
            lambda_init = 0.8 - 0.6 * math.exp(-0.3 * l)
            cx.layer = l
            io = dict(ios[l])
            io.update(shared)
            for nm in pv:
                io[nm] = DT(pv[nm], tok=t_pack)
                io[nm + "_all"] = DT([av[0][nm], av[1][nm]], tok=t_all)
            if l == 0:
                x_src = lambda t: (xT.ap[:, t * TT:(t + 1) * TT], xT.t)
                x_dst = xs1
            else:
                x_src = lambda t: (xs1.ap[:, t * TT:(t + 1) * TT], xs1.t)
                x_dst = xo
            S.dma("sp", out=c["n1g"][:, :], in_=io["n1g"].ap[:, :], reads=[io["n1g"]], writes=[c["n1g"]])
            S.dma("sp", out=c["nbfg"][64:68, :], in_=io["bfg"].ap[:, :], reads=[io["bfg"]], writes=[c["nbfg"]])
            S.op("dve", lambda e: e.tensor_scalar(out=c["nbfg"][64:68, :], in0=c["nbfg"][64:68, :], scalar1=-1.0,
                                                  scalar2=None, op0=ALU.mult), reads=[c["nbfg"]], writes=[c["nbfg"]])
            phase_kv(cx, io, x_src, c)
            S.collective_allgather(kvpack_h.ap().opt(), kvg_h.ap().opt(), reads=[t_pack], writes=[t_g])
            u = c["u"]
            for i in range(NSEL):
                k = i % 4
                toks = [u.s(8 * k + j) for j in range(8)]
                stv = u[:, 8 * k:8 * k + 8, :].rearrange("p k n -> p (k n)")
                S.idma(out=stv, in_=kvg_h.ap()[:, :], idx_ap=c["pidx"][:, i:i + 1], reads=[t_g, c["pidx"]],
                       writes=toks, semtile=toks[0])
                S.dma("sp", out=kvall_h.ap()[i * 128:(i + 1) * 128, :], in_=stv, reads=toks, writes=[t_all],
                      semtile=toks[0])
            setup_q(cx, c, io, lambda_init)
            phase_q(cx, c, io, x_src, x_dst, lambda_init, l == DEPTH - 1, None)
        S.final_all()
        print("ops per engine:", {k: len(v) for k, v in S.ops.items()})
        S.emit()
    return nc


def pair_index(core):
    b = core // 2
    idx = np.zeros((128, NSEL), np.int32)
    nb = RP // 128
    for i in range(NSEL):
        idx[:, i] = (2 * b + i // nb) * RP + (i % nb) * 128 + np.arange(128)
    return idx


def kernel_fused(**inputs):
    inp = {k: np.asarray(v) for k, v in inputs.items()}
    x = inp["x"]
    cores = list(range(N_CORES))
    nc = build_fused()
    per_layer = []
    for l in range(DEPTH):
        d = {}
        for c in range(2):
            m = host_q_inputs(inp, l, c)
            m.update({k: v for k, v in host_kv_inputs(inp, l, c).items() if k in ("wk", "wv", "bfg")})
            d[c] = m
        per_layer.append(d)
    maps = []
    for core in cores:
        b, c = core // 2, core % 2
        m = {"xT": to_local_T(x[b], c), "pidx": pair_index(core)}
        for l in range(DEPTH):
            src = per_layer[l][c]
            for nm, _ in A_IN + Q_IN:
                if nm in ("ropet", "identf", "mk", "idxmask", "selh", "sel", "fg"):
                    m[nm] = per_layer[0][c][nm]
                else:
                    m[f"{nm}{l}"] = per_layer[l][0][nm] if nm not in ("biasA",) else src[nm]
        maps.append(m)
    res = run_bass_kernel_spmd(nc, maps, core_ids=cores)
    xo = [np.asarray(res.results[core]["xo"]) for core in cores]
    out = np.stack([_from_local(xo[2 * b], xo[2 * b + 1]) for b in range(BATCH)], 0)
    return out.astype(np.float32)


def _from_local(loc0, loc1):
    out = np.empty((SEQ, D_MODEL), np.float32)
    o3 = out.reshape(NBLK, 128, D_MODEL)
    o3[0::2] = np.asarray(loc0).T.reshape(LBLK, 128, D_MODEL)
    o3[1::2] = np.asarray(loc1).T.reshape(LBLK, 128, D_MODEL)
    return out


def kernel_unfused(**inputs):
    inp = {k: np.asarray(v) for k, v in inputs.items()}
    x = inp["x"]
    cores = list(range(N_CORES))
    xT = [to_local_T(x[core // 2], core % 2) for core in cores]
    for l in range(DEPTH):
        ncA = build_A()
        mapsA = []
        for core in cores:
            m = host_kv_inputs(inp, l, core % 2)
            m["xT"] = xT[core]
            mapsA.append(m)
        resA = run_bass_kernel_spmd(ncA, mapsA, core_ids=cores)
        del mapsA
        ncB = build_B(l)
        mapsB = []
        for core in cores:
            b, c = core // 2, core % 2
            m = host_q_inputs(inp, l, c)
            m["xT"] = xT[core]
            for nm, shp, dt in KV_ALL:
                m[nm + "_all"] = np.ascontiguousarray(
                    np.stack([resA.results[2 * b][nm], resA.results[2 * b + 1][nm]], 0))
            mapsB.append(m)
        resB = run_bass_kernel_spmd(ncB, mapsB, core_ids=cores)
        del mapsB
        xT = [np.asarray(resB.results[core]["xo"]) for core in cores]
    out = np.stack([_from_local(xT[2 * b], xT[2 * b + 1]) for b in range(BATCH)], 0)
    return out.astype(np.float32)


def kernel(**inputs):
    return kernel_fused(**inputs)


def _sim_sched(S):
    sem = {}
    pos = {e: 0 for e in ENGS}
    total = sum(len(v) for v in S.ops.values())
    done = 0
    while done < total:
        progressed = False
        for e in ENGS:
            ops = S.ops[e]
            while pos[e] < len(ops):
                waits, fn, inc = ops[pos[e]]
                if all(sem.get(k, 0) >= v for k, v in waits):
                    sem[inc[0]] = sem.get(inc[0], 0) + (1 if inc[1] is None else inc[1])
                    pos[e] += 1
                    done += 1
                    progressed = True
                else:
                    break
        if not progressed:
            for e in ENGS:
                if pos[e] < len(S.ops[e]):
                    waits, fn, inc = S.ops[e][pos[e]]
                    bad = [(k, v, sem.get(k, 0)) for k, v in waits if sem.get(k, 0) < v]
                    print("STUCK", e, pos[e], "/", len(S.ops[e]), "unsatisfied (key, need, have):", bad)
            return False
    return True
```

```python
import math
from contextlib import ExitStack

import numpy as np
import concourse.bass as bass
import concourse.mybir as mybir
from concourse.bass_utils import run_bass_kernel_spmd

F32 = mybir.dt.float32
BF16 = mybir.dt.bfloat16
AF = mybir.ActivationFunctionType
ALU = mybir.AluOpType
AX = mybir.AxisListType

D_MODEL = 2048
BATCH = 4
SEQ = 4096
DEPTH = 2
KC = D_MODEL // 128
NBLK = SEQ // 128
LBLK = NBLK // 2
LTOK = LBLK * 128
TT = 256
NT = LTOK // TT
D_FF = 4 * D_MODEL
EPS = 1e-6
NEG = -30000.0
N_CORES = 8


class Tok:
    __slots__ = ("w", "r", "parent", "subs", "dj")

    def __init__(self, parent=None, dj=False):
        self.w = {}
        self.r = {}
        self.parent = parent
        self.subs = []
        self.dj = dj


class Tile:
    def __init__(self, ap, name):
        self.ap = ap
        self.name = name
        self.t = Tok()
        self._subs = {}

    def s(self, key):
        if key not in self._subs:
            tk = Tok(parent=self.t)
            self.t.subs.append(tk)
            self._subs[key] = tk
        return self._subs[key]

    def __getitem__(self, idx):
        return self.ap[idx]


ENGS = ("pe", "act", "dve", "pool", "sp")


class _Rec:
    def __init__(self):
        self.call = None

    def __getattr__(self, name):
        def f(*a, **kw):
            self.call = (name, a, kw)
            return None
        return f


class Sched:
    def __init__(self, nc, es, n_dma_sems=56):
        self.nc = nc
        self.es = es
        self.ops = {e: [] for e in ENGS}
        self.cnt = {e: 0 for e in ENGS}
        self.sems = {}
        for e in ENGS + ("cc",):
            self.sems[e] = es.enter_context(nc.semaphore("sem_" + e))
        self.cc_cnt = 0
        self.dma_sems = [es.enter_context(nc.semaphore(f"dsem{i}")) for i in range(n_dma_sems)]
        self.dma_cnt = [0] * n_dma_sems
        self.dma_rr = 0
        self.tile_sem = {}
        self.seen = {e: {} for e in ENGS}
        self.final_waits = []
        self.ntiles = 0

    def sb(self, name, shape, dtype):
        self.ntiles += 1
        t = self.es.enter_context(self.nc.sbuf_tensor(f"{name}_{self.ntiles}", list(shape), dtype))
        return Tile(t, name)

    def ps(self, name, shape, dtype=F32):
        self.ntiles += 1
        t = self.es.enter_context(self.nc.psum_tensor(f"{name}_{self.ntiles}", list(shape), dtype))
        return Tile(t, name)

    @staticmethod
    def _toks(x):
        out = []
        for i in x:
            out.append(i if isinstance(i, Tok) else i.t)
        return out

    def _collect(self, reads, writes):
        ev = {}

        def add_r(d):
            for k, v in d.items():
                if ev.get(k, 0) < v:
                    ev[k] = v

        add = add_r

        for t in reads:
            add(t.w)
            if t.parent is not None:
                add(t.parent.w)
            for s_ in t.subs:
                add(s_.w)
        for t in writes:
            if not t.dj:
                add(t.w)
            add_r(t.r)
            if t.parent is not None:
                add(t.parent.w)
                add_r(t.parent.r)
            for s_ in t.subs:
                add(s_.w)
                add_r(s_.r)
        return ev

    def _update(self, reads, writes, me):
        k, v = me
        for t in reads:
            if t.r.get(k, 0) < v:
                t.r[k] = v
        for t in writes:
            if t.dj:
                if t.w.get(k, 0) < v:
                    t.w[k] = v
                continue
            t.w = {k: v}
            t.r = {}
            for s_ in t.subs:
                s_.w = {}
                s_.r = {}

    def _waits(self, eng, ev):
        seen = self.seen[eng]
        waits = []
        for k, v in ev.items():
            if eng == "pe" and k == "pe":
                continue
            if seen.get(k, 0) >= v:
                continue
            seen[k] = v
            waits.append((k, v))
        return waits

    def _semof(self, k):
        return self.sems[k] if isinstance(k, str) else self.dma_sems[k]

    def op(self, eng, fn, reads=(), writes=()):
        reads = self._toks(reads)
        writes = self._toks(writes)
        ev = self._collect(reads, writes)
        waits = self._waits(eng, ev)
        self.cnt[eng] += 1
        me = (eng, self.cnt[eng])
        rec = _Rec()
        fn(rec)
        name, a, kw = rec.call

        def fn2(e, name=name, a=a, kw=kw):
            return getattr(e, name)(*a, **kw)

        self.ops[eng].append((waits, fn2, (eng, 1)))
        self._update(reads, writes, me)

    def collective_allgather(self, in_ap, out_ap, reads=(), writes=()):
        reads = self._toks(reads)
        writes = self._toks(writes)
        ev = self._collect(reads, writes)
        waits = self._waits("pool", ev)
        self.cc_cnt += 1
        me = ("cc", self.cc_cnt)

        def fn(e):
            return e.collective_compute("AllGather", ALU.bypass, replica_groups=[list(range(N_CORES))],
                                        ins=[in_ap], outs=[out_ap])

        self.ops["pool"].append((waits, fn, ("cc", None)))
        self._update(reads, writes, me)

    def idma(self, out, in_, idx_ap, reads=(), writes=(), semtile=None):
        def fn(e):
            return e.indirect_dma_start(out=out, out_offset=None, in_=in_,
                                        in_offset=bass.IndirectOffsetOnAxis(ap=idx_ap, axis=0))
        return self.dma("pool", None, None, reads=reads, writes=writes, semtile=semtile, fn=fn)

    def dma(self, queue, out, in_, reads=(), writes=(), semtile=None, fn=None, **kw):
        reads = self._toks(reads)
        writes = self._toks(writes)
        ev = self._collect(reads, writes)
        key = semtile if semtile is not None else (writes[0] if writes else reads[0])
        if key not in self.tile_sem:
            self.tile_sem[key] = self.dma_rr % len(self.dma_sems)
            self.dma_rr += 1
        si = self.tile_sem[key]
        if self.dma_cnt[si] > 0:
            pv = self.dma_cnt[si]
            if ev.get(si, 0) < pv:
                ev[si] = pv
        waits = self._waits(queue, ev)
        self.dma_cnt[si] += 16
        me = (si, self.dma_cnt[si])

        if fn is None:
            def fn(e, out=out, in_=in_, kw=kw):
                return e.dma_start(out=out, in_=in_, **kw)

        self.ops[queue].append((waits, fn, (si, 16)))
        self._update(reads, writes, me)
        return me

    def final_all(self):
        for si, v in enumerate(self.dma_cnt):
            if v > 0:
                self.final_waits.append((si, v))

    def emit(self):
        nc = self.nc
        block = self.es.enter_context(nc.Block())

        def replay(eng_name, e, extra=None):
            for waits, fn, inc in self.ops[eng_name]:
                for k, v in waits:
                    e.wait_ge(self._semof(k), v)
                ins = fn(e)
                if inc[1] is None:
                    ins.then_inc(self._semof(inc[0]))
                else:
                    ins.then_inc(self._semof(inc[0]), inc[1])
            if extra:
                for k, v in extra:
                    e.wait_ge(self._semof(k), v)

        @block.sync
        def _(e):
            replay("sp", e, self.final_waits)

        @block.tensor
        def _(e):
            replay("pe", e)

        @block.scalar
        def _(e):
            replay("act", e)

        @block.vector
        def _(e):
            replay("dve", e)

        @block.gpsimd
        def _(e):
            replay("pool", e)


WB = 512
USE_WSCR = False


class Ctx:
    def __init__(self, S):
        self.S = S
        self.ones_bf = S.sb("ones_bf", [128, 128], BF16)
        S.op("dve", lambda e: e.memset(self.ones_bf[:], 1.0), writes=[self.ones_bf])
        self.wscr = None
        self.wslots = {}
        self.wtoks = {}
        self.first_tile = True
        self.layer = 0
        self.wbuf = [S.sb(f"wbuf{i}", [128, KC, WB], BF16) for i in range(2)]
        self.wrr = 0
        self.wsm = [S.sb(f"wsm{i}", [128, 4, WB], BF16) for i in range(2)]
        self.wsrr = 0
        self.psd = [S.ps(f"psd{i}", [128, 512], F32) for i in range(2)]
        self.psrr = 0
        self.evrr = 0
        self.sqb = [S.sb(f"sqb{i}", [128, TT], BF16) for i in range(2)]
        self.rstd = S.sb("rstd", [128, TT], F32)
        self.fence_t = S.sb("fence", [128, 8], F32)

    def next_w(self, small=False):
        if small:
            w = self.wsm[self.wsrr % len(self.wsm)]
            self.wsrr += 1
            return w
        w = self.wbuf[self.wrr % len(self.wbuf)]
        self.wrr += 1
        return w

    def next_ps(self):
        p = self.psd[self.psrr % len(self.psd)]
        self.psrr += 1
        return p

    def ev_eng(self):
        self.evrr += 1
        return "act" if self.evrr % 2 else "dve"

    def fence(self, tiles):
        self.S.op("pool", lambda e: e.memset(self.fence_t[:, 0:1], 0.0), writes=[self.fence_t] + list(tiles))


def load_w(cx, wdram, col0, ncols, nk=KC, row0=0, wtok=None, key=None):
    S = cx.S
    w = cx.next_w(small=(nk <= 4))
    src = wdram[row0:row0 + nk * 128, col0:col0 + ncols].rearrange("(k p) n -> p k n", p=128)
    if cx.wscr is None or key is None:
        S.dma("pool", out=w[:, 0:nk, 0:ncols], in_=src, reads=([wtok] if wtok is not None else []), writes=[w])
        return w
    k = (cx.layer, key, col0, row0)
    if k not in cx.wslots:
        cx.wslots[k] = len([1 for kk in cx.wslots if kk[0] == cx.layer])
        cx.wtoks[k] = Tok()
    slot = cx.wslots[k]
    sv = cx.wscr[cx.layer][slot][:, 0:nk * ncols].rearrange("p (k n) -> p k n", n=ncols)
    if cx.first_tile:
        S.dma("pool", out=w[:, 0:nk, 0:ncols], in_=src, reads=([wtok] if wtok is not None else []), writes=[w])
        S.dma("sp", out=sv, in_=w[:, 0:nk, 0:ncols], reads=[w], writes=[cx.wtoks[k]], semtile=w.t)
    else:
        S.dma("sp", out=w[:, 0:nk, 0:ncols], in_=sv, reads=[cx.wtoks[k]], writes=[w])
    return w


def evac(cx, out_ap, in_ps, reads, writes, scale=None, eng=None):
    S = cx.S
    eng = eng or cx.ev_eng()
    if eng == "act":
        if scale is None:
            S.op("act", lambda e: e.activation(out=out_ap, in_=in_ps, func=AF.Copy), reads=reads, writes=writes)
        else:
            S.op("act", lambda e: e.activation(out=out_ap, in_=in_ps, func=AF.Copy, scale=float(scale)),
                 reads=reads, writes=writes)
    else:
        if scale is None:
            S.op("dve", lambda e: e.tensor_copy(out=out_ap, in_=in_ps), reads=reads, writes=writes)
        else:
            S.op("dve", lambda e: e.tensor_scalar(out=out_ap, in0=in_ps, scalar1=float(scale), scalar2=None,
                                                  op0=ALU.mult), reads=reads, writes=writes)


def rmsnorm_fm(cx, x, g, hT, ps):
    S = cx.S
    rstd = cx.rstd
    for kc in range(KC):
        sq = cx.sqb[kc % 2]
        S.op("act", lambda e, kc=kc, sq=sq: e.activation(out=sq[:, :], in_=x[:, kc, :], func=AF.Square),
             reads=[x], writes=[sq])
        S.op("pe", lambda e, kc=kc, sq=sq: e.matmul(ps[:, 0:TT], lhsT=cx.ones_bf[:, :], rhs=sq[:, :],
                                                    start=(kc == 0), stop=(kc == KC - 1)),
             reads=[sq, cx.ones_bf], writes=[ps])
    S.op("act", lambda e: e.activation(out=rstd[:, :], in_=ps[:, 0:TT], func=AF.Sqrt, bias=EPS, scale=1.0 / D_MODEL),
         reads=[ps], writes=[rstd])
    S.op("dve", lambda e: e.reciprocal(out=rstd[:, :], in_=rstd[:, :]), reads=[rstd], writes=[rstd])
    for kc in range(KC):
        S.op("dve", lambda e, kc=kc: e.scalar_tensor_tensor(out=hT[:, kc, :], in0=x[:, kc, :], scalar=g[:, kc:kc + 1],
                                                            op0=ALU.mult, in1=rstd[:, :], op1=ALU.mult),
             reads=[x, g, rstd], writes=[hT.s(kc)])


def proj_fm(cx, w, c0, M, hT, ps, nk=KC, ncol=TT, kofs=0):
    S = cx.S
    for kc in range(nk):
        S.op("pe", lambda e, kc=kc: e.matmul(ps[0:M, 0:ncol], lhsT=w[:, kc, c0:c0 + M], rhs=hT[:, kofs + kc, :],
                                             start=(kc == 0), stop=(kc == nk - 1)),
             reads=[w, hT.s(kofs + kc)], writes=[ps])


def rope_fm(cx, ps, P, half, ctab, stab, out_ap, out_toks, tmpA, tmpB):
    S = cx.S
    S.op("dve", lambda e: e.tensor_tensor(out=tmpA[0:P, :], in0=ps[0:P, 0:TT], in1=ctab[0:P, :], op=ALU.mult),
         reads=[ps] + ctab.toks, writes=[tmpA])
    for g in range(P // (2 * half)):
        b = g * 2 * half
        S.op("dve", lambda e, b=b: e.tensor_tensor(out=tmpB[b:b + half, :], in0=ps[b + half:b + 2 * half, 0:TT],
                                                   in1=stab[b + half:b + 2 * half, :], op=ALU.mult),
             reads=[ps] + stab.toks, writes=[tmpB])
        S.op("dve", lambda e, b=b: e.tensor_tensor(out=tmpB[b + half:b + 2 * half, :], in0=ps[b:b + half, 0:TT],
                                                   in1=stab[b:b + half, :], op=ALU.mult),
             reads=[ps] + stab.toks, writes=[tmpB])
    if isinstance(out_ap, list):
        for (r0, r1, oap, otk) in out_ap:
            S.op("dve", lambda e, r0=r0, r1=r1, oap=oap: e.tensor_tensor(out=oap, in0=tmpA[r0:r1, :], in1=tmpB[r0:r1, :],
                                                                      op=ALU.add), reads=[tmpA, tmpB], writes=otk)
    else:
        S.op("dve", lambda e: e.tensor_tensor(out=out_ap, in0=tmpA[0:P, :], in1=tmpB[0:P, :], op=ALU.add),
             reads=[tmpA, tmpB], writes=out_toks)


class View:
    def __init__(self, tile, idx, toks=None):
        self.tile = tile
        self.idx = idx
        self.toks = toks if toks is not None else [tile.t]

    def __getitem__(self, sl):
        rows, cols = sl
        return self.tile.ap[(rows,) + tuple(self.idx) + (cols,)] if isinstance(self.idx, tuple) else \
            self.tile.ap[rows, self.idx, cols]


WK_COLS = 512 * 3 + 128 + 64 + 4
WV_COLS = 512 * 3 + 128


class DT:
    def __init__(self, ap, tok=None, dj=False):
        self.ap = ap
        self.t = tok if tok is not None else Tok(dj=dj)

    def __getitem__(self, i):
        return self.ap[i]


def load_x_tile(cx, c, x_src, t, ropet):
    S = cx.S
    xs, xtok = x_src(t)
    S.dma("sp", out=c["xbuf"][:, :, :], in_=xs.rearrange("(k p) n -> p k n", p=128), reads=[xtok], writes=[c["xbuf"]])
    S.dma("sp", out=c["rt"][:, :, :], in_=ropet.ap[:, :, t * TT:(t + 1) * TT].rearrange("f p n -> p f n"),
          reads=[ropet.t], writes=[c["rt"]])


def phase_kv(cx, io, x_src, c):
    S = cx.S
    x, hT, tmpA, tmpB, kst, vst, lf, rt = (c[k] for k in ("xbuf", "hT", "tmpA", "tmpB", "kst", "vst", "lf", "rt"))
    c128, s128, c64, s64 = (View(rt, i) for i in range(4))
    stn = 0
    for t in range(NT):
        cx.first_tile = (t == 0)
        tc = (t * TT, (t + 1) * TT)
        load_x_tile(cx, c, x_src, t, io["ropet"])
        rmsnorm_fm(cx, x, c["n1g"], hT, cx.next_ps())
        for (c0, dst, rk) in ((0, "kTa", None), (512, "kTb", 32), (1024, "kTc", None)):
            w = load_w(cx, io["wk"].ap, c0, 512, wtok=io["wk"].t, key="wk")
            for jp in range(2):
                st = kst[stn % 2]
                stn += 1
                for j in range(2):
                    ps = cx.next_ps()
                    proj_fm(cx, w, (jp * 2 + j) * 128, 128, hT, ps)
                    if rk is None:
                        evac(cx, st[:, j, :], ps[:, 0:TT], [ps], [st])
                    else:
                        rope_fm(cx, ps, 128, rk, c64, s64, st[:, j, :], [st], tmpA, tmpB)
                r0 = jp * 256
                S.dma("sp", out=io[dst].ap[r0:r0 + 256, tc[0]:tc[1]].rearrange("(j p) n -> p j n", p=128),
                      in_=st[:, :, :], reads=[st], writes=[io[dst]], semtile=st.t)
        w = load_w(cx, io["wk"].ap, 1536, 196, wtok=io["wk"].t, key="wk")
        st = kst[stn % 2]
        stn += 1
        ps = cx.next_ps()
        proj_fm(cx, w, 0, 128, hT, ps)
        rope_fm(cx, ps, 128, 64, c128, s128, st[:, 0, :], [st], tmpA, tmpB)
        ps = cx.next_ps()
        proj_fm(cx, w, 128, 68, hT, ps)
        rope_fm(cx, ps, 64, 32, c64, s64, st[0:64, 1, :], [st], tmpA, tmpB)
        S.op("act", lambda e, ps=ps: e.activation(out=lf[64:68, :], in_=ps[64:68, 0:TT], func=AF.Exp,
                                                 bias=c["nbfg"][64:68, :], scale=-1.0),
             reads=[ps, c["nbfg"]], writes=[lf])
        S.op("act", lambda e: e.activation(out=lf[64:68, :], in_=lf[64:68, :], func=AF.Ln, bias=1.0),
             reads=[lf], writes=[lf])
        S.op("act", lambda e: e.mul(out=lf[64:68, :], in_=lf[64:68, :], mul=-1.0), reads=[lf], writes=[lf])
        S.dma("sp", out=io["kTd"].ap[:, tc[0]:tc[1]], in_=st[:, 0, :], reads=[st], writes=[io["kTd"]], semtile=st.t)
        S.dma("sp", out=io["ikT"].ap[:, tc[0]:tc[1]], in_=st[0:64, 1, :], reads=[st], writes=[io["ikT"]], semtile=st.t)
        S.dma("sp", out=io["logf"].ap[:, tc[0]:tc[1]], in_=lf[64:68, :], reads=[lf], writes=[io["logf"]], semtile=lf.t)
        for (c0, ncols, nm) in ((0, 512, "va"), (512, 512, "vb"), (1024, 512, "vc"), (1536, 128, "vd")):
            w = load_w(cx, io["wv"].ap, c0, ncols, wtok=io["wv"].t, key="wv")
            for sbk in range(TT // 128):
                ps = cx.next_ps()
                for kc in range(KC):
                    S.op("pe", lambda e, kc=kc, ps=ps, w=w, sbk=sbk, ncols=ncols: e.matmul(
                        ps[:, 0:ncols], lhsT=hT[:, kc, sbk * 128:(sbk + 1) * 128], rhs=w[:, kc, 0:ncols],
                        start=(kc == 0), stop=(kc == KC - 1)), reads=[w, hT.s(kc)], writes=[ps])
                st = vst[stn % 2]
                stn += 1
                evac(cx, st[:, 0:ncols], ps[:, 0:ncols], [ps], [st])
                r0 = t * TT + sbk * 128
                S.dma("sp", out=io[nm].ap[r0:r0 + 128, 0:ncols], in_=st[:, 0:ncols], reads=[st], writes=[io[nm]],
                      semtile=st.t)


def alloc_common(cx):
    S = cx.S
    c = {}
    c["xbuf"] = S.sb("xbuf", [128, KC, TT], F32)
    c["hT"] = S.sb("hT", [128, KC, TT], BF16)
    c["tmpA"] = S.sb("tmpA", [128, TT], F32)
    c["tmpB"] = S.sb("tmpB", [128, TT], F32)
    c["rt"] = S.sb("rt", [128, 4, TT], F32)
    c["n1g"] = S.sb("n1g", [128, KC], F32)
    return c


def alloc_kv(cx, c):
    S = cx.S
    c["kst"] = [S.sb(f"kst{i}", [128, 2, TT], BF16) for i in range(2)]
    c["vst"] = [S.sb(f"vst{i}", [128, 512], BF16) for i in range(2)]
    c["lf"] = S.sb("lf", [128, TT], F32)
    c["nbfg"] = S.sb("nbfg", [128, 1], F32)
    return c


def dram_in(nc, name, shape, dtype=F32):
    return DT(nc.dram_tensor(name, list(shape), dtype, kind="ExternalInput").ap())


def dram_out(nc, name, shape, dtype=F32):
    return DT(nc.dram_tensor(name, list(shape), dtype, kind="ExternalOutput").ap(), dj=True)


KV_OUT = (("kTa", [512, LTOK], BF16), ("kTb", [512, LTOK], BF16), ("kTc", [512, LTOK], BF16),
          ("kTd", [128, LTOK], BF16), ("ikT", [64, LTOK], BF16), ("logf", [4, LTOK], F32),
          ("va", [LTOK, 512], BF16), ("vb", [LTOK, 512], BF16), ("vc", [LTOK, 512], BF16),
          ("vd", [LTOK, 128], BF16))


def build_A():
    nc = bass.Bass("TRN2", target_bir_lowering=False)
    es = ExitStack()
    io = {}
    xT = dram_in(nc, "xT", [D_MODEL, LTOK])
    io["wk"] = dram_in(nc, "wk", [D_MODEL, WK_COLS])
    io["wv"] = dram_in(nc, "wv", [D_MODEL, WV_COLS])
    io["ropet"] = dram_in(nc, "ropet", [4, 128, LTOK])
    n1g = dram_in(nc, "n1g", [128, KC])
    bfg = dram_in(nc, "bfg", [4, 1])
    for nm, shp, dt in KV_OUT:
        io[nm] = dram_out(nc, nm, shp, dt)
    with es:
        S = Sched(nc, es)
        cx = Ctx(S)
        c = alloc_kv(cx, alloc_common(cx))
        S.dma("sp", out=c["n1g"][:, :], in_=n1g.ap[:, :], writes=[c["n1g"]])
        S.dma("sp", out=c["nbfg"][64:68, :], in_=bfg.ap[:, :], writes=[c["nbfg"]])
        S.op("dve", lambda e: e.tensor_scalar(out=c["nbfg"][64:68, :], in0=c["nbfg"][64:68, :], scalar1=-1.0,
                                              scalar2=None, op0=ALU.mult), reads=[c["nbfg"]], writes=[c["nbfg"]])
        phase_kv(cx, io, lambda t: (xT.ap[:, t * TT:(t + 1) * TT], xT.t), c)
        S.final_all()
        S.emit()
    return nc


SEG_W = (("a_q", 512), ("a_k", 512), ("a_v", 512), ("b_q", 512), ("b_k", 512), ("b_v", 512),
         ("c_q", 512), ("c_k", 512), ("c_v", 512), ("c_f", 4), ("d_q", 512), ("d_k", 128), ("d_v", 128),
         ("d_iq", 512), ("d_ik", 64), ("d_iw", 8), ("gate", 8192))
SEG = {}
_o = 0
for _n, _w in SEG_W:
    SEG[_n] = (_o, _o + _w)
    _o += _w


def wcols(w, names):
    return np.ascontiguousarray(np.concatenate([w[:, SEG[n][0]:SEG[n][1]] for n in names], axis=1))


def local_pos(c):
    lb = np.arange(LBLK)
    return ((2 * lb + c)[:, None] * 128 + np.arange(128)[None, :]).reshape(-1)


def rope_tabs(c):
    pos = local_pos(c).astype(np.float32)
    out = []
    for dim in (128, 64):
        inv = (np.float32(10000.0) ** (-np.arange(0, dim, 2, dtype=np.float32) / np.float32(dim))).astype(np.float32)
        ang = pos[:, None] * inv[None, :]
        cos = np.cos(ang).astype(np.float32).T
        sin = np.sin(ang).astype(np.float32).T
        rep = 128 // dim
        out.append(np.concatenate([cos, cos] * rep, axis=0))
        out.append(np.concatenate([sin, -sin] * rep, axis=0))
    return np.ascontiguousarray(np.stack(out, 0))


def to_local_T(xb, c):
    blk = xb.reshape(NBLK, 128, -1)[c::2].reshape(LTOK, -1)
    return np.ascontiguousarray(blk.T)


def pcol(v):
    return np.ascontiguousarray(v.reshape(-1, 128).T)


IWS = (8 ** -0.5) * (64 ** -0.5)
BIS_W = 64.0
BIS_IT = 20
KV_ALL = (("kTa", [2, 512, LTOK], BF16), ("kTb", [2, 512, LTOK], BF16), ("kTc", [2, 512, LTOK], BF16),
          ("kTd", [2, 128, LTOK], BF16), ("ikT", [2, 64, LTOK], BF16), ("logf", [2, 4, LTOK], F32),
          ("va", [2, LTOK, 512], BF16), ("vb", [2, LTOK, 512], BF16), ("vc", [2, LTOK, 512], BF16),
          ("vd", [2, LTOK, 128], BF16))


class UView:
    def __init__(self, tile):
        self.tile = tile
        self.t = tile.t
        self.v = tile.ap[:, :].bitcast(BF16).rearrange("p (k n) -> p k n", n=TT)

    def s(self, k):
        return self.tile.s(("u", k))

    def __getitem__(self, idx):
        return self.v[idx]


def alloc_q(cx, c):
    S = cx.S
    c["kTd"] = S.sb("kTd_sb", [128, SEQ], BF16)
    c["ikT2"] = S.sb("ikT2", [128, SEQ], BF16)
    c["vd"] = S.sb("vd_sb", [128, NBLK, 128], BF16)
    c["score"] = S.sb("score", [128, SEQ], F32)
    c["negcum"] = S.sb("negcum", [128, NBLK * 4], F32)
    c["cum_mine"] = S.sb("cum_mine", [4, LTOK], F32)
    c["cumq"] = S.sb("cumq", [128, 4, TT], F32)
    c["biasA"] = S.sb("biasA", [128, 6, 512], BF16)
    c["mk"] = S.sb("mk", [128, 4, 128], BF16)
    c["idxmask"] = S.sb("idxmask", [128, 256], F32)
    c["ident"] = S.sb("ident", [128, 128], BF16)
    c["identf"] = S.sb("identf", [128, 128], F32)
    c["onesf"] = S.sb("onesf", [128, 256], F32)
    c["selh"] = S.sb("selh", [4, 512], F32)
    c["sel"] = S.sb("sel", [4, 2], F32)
    c["n2g"] = S.sb("n2g", [128, KC], F32)
    c["fg"] = S.sb("fg", [128, KC], F32)
    c["bgate"] = S.sb("bgate", [128, 64], F32)
    c["dng"] = S.sb("dng", [128, 1], F32)
    c["neglam"] = S.sb("neglam", [128, 1], F32)
    c["lamv"] = S.sb("lamv", [64, 4], F32)
    c["lamp"] = S.sb("lamp", [128, 2], F32)
    c["wiw"] = S.sb("wiw", [128, KC, 8], BF16)
    c["q"] = S.sb("q", [128, 4, TT], BF16)
    c["iq"] = S.sb("iq", [128, 4, TT], BF16)
    c["qz"] = [S.sb(f"qz{i}", [128, 4, TT], BF16) for i in range(2)]
    c["iw"] = S.sb("iw", [128, 16], F32)
    c["o"] = [S.sb(f"o{i}", [128, 4, TT], BF16) for i in range(4)]
    c["mg"] = S.sb("mg", [128, KC, TT], BF16)
    c["u"] = UView(c["score"])
    c["kch"] = [S.sb(f"kch{i}", [128, 4, 512], BF16) for i in range(2)]
    c["vch"] = [S.sb(f"vch{i}", [128, 4, 512], BF16) for i in range(2)]
    c["pT"] = [S.sb(f"pT{i}", [128, 512], BF16) for i in range(3)]
    c["sp"] = [S.sb(f"sp{i}", [128, 512], F32) for i in range(2)]
    c["nm"] = [S.sb(f"nm{i}", [128, 512], BF16) for i in range(2)]
    c["nmT"] = [S.sb(f"nmT{i}", [128, 4, 128], BF16) for i in range(2)]
    c["rden"] = S.sb("rden", [128, 512], F32)
    c["obr"] = S.sb("obr", [128, TT], F32)
    c["acc"] = [S.sb(f"acc{i}", [128, TT], F32) for i in range(4)]
    c["m8"] = S.sb("m8", [128, 8], F32)
    c["bis"] = S.sb("bis", [128, 4], F32)
    c["ost"] = [S.sb(f"ost{i}", [128, TT], F32) for i in range(2)]
    c["psS"] = [S.ps(f"psS{i}", [128, 512], F32) for i in range(2)]
    c["psO"] = S.ps("psO", [128, 512], F32)
    c["psD"] = S.ps("psD", [128, 512], F32)
    c["psm"] = S.ps("psm", [128, 512], F32)
    c["psT"] = S.ps("psT", [128, 512], BF16)
    c["rr"] = {"S": 0, "pT": 0, "sp": 0, "kv": 0, "nm": 0}
    return c


def rr(c, name, key):
    lst = c[name]
    i = c["rr"][key]
    c["rr"][key] = i + 1
    return lst[i % len(lst)]


def setup_q(cx, c, io, lambda_init):
    S = cx.S
    pq = "pool"
    S.dma(pq, out=c["biasA"][:, :, :], in_=io["biasA"].ap[:, :].rearrange("p (b f) -> p b f", b=6),
          reads=[io["biasA"]], writes=[c["biasA"]])
    S.dma(pq, out=c["mk"][:, :, :], in_=io["mk"].ap[:, :].rearrange("p (b f) -> p b f", b=4), reads=[io["mk"]],
          writes=[c["mk"]])
    S.dma(pq, out=c["ident"][:, :], in_=io["identf"].ap[:, :], reads=[io["identf"]], writes=[c["ident"]])
    S.dma(pq, out=c["wiw"][:, :, :], in_=io["wiw"].ap[:, :].rearrange("(k p) n -> p k n", p=128), reads=[io["wiw"]],
          writes=[c["wiw"]])
    for nm in ("identf", "idxmask", "selh", "sel", "n2g", "fg", "bgate", "dng"):
        S.dma("sp", out=c[nm][:, :], in_=io[nm].ap[:, :], reads=[io[nm]], writes=[c[nm]])
    S.dma("sp", out=c["lamv"][:, :], in_=io["lam4"].ap[:, :], reads=[io["lam4"]], writes=[c["lamv"]])
    S.op("dve", lambda e: e.memset(c["onesf"][:, :], 1.0), writes=[c["onesf"]])
    S.op("dve", lambda e: e.tensor_scalar(out=c["dng"][:, :], in0=c["dng"][:, :], scalar1=float(1.0 - lambda_init),
                                          scalar2=None, op0=ALU.mult), reads=[c["dng"]], writes=[c["dng"]])
    x = c["xbuf"]
    xflat = x[0:4, :, :].rearrange("p k n -> p (k n)")
    for r in range(2):
        S.dma("sp", out=c["kTd"][:, :].rearrange("p (n r j) -> p n r j", r=2, j=128)[:, :, r, :],
              in_=io["kTd_all"].ap[r].rearrange("p (n j) -> p n j", j=128), reads=[io["kTd_all"]], writes=[c["kTd"]])
        for hf in range(2):
            S.dma("sp", out=c["ikT2"][hf * 64:(hf + 1) * 64, :].rearrange("p (n r j) -> p n r j", r=2, j=128)[:, :, r, :],
                  in_=io["ikT_all"].ap[r].rearrange("p (n j) -> p n j", j=128), reads=[io["ikT_all"]],
                  writes=[c["ikT2"]])
        S.dma("sp", out=c["vd"][:, :, :].rearrange("p (n r) d -> p n r d", r=2)[:, :, r, :],
              in_=io["vd_all"].ap[r].rearrange("(n p) d -> p n d", p=128), reads=[io["vd_all"]], writes=[c["vd"]])
        S.dma("sp", out=xflat.rearrange("p (n r j) -> p n r j", r=2, j=128)[:, :, r, :],
              in_=io["logf_all"].ap[r].rearrange("p (n j) -> p n j", j=128), reads=[io["logf_all"]], writes=[x])
    score = c["score"]
    CH = 256
    for i in range(SEQ // CH):
        init = 0.0 if i == 0 else score[0:4, i * CH - 1:i * CH]
        S.op("dve", lambda e, i=i, init=init: e.tensor_tensor_scan(
            out=score[0:4, i * CH:(i + 1) * CH], data0=c["onesf"][0:4, 0:CH], data1=xflat[:, i * CH:(i + 1) * CH],
            initial=init, op0=ALU.mult, op1=ALU.add), reads=[x, c["onesf"], score], writes=[score])
    psm = c["psm"]
    for kb in range(NBLK):
        S.op("pe", lambda e, kb=kb: e.transpose(out=psm[:, kb * 4:(kb + 1) * 4], in_=score[0:4, kb * 128:(kb + 1) * 128],
                                                identity=c["identf"][0:4, 0:4]),
             reads=[score, c["identf"]], writes=[psm])
    S.op("act", lambda e: e.mul(out=c["negcum"][:, :], in_=psm[:, 0:NBLK * 4], mul=-1.0), reads=[psm],
         writes=[c["negcum"]])
    cv = score[0:4, :].rearrange("p (n r j) -> p n r j", r=2, j=128)
    cm = c["cum_mine"][0:4, :].rearrange("p (n j) -> p n j", j=128)
    S.op("dve", lambda e: e.tensor_scalar(out=cm, in0=cv[:, :, 0, :], scalar1=c["sel"][0:4, 0:1], scalar2=None,
                                          op0=ALU.mult), reads=[score, c["sel"]], writes=[c["cum_mine"]])
    S.op("dve", lambda e: e.scalar_tensor_tensor(out=cm, in0=cv[:, :, 1, :], scalar=c["sel"][0:4, 1:2], op0=ALU.mult,
                                                 in1=cm, op1=ALU.add), reads=[score, c["sel"], c["cum_mine"]],
         writes=[c["cum_mine"]])
    lv, lp = c["lamv"], c["lamp"]
    S.op("dve", lambda e: e.tensor_tensor(out=lp[0:64, 0:1], in0=lv[0:64, 0:1], in1=lv[0:64, 1:2], op=ALU.mult),
         reads=[lv], writes=[lp])
    S.op("dve", lambda e: e.tensor_tensor(out=lp[0:64, 1:2], in0=lv[0:64, 2:3], in1=lv[0:64, 3:4], op=ALU.mult),
         reads=[lv, lp], writes=[lp])
    S.op("pe", lambda e: e.matmul(psm[:, 0:2], lhsT=c["onesf"][0:64, 0:128], rhs=lp[0:64, 0:2], start=True, stop=True),
         reads=[c["onesf"], lp], writes=[psm])
    S.op("act", lambda e: e.activation(out=lp[:, 0:2], in_=psm[:, 0:2], func=AF.Exp), reads=[psm], writes=[lp])
    S.op("dve", lambda e: e.tensor_tensor(out=c["neglam"][:, :], in0=lp[:, 1:2], in1=lp[:, 0:1], op=ALU.subtract),
         reads=[lp], writes=[c["neglam"]])
    S.op("dve", lambda e: e.tensor_scalar(out=c["neglam"][:, :], in0=c["neglam"][:, :], scalar1=float(-lambda_init),
                                          scalar2=None, op0=ALU.add), reads=[c["neglam"]], writes=[c["neglam"]])


def q_proj(cx, c, io, col0, dst, mode, scale=None):
    hT, tmpA, tmpB, rt = c["hT"], c["tmpA"], c["tmpB"], c["rt"]
    w = load_w(cx, io["wq"].ap, col0, 512, wtok=io["wq"].t, key="wq")
    for jj in range(4):
        ps = cx.next_ps()
        proj_fm(cx, w, jj * 128, 128, hT, ps)
        if mode == "plain":
            evac(cx, dst[:, jj, :], ps[:, 0:TT], [ps], [dst], scale=scale)
        elif mode == "rope64z":
            qz = c["qz"]
            rope_fm(cx, ps, 128, 32, View(rt, 2), View(rt, 3),
                    [(0, 64, qz[0][0:64, jj, :], [qz[0]]), (64, 128, qz[1][64:128, jj, :], [qz[1]])], None,
                    tmpA, tmpB)
        elif mode == "rope64":
            rope_fm(cx, ps, 128, 32, View(rt, 2), View(rt, 3), dst[:, jj, :], [dst], tmpA, tmpB)
        else:
            rope_fm(cx, ps, 128, 64, View(rt, 0), View(rt, 1), dst[:, jj, :], [dst], tmpA, tmpB)


def attn_A(cx, c, io, t):
    S = cx.S
    q, o = c["q"], c["o"][0]
    psO, psD, rden = c["psO"], c["psD"], c["rden"]
    for m in range(2):
        lb = 2 * t + m
        n0 = max(0, lb - 2)
        nn = lb - n0 + 1
        mc = slice(m * 128, (m + 1) * 128)
        kb_, vb_ = c["kch"], c["vch"]
        for r in range(2):
            S.dma("sp", out=kb_[r][:, :, 0:nn * 128],
                  in_=io["kTa_all"].ap[r].rearrange("(h p) n -> p h n", p=128)[:, :, n0 * 128:(lb + 1) * 128],
                  reads=[io["kTa_all"]], writes=[kb_[r]])
            S.dma("sp", out=vb_[r][:, 0:nn, :],
                  in_=io["va_all"].ap[r][n0 * 128:(lb + 1) * 128, :].rearrange("(n p) f -> p n f", p=128),
                  reads=[io["va_all"]], writes=[vb_[r]])
        tot = 2 * nn
        k = 0
        for r in range(2):
            for ni in range(nn):
                nrel = n0 + ni - (lb - 2)
                pss = rr(c, "psS", "S")
                pT = rr(c, "pT", "pT")
                for h in range(4):
                    S.op("pe", lambda e, h=h, r=r, ni=ni, pss=pss: e.matmul(
                        pss[:, h * 128:(h + 1) * 128], lhsT=kb_[r][:, h, ni * 128:(ni + 1) * 128], rhs=q[:, h, mc],
                        start=(h == 0), stop=False, skip_group_check=True), reads=[kb_[r], q], writes=[pss])
                S.op("pe", lambda e, r=r, nrel=nrel, pss=pss: e.matmul(
                    pss[:, :], lhsT=c["ident"][:, :], rhs=c["biasA"][:, r * 3 + nrel, :], start=False, stop=True,
                    skip_group_check=True), reads=[c["ident"], c["biasA"]], writes=[pss])
                S.op("act", lambda e, pss=pss, pT=pT: e.activation(out=pT[:, :], in_=pss[:, :], func=AF.Exp),
                     reads=[pss], writes=[pT])
                for h in range(4):
                    S.op("pe", lambda e, h=h, r=r, ni=ni, pT=pT, k=k: e.matmul(
                        psO[:, h * 128:(h + 1) * 128], lhsT=vb_[r][:, ni, h * 128:(h + 1) * 128],
                        rhs=pT[:, h * 128:(h + 1) * 128], start=(k == 0 and h == 0), stop=(k == tot - 1 and h == 3),
                        skip_group_check=True), reads=[vb_[r], pT], writes=[psO])
                S.op("pe", lambda e, pT=pT, k=k: e.matmul(psD[:, :], lhsT=cx.ones_bf[:, :], rhs=pT[:, :],
                                                           start=(k == 0), stop=(k == tot - 1)),
                     reads=[cx.ones_bf, pT], writes=[psD])
                k += 1
        S.op("dve", lambda e: e.reciprocal(out=rden[:, :], in_=psD[:, :]), reads=[psD], writes=[rden])
        S.op("dve", lambda e, mc=mc: e.tensor_tensor(out=o[:, :, mc], in0=psO[:, :].rearrange("p (h q) -> p h q", h=4),
                                                     in1=rden[:, :].rearrange("p (h q) -> p h q", h=4), op=ALU.mult),
             reads=[psO, rden], writes=[o])


def kv_chunks(t):
    nb_r = 2 * t + 2
    out = []
    for r in range(2):
        for cst in range(0, nb_r, 4):
            out.append((r, cst, min(4, nb_r - cst)))
    return out


def attn_BC(cx, c, io, t, kind):
    S = cx.S
    q = c["q"]
    psO, psD, rden = c["psO"], c["psD"], c["rden"]
    kname, vname = ("kTb_all", "vb_all") if kind == "B" else ("kTc_all", "vc_all")
    o = c["o"][1] if kind == "B" else c["o"][2]
    chunks = kv_chunks(t)
    nblocks = sum(nb for _, _, nb in chunks)
    ngroups = 4 if kind == "B" else 2
    for g in range(ngroups):
        k = 0
        for (r, cst, nb) in chunks:
            kc_ = rr(c, "kch", "kv")
            vc_ = c["vch"][(c["rr"]["kv"] - 1) % 2]
            cols = slice(cst * 128, (cst + nb) * 128)
            if kind == "B":
                S.dma("sp", out=kc_[:, 0, 0:nb * 128], in_=io[kname].ap[r][g * 128:(g + 1) * 128, cols],
                      reads=[io[kname]], writes=[kc_])
            else:
                S.dma("sp", out=kc_[:, 0:2, 0:nb * 128],
                      in_=io[kname].ap[r].rearrange("(h p) n -> p h n", p=128)[:, 2 * g:2 * g + 2, cols],
                      reads=[io[kname]], writes=[kc_])
            S.dma("sp", out=vc_[:, 0:nb, :], in_=io[vname].ap[r][cols, :].rearrange("(n p) f -> p n f", p=128),
                  reads=[io[vname]], writes=[vc_])
            for bi in range(nb):
                n = cst + bi
                npr = n - 2 * t
                q0 = 128 if npr == 1 else 0
                pss = rr(c, "psS", "S")
                pT = rr(c, "pT", "pT")
                bsl = slice(bi * 128, (bi + 1) * 128)
                first, last = (k == 0), (k == nblocks - 1)
                for s in range(2):
                    osl = slice(s * 256 + q0, (s + 1) * 256)
                    if kind == "B":
                        ps_ = slice(s * 64, (s + 1) * 64)
                        S.op("pe", lambda e, s=s, osl=osl, bsl=bsl, pss=pss, kc_=kc_, npr=npr: e.matmul(
                            pss[:, osl], lhsT=kc_[:, 0, bsl], rhs=c["qz"][s][:, g, q0:TT], start=(s == 0),
                            stop=(s == 1 and npr < 0), skip_group_check=True), reads=[kc_, c["qz"][s]], writes=[pss])
                    else:
                        S.op("pe", lambda e, s=s, osl=osl, bsl=bsl, pss=pss, kc_=kc_, npr=npr: e.matmul(
                            pss[:, osl], lhsT=kc_[:, s, bsl], rhs=q[:, 2 * g + s, q0:TT], start=(s == 0),
                            stop=(s == 1 and npr < 0), skip_group_check=True), reads=[kc_, q], writes=[pss])
                if npr >= 0:
                    mi = r if kind == "B" else 2 + r
                    for s in range(2):
                        msl = slice(s * 256 + npr * 128, s * 256 + (npr + 1) * 128)
                        S.op("pe", lambda e, s=s, msl=msl, mi=mi, pss=pss: e.matmul(
                            pss[:, msl], lhsT=c["ident"][:, :], rhs=c["mk"][:, mi, :], start=False, stop=(s == 1),
                            skip_group_check=True), reads=[c["ident"], c["mk"]], writes=[pss])
                v3 = lambda ap: ap.rearrange("p (s q) -> p s q", s=2)[:, :, q0:TT]
                if kind == "B":
                    S.op("act", lambda e, pss=pss, pT=pT, v3=v3: e.activation(out=v3(pT[:, :]), in_=v3(pss[:, :]),
                                                                             func=AF.Exp, scale=64 ** -0.5),
                         reads=[pss], writes=[pT])
                else:
                    sp = rr(c, "sp", "sp")
                    kbt = 2 * n + r
                    for s in range(2):
                        h = 2 * g + s
                        osl = slice(s * 256 + q0, (s + 1) * 256)
                        S.op("dve", lambda e, osl=osl, h=h, kbt=kbt, pss=pss, sp=sp: e.scalar_tensor_tensor(
                            out=sp[:, osl], in0=pss[:, osl], scalar=c["negcum"][:, kbt * 4 + h:kbt * 4 + h + 1],
                            op0=ALU.add, in1=c["cumq"][:, h, q0:TT], op1=ALU.add),
                             reads=[pss, c["negcum"], c["cumq"]], writes=[sp])
                    S.op("act", lambda e, sp=sp, pT=pT, v3=v3: e.activation(out=v3(pT[:, :]), in_=v3(sp[:, :]),
                                                                           func=AF.Exp), reads=[sp], writes=[pT])
                if kind == "B":
                    S.op("pe", lambda e, pT=pT, vc_=vc_, bi=bi, first=first, last=last, v3=v3: e.matmul(
                        v3(psO[:, :]), lhsT=vc_[:, bi, g * 128:(g + 1) * 128], rhs=v3(pT[:, :]), start=first, stop=last,
                        skip_group_check=True), reads=[vc_, pT], writes=[psO])
                else:
                    for s in range(2):
                        h = 2 * g + s
                        osl = slice(s * 256 + q0, (s + 1) * 256)
                        S.op("pe", lambda e, s=s, h=h, osl=osl, pT=pT, vc_=vc_, bi=bi, first=first, last=last: e.matmul(
                            psO[:, osl], lhsT=vc_[:, bi, h * 128:(h + 1) * 128], rhs=pT[:, osl],
                            start=(first and s == 0), stop=(last and s == 1), skip_group_check=True),
                             reads=[vc_, pT], writes=[psO])
                S.op("pe", lambda e, pT=pT, first=first, last=last, v3=v3: e.matmul(
                    v3(psD[:, :]), lhsT=cx.ones_bf[:, :], rhs=v3(pT[:, :]), start=first, stop=last,
                    skip_group_check=True), reads=[cx.ones_bf, pT], writes=[psD])
                k += 1
        S.op("dve", lambda e: e.reciprocal(out=rden[:, :], in_=psD[:, :]), reads=[psD], writes=[rden])
        if kind == "C":
            S.op("dve", lambda e, g=g: e.tensor_tensor(
                out=o[:, 2 * g:2 * g + 2, :], in0=psO[:, :].rearrange("p (s q) -> p s q", s=2),
                in1=rden[:, :].rearrange("p (s q) -> p s q", s=2), op=ALU.mult), reads=[psO, rden], writes=[o])
        else:
            obr, tA, tB = c["obr"], c["tmpA"], c["tmpB"]
            S.op("dve", lambda e: e.tensor_tensor(out=obr[:, :], in0=psO[:, 0:TT], in1=rden[:, 0:TT], op=ALU.mult),
                 reads=[psO, rden], writes=[obr])
            S.op("dve", lambda e: e.tensor_tensor(out=tA[:, :], in0=psO[:, TT:2 * TT], in1=rden[:, TT:2 * TT],
                                                  op=ALU.mult), reads=[psO, rden], writes=[tA])
            S.op("dve", lambda e: e.scalar_tensor_tensor(out=obr[:, :], in0=tA[:, :], scalar=c["neglam"][:, 0:1],
                                                         op0=ALU.mult, in1=obr[:, :], op1=ALU.add),
                 reads=[tA, c["neglam"], obr], writes=[obr])
            sq = cx.sqb[0]
            psm = c["psm"]
            S.op("act", lambda e: e.activation(out=sq[:, :], in_=obr[:, :], func=AF.Square), reads=[obr], writes=[sq])
            S.op("pe", lambda e: e.matmul(psm[:, 0:TT], lhsT=cx.ones_bf[:, :], rhs=sq[:, :], start=True, stop=True),
                 reads=[cx.ones_bf, sq], writes=[psm])
            S.op("act", lambda e: e.activation(out=tB[:, :], in_=psm[:, 0:TT], func=AF.Sqrt, bias=EPS, scale=1.0 / 128),
                 reads=[psm], writes=[tB])
            S.op("dve", lambda e: e.reciprocal(out=tB[:, :], in_=tB[:, :]), reads=[tB], writes=[tB])
            S.op("dve", lambda e, g=g: e.scalar_tensor_tensor(out=o[:, g, :], in0=obr[:, :], scalar=c["dng"][:, 0:1],
                                                              op0=ALU.mult, in1=tB[:, :], op1=ALU.mult),
                 reads=[obr, c["dng"], tB], writes=[o])


def attn_D(cx, c, io, t):
    S = cx.S
    q, iq, o, score = c["q"], c["iq"], c["o"][3], c["score"]
    psO, psD, psT, rden, m8 = c["psO"], c["psD"], c["psT"], c["rden"], c["m8"]
    for m in range(2):
        lb = 2 * t + m
        nk = (2 * lb + 2) * 128
        mc = slice(m * 128, (m + 1) * 128)
        for cc in range(0, nk, 512):
            ncol = min(512, nk - cc)
            for ih in range(8):
                psI = rr(c, "psS", "S")
                sp = rr(c, "sp", "sp")
                prow = slice((ih % 2) * 64, (ih % 2 + 1) * 64)
                S.op("pe", lambda e, psI=psI, prow=prow, ih=ih, cc=cc, ncol=ncol: e.matmul(
                    psI[:, 0:ncol], lhsT=iq[prow, ih // 2, mc], rhs=c["ikT2"][prow, cc:cc + ncol], start=True, stop=True),
                     reads=[iq, c["ikT2"]], writes=[psI])
                S.op("act", lambda e, psI=psI, sp=sp, ncol=ncol: e.activation(out=sp[:, 0:ncol], in_=psI[:, 0:ncol],
                                                                              func=AF.Relu), reads=[psI], writes=[sp])
                iwc = c["iw"][:, m * 8 + ih:m * 8 + ih + 1]
                if ih == 0:
                    S.op("dve", lambda e, sp=sp, cc=cc, ncol=ncol, iwc=iwc: e.tensor_scalar(
                        out=score[:, cc:cc + ncol], in0=sp[:, 0:ncol], scalar1=iwc, scalar2=None, op0=ALU.mult),
                         reads=[sp, c["iw"]], writes=[score])
                else:
                    S.op("dve", lambda e, sp=sp, cc=cc, ncol=ncol, iwc=iwc: e.scalar_tensor_tensor(
                        out=score[:, cc:cc + ncol], in0=sp[:, 0:ncol], scalar=iwc, op0=ALU.mult,
                        in1=score[:, cc:cc + ncol], op1=ALU.add), reads=[sp, c["iw"], score], writes=[score])
        S.op("dve", lambda e, nk=nk: e.tensor_tensor(out=score[:, nk - 256:nk], in0=score[:, nk - 256:nk],
                                                     in1=c["idxmask"][:, :], op=ALU.add),
             reads=[score, c["idxmask"]], writes=[score])
        if lb >= 1:
            mgj = c["mg"]
            junk = mgj[:, :, :].rearrange("p k n -> p (k n)")
            lo, cand, cnt, gg = (c["bis"][:, i:i + 1] for i in range(4))
            S.op("dve", lambda e, nk=nk: e.max(out=m8[:, :], in_=score[:, 0:nk]), reads=[score], writes=[m8])
            S.op("dve", lambda e: e.tensor_scalar(out=lo, in0=m8[:, 0:1], scalar1=-BIS_W, scalar2=None, op0=ALU.add),
                 reads=[m8], writes=[c["bis"]])
            for it in range(BIS_IT):
                ck = BIS_W / float(2 ** (it + 1))
                S.op("dve", lambda e, ck=ck: e.tensor_scalar(out=cand, in0=lo, scalar1=ck, scalar2=None, op0=ALU.add),
                     reads=[c["bis"]], writes=[c["bis"]])
                S.op("dve", lambda e, nk=nk: e.tensor_scalar(out=junk[:, 0:nk], in0=score[:, 0:nk], scalar1=cand,
                                                             scalar2=None, op0=ALU.is_ge, op1=ALU.add, accum_out=cnt),
                     reads=[score, c["bis"]], writes=[mgj, c["bis"]])
                S.op("dve", lambda e, ck=ck: e.tensor_scalar(out=gg, in0=cnt, scalar1=256.0, scalar2=ck, op0=ALU.is_ge,
                                                             op1=ALU.mult), reads=[c["bis"]], writes=[c["bis"]])
                S.op("dve", lambda e: e.tensor_tensor(out=lo, in0=lo, in1=gg, op=ALU.add), reads=[c["bis"]],
                     writes=[c["bis"]])
        nkb = nk // 128
        k = 0
        for cc in range(0, nkb, 4):
            nb = min(4, nkb - cc)
            nm = rr(c, "nm", "nm")
            nmT = c["nmT"][(c["rr"]["nm"] - 1) % 2]
            csl = slice(cc * 128, (cc + nb) * 128)
            if lb >= 1:
                S.op("dve", lambda e, nm=nm, csl=csl, nb=nb: e.tensor_scalar(
                    out=nm[:, 0:nb * 128], in0=score[:, csl], scalar1=c["bis"][:, 0:1], scalar2=NEG, op0=ALU.is_lt,
                    op1=ALU.mult), reads=[score, c["bis"]], writes=[nm])
            else:
                S.op("dve", lambda e, nm=nm, csl=csl, nb=nb: e.tensor_scalar(
                    out=nm[:, 0:nb * 128], in0=score[:, csl], scalar1=-1.0e29, scalar2=NEG, op0=ALU.is_lt, op1=ALU.mult),
                     reads=[score], writes=[nm])
            for bi in range(nb):
                S.op("pe", lambda e, nm=nm, bi=bi: e.transpose(out=psT[:, bi * 128:(bi + 1) * 128],
                                                               in_=nm[:, bi * 128:(bi + 1) * 128],
                                                               identity=c["ident"][:, :]),
                     reads=[nm, c["ident"]], writes=[psT])
            S.op("act", lambda e, nmT=nmT, nb=nb: e.activation(
                out=nmT[:, 0:nb, :], in_=psT[:, 0:nb * 128].rearrange("p (b q) -> p b q", q=128), func=AF.Copy),
                 reads=[psT], writes=[nmT])
            for bi in range(nb):
                kb = cc + bi
                pss = rr(c, "psS", "S")
                pT = rr(c, "pT", "pT")
                first, last = (k == 0), (k == nkb - 1)
                S.op("pe", lambda e, kb=kb, pss=pss: e.matmul(
                    pss[:, :].rearrange("p (h q) -> p h q", h=4), lhsT=c["kTd"][:, kb * 128:(kb + 1) * 128],
                    rhs=q[:, :, mc], start=True, stop=False, skip_group_check=True), reads=[c["kTd"], q], writes=[pss])
                S.op("pe", lambda e, bi=bi, nmT=nmT, pss=pss: e.matmul(
                    pss[:, :].rearrange("p (h q) -> p h q", h=4), lhsT=c["ident"][:, :],
                    rhs=nmT[:, bi:bi + 1, :].broadcast_to([128, 4, 128]), start=False, stop=True, skip_group_check=True),
                     reads=[c["ident"], nmT], writes=[pss])
                S.op("act", lambda e, pss=pss, pT=pT: e.activation(out=pT[:, :], in_=pss[:, :], func=AF.Exp,
                                                                  scale=128 ** -0.5), reads=[pss], writes=[pT])
                S.op("pe", lambda e, kb=kb, pT=pT, first=first, last=last: e.matmul(
                    psO[:, :], lhsT=c["vd"][:, kb, :], rhs=pT[:, :], start=first, stop=last), reads=[c["vd"], pT],
                     writes=[psO])
                S.op("pe", lambda e, pT=pT, first=first, last=last: e.matmul(
                    psD[:, :], lhsT=cx.ones_bf[:, :], rhs=pT[:, :], start=first, stop=last), reads=[cx.ones_bf, pT],
                     writes=[psD])
                k += 1
        S.op("dve", lambda e: e.reciprocal(out=rden[:, :], in_=psD[:, :]), reads=[psD], writes=[rden])
        S.op("dve", lambda e, mc=mc: e.tensor_tensor(out=o[:, :, mc], in0=psO[:, :].rearrange("p (h q) -> p h q", h=4),
                                                     in1=rden[:, :].rearrange("p (h q) -> p h q", h=4), op=ALU.mult),
             reads=[psO, rden], writes=[o])


def dense_tail(cx, c, io, t, x_dst, last_layer):
    S = cx.S
    x, hT, mg, u, acc = c["xbuf"], c["hT"], c["mg"], c["u"], c["acc"]
    sp = c["sp"]
    for j2 in range(4):
        for i in range(4):
            wg = load_w(cx, io["wg"].ap, i * 2048 + j2 * 512, 512, wtok=io["wg"].t, key="wg")
            wb = load_w(cx, io["wbr"].ap, j2 * 512, 512, nk=4, row0=i * 512, wtok=io["wbr"].t, key="wbr")
            for j in range(4):
                psg = cx.next_ps()
                proj_fm(cx, wg, j * 128, 128, hT, psg)
                psb = cx.next_ps()
                proj_fm(cx, wb, j * 128, 128, c["o"][i], psb, nk=4)
                sg = sp[j // 2][:, (j % 2) * TT:(j % 2 + 1) * TT]
                sgt = sp[j // 2]
                bcol = i * 16 + j2 * 4 + j
                S.op("act", lambda e, psg=psg, sg=sg, bcol=bcol: e.activation(
                    out=sg, in_=psg[:, 0:TT], func=AF.Sigmoid, bias=c["bgate"][:, bcol:bcol + 1]),
                     reads=[psg, c["bgate"]], writes=[sgt])
                if i == 0:
                    S.op("dve", lambda e, psb=psb, sg=sg, j=j: e.tensor_tensor(out=acc[j][:, :], in0=psb[:, 0:TT],
                                                                              in1=sg, op=ALU.mult),
                         reads=[psb, sgt], writes=[acc[j]])
                else:
                    S.op("dve", lambda e, psb=psb, sg=sg: e.tensor_tensor(out=sg, in0=psb[:, 0:TT], in1=sg, op=ALU.mult),
                         reads=[psb, sgt], writes=[sgt])
                    if i < 3:
                        S.op("dve", lambda e, sg=sg, j=j: e.tensor_tensor(out=acc[j][:, :], in0=acc[j][:, :], in1=sg,
                                                                         op=ALU.add),
                             reads=[acc[j], sgt], writes=[acc[j]])
                    else:
                        jj = j2 * 4 + j
                        S.op("dve", lambda e, sg=sg, j=j, jj=jj: e.tensor_tensor(out=mg[:, jj, :], in0=acc[j][:, :],
                                                                                in1=sg, op=ALU.add),
                             reads=[acc[j], sgt], writes=[mg.s(jj)])
    for j2 in range(4):
        w = load_w(cx, io["wo"].ap, j2 * 512, 512, wtok=io["wo"].t, key="wo")
        for j in range(4):
            jj = j2 * 4 + j
            ps = cx.next_ps()
            proj_fm(cx, w, j * 128, 128, mg, ps)
            S.op("dve", lambda e, ps=ps, jj=jj: e.tensor_tensor(out=x[:, jj, :], in0=ps[:, 0:TT], in1=x[:, jj, :],
                                                                op=ALU.add), reads=[ps, x], writes=[x])
    rmsnorm_fm(cx, x, c["n2g"], hT, c["psm"])
    for half in range(2):
        for j2 in range(8):
            w = load_w(cx, io["wf1"].ap, half * 4096 + j2 * 512, 512, wtok=io["wf1"].t, key="wf1")
            for j in range(4):
                jj = j2 * 4 + j
                ps = cx.next_ps()
                proj_fm(cx, w, j * 128, 128, hT, ps)
                r_ = sp[j // 2][:, (j % 2) * TT:(j % 2 + 1) * TT]
                rt_ = sp[j // 2]
                S.op("act", lambda e, ps=ps, r_=r_: e.activation(out=r_, in_=ps[:, 0:TT], func=AF.Relu),
                     reads=[ps], writes=[rt_])
                S.op("dve", lambda e, r_=r_, jj=jj, ps=ps: e.tensor_tensor(out=u[:, jj, :], in0=ps[:, 0:TT], in1=r_,
                                                                          op=ALU.mult),
                     reads=[rt_, ps], writes=[u.s(jj)])
        for j2 in range(4):
            for kk in range(2):
                w = load_w(cx, io["wf2"].ap, j2 * 512, 512, nk=16, row0=half * 4096 + kk * 2048, wtok=io["wf2"].t,
                           key="wf2")
                for j in range(4):
                    jj = j2 * 4 + j
                    ps = cx.next_ps()
                    for kc in range(16):
                        kidx = kk * 16 + kc
                        S.op("pe", lambda e, kc=kc, kidx=kidx, ps=ps, j=j, w=w: e.matmul(
                            ps[:, 0:TT], lhsT=w[:, kc, j * 128:(j + 1) * 128], rhs=u[:, kidx, :],
                            start=(kc == 0), stop=(kc == 15)), reads=[w, u.s(kidx)], writes=[ps])
                    S.op("dve", lambda e, ps=ps, jj=jj: e.tensor_tensor(out=x[:, jj, :], in0=ps[:, 0:TT], in1=x[:, jj, :],
                                                                        op=ALU.add), reads=[ps, x], writes=[x])
    tc0 = t * TT
    if not last_layer:
        S.dma("sp", out=x_dst.ap[:, tc0:tc0 + TT].rearrange("(k p) n -> p k n", p=128), in_=x[:, :, :], reads=[x],
              writes=[x_dst])
    else:
        ps = c["psm"]
        rstd = cx.rstd
        for kc in range(KC):
            sq = cx.sqb[kc % 2]
            S.op("act", lambda e, kc=kc, sq=sq: e.activation(out=sq[:, :], in_=x[:, kc, :], func=AF.Square),
                 reads=[x], writes=[sq])
            S.op("pe", lambda e, kc=kc, sq=sq: e.matmul(ps[:, 0:TT], lhsT=cx.ones_bf[:, :], rhs=sq[:, :],
                                                        start=(kc == 0), stop=(kc == KC - 1)),
                 reads=[sq, cx.ones_bf], writes=[ps])
        S.op("act", lambda e: e.activation(out=rstd[:, :], in_=ps[:, 0:TT], func=AF.Sqrt, bias=EPS,
                                           scale=1.0 / D_MODEL), reads=[ps], writes=[rstd])
        S.op("dve", lambda e: e.reciprocal(out=rstd[:, :], in_=rstd[:, :]), reads=[rstd], writes=[rstd])
        for kc in range(KC):
            S.op("dve", lambda e, kc=kc: e.scalar_tensor_tensor(out=x[:, kc, :], in0=x[:, kc, :],
                                                                scalar=c["fg"][:, kc:kc + 1], op0=ALU.mult,
                                                                in1=rstd[:, :], op1=ALU.mult),
                 reads=[x, c["fg"], rstd], writes=[x])
        S.dma("sp", out=x_dst.ap[:, tc0:tc0 + TT].rearrange("(k p) n -> p k n", p=128), in_=x[:, :, :], reads=[x],
              writes=[x_dst])


def phase_q(cx, c, io, x_src, x_dst, lambda_init, last_layer, dbg=None):
    S = cx.S
    x, hT, psm = c["xbuf"], c["hT"], c["psm"]
    for i in range(2):
        S.op("dve", lambda e, i=i: e.memset(c["qz"][i][:, :, :], 0.0), writes=[c["qz"][i]])
    for t in range(NT):
        cx.first_tile = (t == 0)
        load_x_tile(cx, c, x_src, t, io["ropet"])
        rmsnorm_fm(cx, x, c["n1g"], hT, psm)
        for m in range(2):
            for kc in range(KC):
                S.op("pe", lambda e, m=m, kc=kc: e.matmul(psm[:, m * 8:(m + 1) * 8], lhsT=hT[:, kc, m * 128:(m + 1) * 128],
                                                          rhs=c["wiw"][:, kc, :], start=(kc == 0), stop=(kc == KC - 1)),
                     reads=[hT.s(kc), c["wiw"]], writes=[psm])
        S.op("act", lambda e: e.mul(out=c["iw"][:, :], in_=psm[:, 0:16], mul=IWS), reads=[psm], writes=[c["iw"]])
        q_proj(cx, c, io, 0, c["q"], "plain", scale=128 ** -0.5)
        attn_A(cx, c, io, t)
        q_proj(cx, c, io, 512, None, "rope64z")
        attn_BC(cx, c, io, t, "B")
        q_proj(cx, c, io, 1024, c["q"], "plain", scale=128 ** -0.5)
        for h in range(4):
            S.op("pe", lambda e, h=h, t=t: e.matmul(psm[:, 0:TT], lhsT=c["selh"][0:4, h * 128:(h + 1) * 128],
                                                    rhs=c["cum_mine"][0:4, t * TT:(t + 1) * TT], start=True, stop=True),
                 reads=[c["selh"], c["cum_mine"]], writes=[psm])
            S.op("act", lambda e, h=h: e.activation(out=c["cumq"][:, h, :], in_=psm[:, 0:TT], func=AF.Copy),
                 reads=[psm], writes=[c["cumq"]])
        attn_BC(cx, c, io, t, "C")
        q_proj(cx, c, io, 1536, c["q"], "rope128")
        q_proj(cx, c, io, 2048, c["iq"], "rope64")
        attn_D(cx, c, io, t)
        if dbg is not None:
            for i in range(4):
                S.dma("sp", out=dbg[i].ap[:, t * TT:(t + 1) * TT].rearrange("(h p) n -> p h n", p=128),
                      in_=c["o"][i][:, :, :], reads=[c["o"][i]], writes=[dbg[i]])
        dense_tail(cx, c, io, t, x_dst, last_layer)


Q_IN = (("wq", [D_MODEL, 2560]), ("wiw", [D_MODEL, 8]), ("wg", [D_MODEL, 8192]), ("wbr", [2048, D_MODEL]),
        ("wo", [D_MODEL, D_MODEL]), ("wf1", [D_MODEL, D_FF]), ("wf2", [D_FF, D_MODEL]),
        ("ropet", [4, 128, LTOK]), ("biasA", [128, 6 * 512]), ("mk", [128, 4 * 128]), ("identf", [128, 128]),
        ("idxmask", [128, 256]), ("selh", [4, 512]), ("sel", [4, 2]), ("n1g", [128, KC]), ("n2g", [128, KC]),
        ("fg", [128, KC]), ("bgate", [128, 64]), ("dng", [128, 1]), ("lam4", [64, 4]))


def build_B(layer, dbg=False):
    lambda_init = 0.8 - 0.6 * math.exp(-0.3 * layer)
    nc = bass.Bass("TRN2", target_bir_lowering=False)
    es = ExitStack()
    io = {}
    xT = dram_in(nc, "xT", [D_MODEL, LTOK])
    for nm, shp in Q_IN:
        io[nm] = dram_in(nc, nm, shp)
    for nm, shp, dt in KV_ALL:
        io[nm + "_all"] = dram_in(nc, nm + "_all", shp, dt)
    xo = dram_out(nc, "xo", [D_MODEL, LTOK])
    dbgs = [dram_out(nc, f"dbg_o{i}", [512, LTOK], BF16) for i in range(4)] if dbg else None
    with es:
        S = Sched(nc, es)
        cx = Ctx(S)
        c = alloc_q(cx, alloc_common(cx))
        S.dma("sp", out=c["n1g"][:, :], in_=io["n1g"].ap[:, :], reads=[io["n1g"]], writes=[c["n1g"]])
        setup_q(cx, c, io, lambda_init)
        phase_q(cx, c, io, lambda t: (xT.ap[:, t * TT:(t + 1) * TT], xT.t), xo, lambda_init, layer == DEPTH - 1, dbgs)
        S.final_all()
        S.emit()
    return nc


def table_biasA(rel_bias_l, c):
    out = np.full((128, 6, 4, 128), NEG, np.float32)
    j = np.arange(128)[:, None]
    qi = np.arange(128)[None, :]
    for r in range(2):
        for nrel in range(3):
            dblk = (2 * (nrel - 2) + r) - c
            dist = -dblk * 128 + qi - j
            kch = 2 * dblk + j // 64
            qch = qi // 64
            valid = (kch <= qch) & (kch >= qch - 8)
            idx = np.clip(dist, -128, 128) + 128
            for h in range(4):
                vals = rel_bias_l[h][idx]
                out[:, r * 3 + nrel, h, :] = np.where(valid, vals, np.float32(NEG))
    return np.ascontiguousarray(out.reshape(128, 6 * 512))


def table_mk(c):
    out = np.zeros((128, 4, 128), np.float32)
    j = np.arange(128)[:, None]
    qi = np.arange(128)[None, :]
    diagB = np.where(j // 64 <= qi // 64, 0.0, NEG).astype(np.float32)
    diagC = np.where(j <= qi, 0.0, NEG).astype(np.float32)
    for r in range(2):
        if r < c:
            mB = mC = np.zeros((128, 128), np.float32)
        elif r == c:
            mB, mC = diagB, diagC
        else:
            mB = mC = np.full((128, 128), NEG, np.float32)
        out[:, r, :] = mB
        out[:, 2 + r, :] = mC
    return np.ascontiguousarray(out.reshape(128, 512))


def table_idxmask(c):
    out = np.zeros((128, 2, 128), np.float32)
    qi = np.arange(128)[:, None]
    j = np.arange(128)[None, :]
    for r in range(2):
        if r < c:
            pass
        elif r == c:
            out[:, r, :] = np.where(j // 64 <= qi // 64, 0.0, -1.0e30)
        else:
            out[:, r, :] = -1.0e30
    return np.ascontiguousarray(out.reshape(128, 256))


def host_q_inputs(inp, l, c):
    w_in = inp["w_in"][l]
    m = {}
    m["wq"] = wcols(w_in, ["a_q", "b_q", "c_q", "d_q", "d_iq"])
    m["wiw"] = wcols(w_in, ["d_iw"])
    m["wg"] = wcols(w_in, ["gate"])
    m["wbr"] = np.ascontiguousarray(inp["w_branch"][l].reshape(4 * 512, D_MODEL))
    m["wo"] = np.ascontiguousarray(inp["w_out"][l])
    m["wf1"] = np.ascontiguousarray(inp["w_ff1"][l])
    m["wf2"] = np.ascontiguousarray(inp["w_ff2"][l])
    m["ropet"] = rope_tabs(c)
    m["biasA"] = table_biasA(inp["rel_bias"][l], c)
    m["mk"] = table_mk(c)
    m["identf"] = np.eye(128, dtype=np.float32)
    m["idxmask"] = table_idxmask(c)
    selh = np.zeros((4, 4, 128), np.float32)
    for h in range(4):
        selh[h, h, :] = 1.0
    m["selh"] = selh.reshape(4, 512)
    sel = np.zeros((4, 2), np.float32)
    sel[:, c] = 1.0
    m["sel"] = sel
    m["n1g"] = pcol(inp["norm1_g"][l])
    m["n2g"] = pcol(inp["norm2_g"][l])
    m["fg"] = pcol(inp["final_g"])
    m["bgate"] = pcol(inp["b_gate"][l])
    m["dng"] = np.ascontiguousarray(inp["diff_norm_g"][l].reshape(128, 1))
    m["lam4"] = np.ascontiguousarray(np.stack([inp["lambda_q1"][l], inp["lambda_k1"][l], inp["lambda_q2"][l],
                                               inp["lambda_k2"][l]], axis=1))
    return m


def host_kv_inputs(inp, l, c):
    w_in = inp["w_in"][l]
    return {"wk": wcols(w_in, ["a_k", "b_k", "c_k", "d_k", "d_ik", "c_f"]),
            "wv": wcols(w_in, ["a_v", "b_v", "c_v", "d_v"]),
            "ropet": rope_tabs(c), "n1g": pcol(inp["norm1_g"][l]),
            "bfg": np.ascontiguousarray(inp["b_forget"][l].reshape(4, 1))}


I32 = mybir.dt.int32
PK = {"kTa": 0, "kTb": 512, "kTc": 1024, "kTd": 1536, "ikT": 1664, "logf": 1728, "va": 1792, "vb": 2304,
      "vc": 2816, "vd": 3328}
RP = 3456
NSEL = 2 * RP // 128


def pack_views(ap2d, base):
    v = {}
    for nm, rows in (("kTa", 512), ("kTb", 512), ("kTc", 512), ("kTd", 128), ("ikT", 64)):
        v[nm] = ap2d[base + PK[nm]:base + PK[nm] + rows, :]
    v["logf"] = ap2d[base + PK["logf"]:base + PK["logf"] + 8, :].bitcast(F32).rearrange("(h a) n -> h (a n)", a=2)
    for nm in ("va", "vb", "vc"):
        v[nm] = ap2d[base + PK[nm]:base + PK[nm] + 512, :].rearrange("r (a f) -> (r a) f", f=512)
    v["vd"] = ap2d[base + PK["vd"]:base + PK["vd"] + 128, :].rearrange("r (a f) -> (r a) f", f=128)
    return v


A_IN = (("wk", [D_MODEL, WK_COLS]), ("wv", [D_MODEL, WV_COLS]), ("bfg", [4, 1]))


def build_fused():
    nc = bass.Bass("TRN2", target_bir_lowering=False)
    es = ExitStack()
    xT = dram_in(nc, "xT", [D_MODEL, LTOK])
    pidx_d = dram_in(nc, "pidx", [128, NSEL], I32)
    ios = []
    for l in range(DEPTH):
        io = {}
        for nm, shp in A_IN + Q_IN:
            if nm in ("ropet", "identf", "mk", "idxmask", "selh", "sel", "fg"):
                continue
            io[nm] = dram_in(nc, f"{nm}{l}", shp)
        ios.append(io)
    shared = {nm: dram_in(nc, nm, dict(Q_IN)[nm]) for nm in ("ropet", "identf", "mk", "idxmask", "selh", "sel", "fg")}
    xo = dram_out(nc, "xo", [D_MODEL, LTOK])
    kvpack_h = nc.dram_tensor("kvpack", [RP, LTOK], BF16)
    kvg_h = nc.dram_tensor("kvg", [N_CORES * RP, LTOK], BF16)
    kvall_h = nc.dram_tensor("kvall", [2 * RP, LTOK], BF16)
    xs1_h = nc.dram_tensor("xs1", [D_MODEL, LTOK], F32)
    NSLOT = 160
    wscr_h = [nc.dram_tensor(f"wscr{l}", [NSLOT, 128, KC * WB], BF16) for l in range(DEPTH)] if USE_WSCR else []
    t_pack, t_g, t_all = Tok(dj=True), Tok(dj=True), Tok(dj=True)
    xs1 = DT(xs1_h.ap(), dj=True)
    pv = pack_views(kvpack_h.ap(), 0)
    av = [pack_views(kvall_h.ap(), r * RP) for r in range(2)]
    with es:
        S = Sched(nc, es)
        cx = Ctx(S)
        c = alloc_q(cx, alloc_kv(cx, alloc_common(cx)))
        c["pidx"] = S.sb("pidx", [128, NSEL], I32)
        cx.psd = cx.psd + [c["psS"][0], c["psS"][1], c["psO"], c["psD"]]
        cx.wscr = [h.ap() for h in wscr_h] if USE_WSCR else None
        S.dma("sp", out=c["pidx"][:, :], in_=pidx_d.ap[:, :], writes=[c["pidx"]])
        for l in range(DEPTH):
            lambda_init = 0.8 - 0.6 * math.exp(-0.3 * l)
            cx.layer = l
            io = dict(ios[l])
            io.update(shared)
            for nm in pv:
                io[nm] = DT(pv[nm], tok=t_pack)
                io[nm + "_all"] = DT([av[0][nm], av[1][nm]], tok=t_all)
            if l == 0:
                x_src = lambda t: (xT.ap[:, t * TT:(t + 1) * TT], xT.t)
                x_dst = xs1
            else:
                x_src = lambda t: (xs1.ap[:, t * TT:(t + 1) * TT], xs1.t)
                x_dst = xo
            S.dma("sp", out=c["n1g"][:, :], in_=io["n1g"].ap[:, :], reads=[io["n1g"]], writes=[c["n1g"]])
            S.dma("sp", out=c["nbfg"][64:68, :], in_=io["bfg"].ap[:, :], reads=[io["bfg"]], writes=[c["nbfg"]])
            S.op("dve", lambda e: e.tensor_scalar(out=c["nbfg"][64:68, :], in0=c["nbfg"][64:68, :], scalar1=-1.0,
                                                  scalar2=None, op0=ALU.mult), reads=[c["nbfg"]], writes=[c["nbfg"]])
            phase_kv(cx, io, x_src, c)
            S.collective_allgather(kvpack_h.ap().opt(), kvg_h.ap().opt(), reads=[t_pack], writes=[t_g])
            u = c["u"]
            for i in range(NSEL):
                k = i % 4
                toks = [u.s(8 * k + j) for j in range(8)]
                stv = u[:, 8 * k:8 * k + 8, :].rearrange("p k n -> p (k n)")
                S.idma(out=stv, in_=kvg_h.ap()[:, :], idx_ap=c["pidx"][:, i:i + 1], reads=[t_g, c["pidx"]],
                       writes=toks, semtile=toks[0])
                S.dma("sp", out=kvall_h.ap()[i * 128:(i + 1) * 128, :], in_=stv, reads=toks, writes=[t_all],
                      semtile=toks[0])
            setup_q(cx, c, io, lambda_init)
            phase_q(cx, c, io, x_src, x_dst, lambda_init, l == DEPTH - 1, None)
        S.final_all()
        print("ops per engine:", {k: len(v) for k, v in S.ops.items()})
        S.emit()
    return nc


def pair_index(core):
    b = core // 2
    idx = np.zeros((128, NSEL), np.int32)
    nb = RP // 128
    for i in range(NSEL):
        idx[:, i] = (2 * b + i // nb) * RP + (i % nb) * 128 + np.arange(128)
    return idx


def kernel_fused(**inputs):
    inp = {k: np.asarray(v) for k, v in inputs.items()}
    x = inp["x"]
    cores = list(range(N_CORES))
    nc = build_fused()
    per_layer = []
    for l in range(DEPTH):
        d = {}
        for c in range(2):
            m = host_q_inputs(inp, l, c)
            m.update({k: v for k, v in host_kv_inputs(inp, l, c).items() if k in ("wk", "wv", "bfg")})
            d[c] = m
        per_layer.append(d)
    maps = []
    for core in cores:
        b, c = core // 2, core % 2
        m = {"xT": to_local_T(x[b], c), "pidx": pair_index(core)}
        for l in range(DEPTH):
            src = per_layer[l][c]
            for nm, _ in A_IN + Q_IN:
                if nm in ("ropet", "identf", "mk", "idxmask", "selh", "sel", "fg"):
                    m[nm] = per_layer[0][c][nm]
                else:
                    m[f"{nm}{l}"] = per_layer[l][0][nm] if nm not in ("biasA",) else src[nm]
        maps.append(m)
    res = run_bass_kernel_spmd(nc, maps, core_ids=cores)
    xo = [np.asarray(res.results[core]["xo"]) for core in cores]
    out = np.stack([_from_local(xo[2 * b], xo[2 * b + 1]) for b in range(BATCH)], 0)
    return out.astype(np.float32)


def _from_local(loc0, loc1):
    out = np.empty((SEQ, D_MODEL), np.float32)
    o3 = out.reshape(NBLK, 128, D_MODEL)
    o3[0::2] = np.asarray(loc0).T.reshape(LBLK, 128, D_MODEL)
    o3[1::2] = np.asarray(loc1).T.reshape(LBLK, 128, D_MODEL)
    return out


def kernel_unfused(**inputs):
    inp = {k: np.asarray(v) for k, v in inputs.items()}
    x = inp["x"]
    cores = list(range(N_CORES))
    xT = [to_local_T(x[core // 2], core % 2) for core in cores]
    for l in range(DEPTH):
        ncA = build_A()
        mapsA = []
        for core in cores:
            m = host_kv_inputs(inp, l, core % 2)
            m["xT"] = xT[core]
            mapsA.append(m)
        resA = run_bass_kernel_spmd(ncA, mapsA, core_ids=cores)
        del mapsA
        ncB = build_B(l)
        mapsB = []
        for core in cores:
            b, c = core // 2, core % 2
            m = host_q_inputs(inp, l, c)
            m["xT"] = xT[core]
            for nm, shp, dt in KV_ALL:
                m[nm + "_all"] = np.ascontiguousarray(
                    np.stack([resA.results[2 * b][nm], resA.results[2 * b + 1][nm]], 0))
            mapsB.append(m)
        resB = run_bass_kernel_spmd(ncB, mapsB, core_ids=cores)
        del mapsB
        xT = [np.asarray(resB.results[core]["xo"]) for core in cores]
    out = np.stack([_from_local(xT[2 * b], xT[2 * b + 1]) for b in range(BATCH)], 0)
    return out.astype(np.float32)


def kernel(**inputs):
    return kernel_fused(**inputs)


def _sim_sched(S):
    sem = {}
    pos = {e: 0 for e in ENGS}
    total = sum(len(v) for v in S.ops.values())
    done = 0
    while done < total:
        progressed = False
        for e in ENGS:
            ops = S.ops[e]
            while pos[e] < len(ops):
                waits, fn, inc = ops[pos[e]]
                if all(sem.get(k, 0) >= v for k, v in waits):
                    sem[inc[0]] = sem.get(inc[0], 0) + (1 if inc[1] is None else inc[1])
                    pos[e] += 1
                    done += 1
                    progressed = True
                else:
                    break
        if not progressed:
            for e in ENGS:
                if pos[e] < len(S.ops[e]):
                    waits, fn, inc = S.ops[e][pos[e]]
                    bad = [(k, v, sem.get(k, 0)) for k, v in waits if sem.get(k, 0) < v]
                    print("STUCK", e, pos[e], "/", len(S.ops[e]), "unsatisfied (key, need, have):", bad)
            return False
    return True
```
